# Optimizing a Trainium2 kernel written in Bass

```python
import jax, jax.numpy as jnp
from jax import lax
import numpy as np

D_MODEL = 1024
BATCH = 8
SEQ = 4096
DEPTH = 1

N_META = 16
CONV_CH = 512
CONV_KERNEL = 31
RWKV_WIDTH = 512
RWKV_HEAD = 64
RWKV_HEADS = RWKV_WIDTH // RWKV_HEAD
RANK_W = 64
RANK_A = 64
RANK_G = 128
N_BRANCH = 2
SHIFT_TOTAL = 3 * RWKV_WIDTH + RANK_W + RANK_A + RANK_G
IN_SPLITS = [CONV_CH, 2 * CONV_CH, 2 * CONV_CH + SHIFT_TOTAL]
IN_TOTAL = 2 * CONV_CH + SHIFT_TOTAL + N_BRANCH * D_MODEL
RWKV_SPLITS = [RWKV_WIDTH, 2 * RWKV_WIDTH, 3 * RWKV_WIDTH, 3 * RWKV_WIDTH + RANK_W, 3 * RWKV_WIDTH + RANK_W + RANK_A]
PEER_HEADS = 8
PEER_NKEYS = 128
PEER_EXPERTS = PEER_NKEYS * PEER_NKEYS
PEER_DHALF = 128
PEER_TOPK = 16
TOK_BLOCK = 256
RMS_EPS = 1e-6
LN_EPS = 1e-5
GN_EPS = 64e-5

kernel_name = "hybrid_conformer_rwkv7_peer_block"


def rmsnorm(x, g):
    xf = x.astype(jnp.float32)
    y = xf * lax.rsqrt(jnp.mean(xf * xf, axis=-1, keepdims=True) + RMS_EPS)
    return (y * g.astype(jnp.float32)).astype(x.dtype)


def token_shift(z):
    return jnp.pad(z[:, :-1], ((0, 0), (1, 0), (0, 0)))


def conformer_conv(val, gate, conv_w, conv_b, ln_g, ln_b, w_out):
    u = val * jax.nn.sigmoid(gate)
    y = lax.conv_general_dilated(
        u, conv_w[:, None, :].astype(u.dtype), window_strides=(1,),
        padding=[(CONV_KERNEL - 1, 0)], dimension_numbers=('NWC', 'WIO', 'NWC'),
        feature_group_count=CONV_CH) + conv_b
    yf = y.astype(jnp.float32)
    mean = jnp.mean(yf, axis=-1, keepdims=True)
    var = jnp.mean(jnp.square(yf - mean), axis=-1, keepdims=True)
    yn = (yf - mean) * lax.rsqrt(var + LN_EPS) * ln_g + ln_b
    return jax.nn.silu(yn).astype(val.dtype) @ w_out


def wkv7_scan(r, decay, k, v, kk, a):
    def step(S, inp):
        r_t, w_t, k_t, v_t, kk_t, a_t = inp
        sa = jnp.einsum('bhvk,bhk->bhv', S, kk_t)
        S = (S * w_t[:, :, None, :]
             - sa[..., None] * (kk_t * a_t)[:, :, None, :]
             + v_t[..., None] * k_t[:, :, None, :])
        return S, jnp.einsum('bhvk,bhk->bhv', S, r_t)
    B, T, H, N = r.shape
    S0 = jnp.zeros((B, H, N, N), jnp.float32)
    xs = tuple(jnp.moveaxis(z, 1, 0) for z in (r, decay, k, v, kk, a))
    _, o = lax.scan(step, S0, xs)
    return jnp.moveaxis(o, 0, 1)


def rwkv7_mix(z, mu, w0, w_up, a0, a_up, g_up, k_k, k_a, r_k, gn_g, gn_b, w_out):
    B, T, _ = z.shape
    zf = z.astype(jnp.float32)
    zf = zf + (token_shift(zf) - zf) * mu
    r, k, v, lw, la, lg = jnp.split(zf, RWKV_SPLITS, axis=-1)
    w = -jax.nn.softplus(-(w0 + jnp.tanh(lw) @ w_up)) - 0.5
    decay = jnp.exp(-jnp.exp(w))
    a = jax.nn.sigmoid(a0 + la @ a_up)
    g = jax.nn.sigmoid(lg) @ g_up
    hd = lambda t: t.reshape(B, T, RWKV_HEADS, RWKV_HEAD)
    kk = hd(k * k_k)
    kk = kk * lax.rsqrt(jnp.maximum(jnp.sum(kk * kk, axis=-1, keepdims=True), 1e-24))
    k = k * (1.0 + (a - 1.0) * k_a)
    r_h, k_h, v_h, a_h = hd(r), hd(k), hd(v), hd(a)
    o = wkv7_scan(r_h, hd(decay), k_h, v_h, kk, a_h)
    mean = jnp.mean(o, axis=-1, keepdims=True)
    var = jnp.mean(jnp.square(o - mean), axis=-1, keepdims=True)
    o = ((o - mean) * lax.rsqrt(var + GN_EPS)).reshape(B, T, RWKV_WIDTH) * gn_g + gn_b
    bonus = jnp.sum(r_h * k_h * r_k, axis=-1, keepdims=True) * v_h
    o = (o + bonus.reshape(B, T, RWKV_WIDTH)) * g
    return o.astype(z.dtype) @ w_out


def peer_ffn(x, w_q, sub_keys, expert_u, expert_v):
    B, T, D = x.shape
    n_tok = B * T
    n_blocks = -(-n_tok // TOK_BLOCK)
    pad = n_blocks * TOK_BLOCK - n_tok
    xt = jnp.pad(x.reshape(n_tok, D), ((0, pad), (0, 0))).reshape(n_blocks, TOK_BLOCK, D)
    K = PEER_TOPK

    def block(xb):
        q = (xb @ w_q).reshape(TOK_BLOCK, PEER_HEADS, 2, PEER_DHALF)
        s = jnp.einsum('thcd,hcnd->thcn', q, sub_keys).astype(jnp.float32)
        top_s, top_i = lax.top_k(s, K)
        cand = top_s[:, :, 0, :, None] + top_s[:, :, 1, None, :]
        best_s, best_c = lax.top_k(cand.reshape(TOK_BLOCK, PEER_HEADS, K * K), K)
        i1 = jnp.take_along_axis(top_i[:, :, 0], best_c // K, axis=-1)
        i2 = jnp.take_along_axis(top_i[:, :, 1], best_c % K, axis=-1)
        e = (i1 * PEER_NKEYS + i2).reshape(TOK_BLOCK, PEER_HEADS * K)
        gate = jax.nn.softmax(best_s, axis=-1).reshape(TOK_BLOCK, PEER_HEADS * K)
        u = expert_u[e]
        act = jax.nn.gelu(jnp.einsum('td,ted->te', xb, u), approximate=False)
        coef = (gate * act.astype(jnp.float32)).astype(xb.dtype)
        return jnp.einsum('te,ted->td', coef, expert_v[e])

    y = lax.map(block, xt)
    return y.reshape(n_blocks * TOK_BLOCK, D)[:n_tok].reshape(B, T, D)


def setup_inputs(seed: int = 0) -> dict:
    key = jax.random.key(seed)
    ks = jax.random.split(key, 32)
    f32 = jnp.float32
    L = DEPTH
    D = D_MODEL

    def nrm(k, shape, s):
        return jax.random.normal(k, shape, f32) * s

    return {
        "x": nrm(ks[0], (BATCH, SEQ, D), 1.0),
        "meta_tokens": nrm(ks[1], (N_META, D), 1.0),
        "g_mix": 1.0 + nrm(ks[2], (L, D), 0.01),
        "w_in": nrm(ks[3], (L, D, IN_TOTAL), D ** -0.5),
        "conv_w": nrm(ks[4], (L, CONV_KERNEL, CONV_CH), CONV_KERNEL ** -0.5),
        "conv_b": nrm(ks[5], (L, CONV_CH), 0.01),
        "conv_ln_g": 1.0 + nrm(ks[6], (L, CONV_CH), 0.01),
        "conv_ln_b": nrm(ks[7], (L, CONV_CH), 0.01),
        "w_conv_out": nrm(ks[8], (L, CONV_CH, D), CONV_CH ** -0.5),
        "mu_shift": jax.random.uniform(ks[9], (L, SHIFT_TOTAL), f32),
        "w0": jax.random.uniform(ks[10], (L, RWKV_WIDTH), f32, -6.0, -1.0),
        "w_up": nrm(ks[11], (L, RANK_W, RWKV_WIDTH), 0.1 * RANK_W ** -0.5),
        "a0": nrm(ks[12], (L, RWKV_WIDTH), 0.1),
        "a_up": nrm(ks[13], (L, RANK_A, RWKV_WIDTH), 0.1 * RANK_A ** -0.5),
        "g_up": nrm(ks[14], (L, RANK_G, RWKV_WIDTH), RANK_G ** -0.5),
        "k_k": 0.85 + nrm(ks[15], (L, RWKV_WIDTH), 0.02),
        "k_a": 1.0 + nrm(ks[16], (L, RWKV_WIDTH), 0.02),
        "r_k": nrm(ks[17], (L, RWKV_HEADS, RWKV_HEAD), 0.1),
        "gn_g": 1.0 + nrm(ks[18], (L, RWKV_WIDTH), 0.01),
        "gn_b": nrm(ks[19], (L, RWKV_WIDTH), 0.01),
        "w_rwkv_out": nrm(ks[20], (L, RWKV_WIDTH, D), RWKV_WIDTH ** -0.5),
        "w_o": nrm(ks[21], (L, D, D), D ** -0.5),
        "g_ffn": 1.0 + nrm(ks[22], (L, D), 0.01),
        "w_q": nrm(ks[23], (L, D, PEER_HEADS * 2 * PEER_DHALF), D ** -0.5),
        "sub_keys": nrm(ks[24], (L, PEER_HEADS, 2, PEER_NKEYS, PEER_DHALF), PEER_DHALF ** -0.5),
        "expert_u": nrm(ks[25], (L, PEER_EXPERTS, D), D ** -0.5),
        "expert_v": nrm(ks[26], (L, PEER_EXPERTS, D), PEER_HEADS ** -0.5),
        "g_final": 1.0 + nrm(ks[27], (D,), 0.01),
    }


def reference(x, meta_tokens, g_mix, w_in, conv_w, conv_b, conv_ln_g, conv_ln_b, w_conv_out,
              mu_shift, w0, w_up, a0, a_up, g_up, k_k, k_a, r_k, gn_g, gn_b, w_rwkv_out,
              w_o, g_ffn, w_q, sub_keys, expert_u, expert_v, g_final):
    B = x.shape[0]
    meta = jnp.broadcast_to(meta_tokens[None].astype(x.dtype), (B, N_META, D_MODEL))
    h = jnp.concatenate([meta, x], axis=1)
    for l in range(DEPTH):
        z = rmsnorm(h, g_mix[l]) @ w_in[l]
        conv_val, conv_gate, rwkv_in, gate_logits = jnp.split(z, IN_SPLITS, axis=-1)
        y_a = conformer_conv(conv_val, conv_gate, conv_w[l], conv_b[l], conv_ln_g[l], conv_ln_b[l], w_conv_out[l])
        y_b = rwkv7_mix(rwkv_in, mu_shift[l], w0[l], w_up[l], a0[l], a_up[l], g_up[l],
                        k_k[l], k_a[l], r_k[l], gn_g[l], gn_b[l], w_rwkv_out[l])
        gate_a, gate_b = jnp.split(jax.nn.sigmoid(gate_logits), N_BRANCH, axis=-1)
        h = h + (gate_a * y_a + gate_b * y_b) @ w_o[l]
        h = h + peer_ffn(rmsnorm(h, g_ffn[l]), w_q[l], sub_keys[l], expert_u[l], expert_v[l])
    return rmsnorm(h, g_final)[:, N_META:]
```

```python
import numpy as np
import concourse.bass as bass
import concourse.mybir as mybir
from concourse.bass_utils import run_bass_kernel_spmd
from contextlib import ExitStack

F32 = mybir.dt.float32
BF16 = mybir.dt.bfloat16
U32 = mybir.dt.uint32
ALU = mybir.AluOpType
AF = mybir.ActivationFunctionType
AX = mybir.AxisListType


class Buf:
    __slots__ = ("name", "w", "r", "excl")

    def __init__(self, name, excl=False):
        self.name = name
        self.w = None
        self.r = []
        self.excl = excl


class Op:
    __slots__ = ("eng", "fn", "deps", "signal", "is_dma", "chan", "sem", "val")

    def __init__(self, eng, fn, is_dma=False, chan=None):
        self.eng = eng
        self.fn = fn
        self.deps = []
        self.signal = False
        self.is_dma = is_dma
        self.chan = chan
        self.sem = None
        self.val = 0


class Chan:
    __slots__ = ("name", "last", "count", "sem")

    def __init__(self, name):
        self.name = name
        self.last = None
        self.count = 0
        self.sem = None


EPOCH = 30000
CAST = True
ENGS = ("pe", "dve", "act", "pool", "sp")


class Sched:
    def __init__(self, nc):
        self.nc = nc
        self.ops = {e: [] for e in ENGS}
        self.chans = []
        self.final_waits = []

    def chan(self, name):
        c = Chan(name)
        self.chans.append(c)
        return c

    def _record(self, op, reads, writes):
        writes = writes + [b for b in reads if b.excl and not any(b is x for x in writes)]
        deps = []
        for b in reads:
            if b.w is not None:
                deps.append(b.w)
        for b in writes:
            if b.w is not None:
                deps.append(b.w)
            for r in b.r:
                if r.eng == op.eng and not r.is_dma and not op.is_dma:
                    continue
                deps.append(r)
        seen = set(id(d) for d in op.deps)
        for d in deps:
            if d is op or id(d) in seen:
                continue
            if d.eng == "pe" and op.eng == "pe" and not d.is_dma and not op.is_dma:
                continue
            seen.add(id(d))
            op.deps.append(d)
            d.signal = True
        for b in reads:
            b.r.append(op)
        for b in writes:
            b.w = op
            b.r = []
        self.ops[op.eng].append(op)
        return op

    def op(self, eng, fn, reads=(), writes=()):
        return self._record(Op(eng, fn), list(reads), list(writes))

    def dma(self, chan, out, in_, reads=(), writes=(), eng="sp", **kw):
        op = Op(eng, lambda e: e.dma_start(out=out, in_=in_, **kw), is_dma=True, chan=chan)
        if chan.last is not None:
            op.deps.append(chan.last)
            chan.last.signal = True
        chan.last = op
        op.signal = True
        return self._record(op, list(reads), list(writes))

    def barrier(self):
        lasts = []
        for e in ENGS:
            for o in reversed(self.ops[e]):
                if not o.is_dma and o.fn is not None:
                    lasts.append(o)
                    break
        for c in self.chans:
            if c.last is not None:
                lasts.append(c.last)
        for e in ENGS:
            op = Op(e, None)
            for d in lasts:
                if d.eng == e and not d.is_dma:
                    continue
                op.deps.append(d)
                d.signal = True
            self.ops[e].append(op)

    def emit(self, stack):
        nc = self.nc
        for c in self.chans:
            c.sem = stack.enter_context(nc.semaphore("c_" + c.name))
        for d in self.final_waits:
            d.signal = True
        for e, lst in self.ops.items():
            cur = None
            cnt = 0
            k = 0
            for op in lst:
                if op.is_dma:
                    op.chan.count += 16
                    op.sem = op.chan.sem
                    op.val = op.chan.count
                elif op.signal:
                    if cur is None or cnt >= EPOCH:
                        cur = stack.enter_context(nc.semaphore("e_%s_%d" % (e, k)))
                        k += 1
                        cnt = 0
                    cnt += 1
                    op.sem = cur
                    op.val = cnt
        block = stack.enter_context(nc.Block())

        def run(engine_obj, lst, tail=None):
            waited = {}

            def w(d):
                key = d.sem.name
                if waited.get(key, 0) >= d.val:
                    return
                engine_obj.wait_ge(d.sem, d.val)
                waited[key] = d.val

            for op in lst:
                for d in op.deps:
                    w(d)
                if op.fn is None:
                    continue
                ins = op.fn(engine_obj)
                if op.is_dma:
                    ins.then_inc(op.sem, 16)
                elif op.signal:
                    ins.then_inc(op.sem, 1)
            if tail:
                for d in tail:
                    w(d)

        ops = self.ops
        fw = self.final_waits

        @block.tensor
        def _(e):
            run(e, ops["pe"])

        @block.vector
        def _(e):
            run(e, ops["dve"])

        @block.scalar
        def _(e):
            run(e, ops["act"])

        @block.gpsimd
        def _(e):
            run(e, ops["pool"])

        @block.sync
        def _(e):
            run(e, ops["sp"], tail=fw)


D = 1024
NPAD = 112
C_IDENT, C_ONES, C_O512, C_BLK, C_TRII, C_TRIE, C_MS, C_MSN, C_MLN, C_MI, C_IOTA, C_O1024 = range(12)
NCST = 12
P_GMIX = 0
P_MU = 8
P_A0 = 22
P_KK = 26
P_KA = 30
P_RK = 34
P_GNG = 38
P_GNB = 42
P_CB = 46
P_LNG = 50
P_LNB = 54
P_GFFN = 58
P_EPS6 = 66
P_EPS5 = 67
P_EPSG = 68
P_CW = 69
NPRM = 69 + 124


def build_program(NT, debug=False):
    NRT = NT - 1
    assert NRT % 2 == 0
    nc = bass.Bass("TRN2", target_bir_lowering=False)
    din = lambda n, s, dt=F32: nc.dram_tensor(n, s, dt, kind="ExternalInput").ap()
    xT_d = din("xT", [NT, 128, 8, 128])
    w_in_d = din("w_in", [1024, 4864])
    wco_d = din("w_conv_out", [512, 1024])
    wro_d = din("w_rwkv_out", [512, 1024])
    wo_d = din("w_o", [1024, 1024])
    wq_d = din("w_q", [1024, 2048])
    skT_d = din("skT", [128, 16, 128])
    uT_d = din("uT", [128, 128, 8 * 128])
    vP_d = din("vP", [128, 128, 1024])
    waup_d = din("wa_up", [128, 512])
    gup_d = din("g_up", [128, 512])
    prm_d = din("params", [128, NPRM])
    w0_d = din("w0row", [1, 512])
    cst_d = din("cst", [128, NCST, 128])
    gfin_d = din("gfin", [1, 1024])
    out_d = nc.dram_tensor("out", [NRT * 128, 1024], F32, kind="ExternalOutput").ap()
    dscr = lambda n, s, dt: nc.dram_tensor(n, s, dt, kind="Internal").ap()
    orw_s = dscr("orw_s", [NT, 128, 4, 128], BF16)
    h1tok_s = dscr("h1tok_s", [NRT, 128, 1024], F32)
    h1nT_s = dscr("h1nT_s", [NRT, 128, 8, 128], BF16)
    sel_s = dscr("sel_s", [NRT, 128, 3, 128], F32)
    uT_b = dscr("uT_b", [128, 128, 8 * 128], BF16)
    vP_b = dscr("vP_b", [128, 128, 1024], BF16)
    dbg = {}

    st = ExitStack()
    with st:
        S = Sched(nc)
        ARENA = 53000
        arena = st.enter_context(nc.sbuf_tensor("arena", [128, ARENA], F32))
        PS = [st.enter_context(nc.psum_tensor("ps%d" % k, [128, 512], F32)) for k in range(8)]
        PB = []
        for k in range(8):
            _b = Buf("ps%d" % k, excl=True)
            PB.append([_b, _b, _b, _b])
        top = [0]

        def alloc(name, free, dt=F32):
            n = int(np.prod(free))
            words = n if dt in (F32, U32) else (n + 1) // 2
            words = (words + 7) // 8 * 8
            a = arena[:, top[0]:top[0] + words]
            top[0] += words
            assert top[0] <= ARENA, (name, top[0])
            if dt != F32:
                a = a.bitcast(dt)
            a = a[:, 0:n]
            if len(free) == 2:
                a = a.rearrange("p (a b) -> p a b", b=free[1])
            elif len(free) == 3:
                a = a.rearrange("p (a b c) -> p a b c", b=free[1], c=free[2])
            return a, Buf(name)

        def pq(k, q):
            return PS[k][:, q * 128:(q + 1) * 128]

        def tap(name, ap, buf, shape, dt=F32):
            if not debug:
                return
            d = nc.dram_tensor("dbg_" + name, list(shape), dt, kind="ExternalOutput").ap()
            c = S.chan("dbg_" + name)
            o = S.dma(c, d, ap, reads=[buf])
            S.final_waits.append(o)
            dbg[name] = (shape, dt)

        cst, B_cst = alloc("cst", [NCST, 128])
        prm, B_prm = alloc("prm", [NPRM])
        ones_b, B_onesb = alloc("ones_b", [128], BF16)
        ch_c = S.chan("cst")
        S.dma(ch_c, cst, cst_d, writes=[B_cst])
        ch_p = S.chan("prm")
        S.dma(ch_p, prm, prm_d, writes=[B_prm])
        S.op("dve", lambda e: e.tensor_copy(out=ones_b, in_=cst[:, C_ONES, :]), [B_cst], [B_onesb])
        ident = cst[:, C_IDENT, :]
        base_top = top[0]

        ch_cast = [S.chan("cast%d" % k) for k in range(4)]
        B_uTb = Buf("uTb")
        B_vPb = Buf("vPb")
        cast_ops = []
        def issue_cast(k):
            if not CAST:
                return
            sl = slice(k * 8, (k + 1) * 8)
            cast_ops.append(S.dma(ch_cast[k % 2], uT_b[sl], uT_d[sl], eng="pool"))
            cast_ops.append(S.dma(ch_cast[2 + k % 2], vP_b[sl], vP_d[sl], eng="pool"))

        def load_x(i, xt, B_xt, ch, xb, B_xb, sq, B_sq, rs, B_rs, psq):
            S.dma(ch, xt, xT_d[i], writes=[B_xt])
            S.op("pool", lambda e: e.tensor_tensor(out=xb, in0=xt, in1=prm[:, P_GMIX:P_GMIX + 8].unsqueeze(2).to_broadcast([128, 8, 128]), op=ALU.mult),
                 [B_xt, B_prm], [B_xb])
            S.op("act", lambda e: e.activation(out=sq, in_=xt, func=AF.Square), [B_xt], [B_sq])
            k, q = psq
            for dc in range(8):
                S.op("pe", lambda e, dc=dc: e.matmul(pq(k, q), lhsT=ones_b, rhs=sq[:, dc, :], start=(dc == 0), stop=(dc == 7)),
                     [B_onesb, B_sq], [PB[k][q]])
            S.op("act", lambda e: e.activation(out=rs, in_=pq(k, q), func=AF.Sqrt, scale=1.0 / 1024, bias=prm[:, P_EPS6:P_EPS6 + 1]),
                 [PB[k][q], B_prm], [B_rs])
            S.op("dve", lambda e: e.reciprocal(out=rs, in_=rs), [B_rs], [B_rs])

        B_orws = Buf("orws")
        ch_orw = S.chan("orw")

        def phase_R():
            wR, B_wR = alloc("wR", [8, 1792], BF16)
            waup, B_waup = alloc("waup", [512])
            gup, B_gup = alloc("gup", [512])
            w0r, B_w0r = alloc("w0r", [512])
            chw = S.chan("wR")
            S.dma(chw, wR, w_in_d[:, 1024:2816].rearrange("(c p) n -> p c n", p=128), writes=[B_wR], eng="pool")
            S.dma(S.chan("waup"), waup, waup_d, writes=[B_waup])
            S.dma(S.chan("gup"), gup, gup_d, writes=[B_gup])
            S.dma(S.chan("w0r"), w0r[0:1, :], w0_d, writes=[B_w0r])
            xts = [alloc("xt0", [8, 128])] * 2
            chx = [S.chan("x%d" % k) for k in range(2)]
            xb, B_xb = alloc("xb", [8, 128], BF16)
            sq, B_sq = alloc("sq", [8, 128], BF16)
            rs, B_rs = alloc("rs", [128])
            zr, B_zr = alloc("zr", [14, 129])
            zs, B_zs = alloc("zs", [14, 128])
            tl, B_tl = alloc("tl", [128])
            sgt, B_sgt = alloc("sgt", [512])
            cexc, B_cexc = alloc("cexc", [4, 128])
            cinv, B_cinv = alloc("cinv", [4, 128])
            aa, B_aa = alloc("aa", [4, 128])
            sgl, B_sgl = alloc("sgl", [128])
            kk, B_kk = alloc("kk", [4, 128])
            tmp, B_tmp = alloc("tmp", [4, 128])
            k2, B_k2 = alloc("k2", [4, 128])
            FS = [{n_: alloc("%s_%d" % (n_, k_), shp_) for n_, shp_ in (("rt", [4, 128]), ("kkt", [4, 128]), ("kt", [4, 128]), ("bt", [4, 128]), ("bon", [4, 128]), ("gT", [4, 128]), ("vtok", [512]), ("ktok", [512]), ("btok", [512]), ("cinc", [4, 128]))} for k_ in range(3)]
            tmp2, B_tmp2 = alloc("tmp2", [4, 128])
            osq, B_osq = tmp2.rearrange("p a b -> p (a b)"), B_tmp2
            ST, _ = alloc("ST", [4, 64])
            osb, B_osb = alloc("osb", [512])
            gst, B_gst = alloc("gst", [32])
            orw, B_orw = alloc("orw", [4, 128], BF16)
            KAS = {}
            for nm in ("Xa", "XTa", "Xb", "XTb", "Tb"):
                ap_, _ = alloc(nm + "8", [8, 128])
                KAS[nm] = (ap_, [Buf(nm + "_e"), Buf(nm + "_o")])
            KAD = []
            for k_ in range(2):
                d_ = {}
                for nm in ("Mk", "Ak", "Ab", "Ta"):
                    ap_, _ = alloc("%s8_%d" % (nm, k_), [8, 128])
                    d_[nm] = (ap_, [Buf("%s_e%d" % (nm, k_)), Buf("%s_o%d" % (nm, k_))])
                KAD.append(d_)
            XTs8, B_XTs8 = alloc("XTs8", [8, 64])
            NTs8, B_NTs8 = alloc("NTs8", [8, 64])
            tmpS, B_tmpS = alloc("tmpS", [4, 64])
            B_STp = [Buf("STe"), Buf("STo")]
            S.op("dve", lambda e: e.memset(zr, 0.0), [], [B_zr])
            S.op("dve", lambda e: e.memset(ST, 0.0), [], B_STp)
            slot_ctr = [0]

            def nslot():
                s_ = slot_ctr[0] % 3
                slot_ctr[0] += 1
                return 2 + s_

            def front(i):
                rt, B_rt = FS[i % 3]["rt"]
                kkt, B_kkt = FS[i % 3]["kkt"]
                kt, B_kt = FS[i % 3]["kt"]
                bt, B_bt = FS[i % 3]["bt"]
                bon, B_bon = FS[i % 3]["bon"]
                gT, B_gT = FS[i % 3]["gT"]
                vtok, B_vtok = FS[i % 3]["vtok"]
                ktok, B_ktok = FS[i % 3]["ktok"]
                btok, B_btok = FS[i % 3]["btok"]
                cinc, B_cinc = FS[i % 3]["cinc"]
                xt, B_xt = xts[i % 2]
                load_x(i, xt, B_xt, chx[i % 2], xb, B_xb, sq, B_sq, rs, B_rs, (1, 0))
                yield
                for gi, (j0, nj) in enumerate(((0, 4), (4, 4), (8, 4), (12, 2))):
                    for jj in range(nj):
                        j = j0 + jj
                        for dc in range(8):
                            S.op("pe", lambda e, j=j, jj=jj, dc=dc, gi=gi: e.matmul(pq(gi % 2, jj), lhsT=wR[:, dc, j * 128:(j + 1) * 128], rhs=xb[:, dc, :], start=(dc == 0), stop=(dc == 7)),
                                 [B_wR, B_xb], [PB[gi % 2][jj]])
                    S.op("dve", lambda e, gi=gi, j0=j0, nj=nj: e.tensor_tensor(out=zr[:, j0:j0 + nj, 1:129], in0=PS[gi % 2][:, 0:nj * 128].rearrange("p (a b) -> p a b", b=128),
                                                                           in1=rs.unsqueeze(1).to_broadcast([128, nj, 128]), op=ALU.mult),
                         PB[gi % 2][0:nj] + [B_rs], [B_zr])
                yield
                mu_bc = prm[:, P_MU:P_MU + 14].unsqueeze(2).to_broadcast([128, 14, 128])
                S.op("pool", lambda e: e.tensor_tensor(out=zs, in0=zr[:, :, 0:128], in1=zr[:, :, 1:129], op=ALU.subtract), [B_zr], [B_zs])
                S.op("pool", lambda e: e.tensor_tensor(out=zs, in0=zs, in1=mu_bc, op=ALU.mult), [B_zs, B_prm], [B_zs])
                S.op("pool", lambda e: e.tensor_tensor(out=zs, in0=zs, in1=zr[:, :, 1:129], op=ALU.add), [B_zs, B_zr], [B_zs])
                S.op("pool", lambda e: e.tensor_copy(out=zr[:, :, 0:1], in_=zr[:, :, 128:129]), [B_zr, B_zs], [B_zr])
                if i == 1:
                    tap("zs", zs, B_zs, [128, 14, 128])
                r_ = zs[:, 0:4, :]
                k_ = zs[:, 4:8, :]
                v_ = zs[:, 8:12, :]
                yield
                S.op("act", lambda e: e.activation(out=tl[0:64, :], in_=zs[0:64, 12, :], func=AF.Tanh), [B_zs], [B_tl])
                S.op("pe", lambda e: e.matmul(PS[0][:, :], lhsT=tl[0:64, :], rhs=waup[0:64, :], start=True, stop=False), [B_tl, B_waup], PB[0])
                S.op("pe", lambda e: e.matmul(PS[0][:, :], lhsT=cst[0:1, C_ONES, :], rhs=w0r[0:1, :], start=False, stop=True), [B_cst, B_w0r], PB[0])
                S.op("act", lambda e: e.activation(out=sgt, in_=PS[0][:, :], func=AF.Sigmoid), PB[0], [B_sgt])
                for cc in range(4):
                    S.op("pe", lambda e, cc=cc: e.matmul(pq(1, cc), lhsT=sgt[:, cc * 128:(cc + 1) * 128], rhs=cst[:, C_TRII, :], start=True, stop=True), [B_sgt, B_cst], [PB[1][cc]])
                    S.op("pe", lambda e, cc=cc: e.matmul(pq(0, cc), lhsT=sgt[:, cc * 128:(cc + 1) * 128], rhs=cst[:, C_TRIE, :], start=True, stop=True), [B_sgt, B_cst], [PB[0][cc]])
                S.op("act", lambda e: e.activation(out=cinc.rearrange("p a b -> p (a b)"), in_=PS[1][:, :], func=AF.Exp), PB[1], [B_cinc])
                S.op("act", lambda e: e.activation(out=cinv.rearrange("p a b -> p (a b)"), in_=PS[1][:, :], func=AF.Exp, scale=-1.0), PB[1], [B_cinv])
                S.op("act", lambda e: e.activation(out=cexc.rearrange("p a b -> p (a b)"), in_=PS[0][:, :], func=AF.Exp), PB[0], [B_cexc])
                yield
                for cc in range(4):
                    S.op("pe", lambda e, cc=cc: e.matmul(pq(1, cc), lhsT=waup[64:128, cc * 128:(cc + 1) * 128], rhs=zs[64:128, 12, :], start=True, stop=True), [B_waup, B_zs], [PB[1][cc]])
                    S.op("act", lambda e, cc=cc: e.activation(out=aa[:, cc, :], in_=pq(1, cc), func=AF.Sigmoid, bias=prm[:, P_A0 + cc:P_A0 + cc + 1]), [PB[1][cc], B_prm], [B_aa])
                S.op("act", lambda e: e.activation(out=sgl, in_=zs[:, 13, :], func=AF.Sigmoid), [B_zs], [B_sgl])
                for cc in range(4):
                    S.op("pe", lambda e, cc=cc: e.matmul(pq(0, cc), lhsT=gup[:, cc * 128:(cc + 1) * 128], rhs=sgl, start=True, stop=True), [B_gup, B_sgl], [PB[0][cc]])
                S.op("act", lambda e: e.copy(out=gT.rearrange("p a b -> p (a b)"), in_=PS[0][:, :]), PB[0], [B_gT])
                yield
                bc4 = lambda c0: prm[:, c0:c0 + 4].unsqueeze(2).to_broadcast([128, 4, 128])
                S.op("dve", lambda e: e.tensor_tensor(out=kk, in0=k_, in1=bc4(P_KK), op=ALU.mult), [B_zs, B_prm], [B_kk])
                S.op("pool", lambda e: e.tensor_tensor(out=tmp, in0=kk, in1=kk, op=ALU.mult), [B_kk], [B_tmp])
                for cc in range(4):
                    S.op("pe", lambda e, cc=cc: e.matmul(pq(1, cc), lhsT=cst[:, C_BLK, :], rhs=tmp[:, cc, :], start=True, stop=True), [B_cst, B_tmp], [PB[1][cc]])
                S.op("dve", lambda e: e.tensor_scalar(out=tmp.rearrange("p a b -> p (a b)"), in0=PS[1][:, :], scalar1=1e-24, scalar2=None, op0=ALU.max), PB[1], [B_tmp])
                S.op("act", lambda e: e.activation(out=tmp, in_=tmp, func=AF.Sqrt), [B_tmp], [B_tmp])
                S.op("dve", lambda e: e.reciprocal(out=tmp, in_=tmp), [B_tmp], [B_tmp])
                S.op("dve", lambda e: e.tensor_tensor(out=kk, in0=kk, in1=tmp, op=ALU.mult), [B_kk, B_tmp], [B_kk])
                S.op("pool", lambda e: e.tensor_scalar(out=k2, in0=aa, scalar1=-1.0, scalar2=None, op0=ALU.add), [B_aa], [B_k2])
                S.op("pool", lambda e: e.tensor_tensor(out=k2, in0=k2, in1=bc4(P_KA), op=ALU.mult), [B_k2, B_prm], [B_k2])
                S.op("dve", lambda e: e.scalar_tensor_tensor(out=k2, in0=k2, scalar=1.0, in1=k_, op0=ALU.add, op1=ALU.mult), [B_k2, B_zs], [B_k2])
                yield
                S.op("dve", lambda e: e.tensor_tensor(out=kkt, in0=kk, in1=cexc, op=ALU.mult), [B_kk, B_cexc], [B_kkt])
                S.op("dve", lambda e: e.tensor_tensor(out=bt, in0=kk, in1=aa, op=ALU.mult), [B_kk, B_aa], [B_bt])
                S.op("dve", lambda e: e.tensor_tensor(out=bt, in0=bt, in1=cinv, op=ALU.mult), [B_bt, B_cinv], [B_bt])
                S.op("pool", lambda e: e.tensor_tensor(out=kt, in0=k2, in1=cinv, op=ALU.mult), [B_k2, B_cinv], [B_kt])
                S.op("pool", lambda e: e.tensor_tensor(out=rt, in0=r_, in1=cinc, op=ALU.mult), [B_zs, B_cinc], [B_rt])
                S.op("pool", lambda e: e.tensor_tensor(out=tmp, in0=r_, in1=k2, op=ALU.mult), [B_zs, B_k2, B_kk], [B_tmp])
                S.op("pool", lambda e: e.tensor_tensor(out=tmp, in0=tmp, in1=bc4(P_RK), op=ALU.mult), [B_tmp, B_prm], [B_tmp])
                for cc in range(4):
                    S.op("pe", lambda e, cc=cc: e.matmul(pq(0, cc), lhsT=cst[:, C_BLK, :], rhs=tmp[:, cc, :], start=True, stop=True), [B_cst, B_tmp], [PB[0][cc]])
                S.op("dve", lambda e: e.tensor_tensor(out=bon, in0=PS[0][:, :].rearrange("p (a b) -> p a b", b=128), in1=v_, op=ALU.mult), PB[0] + [B_zs], [B_bon])
                yield
                for (src, Bsrc, dst, Bdst, bank) in ((v_, B_zs, vtok, B_vtok, 0), (kt, B_kt, ktok, B_ktok, 1), (bt, B_bt, btok, B_btok, 0)):
                    for cc in range(4):
                        S.op("pe", lambda e, src=src, cc=cc, bank=bank: e.transpose(pq(bank, cc), src[:, cc, :], ident), [Bsrc, B_cst], [PB[bank][cc]])
                    S.op("act", lambda e, dst=dst, bank=bank: e.copy(out=dst, in_=PS[bank][:, :]), PB[bank], [Bdst])
                if i == 1:
                    tap("kkt", kkt, B_kkt, [128, 4, 128])
                    tap("rt", rt, B_rt, [128, 4, 128])
                    tap("vtok", vtok, B_vtok, [128, 512])
                yield

            def hv(ap3, par, cc):
                return ap3[64 * par:64 * par + 64, cc, :]

            def neu(i):
                rt, B_rt = FS[i % 3]["rt"]
                kkt, B_kkt = FS[i % 3]["kkt"]
                kt, B_kt = FS[i % 3]["kt"]
                bt, B_bt = FS[i % 3]["bt"]
                bon, B_bon = FS[i % 3]["bon"]
                gT, B_gT = FS[i % 3]["gT"]
                vtok, B_vtok = FS[i % 3]["vtok"]
                ktok, B_ktok = FS[i % 3]["ktok"]
                btok, B_btok = FS[i % 3]["btok"]
                cinc, B_cinc = FS[i % 3]["cinc"]
                KA = dict(KAS)
                KA.update(KAD[i % 2])
                def hv(ap3, par, cc):
                    return ap3[64 * par:64 * par + 64, cc, :]

                def mm_mask(lsrc, Bl, rsrc, Br, mask_idx, kind, eng="dve"):
                    dst, Bd = KA[kind]
                    for par in range(2):
                        bank = nslot()
                        for cc in range(4):
                            S.op("pe", lambda e, par=par, cc=cc, bank=bank: e.matmul(pq(bank, cc), lhsT=hv(lsrc, par, cc), rhs=hv(rsrc, par, cc), start=True, stop=True), [Bl, Br], [PB[bank][0]])
                        S.op(eng, lambda e, par=par, bank=bank: e.tensor_tensor(out=dst[:, par * 4:par * 4 + 4, :], in0=PS[bank][:, :].rearrange("p (a b) -> p a b", b=128),
                                                                           in1=cst[:, mask_idx, :].unsqueeze(1).to_broadcast([128, 4, 128]), op=ALU.mult), [PB[bank][0], B_cst], [Bd[par]])

                mm_mask(bt, B_bt, kkt, B_kkt, C_MSN, "Xa")
                yield
                mm_mask(kkt, B_kkt, bt, B_bt, C_MLN, "XTa")
                yield
                mm_mask(kt, B_kt, kkt, B_kkt, C_MS, "Mk")
                yield
                mm_mask(kt, B_kt, rt, B_rt, C_MI, "Ak")
                yield
                mm_mask(bt, B_bt, rt, B_rt, C_MI, "Ab")
                yield
                for par in range(2):
                    S.op("dve", lambda e, par=par: e.tensor_tensor(out=KA["Ta"][0][:, par * 4:par * 4 + 4, :], in0=KA["Xa"][0][:, par * 4:par * 4 + 4, :],
                                                                in1=ident.unsqueeze(1).to_broadcast([128, 4, 128]), op=ALU.add), [KA["Xa"][1][par], B_cst], [KA["Ta"][1][par]])
                X, XT, T = "Xa", "XTa", "Ta"
                Xn, XTn, Tn = "Xb", "XTb", "Tb"

                def lvl_mm(lk, rk, outk, eng, addk=None):
                    la, lB = KA[lk]
                    ra, rB = KA[rk]
                    oa, oB = KA[outk]
                    for par in range(2):
                        bank = nslot()
                        for cc in range(4):
                            hi = par * 4 + cc
                            S.op("pe", lambda e, hi=hi, cc=cc, bank=bank: e.matmul(pq(bank, cc), lhsT=la[:, hi, :], rhs=ra[:, hi, :], start=True, stop=True), [lB[par], rB[par]], [PB[bank][0]])
                        if addk is None:
                            S.op(eng, lambda e, par=par, bank=bank: e.copy(out=oa[:, par * 4:par * 4 + 4, :], in_=PS[bank][:, :].rearrange("p (a b) -> p a b", b=128)), [PB[bank][0]], [oB[par]])
                        else:
                            aa_, aB = KA[addk]
                            S.op(eng, lambda e, par=par, bank=bank: e.tensor_tensor(out=oa[:, par * 4:par * 4 + 4, :], in0=PS[bank][:, :].rearrange("p (a b) -> p a b", b=128),
                                                                               in1=aa_[:, par * 4:par * 4 + 4, :], op=ALU.add), [PB[bank][0], aB[par]], [oB[par]])

                for l in range(6):
                    if l < 5:
                        lvl_mm(XT, X, Xn, "act")
                    lvl_mm(X, XT, XTn, "act")
                    yield
                    lvl_mm(XTn, T, Tn, "dve", addk=T)
                    yield
                    X, Xn = Xn, X
                    XT, XTn = XTn, XT
                    T, Tn = Tn, T
                yield
                assert T == "Ta"
                yield

            def stt(i):
                rt, B_rt = FS[i % 3]["rt"]
                kkt, B_kkt = FS[i % 3]["kkt"]
                kt, B_kt = FS[i % 3]["kt"]
                bt, B_bt = FS[i % 3]["bt"]
                bon, B_bon = FS[i % 3]["bon"]
                gT, B_gT = FS[i % 3]["gT"]
                vtok, B_vtok = FS[i % 3]["vtok"]
                ktok, B_ktok = FS[i % 3]["ktok"]
                btok, B_btok = FS[i % 3]["btok"]
                cinc, B_cinc = FS[i % 3]["cinc"]
                KA = KAD[i % 2]
                Tinv, B_Tinv = KA["Ta"]
                Mk8, B_Mk8 = KA["Mk"]
                Ak8, B_Ak8 = KA["Ak"]
                Ab8, B_Ab8 = KA["Ab"]
                heads = [(par, cc) for par in range(2) for cc in range(4)]
                for (par, cc) in heads:
                    hi = par * 4 + cc
                    vt_h = vtok[:, cc * 128 + 64 * par: cc * 128 + 64 * par + 64]
                    S.op("pe", lambda e, par=par, cc=cc, hi=hi: e.matmul(PS[6][:, hi * 64:(hi + 1) * 64], lhsT=hv(kkt, par, cc), rhs=ST[64 * par:64 * par + 64, cc, :], start=True, stop=False),
                         [B_kkt, B_STp[par]], [PB[6][0]])
                    S.op("pe", lambda e, hi=hi, vt_h=vt_h: e.matmul(PS[6][:, hi * 64:(hi + 1) * 64], lhsT=Mk8[:, hi, :], rhs=vt_h, start=False, stop=True), [B_Mk8[par], B_vtok], [PB[6][0]])
                S.op("act", lambda e: e.mul(XTs8.rearrange("p a b -> p (a b)"), PS[6][:, :], -1.0), [PB[6][0]], [B_XTs8])
                yield
                for (par, cc) in heads:
                    hi = par * 4 + cc
                    S.op("pe", lambda e, hi=hi: e.matmul(PS[7][:, hi * 64:(hi + 1) * 64], lhsT=Tinv[:, hi, :], rhs=XTs8[:, hi, :], start=True, stop=True), [B_Tinv[par], B_XTs8], [PB[7][0]])
                S.op("act", lambda e: e.copy(out=NTs8.rearrange("p a b -> p (a b)"), in_=PS[7][:, :]), [PB[7][0]], [B_NTs8])
                yield
                for (par, cc) in heads:
                    hi = par * 4 + cc
                    h = 2 * cc + par
                    vt_h = vtok[:, cc * 128 + 64 * par: cc * 128 + 64 * par + 64]
                    osl = PS[5][:, h * 64:(h + 1) * 64]
                    S.op("pe", lambda e, par=par, cc=cc, osl=osl: e.matmul(osl, lhsT=hv(rt, par, cc), rhs=ST[64 * par:64 * par + 64, cc, :], start=True, stop=False), [B_rt, B_STp[par]], [PB[5][0]])
                    S.op("pe", lambda e, hi=hi, osl=osl: e.matmul(osl, lhsT=Ab8[:, hi, :], rhs=NTs8[:, hi, :], start=False, stop=False), [B_Ab8[par], B_NTs8], [PB[5][0]])
                    S.op("pe", lambda e, hi=hi, osl=osl, vt_h=vt_h: e.matmul(osl, lhsT=Ak8[:, hi, :], rhs=vt_h, start=False, stop=True), [B_Ak8[par], B_vtok], [PB[5][0]])
                yield
                for (par, cc) in heads:
                    hi = par * 4 + cc
                    vt_h = vtok[:, cc * 128 + 64 * par: cc * 128 + 64 * par + 64]
                    S.op("pe", lambda e, hi=hi, cc=cc: e.matmul(PS[6][:, hi * 64:(hi + 1) * 64], lhsT=btok[:, cc * 128:(cc + 1) * 128], rhs=NTs8[:, hi, :], start=True, stop=False), [B_btok, B_NTs8], [PB[6][0]])
                    S.op("pe", lambda e, hi=hi, cc=cc, vt_h=vt_h: e.matmul(PS[6][:, hi * 64:(hi + 1) * 64], lhsT=ktok[:, cc * 128:(cc + 1) * 128], rhs=vt_h, start=False, stop=True), [B_ktok, B_vtok], [PB[6][0]])
                for par in range(2):
                    sl = slice(64 * par, 64 * par + 64)
                    S.op("dve", lambda e, par=par, sl=sl: e.tensor_tensor(out=tmpS[sl, :, :], in0=PS[6][sl, par * 256:(par + 1) * 256].rearrange("p (c v) -> p c v", v=64), in1=ST[sl, :, :], op=ALU.add),
                         [PB[6][0], B_STp[par]], [B_tmpS])
                    S.op("dve", lambda e, par=par, sl=sl: e.tensor_tensor(out=ST[sl, :, :], in0=tmpS[sl, :, :], in1=cinc[sl, :, 127:128].to_broadcast([64, 4, 64]), op=ALU.mult),
                         [B_tmpS, B_cinc], [B_STp[par]])
                if i == 0:
                    return
                yield
                S.op("act", lambda e: e.copy(out=osb, in_=PS[5][:, :]), PB[5], [B_osb])
                S.op("act", lambda e: e.activation(out=osq, in_=PS[5][:, :], func=AF.Square), PB[5], [B_osq])
                if i == 1:
                    tap("osb", osb, B_osb, [128, 512])
                o3 = osb.rearrange("p (h v) -> p h v", v=64)
                S.op("dve", lambda e: e.tensor_reduce(out=gst[:, 0:8], in_=o3, axis=AX.X, op=ALU.add), [B_osb], [B_gst])
                S.op("dve", lambda e: e.tensor_reduce(out=gst[:, 8:16], in_=osq.rearrange("p (h v) -> p h v", v=64), axis=AX.X, op=ALU.add), [B_osq, B_gst], [B_gst])
                S.op("dve", lambda e: e.tensor_scalar(out=gst[:, 0:16], in0=gst[:, 0:16], scalar1=1.0 / 64, scalar2=None, op0=ALU.mult), [B_gst], [B_gst])
                S.op("dve", lambda e: e.tensor_tensor(out=gst[:, 16:24], in0=gst[:, 0:8], in1=gst[:, 0:8], op=ALU.mult), [B_gst], [B_gst])
                S.op("dve", lambda e: e.tensor_tensor(out=gst[:, 16:24], in0=gst[:, 8:16], in1=gst[:, 16:24], op=ALU.subtract), [B_gst], [B_gst])
                S.op("act", lambda e: e.activation(out=gst[:, 24:32], in_=gst[:, 16:24], func=AF.Sqrt, bias=prm[:, P_EPSG:P_EPSG + 1]), [B_gst, B_prm], [B_gst])
                S.op("dve", lambda e: e.reciprocal(out=gst[:, 24:32], in_=gst[:, 24:32]), [B_gst], [B_gst])
                S.op("dve", lambda e: e.tensor_tensor(out=o3, in0=o3, in1=gst[:, 0:8].unsqueeze(2).to_broadcast([128, 8, 64]), op=ALU.subtract), [B_osb, B_gst], [B_osb])
                S.op("dve", lambda e: e.tensor_tensor(out=o3, in0=o3, in1=gst[:, 24:32].unsqueeze(2).to_broadcast([128, 8, 64]), op=ALU.mult), [B_osb, B_gst], [B_osb])
                yield
                for cc in range(4):
                    S.op("pe", lambda e, cc=cc: e.transpose(pq(7, cc), osb[:, cc * 128:(cc + 1) * 128], ident), [B_osb, B_cst], [PB[7][cc]])
                    S.op("dve", lambda e, cc=cc: e.tensor_scalar(out=tmp2[:, cc, :], in0=pq(7, cc), scalar1=prm[:, P_GNG + cc:P_GNG + cc + 1], scalar2=prm[:, P_GNB + cc:P_GNB + cc + 1], op0=ALU.mult, op1=ALU.add),
                         [PB[7][cc], B_prm], [B_tmp2])
                S.op("pool", lambda e: e.tensor_tensor(out=tmp2, in0=tmp2, in1=bon, op=ALU.add), [B_tmp2, B_bon], [B_tmp2])
                S.op("pool", lambda e: e.tensor_tensor(out=orw, in0=tmp2, in1=gT, op=ALU.mult), [B_tmp2, B_gT], [B_orw])
                if i == 1:
                    tap("orw", orw, B_orw, [128, 4, 128], BF16)
                S.dma(ch_orw, orw_s[i], orw, reads=[B_orw], writes=[B_orws])
                yield

            def run_all(g):
                for _ in g:
                    pass

            def adv(g):
                try:
                    next(g)
                    return True
                except StopIteration:
                    return False

            run_all(front(0))
            if NT > 1:
                run_all(front(1))
            run_all(neu(0))
            NNEU = 21
            NFRONT = 10
            for i in range(NT):
                if i < 16:
                    issue_cast(i)
                g_st = stt(i)
                g_neu = neu(i + 1) if i + 1 < NT else iter(())
                g_fr = front(i + 2) if i + 2 < NT else iter(())
                a_st = a_neu = a_fr = True
                rnd = 0
                fdone = 0
                while a_st or a_neu or a_fr:
                    if a_st:
                        a_st = adv(g_st)
                    if a_neu:
                        a_neu = adv(g_neu)
                    want = (rnd + 1) * NFRONT // NNEU + 1 if (a_neu or a_st) else 10 ** 9
                    while a_fr and fdone < want:
                        a_fr = adv(g_fr)
                        fdone += 1
                    rnd += 1
            for k_ in range(min(NT, 16), 16):
                issue_cast(k_)
            tap("STfin", ST, B_STp[0], [128, 4, 64])
        phase_R()
        S.barrier()
        B_h1toks = Buf("h1toks")
        B_h1ns = Buf("h1ns")

        def phase_C():
            top[0] = base_top
            wC, B_wC = alloc("wC", [8, 3072], BF16)
            wco, B_wco = alloc("wco", [4, 1024], BF16)
            wro, B_wro = alloc("wro", [4, 1024], BF16)
            wo, B_wo = alloc("wo", [8, 1024], BF16)
            diag, B_diag = alloc("diag", [124, 128], BF16)
            S.dma(S.chan("wC0"), wC[:, :, 0:1024], w_in_d[:, 0:1024].rearrange("(c p) n -> p c n", p=128), writes=[B_wC], eng="pool")
            S.dma(S.chan("wC1"), wC[:, :, 1024:3072], w_in_d[:, 2816:4864].rearrange("(c p) n -> p c n", p=128), writes=[B_wC], eng="pool")
            S.dma(S.chan("wco"), wco, wco_d.rearrange("(c p) n -> p c n", p=128), writes=[B_wco], eng="pool")
            S.dma(S.chan("wro"), wro, wro_d.rearrange("(c p) n -> p c n", p=128), writes=[B_wro], eng="pool")
            S.dma(S.chan("wo"), wo, wo_d.rearrange("(c p) n -> p c n", p=128), writes=[B_wo], eng="pool")
            for cc in range(4):
                for j in range(31):
                    S.op("dve", lambda e, cc=cc, j=j: e.tensor_scalar(out=diag[:, cc * 31 + j, :], in0=ident, scalar1=prm[:, P_CW + cc * 31 + j:P_CW + cc * 31 + j + 1], scalar2=None, op0=ALU.mult),
                         [B_cst, B_prm], [B_diag])
            xts = [alloc("cxt%d" % k, [8, 128]) for k in range(3)]
            chx = [S.chan("cx%d" % k) for k in range(3)]
            xb, B_xb = alloc("cxb", [8, 128], BF16)
            sq, B_sq = alloc("csq", [8, 128], BF16)
            rs, B_rs = alloc("crs", [128])
            zcs = [alloc("zc%d" % k, [24, 128]) for k in range(2)]
            ubuf, B_ubuf = alloc("ubuf", [4, 158], BF16)
            ysb, B_ysb = alloc("ysb", [4, 128])
            ysq, B_ysq = alloc("ysq", [4, 128])
            mv, B_mv = alloc("mv", [128])
            actc, B_actc = alloc("actc", [4, 128], BF16)
            orwt, B_orwt = alloc("orwt", [4, 128], BF16)
            t1, B_t1 = alloc("t1", [8, 128])
            t2, B_t2 = alloc("t2", [8, 128])
            gateds = [alloc("gated%d" % k, [8, 128], BF16) for k in range(2)]
            t1b, B_t1b = alloc("t1b", [8, 128])
            h1T, B_h1T = alloc("h1T", [8, 128])
            h1sq, B_h1sq = alloc("h1sq", [8, 128], BF16)
            rs2, B_rs2 = alloc("rs2", [128])
            h1n, B_h1n = alloc("h1n", [8, 128], BF16)
            h1tok, B_h1tok = alloc("h1tok", [1024])
            ch_orwl = S.chan("orwl")
            ch_h1tok = S.chan("h1tok")
            ch_h1n = S.chan("h1n")
            S.op("dve", lambda e: e.memset(ubuf, 0.0), [], [B_ubuf])
            def cfront(i):
                zc, B_zc = zcs[i % 2]
                xt, B_xt = xts[i % 3]
                load_x(i, xt, B_xt, chx[i % 3], xb, B_xb, sq, B_sq, rs, B_rs, (1, 0))
                nchunk = 8 if i == 0 else 24
                for gi in range(nchunk // 4):
                    bank = gi % 2
                    for jj in range(4):
                        j = gi * 4 + jj
                        col = j * 128
                        for dc in range(8):
                            S.op("pe", lambda e, bank=bank, jj=jj, dc=dc, col=col: e.matmul(pq(bank, jj), lhsT=wC[:, dc, col:col + 128], rhs=xb[:, dc, :], start=(dc == 0), stop=(dc == 7)),
                                 [B_wC, B_xb], [PB[bank][0]])
                    S.op("dve", lambda e, bank=bank, gi=gi: e.tensor_tensor(out=zc[:, gi * 4:gi * 4 + 4, :], in0=PS[bank][:, :].rearrange("p (a b) -> p a b", b=128),
                                                                         in1=rs.unsqueeze(1).to_broadcast([128, 4, 128]), op=ALU.mult),
                         [PB[bank][0], B_rs], [B_zc])
                yield

            def cb1(i):
                zc, B_zc = zcs[i % 2]
                gated, B_gated = gateds[i % 2]
                S.op("act", lambda e: e.activation(out=zc[:, 4:8, :], in_=zc[:, 4:8, :], func=AF.Sigmoid), [B_zc], [B_zc])
                S.op("dve", lambda e: e.tensor_tensor(out=ubuf[:, :, 30:158], in0=zc[:, 0:4, :], in1=zc[:, 4:8, :], op=ALU.mult), [B_zc], [B_ubuf])
                yield
                for cc in range(4):
                    for j in range(31):
                        S.op("pe", lambda e, cc=cc, j=j: e.matmul(pq(2, cc), lhsT=diag[:, cc * 31 + j, :], rhs=ubuf[:, cc, j:j + 128], start=(j == 0), stop=(j == 30)),
                             [B_diag, B_ubuf], [PB[2][0]])
                S.op("pool", lambda e: e.tensor_copy(out=ubuf[:, :, 0:30], in_=ubuf[:, :, 128:158]), [B_ubuf], [B_ubuf])
                if i == 0:
                    S.op("act", lambda e: e.copy(out=ysb.rearrange("p a b -> p (a b)"), in_=PS[2][:, :]), [PB[2][0]], [B_ysb])
                    return
                yield
                for cc in range(4):
                    S.op("act", lambda e, cc=cc: e.activation(out=ysb[:, cc, :], in_=pq(2, cc), func=AF.Identity, bias=prm[:, P_CB + cc:P_CB + cc + 1]), [PB[2][0], B_prm], [B_ysb])
                    S.op("act", lambda e, cc=cc: e.activation(out=ysq[:, cc, :], in_=pq(2, cc), func=AF.Square, bias=prm[:, P_CB + cc:P_CB + cc + 1]), [PB[2][0], B_prm], [B_ysq])
                for cc in range(4):
                    S.op("pe", lambda e, cc=cc: e.matmul(pq(3, 0), lhsT=cst[:, C_O512, :], rhs=ysb[:, cc, :], start=(cc == 0), stop=(cc == 3)), [B_cst, B_ysb], [PB[3][0]])
                for cc in range(4):
                    S.op("pe", lambda e, cc=cc: e.matmul(pq(3, 1), lhsT=cst[:, C_O512, :], rhs=ysq[:, cc, :], start=(cc == 0), stop=(cc == 3)), [B_cst, B_ysq], [PB[3][0]])
                S.op("act", lambda e: e.activation(out=mv, in_=pq(3, 0), func=AF.Square), [PB[3][0]], [B_mv])
                S.op("dve", lambda e: e.tensor_tensor(out=mv, in0=pq(3, 1), in1=mv, op=ALU.subtract), [PB[3][0], B_mv], [B_mv])
                S.op("act", lambda e: e.activation(out=mv, in_=mv, func=AF.Sqrt, bias=prm[:, P_EPS5:P_EPS5 + 1]), [B_mv, B_prm], [B_mv])
                S.op("dve", lambda e: e.reciprocal(out=mv, in_=mv), [B_mv], [B_mv])
                S.op("dve", lambda e: e.tensor_tensor(out=ysb, in0=ysb, in1=pq(3, 0).unsqueeze(1).to_broadcast([128, 4, 128]), op=ALU.subtract), [B_ysb, PB[3][0]], [B_ysb])
                S.op("dve", lambda e: e.tensor_tensor(out=ysb, in0=ysb, in1=mv.unsqueeze(1).to_broadcast([128, 4, 128]), op=ALU.mult), [B_ysb, B_mv], [B_ysb])
                for cc in range(4):
                    S.op("act", lambda e, cc=cc: e.activation(out=actc[:, cc, :], in_=ysb[:, cc, :], func=AF.Silu, scale=prm[:, P_LNG + cc:P_LNG + cc + 1], bias=prm[:, P_LNB + cc:P_LNB + cc + 1]),
                         [B_ysb, B_prm], [B_actc])
                yield
                S.dma(ch_orwl, orwt, orw_s[i], reads=[B_orws], writes=[B_orwt])
                for m_ in range(8):
                    for cc in range(4):
                        S.op("pe", lambda e, m_=m_, cc=cc: e.matmul(pq(4 + m_ // 4, m_ % 4), lhsT=wco[:, cc, m_ * 128:(m_ + 1) * 128], rhs=actc[:, cc, :], start=(cc == 0), stop=(cc == 3)),
                             [B_wco, B_actc], [PB[4 + m_ // 4][0]])
                S.op("act", lambda e: e.activation(out=zc[:, 8:24, :], in_=zc[:, 8:24, :], func=AF.Sigmoid), [B_zc], [B_zc])
                yield
                for hb_ in range(2):
                    S.op("dve", lambda e, hb_=hb_: e.tensor_tensor(out=t1[:, hb_ * 4:hb_ * 4 + 4, :], in0=PS[4 + hb_][:, :].rearrange("p (a b) -> p a b", b=128), in1=zc[:, 8 + hb_ * 4:12 + hb_ * 4, :], op=ALU.mult),
                         [PB[4 + hb_][0], B_zc], [B_t1])
                for m_ in range(8):
                    for cc in range(4):
                        S.op("pe", lambda e, m_=m_, cc=cc: e.matmul(pq(4 + m_ // 4, m_ % 4), lhsT=wro[:, cc, m_ * 128:(m_ + 1) * 128], rhs=orwt[:, cc, :], start=(cc == 0), stop=(cc == 3)),
                             [B_wro, B_orwt], [PB[4 + m_ // 4][0]])
                yield
                for hb_ in range(2):
                    S.op("dve", lambda e, hb_=hb_: e.tensor_tensor(out=t2[:, hb_ * 4:hb_ * 4 + 4, :], in0=PS[4 + hb_][:, :].rearrange("p (a b) -> p a b", b=128), in1=zc[:, 16 + hb_ * 4:20 + hb_ * 4, :], op=ALU.mult),
                         [PB[4 + hb_][0], B_zc], [B_t2])
                S.op("pool", lambda e: e.tensor_tensor(out=gated, in0=t1, in1=t2, op=ALU.add), [B_t1, B_t2], [B_gated])
                yield


            def cb2(i):
                if i == 0:
                    return
                    yield
                xt, B_xt = xts[i % 3]
                gated, B_gated = gateds[i % 2]
                for m_ in range(8):
                    for kc in range(8):
                        S.op("pe", lambda e, m_=m_, kc=kc: e.matmul(pq(6 + m_ // 4, m_ % 4), lhsT=wo[:, kc, m_ * 128:(m_ + 1) * 128], rhs=gated[:, kc, :], start=(kc == 0), stop=(kc == 7)),
                             [B_wo, B_gated], [PB[6 + m_ // 4][0]])
                for hb_ in range(2):
                    S.op("dve", lambda e, hb_=hb_, xt=xt: e.tensor_tensor(out=h1T[:, hb_ * 4:hb_ * 4 + 4, :], in0=PS[6 + hb_][:, :].rearrange("p (a b) -> p a b", b=128), in1=xt[:, hb_ * 4:hb_ * 4 + 4, :], op=ALU.add),
                         [PB[6 + hb_][0], B_xt], [B_h1T])
                yield
                for m_ in range(8):
                    S.op("pe", lambda e, m_=m_: e.transpose(pq(6 + m_ // 4, m_ % 4), h1T[:, m_, :], ident), [B_h1T, B_cst], [PB[6 + m_ // 4][0]])
                for hb_ in range(2):
                    S.op("act", lambda e, hb_=hb_: e.copy(out=h1tok[:, hb_ * 512:(hb_ + 1) * 512], in_=PS[6 + hb_][:, :]), [PB[6 + hb_][0]], [B_h1tok])
                S.dma(ch_h1tok, h1tok_s[i - 1], h1tok, reads=[B_h1tok], writes=[B_h1toks])
                yield
                S.op("act", lambda e: e.activation(out=h1sq, in_=h1T, func=AF.Square), [B_h1T], [B_h1sq])
                for dc in range(8):
                    S.op("pe", lambda e, dc=dc: e.matmul(pq(6, 0), lhsT=ones_b, rhs=h1sq[:, dc, :], start=(dc == 0), stop=(dc == 7)), [B_onesb, B_h1sq], [PB[6][0]])
                S.op("act", lambda e: e.activation(out=rs2, in_=pq(6, 0), func=AF.Sqrt, scale=1.0 / 1024, bias=prm[:, P_EPS6:P_EPS6 + 1]), [PB[6][0], B_prm], [B_rs2])
                S.op("dve", lambda e: e.reciprocal(out=rs2, in_=rs2), [B_rs2], [B_rs2])
                S.op("dve", lambda e: e.tensor_tensor(out=t1b, in0=h1T, in1=rs2.unsqueeze(1).to_broadcast([128, 8, 128]), op=ALU.mult), [B_h1T, B_rs2], [B_t1b])
                S.op("pool", lambda e: e.tensor_tensor(out=h1n, in0=t1b, in1=prm[:, P_GFFN:P_GFFN + 8].unsqueeze(2).to_broadcast([128, 8, 128]), op=ALU.mult), [B_t1b, B_prm], [B_h1n])
                S.dma(ch_h1n, h1nT_s[i - 1], h1n, reads=[B_h1n], writes=[B_h1ns])
                if i == 1:
                    tap("h1tok", h1tok, B_h1tok, [128, 1024])
                    tap("h1n", h1n, B_h1n, [128, 8, 128], BF16)
                yield

            def _adv(g):
                try:
                    next(g)
                    return True
                except StopIteration:
                    return False

            def _run(g):
                for _ in g:
                    pass

            _run(cfront(0))
            if NT > 1:
                _run(cfront(1))
            _run(cb1(0))
            for i in range(NT):
                g2 = cb2(i)
                g1 = cb1(i + 1) if i + 1 < NT else iter(())
                gf = cfront(i + 2) if i + 2 < NT else iter(())
                a1 = a2 = af = True
                while a1 or a2 or af:
                    if a2:
                        a2 = _adv(g2)
                    if a1:
                        a1 = _adv(g1)
                    if af:
                        af = _adv(gf)
        phase_C()
        S.barrier()

        B_sels = Buf("sels")

        def phase_Q():
            top[0] = base_top
            wq, B_wq = alloc("wq", [8, 2048], BF16)
            skT, B_skT = alloc("skT", [16, 128], BF16)
            S.dma(S.chan("wq"), wq, wq_d.rearrange("(c p) n -> p c n", p=128), writes=[B_wq], eng="pool")
            S.dma(S.chan("skT"), skT, skT_d, writes=[B_skT], eng="pool")
            hns = [alloc("hn%d" % k, [8, 128], BF16) for k in range(2)]
            ch_hns = [S.chan("hn%d" % k) for k in range(2)]
            qT, B_qT = alloc("qT", [16, 128], BF16)
            ssbs = [alloc("ssb%d" % k, [16, 128]) for k in range(2)]
            wk16, _ = alloc("wk16", [16, 128])
            B_wkg = [Buf("wk%d" % g_) for g_ in range(16)]
            B_ssb4s = [[Buf("ssb%d_%d" % (k, g_)) for g_ in range(4)] for k in range(2)]
            B_topsg = [Buf("tops%d" % g_) for g_ in range(16)]
            B_topig = [Buf("topi%d" % g_) for g_ in range(16)]
            B_candh = [Buf("cand%d" % g_) for g_ in range(8)]
            B_bestsh = [Buf("bests%d" % g_) for g_ in range(8)]
            B_bestch = [Buf("bestc%d" % g_) for g_ in range(8)]
            B_eqh = [Buf("eq%d" % g_) for g_ in range(16)]
            B_sel3h = [Buf("sel3_%d" % g_) for g_ in range(16)]
            B_sel3g = Buf("sel3g")
            B_ju2 = Buf("ju2")
            TS3 = [(alloc("tops%d" % k, [16, 16])[0], alloc("topi%d" % k, [16, 16], U32)[0], alloc("topif%d" % k, [16, 16])[0]) for k in range(2)]
            TB3 = [([Buf("tops%d_%d" % (k, g_)) for g_ in range(16)], [Buf("topi%d_%d" % (k, g_)) for g_ in range(16)], Buf("topif%d" % k)) for k in range(2)]
            wk2, _ = alloc("wk2", [8, 256])
            B_wk2h = [Buf("wk2_%d" % h) for h in range(8)]
            cand, B_cand = alloc("cand", [8, 256])
            bests, B_bests = alloc("bests", [8, 16])
            bestc, B_bestc = alloc("bestc", [8, 16], U32)
            ju, B_ju = alloc("ju", [2, 8, 16], U32)
            j1, B_j1 = alloc("j1", [8, 16])
            j2, B_j2 = alloc("j2", [8, 16])
            eq, B_eq = alloc("eq", [16, 16, 16])
            ee, B_ee = alloc("ee", [8, 16])
            zz, B_zz = alloc("zz", [8])
            sel3, B_sel3 = alloc("sel3", [3, 128])
            selT, B_selT = alloc("selT", [3, 128])
            ch_hn = S.chan("hn")
            ch_sel = S.chan("sel")
            iota16 = cst[:, C_IOTA, 0:16]
            def q_front(i):
                hn, B_hn = hns[i % 2]
                ssb = ssbs[i % 2][0]
                B_ssb4 = B_ssb4s[i % 2]
                S.dma(ch_hns[i % 2], hn, h1nT_s[i], reads=[B_h1ns], writes=[B_hn])
                for g_ in range(16):
                    for dc in range(8):
                        S.op("pe", lambda e, g_=g_, dc=dc: e.matmul(pq(g_ // 4, g_ % 4), lhsT=wq[:, dc, g_ * 128:(g_ + 1) * 128], rhs=hn[:, dc, :], start=(dc == 0), stop=(dc == 7)),
                             [B_wq, B_hn], [PB[g_ // 4][0]])
                    if g_ % 4 == 3:
                        S.op("act", lambda e, g_=g_: e.copy(out=qT[:, g_ - 3:g_ + 1, :], in_=PS[g_ // 4][:, :].rearrange("p (a b) -> p a b", b=128)), [PB[g_ // 4][0]], [B_qT])
                for g_ in range(16):
                    S.op("pe", lambda e, g_=g_: e.matmul(pq(4 + g_ // 4, g_ % 4), lhsT=qT[:, g_, :], rhs=skT[:, g_, :], start=True, stop=True), [B_qT, B_skT], [PB[4 + g_ // 4][0]])
                    if g_ % 4 == 3:
                        S.op("act", lambda e, g_=g_: e.copy(out=ssb[:, g_ - 3:g_ + 1, :], in_=PS[4 + g_ // 4][:, :].rearrange("p (a b) -> p a b", b=128)), [PB[4 + g_ // 4][0]], [B_ssb4[g_ // 4]])

            def q_b1(i):
                tops, topi, topif = TS3[i % 2]
                B_topsg, B_topig, B_topif = TB3[i % 2]
                ssb = ssbs[i % 2][0]
                B_ssb4 = B_ssb4s[i % 2]
                for g_ in range(16):
                    S.op("dve", lambda e, g_=g_: e.max(out=tops[:, g_, 0:8], in_=ssb[:, g_, :]), [B_ssb4[g_ // 4]], [B_topsg[g_]])
                yield
                for g_ in range(16):
                    S.op("dve", lambda e, g_=g_: e.max_index(out=topi[:, g_, 0:8], in_max=tops[:, g_, 0:8], in_values=ssb[:, g_, :]), [B_ssb4[g_ // 4], B_topsg[g_]], [B_topig[g_]])
                yield
                for g_ in range(16):
                    S.op("dve", lambda e, g_=g_: e.match_replace(out=wk16[:, g_, :], in_to_replace=tops[:, g_, 0:8], in_values=ssb[:, g_, :], imm_value=-1e30), [B_ssb4[g_ // 4], B_topsg[g_]], [B_wkg[g_]])
                yield
                for g_ in range(16):
                    S.op("dve", lambda e, g_=g_: e.max(out=tops[:, g_, 8:16], in_=wk16[:, g_, :]), [B_wkg[g_]], [B_topsg[g_]])
                yield
                for g_ in range(16):
                    S.op("dve", lambda e, g_=g_: e.max_index(out=topi[:, g_, 8:16], in_max=tops[:, g_, 8:16], in_values=wk16[:, g_, :]), [B_wkg[g_], B_topsg[g_]], [B_topig[g_]])
                S.op("pool", lambda e: e.tensor_copy(out=topif, in_=topi), B_topig, [B_topif])
                yield

            def q_b2(i):
                tops, topi, topif = TS3[i % 2]
                B_topsg, B_topig, B_topif = TB3[i % 2]
                yield
                for h in range(8):
                    S.op("pool", lambda e, h=h: e.tensor_tensor(out=cand[:, h, :].rearrange("p (a b) -> p a b", b=16),
                                                              in0=tops[:, 2 * h, :].unsqueeze(2).to_broadcast([128, 16, 16]),
                                                              in1=tops[:, 2 * h + 1, :].unsqueeze(1).to_broadcast([128, 16, 16]), op=ALU.add), [B_topsg[2 * h], B_topsg[2 * h + 1]], [B_candh[h]])
                yield
                for h in range(8):
                    S.op("dve", lambda e, h=h: e.max(out=bests[:, h, 0:8], in_=cand[:, h, :]), [B_candh[h]], [B_bestsh[h]])
                yield
                for h in range(8):
                    S.op("dve", lambda e, h=h: e.max_index(out=bestc[:, h, 0:8], in_max=bests[:, h, 0:8], in_values=cand[:, h, :]), [B_candh[h], B_bestsh[h]], [B_bestch[h]])
                yield
                for h in range(8):
                    S.op("dve", lambda e, h=h: e.match_replace(out=wk2[:, h, :], in_to_replace=bests[:, h, 0:8], in_values=cand[:, h, :], imm_value=-1e30),
                         [B_candh[h], B_bestsh[h]], [B_wk2h[h]])
                yield
                for h in range(8):
                    S.op("dve", lambda e, h=h: e.max(out=bests[:, h, 8:16], in_=wk2[:, h, :]), [B_wk2h[h]], [B_bestsh[h]])
                yield
                for h in range(8):
                    S.op("dve", lambda e, h=h: e.max_index(out=bestc[:, h, 8:16], in_max=bests[:, h, 8:16], in_values=wk2[:, h, :]),
                         [B_wk2h[h], B_bestsh[h]], [B_bestch[h]])
                S.op("dve", lambda e: e.tensor_single_scalar(out=ju[:, 0, :, :], in_=bestc, scalar=4, op=ALU.logical_shift_right), B_bestch, [B_ju])
                S.op("dve", lambda e: e.tensor_single_scalar(out=ju[:, 1, :, :], in_=bestc, scalar=15, op=ALU.bitwise_and), B_bestch, [B_ju2])
                S.op("pool", lambda e: e.tensor_copy(out=j1, in_=ju[:, 0, :, :]), [B_ju], [B_j1])
                S.op("pool", lambda e: e.tensor_copy(out=j2, in_=ju[:, 1, :, :]), [B_ju2], [B_j2])
                yield
                for half, jj_ in ((0, j1), (1, j2)):
                    Bj = B_j1 if half == 0 else B_j2
                    for h in range(8):
                        S.op("dve", lambda e, h=h, jj_=jj_, half=half: e.tensor_tensor(out=eq[:, half * 8 + h, :, :], in0=jj_[:, h, :].unsqueeze(2).to_broadcast([128, 16, 16]),
                                                                          in1=iota16.unsqueeze(1).to_broadcast([128, 16, 16]), op=ALU.is_equal), [Bj, B_cst], [B_eqh[half * 8 + h]])
                    for h in range(8):
                        S.op("pool", lambda e, h=h, half=half: e.tensor_tensor(out=eq[:, half * 8 + h, :, :], in0=eq[:, half * 8 + h, :, :],
                                                                            in1=topif[:, 2 * h + half, :].unsqueeze(1).to_broadcast([128, 16, 16]), op=ALU.mult), [B_eqh[half * 8 + h], B_topif], [B_eqh[half * 8 + h]])
                yield
                for half in range(2):
                    for h in range(8):
                        S.op("dve", lambda e, h=h, half=half: e.tensor_reduce(out=sel3[:, half, h * 16:(h + 1) * 16], in_=eq[:, half * 8 + h, :, :], axis=AX.X, op=ALU.add), [B_eqh[half * 8 + h]], [B_sel3h[half * 8 + h]])
                B_bests_all = B_bestsh
                yield
                S.op("dve", lambda e: e.tensor_tensor(out=ee, in0=bests, in1=bests[:, :, 0:1].to_broadcast([128, 8, 16]), op=ALU.subtract), B_bestsh, [B_ee])
                S.op("act", lambda e: e.activation(out=ee, in_=ee, func=AF.Exp), [B_ee], [B_ee])
                S.op("dve", lambda e: e.tensor_reduce(out=zz, in_=ee, axis=AX.X, op=ALU.add), [B_ee], [B_zz])
                S.op("dve", lambda e: e.reciprocal(out=zz, in_=zz), [B_zz], [B_zz])
                S.op("dve", lambda e: e.tensor_tensor(out=sel3[:, 2, :].rearrange("p (h j) -> p h j", j=16), in0=ee, in1=zz.unsqueeze(2).to_broadcast([128, 8, 16]), op=ALU.mult), [B_ee, B_zz], [B_sel3g])
                yield
                for k in range(3):
                    S.op("pe", lambda e, k=k: e.transpose(pq(0, k), sel3[:, k, :], ident), B_sel3h + [B_sel3g, B_cst], [PB[0][0]])
                S.op("act", lambda e: e.copy(out=selT.rearrange("p a b -> p (a b)"), in_=PS[0][:, 0:384]), [PB[0][0]], [B_selT])
                S.dma(ch_sel, sel_s[i], selT, reads=[B_selT], writes=[B_sels])
                if i == 0:
                    tap("sel3", sel3, B_sel3g, [128, 3, 128])


                yield
            def _adv(g):
                try:
                    next(g)
                    return True
                except StopIteration:
                    return False

            q_front(0)
            if NRT > 1:
                q_front(1)
            for _ in q_b1(0):
                pass
            for i in range(NRT):
                if i + 2 < NRT:
                    q_front(i + 2)
                g2 = q_b2(i)
                g1 = q_b1(i + 1) if i + 1 < NRT else iter(())
                a1 = a2 = True
                while a1 or a2:
                    if a2:
                        a2 = _adv(g2)
                    if a2:
                        a2 = _adv(g2)
                    if a1:
                        a1 = _adv(g1)
        phase_Q()
        S.barrier()

        def phase_E():
            top[0] = base_top
            NS = NRT // 2
            act3s = [alloc("act3_%d" % k, [256, 128], BF16) for k in range(2)]
            hn2s = [alloc("hn2_%d" % k, [8, 256], BF16) for k in range(2)]
            selAs = [alloc("selA_%d" % k, [2, 3, 128]) for k in range(2)]
            Ub = [alloc("Ub%d" % k, [8, 128], BF16) for k in range(8)]
            Vb = [alloc("Vb%d" % k, [1024], BF16) for k in range(8)]
            Aoh = [alloc("Aoh%d" % k, [4, 128], BF16) for k in range(4)]
            Boh = [alloc("Boh%d" % k, [4, 128], BF16) for k in range(4)]
            ysb, B_ysb = alloc("eysb", [1024])
            h1t, B_h1t = alloc("h1t", [1024])
            gfin, B_gfin = alloc("gfin", [1024])
            ob, B_ob = alloc("ob", [1024])
            stat, B_stat = alloc("stat", [4])
            S.dma(S.chan("gfin"), gfin, gfin_d.partition_broadcast(128)[:, 0, :], writes=[B_gfin])
            ch_hn2 = [S.chan("hn2_%d" % k) for k in range(2)]
            ch_selA = [S.chan("selA_%d" % k) for k in range(2)]
            ch_U = [S.chan("U%d" % k) for k in range(8)]
            ch_V = [S.chan("V%d" % k) for k in range(8)]
            ch_h1t = S.chan("h1t")
            ch_out = S.chan("out")
            iota_bc = cst[:, C_IOTA, :].unsqueeze(1).to_broadcast([128, 4, 128])
            uctr = [0]
            actr = [0]

            def loads(s_):
                hn2, B_hn2 = hn2s[s_ % 2]
                selA, B_selA = selAs[s_ % 2]
                for ts in range(2):
                    S.dma(ch_hn2[s_ % 2], hn2[:, :, ts * 128:(ts + 1) * 128], h1nT_s[s_ * 2 + ts], reads=[B_h1ns], writes=[B_hn2])
                    S.dma(ch_selA[s_ % 2], selA[:, ts, :, :], sel_s[s_ * 2 + ts], reads=[B_sels], writes=[B_selA])

            st_seq = [0, 0]
            st_fifo = [[], []]
            st_total = NS * 128

            def stream_issue(w):
                r = st_seq[w]
                st_seq[w] += 1
                slot = r % 8
                if w == 0:
                    U_, B_U = Ub[slot]
                    S.dma(ch_U[slot], U_.rearrange("p b c -> p (b c)"), uT_b[r % 128], reads=[B_uTb], writes=[B_U])
                else:
                    V_, B_V = Vb[slot]
                    S.dma(ch_V[slot], V_, vP_b[r % 128], reads=[B_vPb], writes=[B_V])
                st_fifo[w].append(slot)

            def stream_refill(w):
                while len(st_fifo[w]) < 7 and st_seq[w] < st_total:
                    stream_issue(w)

            def stream_get(w):
                if not st_fifo[w]:
                    stream_issue(w)
                slot = st_fifo[w].pop(0)
                return (Ub if w == 0 else Vb)[slot]

            def a_iter(s_, i2):
                act3, B_act3 = act3s[s_ % 2]
                hn2, B_hn2 = hn2s[s_ % 2]
                U_, B_U = stream_get(0)
                bank = (actr[0] // 2) % 2
                half = actr[0] % 2
                actr[0] += 1
                for dc in range(8):
                    S.op("pe", lambda e, dc=dc: e.matmul(PS[bank][:, half * 256:(half + 1) * 256], lhsT=U_[:, dc, :], rhs=hn2[:, dc, :], start=(dc == 0), stop=(dc == 7)), [B_U, B_hn2], [PB[bank][0]])
                if half == 1:
                    S.op("act", lambda e: e.activation(out=act3[:, :, i2 - 1:i2 + 1], in_=PS[bank][:, :].rearrange("p (i t) -> p t i", i=2), func=AF.Gelu), [PB[bank][0]], [B_act3])
                stream_refill(0)

            def b_vars(s_, tg):
                act3, B_act3 = act3s[s_ % 2]
                selA, B_selA = selAs[s_ % 2]
                t0 = tg * 4
                A_, B_A = Aoh[tg % 4]
                Bm, B_B = Boh[tg % 4]
                return act3, B_act3, selA, B_selA, t0, t0 // 128, t0 % 128, A_, B_A, Bm, B_B, 2 + tg % 2

            def b_stage1(s_, tg):
                act3, B_act3, selA, B_selA, t0, ti, tt, A_, B_A, Bm, B_B, gb = b_vars(s_, tg)
                S.op("dve", lambda e: e.tensor_tensor(out=A_, in0=iota_bc, in1=selA[:, ti, 0, tt:tt + 4].unsqueeze(2).to_broadcast([128, 4, 128]), op=ALU.is_equal), [B_cst, B_selA], [B_A])
                S.op("pool", lambda e: e.tensor_tensor(out=A_, in0=A_, in1=selA[:, ti, 2, tt:tt + 4].unsqueeze(2).to_broadcast([128, 4, 128]), op=ALU.mult), [B_A, B_selA], [B_A])
                S.op("dve", lambda e: e.tensor_tensor(out=Bm, in0=iota_bc, in1=selA[:, ti, 1, tt:tt + 4].unsqueeze(2).to_broadcast([128, 4, 128]), op=ALU.is_equal), [B_cst, B_selA], [B_B])

            def b_stage2(s_, tg):
                act3, B_act3, selA, B_selA, t0, ti, tt, A_, B_A, Bm, B_B, gb = b_vars(s_, tg)
                for tk in range(4):
                    S.op("pe", lambda e, tk=tk: e.matmul(pq(gb, tk), lhsT=A_[:, tk, :], rhs=Bm[:, tk, :], start=True, stop=True), [B_A, B_B], [PB[gb][0]])

            def b_stage3(s_, tg):
                act3, B_act3, selA, B_selA, t0, ti, tt, A_, B_A, Bm, B_B, gb = b_vars(s_, tg)
                S.op("dve", lambda e: e.tensor_tensor(out=act3[:, t0:t0 + 4, :], in0=PS[gb][:, :].rearrange("p (a b) -> p a b", b=128), in1=act3[:, t0:t0 + 4, :], op=ALU.mult),
                     [PB[gb][0], B_act3], [B_act3])

            def c_phase(s_):
                act3, B_act3 = act3s[s_ % 2]
                vctr = 0
                for i2 in range(128):
                    V_, B_V = stream_get(1)
                    for ts in range(2):
                        for dh in range(2):
                            bk = 4 + ts * 2 + dh
                            S.op("pe", lambda e, V_=V_, i2=i2, ts=ts, dh=dh, bk=bk: e.matmul(PS[bk][:, :], lhsT=act3[:, ts * 128:(ts + 1) * 128, i2], rhs=V_[:, dh * 512:(dh + 1) * 512],
                                                                                       start=(i2 == 0), stop=(i2 == 127)), [B_act3, B_V], [PB[bk][0]])
                    stream_refill(1)

            def d_phase(s_):
                for ts in range(2):
                    gi_ = s_ * 2 + ts
                    S.dma(ch_h1t, h1t, h1tok_s[gi_], reads=[B_h1toks], writes=[B_h1t])
                    for dh in range(2):
                        bk = 4 + ts * 2 + dh
                        S.op("dve", lambda e, bk=bk, dh=dh: e.tensor_tensor(out=ysb[:, dh * 512:(dh + 1) * 512], in0=PS[bk][:, :], in1=h1t[:, dh * 512:(dh + 1) * 512], op=ALU.add),
                             [PB[bk][0], B_h1t], [B_ysb])
                    S.op("pool", lambda e: e.tensor_tensor(out=ob, in0=ysb, in1=ysb, op=ALU.mult), [B_ysb], [B_ob])
                    S.op("dve", lambda e: e.tensor_reduce(out=stat[:, 0:1], in_=ob, axis=AX.X, op=ALU.add), [B_ob], [B_stat])
                    S.op("act", lambda e: e.activation(out=stat[:, 1:2], in_=stat[:, 0:1], func=AF.Sqrt, scale=1.0 / 1024, bias=prm[:, P_EPS6:P_EPS6 + 1]), [B_stat, B_prm], [B_stat])
                    S.op("dve", lambda e: e.reciprocal(out=stat[:, 2:3], in_=stat[:, 1:2]), [B_stat], [B_stat])
                    S.op("dve", lambda e: e.scalar_tensor_tensor(out=ob, in0=ysb, scalar=stat[:, 2:3], in1=gfin, op0=ALU.mult, op1=ALU.mult), [B_ysb, B_stat, B_gfin], [B_ob])
                    o_ = S.dma(ch_out, out_d[gi_ * 128:(gi_ + 1) * 128, :], ob, reads=[B_ob])
                    S.final_waits.append(o_)

            loads(0)
            stream_refill(0)
            for i2 in range(128):
                a_iter(0, i2)
            stream_refill(1)
            for s_ in range(NS):
                nxt = s_ + 1 < NS
                if nxt:
                    loads(s_ + 1)
                b_stage1(s_, 0)
                b_stage1(s_, 1)
                b_stage2(s_, 0)
                for tg in range(64):
                    if tg + 2 < 64:
                        b_stage1(s_, tg + 2)
                    if tg + 1 < 64:
                        b_stage2(s_, tg + 1)
                    if nxt:
                        a_iter(s_ + 1, 2 * tg)
                        a_iter(s_ + 1, 2 * tg + 1)
                    b_stage3(s_, tg)
                    if tg == 3 and s_ > 0:
                        d_phase(s_ - 1)
                c_phase(s_)
            d_phase(NS - 1)
        phase_E()
        S.barrier()
        S.emit(st)
    return nc, dbg


def host_prep(inp, b, NT):
    f = np.float32
    x = np.asarray(inp["x"])[b]
    nreal = (NT - 1) * 128
    seq = np.concatenate([np.zeros((NPAD, D), f), np.asarray(inp["meta_tokens"], f), x[:nreal]], axis=0)
    xT = np.ascontiguousarray(seq.reshape(NT, 128, 8, 128).transpose(0, 3, 2, 1))
    m = {"xT": xT}
    return m


def shared_prep(inp):
    f = np.float32
    g = lambda k: np.asarray(inp[k], f)
    m = {}
    m["w_in"] = np.ascontiguousarray(g("w_in")[0])
    m["w_conv_out"] = np.ascontiguousarray(g("w_conv_out")[0])
    m["w_rwkv_out"] = np.ascontiguousarray(g("w_rwkv_out")[0])
    m["w_o"] = np.ascontiguousarray(g("w_o")[0])
    m["w_q"] = np.ascontiguousarray(g("w_q")[0])
    m["skT"] = np.ascontiguousarray(g("sub_keys")[0].transpose(3, 0, 1, 2).reshape(128, 16, 128))
    u = g("expert_u")[0]
    m["uT"] = np.ascontiguousarray(u.reshape(128, 128, 8, 128).transpose(1, 3, 2, 0)).reshape(128, 128, 1024)
    v = g("expert_v")[0]
    m["vP"] = np.ascontiguousarray(v.reshape(128, 128, 1024).transpose(1, 0, 2))
    m["wa_up"] = np.ascontiguousarray(np.concatenate([g("w_up")[0], g("a_up")[0]], axis=0))
    m["g_up"] = np.ascontiguousarray(g("g_up")[0])
    m["w0row"] = np.ascontiguousarray(g("w0")[0].reshape(1, 512))
    m["gfin"] = np.ascontiguousarray(g("g_final").reshape(1, 1024))
    prm = np.zeros((128, NPRM), f)
    col = lambda a, n: np.asarray(a, f).reshape(n, 128).T
    prm[:, P_GMIX:P_GMIX + 8] = col(g("g_mix")[0], 8)
    prm[:, P_MU:P_MU + 14] = col(g("mu_shift")[0], 14)
    prm[:, P_A0:P_A0 + 4] = col(g("a0")[0], 4)
    prm[:, P_KK:P_KK + 4] = col(g("k_k")[0], 4)
    prm[:, P_KA:P_KA + 4] = col(g("k_a")[0], 4)
    prm[:, P_RK:P_RK + 4] = col(g("r_k")[0].reshape(512), 4)
    prm[:, P_GNG:P_GNG + 4] = col(g("gn_g")[0], 4)
    prm[:, P_GNB:P_GNB + 4] = col(g("gn_b")[0], 4)
    prm[:, P_CB:P_CB + 4] = col(g("conv_b")[0], 4)
    prm[:, P_LNG:P_LNG + 4] = col(g("conv_ln_g")[0], 4)
    prm[:, P_LNB:P_LNB + 4] = col(g("conv_ln_b")[0], 4)
    prm[:, P_GFFN:P_GFFN + 8] = col(g("g_ffn")[0], 8)
    prm[:, P_EPS6] = 1e-6
    prm[:, P_EPS5] = 1e-5
    prm[:, P_EPSG] = 64e-5
    cw = g("conv_w")[0]
    prm[:, P_CW:P_CW + 124] = cw.reshape(31, 4, 128).transpose(2, 1, 0).reshape(128, 124)
    m["params"] = prm
    cst = np.zeros((128, NCST, 128), f)
    ar = np.arange(128)
    cst[:, C_IDENT] = np.eye(128)
    cst[:, C_ONES] = 1.0
    cst[:, C_O512] = 1.0 / 512
    cst[:, C_BLK] = (ar[:, None] // 64 == ar[None, :] // 64)
    e05 = float(np.exp(np.float32(-0.5)))
    cst[:, C_TRII] = -e05 * (ar[:, None] <= ar[None, :])
    cst[:, C_TRIE] = -e05 * (ar[:, None] < ar[None, :])
    cst[:, C_MS] = (ar[:, None] < ar[None, :])
    cst[:, C_MSN] = -1.0 * (ar[:, None] < ar[None, :])
    cst[:, C_MLN] = -1.0 * (ar[None, :] < ar[:, None])
    cst[:, C_MI] = (ar[:, None] <= ar[None, :])
    cst[:, C_IOTA] = ar[None, :]
    cst[:, C_O1024] = 1.0 / 1024
    m["cst"] = cst
    return m


_CACHE = {}


def kernel(**inputs):
    NT = 33
    if "nc" not in _CACHE:
        _CACHE["nc"] = build_program(NT)[0]
    nc = _CACHE["nc"]
    sh = shared_prep(inputs)
    in_maps = []
    for b in range(8):
        m = dict(sh)
        m.update(host_prep(inputs, b, NT))
        in_maps.append(m)
    res = run_bass_kernel_spmd(nc, in_maps, core_ids=list(range(8)))
    return np.stack([r["out"] for r in res.results], axis=0)
```

```python
import numpy as np
import concourse.bass as bass
import concourse.mybir as mybir
from concourse.bass_utils import run_bass_kernel_spmd
from contextlib import ExitStack

F32 = mybir.dt.float32
BF16 = mybir.dt.bfloat16
U32 = mybir.dt.uint32
ALU = mybir.AluOpType
AF = mybir.ActivationFunctionType
AX = mybir.AxisListType


class Buf:
    __slots__ = ("name", "w", "r", "excl")

    def __init__(self, name, excl=False):
        self.name = name
        self.w = None
        self.r = []
        self.excl = excl


class Op:
    __slots__ = ("eng", "fn", "deps", "signal", "is_dma", "chan", "sem", "val")

    def __init__(self, eng, fn, is_dma=False, chan=None):
        self.eng = eng
        self.fn = fn
        self.deps = []
        self.signal = False
        self.is_dma = is_dma
        self.chan = chan
        self.sem = None
        self.val = 0


class Chan:
    __slots__ = ("name", "last", "count", "sem")

    def __init__(self, name):
        self.name = name
        self.last = None
        self.count = 0
        self.sem = None


EPOCH = 30000
CAST = True
ENGS = ("pe", "dve", "act", "pool", "sp")


class Sched:
    def __init__(self, nc):
        self.nc = nc
        self.ops = {e: [] for e in ENGS}
        self.chans = []
        self.final_waits = []

    def chan(self, name):
        c = Chan(name)
        self.chans.append(c)
        return c

    def _record(self, op, reads, writes):
        writes = writes + [b for b in reads if b.excl and not any(b is x for x in writes)]
        deps = []
        for b in reads:
            if b.w is not None:
                deps.append(b.w)
        for b in writes:
            if b.w is not None:
                deps.append(b.w)
            for r in b.r:
                if r.eng == op.eng and not r.is_dma and not op.is_dma:
                    continue
                deps.append(r)
        seen = set(id(d) for d in op.deps)
        for d in deps:
            if d is op or id(d) in seen:
                continue
            if d.eng == "pe" and op.eng == "pe" and not d.is_dma and not op.is_dma:
                continue
            seen.add(id(d))
            op.deps.append(d)
            d.signal = True
        for b in reads:
            b.r.append(op)
        for b in writes:
            b.w = op
            b.r = []
        self.ops[op.eng].append(op)
        return op

    def op(self, eng, fn, reads=(), writes=()):
        return self._record(Op(eng, fn), list(reads), list(writes))

    def dma(self, chan, out, in_, reads=(), writes=(), eng="sp", **kw):
        op = Op(eng, lambda e: e.dma_start(out=out, in_=in_, **kw), is_dma=True, chan=chan)
        if chan.last is not None:
            op.deps.append(chan.last)
            chan.last.signal = True
        chan.last = op
        op.signal = True
        return self._record(op, list(reads), list(writes))

    def barrier(self):
        lasts = []
        for e in ENGS:
            for o in reversed(self.ops[e]):
                if not o.is_dma and o.fn is not None:
                    lasts.append(o)
                    break
        for c in self.chans:
            if c.last is not None:
                lasts.append(c.last)
        for e in ENGS:
            op = Op(e, None)
            for d in lasts:
                if d.eng == e and not d.is_dma:
                    continue
                op.deps.append(d)
                d.signal = True
            self.ops[e].append(op)

    def emit(self, stack):
        nc = self.nc
        for c in self.chans:
            c.sem = stack.enter_context(nc.semaphore("c_" + c.name))
        for d in self.final_waits:
            d.signal = True
        for e, lst in self.ops.items():
            cur = None
            cnt = 0
            k = 0
            for op in lst:
                if op.is_dma:
                    op.chan.count += 16
                    op.sem = op.chan.sem
                    op.val = op.chan.count
                elif op.signal:
                    if cur is None or cnt >= EPOCH:
                        cur = stack.enter_context(nc.semaphore("e_%s_%d" % (e, k)))
                        k += 1
                        cnt = 0
                    cnt += 1
                    op.sem = cur
                    op.val = cnt
        block = stack.enter_context(nc.Block())

        def run(engine_obj, lst, tail=None):
            waited = {}

            def w(d):
                key = d.sem.name
                if waited.get(key, 0) >= d.val:
                    return
                engine_obj.wait_ge(d.sem, d.val)
                waited[key] = d.val

            for op in lst:
                for d in op.deps:
                    w(d)
                if op.fn is None:
                    continue
                ins = op.fn(engine_obj)
                if op.is_dma:
                    ins.then_inc(op.sem, 16)
                elif op.signal:
                    ins.then_inc(op.sem, 1)
            if tail:
                for d in tail:
                    w(d)

        ops = self.ops
        fw = self.final_waits

        @block.tensor
        def _(e):
            run(e, ops["pe"])

        @block.vector
        def _(e):
            run(e, ops["dve"])

        @block.scalar
        def _(e):
            run(e, ops["act"])

        @block.gpsimd
        def _(e):
            run(e, ops["pool"])

        @block.sync
        def _(e):
            run(e, ops["sp"], tail=fw)


D = 1024
NPAD = 112
C_IDENT, C_ONES, C_O512, C_BLK, C_TRII, C_TRIE, C_MS, C_MSN, C_MLN, C_MI, C_IOTA, C_O1024 = range(12)
NCST = 12
P_GMIX = 0
P_MU = 8
P_A0 = 22
P_KK = 26
P_KA = 30
P_RK = 34
P_GNG = 38
P_GNB = 42
P_CB = 46
P_LNG = 50
P_LNB = 54
P_GFFN = 58
P_EPS6 = 66
P_EPS5 = 67
P_EPSG = 68
P_CW = 69
NPRM = 69 + 124


def build_program(NT, debug=False):
    NRT = NT - 1
    assert NRT % 2 == 0
    nc = bass.Bass("TRN2", target_bir_lowering=False)
    din = lambda n, s, dt=F32: nc.dram_tensor(n, s, dt, kind="ExternalInput").ap()
    xT_d = din("xT", [NT, 128, 8, 128])
    w_in_d = din("w_in", [1024, 4864])
    wco_d = din("w_conv_out", [512, 1024])
    wro_d = din("w_rwkv_out", [512, 1024])
    wo_d = din("w_o", [1024, 1024])
    wq_d = din("w_q", [1024, 2048])
    skT_d = din("skT", [128, 16, 128])
    uT_d = din("uT", [128, 128, 8 * 128])
    vP_d = din("vP", [128, 128, 1024])
    waup_d = din("wa_up", [128, 512])
    gup_d = din("g_up", [128, 512])
    prm_d = din("params", [128, NPRM])
    w0_d = din("w0row", [1, 512])
    cst_d = din("cst", [128, NCST, 128])
    gfin_d = din("gfin", [1, 1024])
    out_d = nc.dram_tensor("out", [NRT * 128, 1024], F32, kind="ExternalOutput").ap()
    dscr = lambda n, s, dt: nc.dram_tensor(n, s, dt, kind="Internal").ap()
    orw_s = dscr("orw_s", [NT, 128, 4, 128], BF16)
    h1tok_s = dscr("h1tok_s", [NRT, 128, 1024], F32)
    h1nT_s = dscr("h1nT_s", [NRT, 128, 8, 128], BF16)
    sel_s = dscr("sel_s", [NRT, 128, 3, 128], F32)
    uT_b = dscr("uT_b", [128, 128, 8 * 128], BF16)
    vP_b = dscr("vP_b", [128, 128, 1024], BF16)
    dbg = {}

    st = ExitStack()
    with st:
        S = Sched(nc)
        ARENA = 53000
        arena = st.enter_context(nc.sbuf_tensor("arena", [128, ARENA], F32))
        PS = [st.enter_context(nc.psum_tensor("ps%d" % k, [128, 512], F32)) for k in range(8)]
        PB = []
        for k in range(8):
            _b = Buf("ps%d" % k, excl=True)
            PB.append([_b, _b, _b, _b])
        top = [0]

        def alloc(name, free, dt=F32):
            n = int(np.prod(free))
            words = n if dt in (F32, U32) else (n + 1) // 2
            words = (words + 7) // 8 * 8
            a = arena[:, top[0]:top[0] + words]
            top[0] += words
            assert top[0] <= ARENA, (name, top[0])
            if dt != F32:
                a = a.bitcast(dt)
            a = a[:, 0:n]
            if len(free) == 2:
                a = a.rearrange("p (a b) -> p a b", b=free[1])
            elif len(free) == 3:
                a = a.rearrange("p (a b c) -> p a b c", b=free[1], c=free[2])
            return a, Buf(name)

        def pq(k, q):
            return PS[k][:, q * 128:(q + 1) * 128]

        def tap(name, ap, buf, shape, dt=F32):
            if not debug:
                return
            d = nc.dram_tensor("dbg_" + name, list(shape), dt, kind="ExternalOutput").ap()
            c = S.chan("dbg_" + name)
            o = S.dma(c, d, ap, reads=[buf])
            S.final_waits.append(o)
            dbg[name] = (shape, dt)

        cst, B_cst = alloc("cst", [NCST, 128])
        prm, B_prm = alloc("prm", [NPRM])
        ones_b, B_onesb = alloc("ones_b", [128], BF16)
        ch_c = S.chan("cst")
        S.dma(ch_c, cst, cst_d, writes=[B_cst])
        ch_p = S.chan("prm")
        S.dma(ch_p, prm, prm_d, writes=[B_prm])
        S.op("dve", lambda e: e.tensor_copy(out=ones_b, in_=cst[:, C_ONES, :]), [B_cst], [B_onesb])
        ident = cst[:, C_IDENT, :]
        base_top = top[0]

        ch_cast = [S.chan("cast%d" % k) for k in range(4)]
        B_uTb = Buf("uTb")
        B_vPb = Buf("vPb")
        cast_ops = []
        def issue_cast(k):
            if not CAST:
                return
            sl = slice(k * 8, (k + 1) * 8)
            cast_ops.append(S.dma(ch_cast[k % 2], uT_b[sl], uT_d[sl], eng="pool"))
            cast_ops.append(S.dma(ch_cast[2 + k % 2], vP_b[sl], vP_d[sl], eng="pool"))

        def load_x(i, xt, B_xt, ch, xb, B_xb, sq, B_sq, rs, B_rs, psq):
            S.dma(ch, xt, xT_d[i], writes=[B_xt])
            S.op("pool", lambda e: e.tensor_tensor(out=xb, in0=xt, in1=prm[:, P_GMIX:P_GMIX + 8].unsqueeze(2).to_broadcast([128, 8, 128]), op=ALU.mult),
                 [B_xt, B_prm], [B_xb])
            S.op("act", lambda e: e.activation(out=sq, in_=xt, func=AF.Square), [B_xt], [B_sq])
            k, q = psq
            for dc in range(8):
                S.op("pe", lambda e, dc=dc: e.matmul(pq(k, q), lhsT=ones_b, rhs=sq[:, dc, :], start=(dc == 0), stop=(dc == 7)),
                     [B_onesb, B_sq], [PB[k][q]])
            S.op("act", lambda e: e.activation(out=rs, in_=pq(k, q), func=AF.Sqrt, scale=1.0 / 1024, bias=prm[:, P_EPS6:P_EPS6 + 1]),
                 [PB[k][q], B_prm], [B_rs])
            S.op("dve", lambda e: e.reciprocal(out=rs, in_=rs), [B_rs], [B_rs])

        B_orws = Buf("orws")
        ch_orw = S.chan("orw")

        def phase_R():
            wR, B_wR = alloc("wR", [8, 1792], BF16)
            waup, B_waup = alloc("waup", [512])
            gup, B_gup = alloc("gup", [512])
            w0r, B_w0r = alloc("w0r", [512])
            chw = S.chan("wR")
            S.dma(chw, wR, w_in_d[:, 1024:2816].rearrange("(c p) n -> p c n", p=128), writes=[B_wR], eng="pool")
            S.dma(S.chan("waup"), waup, waup_d, writes=[B_waup])
            S.dma(S.chan("gup"), gup, gup_d, writes=[B_gup])
            S.dma(S.chan("w0r"), w0r[0:1, :], w0_d, writes=[B_w0r])
            xts = [alloc("xt0", [8, 128])] * 2
            chx = [S.chan("x%d" % k) for k in range(2)]
            xb, B_xb = alloc("xb", [8, 128], BF16)
            sq, B_sq = alloc("sq", [8, 128], BF16)
            rs, B_rs = alloc("rs", [128])
            zr, B_zr = alloc("zr", [14, 129])
            zs, B_zs = alloc("zs", [14, 128])
            tl, B_tl = alloc("tl", [128])
            sgt, B_sgt = alloc("sgt", [512])
            cexc, B_cexc = alloc("cexc", [4, 128])
            cinv, B_cinv = alloc("cinv", [4, 128])
            aa, B_aa = alloc("aa", [4, 128])
            sgl, B_sgl = alloc("sgl", [128])
            kk, B_kk = alloc("kk", [4, 128])
            tmp, B_tmp = alloc("tmp", [4, 128])
            k2, B_k2 = alloc("k2", [4, 128])
            FS = [{n_: alloc("%s_%d" % (n_, k_), shp_) for n_, shp_ in (("rt", [4, 128]), ("kkt", [4, 128]), ("kt", [4, 128]), ("bt", [4, 128]), ("bon", [4, 128]), ("gT", [4, 128]), ("vtok", [512]), ("ktok", [512]), ("btok", [512]), ("cinc", [4, 128]))} for k_ in range(3)]
            tmp2, B_tmp2 = alloc("tmp2", [4, 128])
            osq, B_osq = tmp2.rearrange("p a b -> p (a b)"), B_tmp2
            ST, _ = alloc("ST", [4, 64])
            osb, B_osb = alloc("osb", [512])
            gst, B_gst = alloc("gst", [32])
            orw, B_orw = alloc("orw", [4, 128], BF16)
            KAS = {}
            for nm in ("Xa", "XTa", "Xb", "XTb", "Tb"):
                ap_, _ = alloc(nm + "8", [8, 128])
                KAS[nm] = (ap_, [Buf(nm + "_e"), Buf(nm + "_o")])
            KAD = []
            for k_ in range(2):
                d_ = {}
                for nm in ("Mk", "Ak", "Ab", "Ta"):
                    ap_, _ = alloc("%s8_%d" % (nm, k_), [8, 128])
                    d_[nm] = (ap_, [Buf("%s_e%d" % (nm, k_)), Buf("%s_o%d" % (nm, k_))])
                KAD.append(d_)
            XTs8, B_XTs8 = alloc("XTs8", [8, 64])
            NTs8, B_NTs8 = alloc("NTs8", [8, 64])
            tmpS, B_tmpS = alloc("tmpS", [4, 64])
            B_STp = [Buf("STe"), Buf("STo")]
            S.op("dve", lambda e: e.memset(zr, 0.0), [], [B_zr])
            S.op("dve", lambda e: e.memset(ST, 0.0), [], B_STp)
            slot_ctr = [0]

            def nslot():
                s_ = slot_ctr[0] % 3
                slot_ctr[0] += 1
                return 2 + s_

            def front(i):
                rt, B_rt = FS[i % 3]["rt"]
                kkt, B_kkt = FS[i % 3]["kkt"]
                kt, B_kt = FS[i % 3]["kt"]
                bt, B_bt = FS[i % 3]["bt"]
                bon, B_bon = FS[i % 3]["bon"]
                gT, B_gT = FS[i % 3]["gT"]
                vtok, B_vtok = FS[i % 3]["vtok"]
                ktok, B_ktok = FS[i % 3]["ktok"]
                btok, B_btok = FS[i % 3]["btok"]
                cinc, B_cinc = FS[i % 3]["cinc"]
                xt, B_xt = xts[i % 2]
                load_x(i, xt, B_xt, chx[i % 2], xb, B_xb, sq, B_sq, rs, B_rs, (1, 0))
                yield
                for gi, (j0, nj) in enumerate(((0, 4), (4, 4), (8, 4), (12, 2))):
                    for jj in range(nj):
                        j = j0 + jj
                        for dc in range(8):
                            S.op("pe", lambda e, j=j, jj=jj, dc=dc, gi=gi: e.matmul(pq(gi % 2, jj), lhsT=wR[:, dc, j * 128:(j + 1) * 128], rhs=xb[:, dc, :], start=(dc == 0), stop=(dc == 7)),
                                 [B_wR, B_xb], [PB[gi % 2][jj]])
                    S.op("dve", lambda e, gi=gi, j0=j0, nj=nj: e.tensor_tensor(out=zr[:, j0:j0 + nj, 1:129], in0=PS[gi % 2][:, 0:nj * 128].rearrange("p (a b) -> p a b", b=128),
                                                                           in1=rs.unsqueeze(1).to_broadcast([128, nj, 128]), op=ALU.mult),
                         PB[gi % 2][0:nj] + [B_rs], [B_zr])
                yield
                mu_bc = prm[:, P_MU:P_MU + 14].unsqueeze(2).to_broadcast([128, 14, 128])
                S.op("pool", lambda e: e.tensor_tensor(out=zs, in0=zr[:, :, 0:128], in1=zr[:, :, 1:129], op=ALU.subtract), [B_zr], [B_zs])
                S.op("pool", lambda e: e.tensor_tensor(out=zs, in0=zs, in1=mu_bc, op=ALU.mult), [B_zs, B_prm], [B_zs])
                S.op("pool", lambda e: e.tensor_tensor(out=zs, in0=zs, in1=zr[:, :, 1:129], op=ALU.add), [B_zs, B_zr], [B_zs])
                S.op("pool", lambda e: e.tensor_copy(out=zr[:, :, 0:1], in_=zr[:, :, 128:129]), [B_zr, B_zs], [B_zr])
                if i == 1:
                    tap("zs", zs, B_zs, [128, 14, 128])
                r_ = zs[:, 0:4, :]
                k_ = zs[:, 4:8, :]
                v_ = zs[:, 8:12, :]
                yield
                S.op("act", lambda e: e.activation(out=tl[0:64, :], in_=zs[0:64, 12, :], func=AF.Tanh), [B_zs], [B_tl])
                S.op("pe", lambda e: e.matmul(PS[0][:, :], lhsT=tl[0:64, :], rhs=waup[0:64, :], start=True, stop=False), [B_tl, B_waup], PB[0])
                S.op("pe", lambda e: e.matmul(PS[0][:, :], lhsT=cst[0:1, C_ONES, :], rhs=w0r[0:1, :], start=False, stop=True), [B_cst, B_w0r], PB[0])
                S.op("act", lambda e: e.activation(out=sgt, in_=PS[0][:, :], func=AF.Sigmoid), PB[0], [B_sgt])
                for cc in range(4):
                    S.op("pe", lambda e, cc=cc: e.matmul(pq(1, cc), lhsT=sgt[:, cc * 128:(cc + 1) * 128], rhs=cst[:, C_TRII, :], start=True, stop=True), [B_sgt, B_cst], [PB[1][cc]])
                    S.op("pe", lambda e, cc=cc: e.matmul(pq(0, cc), lhsT=sgt[:, cc * 128:(cc + 1) * 128], rhs=cst[:, C_TRIE, :], start=True, stop=True), [B_sgt, B_cst], [PB[0][cc]])
                S.op("act", lambda e: e.activation(out=cinc.rearrange("p a b -> p (a b)"), in_=PS[1][:, :], func=AF.Exp), PB[1], [B_cinc])
                S.op("act", lambda e: e.activation(out=cinv.rearrange("p a b -> p (a b)"), in_=PS[1][:, :], func=AF.Exp, scale=-1.0), PB[1], [B_cinv])
                S.op("act", lambda e: e.activation(out=cexc.rearrange("p a b -> p (a b)"), in_=PS[0][:, :], func=AF.Exp), PB[0], [B_cexc])
                yield
                for cc in range(4):
                    S.op("pe", lambda e, cc=cc: e.matmul(pq(1, cc), lhsT=waup[64:128, cc * 128:(cc + 1) * 128], rhs=zs[64:128, 12, :], start=True, stop=True), [B_waup, B_zs], [PB[1][cc]])
                    S.op("act", lambda e, cc=cc: e.activation(out=aa[:, cc, :], in_=pq(1, cc), func=AF.Sigmoid, bias=prm[:, P_A0 + cc:P_A0 + cc + 1]), [PB[1][cc], B_prm], [B_aa])
                S.op("act", lambda e: e.activation(out=sgl, in_=zs[:, 13, :], func=AF.Sigmoid), [B_zs], [B_sgl])
                for cc in range(4):
                    S.op("pe", lambda e, cc=cc: e.matmul(pq(0, cc), lhsT=gup[:, cc * 128:(cc + 1) * 128], rhs=sgl, start=True, stop=True), [B_gup, B_sgl], [PB[0][cc]])
                S.op("act", lambda e: e.copy(out=gT.rearrange("p a b -> p (a b)"), in_=PS[0][:, :]), PB[0], [B_gT])
                yield
                bc4 = lambda c0: prm[:, c0:c0 + 4].unsqueeze(2).to_broadcast([128, 4, 128])
                S.op("dve", lambda e: e.tensor_tensor(out=kk, in0=k_, in1=bc4(P_KK), op=ALU.mult), [B_zs, B_prm], [B_kk])
                S.op("pool", lambda e: e.tensor_tensor(out=tmp, in0=kk, in1=kk, op=ALU.mult), [B_kk], [B_tmp])
                for cc in range(4):
                    S.op("pe", lambda e, cc=cc: e.matmul(pq(1, cc), lhsT=cst[:, C_BLK, :], rhs=tmp[:, cc, :], start=True, stop=True), [B_cst, B_tmp], [PB[1][cc]])
                S.op("dve", lambda e: e.tensor_scalar(out=tmp.rearrange("p a b -> p (a b)"), in0=PS[1][:, :], scalar1=1e-24, scalar2=None, op0=ALU.max), PB[1], [B_tmp])
                S.op("act", lambda e: e.activation(out=tmp, in_=tmp, func=AF.Sqrt), [B_tmp], [B_tmp])
                S.op("dve", lambda e: e.reciprocal(out=tmp, in_=tmp), [B_tmp], [B_tmp])
                S.op("dve", lambda e: e.tensor_tensor(out=kk, in0=kk, in1=tmp, op=ALU.mult), [B_kk, B_tmp], [B_kk])
                S.op("pool", lambda e: e.tensor_scalar(out=k2, in0=aa, scalar1=-1.0, scalar2=None, op0=ALU.add), [B_aa], [B_k2])
                S.op("pool", lambda e: e.tensor_tensor(out=k2, in0=k2, in1=bc4(P_KA), op=ALU.mult), [B_k2, B_prm], [B_k2])
                S.op("dve", lambda e: e.scalar_tensor_tensor(out=k2, in0=k2, scalar=1.0, in1=k_, op0=ALU.add, op1=ALU.mult), [B_k2, B_zs], [B_k2])
                yield
                S.op("dve", lambda e: e.tensor_tensor(out=kkt, in0=kk, in1=cexc, op=ALU.mult), [B_kk, B_cexc], [B_kkt])
                S.op("dve", lambda e: e.tensor_tensor(out=bt, in0=kk, in1=aa, op=ALU.mult), [B_kk, B_aa], [B_bt])
                S.op("dve", lambda e: e.tensor_tensor(out=bt, in0=bt, in1=cinv, op=ALU.mult), [B_bt, B_cinv], [B_bt])
                S.op("pool", lambda e: e.tensor_tensor(out=kt, in0=k2, in1=cinv, op=ALU.mult), [B_k2, B_cinv], [B_kt])
                S.op("pool", lambda e: e.tensor_tensor(out=rt, in0=r_, in1=cinc, op=ALU.mult), [B_zs, B_cinc], [B_rt])
                S.op("pool", lambda e: e.tensor_tensor(out=tmp, in0=r_, in1=k2, op=ALU.mult), [B_zs, B_k2, B_kk], [B_tmp])
                S.op("pool", lambda e: e.tensor_tensor(out=tmp, in0=tmp, in1=bc4(P_RK), op=ALU.mult), [B_tmp, B_prm], [B_tmp])
                for cc in range(4):
                    S.op("pe", lambda e, cc=cc: e.matmul(pq(0, cc), lhsT=cst[:, C_BLK, :], rhs=tmp[:, cc, :], start=True, stop=True), [B_cst, B_tmp], [PB[0][cc]])
                S.op("dve", lambda e: e.tensor_tensor(out=bon, in0=PS[0][:, :].rearrange("p (a b) -> p a b", b=128), in1=v_, op=ALU.mult), PB[0] + [B_zs], [B_bon])
                yield
                for (src, Bsrc, dst, Bdst, bank) in ((v_, B_zs, vtok, B_vtok, 0), (kt, B_kt, ktok, B_ktok, 1), (bt, B_bt, btok, B_btok, 0)):
                    for cc in range(4):
                        S.op("pe", lambda e, src=src, cc=cc, bank=bank: e.transpose(pq(bank, cc), src[:, cc, :], ident), [Bsrc, B_cst], [PB[bank][cc]])
                    S.op("act", lambda e, dst=dst, bank=bank: e.copy(out=dst, in_=PS[bank][:, :]), PB[bank], [Bdst])
                if i == 1:
                    tap("kkt", kkt, B_kkt, [128, 4, 128])
                    tap("rt", rt, B_rt, [128, 4, 128])
                    tap("vtok", vtok, B_vtok, [128, 512])
                yield

            def hv(ap3, par, cc):
                return ap3[64 * par:64 * par + 64, cc, :]

            def neu(i):
                rt, B_rt = FS[i % 3]["rt"]
                kkt, B_kkt = FS[i % 3]["kkt"]
                kt, B_kt = FS[i % 3]["kt"]
                bt, B_bt = FS[i % 3]["bt"]
                bon, B_bon = FS[i % 3]["bon"]
                gT, B_gT = FS[i % 3]["gT"]
                vtok, B_vtok = FS[i % 3]["vtok"]
                ktok, B_ktok = FS[i % 3]["ktok"]
                btok, B_btok = FS[i % 3]["btok"]
                cinc, B_cinc = FS[i % 3]["cinc"]
                KA = dict(KAS)
                KA.update(KAD[i % 2])
                def hv(ap3, par, cc):
                    return ap3[64 * par:64 * par + 64, cc, :]

                def mm_mask(lsrc, Bl, rsrc, Br, mask_idx, kind, eng="dve"):
                    dst, Bd = KA[kind]
                    for par in range(2):
                        bank = nslot()
                        for cc in range(4):
                            S.op("pe", lambda e, par=par, cc=cc, bank=bank: e.matmul(pq(bank, cc), lhsT=hv(lsrc, par, cc), rhs=hv(rsrc, par, cc), start=True, stop=True), [Bl, Br], [PB[bank][0]])
                        S.op(eng, lambda e, par=par, bank=bank: e.tensor_tensor(out=dst[:, par * 4:par * 4 + 4, :], in0=PS[bank][:, :].rearrange("p (a b) -> p a b", b=128),
                                                                           in1=cst[:, mask_idx, :].unsqueeze(1).to_broadcast([128, 4, 128]), op=ALU.mult), [PB[bank][0], B_cst], [Bd[par]])

                mm_mask(bt, B_bt, kkt, B_kkt, C_MSN, "Xa")
                yield
                mm_mask(kkt, B_kkt, bt, B_bt, C_MLN, "XTa")
                yield
                mm_mask(kt, B_kt, kkt, B_kkt, C_MS, "Mk")
                yield
                mm_mask(kt, B_kt, rt, B_rt, C_MI, "Ak")
                yield
                mm_mask(bt, B_bt, rt, B_rt, C_MI, "Ab")
                yield
                for par in range(2):
                    S.op("dve", lambda e, par=par: e.tensor_tensor(out=KA["Ta"][0][:, par * 4:par * 4 + 4, :], in0=KA["Xa"][0][:, par * 4:par * 4 + 4, :],
                                                                in1=ident.unsqueeze(1).to_broadcast([128, 4, 128]), op=ALU.add), [KA["Xa"][1][par], B_cst], [KA["Ta"][1][par]])
                X, XT, T = "Xa", "XTa", "Ta"
                Xn, XTn, Tn = "Xb", "XTb", "Tb"

                def lvl_mm(lk, rk, outk, eng, addk=None):
                    la, lB = KA[lk]
                    ra, rB = KA[rk]
                    oa, oB = KA[outk]
                    for par in range(2):
                        bank = nslot()
                        for cc in range(4):
                            hi = par * 4 + cc
                            S.op("pe", lambda e, hi=hi, cc=cc, bank=bank: e.matmul(pq(bank, cc), lhsT=la[:, hi, :], rhs=ra[:, hi, :], start=True, stop=True), [lB[par], rB[par]], [PB[bank][0]])
                        if addk is None:
                            S.op(eng, lambda e, par=par, bank=bank: e.copy(out=oa[:, par * 4:par * 4 + 4, :], in_=PS[bank][:, :].rearrange("p (a b) -> p a b", b=128)), [PB[bank][0]], [oB[par]])
                        else:
                            aa_, aB = KA[addk]
                            S.op(eng, lambda e, par=par, bank=bank: e.tensor_tensor(out=oa[:, par * 4:par * 4 + 4, :], in0=PS[bank][:, :].rearrange("p (a b) -> p a b", b=128),
                                                                               in1=aa_[:, par * 4:par * 4 + 4, :], op=ALU.add), [PB[bank][0], aB[par]], [oB[par]])

                for l in range(6):
                    if l < 5:
                        lvl_mm(XT, X, Xn, "act")
                    lvl_mm(X, XT, XTn, "act")
                    yield
                    lvl_mm(XTn, T, Tn, "dve", addk=T)
                    yield
                    X, Xn = Xn, X
                    XT, XTn = XTn, XT
                    T, Tn = Tn, T
                yield
                assert T == "Ta"
                yield

            def stt(i):
                rt, B_rt = FS[i % 3]["rt"]
                kkt, B_kkt = FS[i % 3]["kkt"]
                kt, B_kt = FS[i % 3]["kt"]
                bt, B_bt = FS[i % 3]["bt"]
                bon, B_bon = FS[i % 3]["bon"]
                gT, B_gT = FS[i % 3]["gT"]
                vtok, B_vtok = FS[i % 3]["vtok"]
                ktok, B_ktok = FS[i % 3]["ktok"]
                btok, B_btok = FS[i % 3]["btok"]
                cinc, B_cinc = FS[i % 3]["cinc"]
                KA = KAD[i % 2]
                Tinv, B_Tinv = KA["Ta"]
                Mk8, B_Mk8 = KA["Mk"]
                Ak8, B_Ak8 = KA["Ak"]
                Ab8, B_Ab8 = KA["Ab"]
                heads = [(par, cc) for par in range(2) for cc in range(4)]
                for (par, cc) in heads:
                    hi = par * 4 + cc
                    vt_h = vtok[:, cc * 128 + 64 * par: cc * 128 + 64 * par + 64]
                    S.op("pe", lambda e, par=par, cc=cc, hi=hi: e.matmul(PS[6][:, hi * 64:(hi + 1) * 64], lhsT=hv(kkt, par, cc), rhs=ST[64 * par:64 * par + 64, cc, :], start=True, stop=False),
                         [B_kkt, B_STp[par]], [PB[6][0]])
                    S.op("pe", lambda e, hi=hi, vt_h=vt_h: e.matmul(PS[6][:, hi * 64:(hi + 1) * 64], lhsT=Mk8[:, hi, :], rhs=vt_h, start=False, stop=True), [B_Mk8[par], B_vtok], [PB[6][0]])
                S.op("act", lambda e: e.mul(XTs8.rearrange("p a b -> p (a b)"), PS[6][:, :], -1.0), [PB[6][0]], [B_XTs8])
                yield
                for (par, cc) in heads:
                    hi = par * 4 + cc
                    S.op("pe", lambda e, hi=hi: e.matmul(PS[7][:, hi * 64:(hi + 1) * 64], lhsT=Tinv[:, hi, :], rhs=XTs8[:, hi, :], start=True, stop=True), [B_Tinv[par], B_XTs8], [PB[7][0]])
                S.op("act", lambda e: e.copy(out=NTs8.rearrange("p a b -> p (a b)"), in_=PS[7][:, :]), [PB[7][0]], [B_NTs8])
                yield
                for (par, cc) in heads:
                    hi = par * 4 + cc
                    h = 2 * cc + par
                    vt_h = vtok[:, cc * 128 + 64 * par: cc * 128 + 64 * par + 64]
                    osl = PS[5][:, h * 64:(h + 1) * 64]
                    S.op("pe", lambda e, par=par, cc=cc, osl=osl: e.matmul(osl, lhsT=hv(rt, par, cc), rhs=ST[64 * par:64 * par + 64, cc, :], start=True, stop=False), [B_rt, B_STp[par]], [PB[5][0]])
                    S.op("pe", lambda e, hi=hi, osl=osl: e.matmul(osl, lhsT=Ab8[:, hi, :], rhs=NTs8[:, hi, :], start=False, stop=False), [B_Ab8[par], B_NTs8], [PB[5][0]])
                    S.op("pe", lambda e, hi=hi, osl=osl, vt_h=vt_h: e.matmul(osl, lhsT=Ak8[:, hi, :], rhs=vt_h, start=False, stop=True), [B_Ak8[par], B_vtok], [PB[5][0]])
                yield
                for (par, cc) in heads:
                    hi = par * 4 + cc
                    vt_h = vtok[:, cc * 128 + 64 * par: cc * 128 + 64 * par + 64]
                    S.op("pe", lambda e, hi=hi, cc=cc: e.matmul(PS[6][:, hi * 64:(hi + 1) * 64], lhsT=btok[:, cc * 128:(cc + 1) * 128], rhs=NTs8[:, hi, :], start=True, stop=False), [B_btok, B_NTs8], [PB[6][0]])
                    S.op("pe", lambda e, hi=hi, cc=cc, vt_h=vt_h: e.matmul(PS[6][:, hi * 64:(hi + 1) * 64], lhsT=ktok[:, cc * 128:(cc + 1) * 128], rhs=vt_h, start=False, stop=True), [B_ktok, B_vtok], [PB[6][0]])
                for par in range(2):
                    sl = slice(64 * par, 64 * par + 64)
                    S.op("dve", lambda e, par=par, sl=sl: e.tensor_tensor(out=tmpS[sl, :, :], in0=PS[6][sl, par * 256:(par + 1) * 256].rearrange("p (c v) -> p c v", v=64), in1=ST[sl, :, :], op=ALU.add),
                         [PB[6][0], B_STp[par]], [B_tmpS])
                    S.op("dve", lambda e, par=par, sl=sl: e.tensor_tensor(out=ST[sl, :, :], in0=tmpS[sl, :, :], in1=cinc[sl, :, 127:128].to_broadcast([64, 4, 64]), op=ALU.mult),
                         [B_tmpS, B_cinc], [B_STp[par]])
                if i == 0:
                    return
                yield
                S.op("act", lambda e: e.copy(out=osb, in_=PS[5][:, :]), PB[5], [B_osb])
                S.op("act", lambda e: e.activation(out=osq, in_=PS[5][:, :], func=AF.Square), PB[5], [B_osq])
                if i == 1:
                    tap("osb", osb, B_osb, [128, 512])
                o3 = osb.rearrange("p (h v) -> p h v", v=64)
                S.op("dve", lambda e: e.tensor_reduce(out=gst[:, 0:8], in_=o3, axis=AX.X, op=ALU.add), [B_osb], [B_gst])
                S.op("dve", lambda e: e.tensor_reduce(out=gst[:, 8:16], in_=osq.rearrange("p (h v) -> p h v", v=64), axis=AX.X, op=ALU.add), [B_osq, B_gst], [B_gst])
                S.op("dve", lambda e: e.tensor_scalar(out=gst[:, 0:16], in0=gst[:, 0:16], scalar1=1.0 / 64, scalar2=None, op0=ALU.mult), [B_gst], [B_gst])
                S.op("dve", lambda e: e.tensor_tensor(out=gst[:, 16:24], in0=gst[:, 0:8], in1=gst[:, 0:8], op=ALU.mult), [B_gst], [B_gst])
                S.op("dve", lambda e: e.tensor_tensor(out=gst[:, 16:24], in0=gst[:, 8:16], in1=gst[:, 16:24], op=ALU.subtract), [B_gst], [B_gst])
                S.op("act", lambda e: e.activation(out=gst[:, 24:32], in_=gst[:, 16:24], func=AF.Sqrt, bias=prm[:, P_EPSG:P_EPSG + 1]), [B_gst, B_prm], [B_gst])
                S.op("dve", lambda e: e.reciprocal(out=gst[:, 24:32], in_=gst[:, 24:32]), [B_gst], [B_gst])
                S.op("dve", lambda e: e.tensor_tensor(out=o3, in0=o3, in1=gst[:, 0:8].unsqueeze(2).to_broadcast([128, 8, 64]), op=ALU.subtract), [B_osb, B_gst], [B_osb])
                S.op("dve", lambda e: e.tensor_tensor(out=o3, in0=o3, in1=gst[:, 24:32].unsqueeze(2).to_broadcast([128, 8, 64]), op=ALU.mult), [B_osb, B_gst], [B_osb])
                yield
                for cc in range(4):
                    S.op("pe", lambda e, cc=cc: e.transpose(pq(7, cc), osb[:, cc * 128:(cc + 1) * 128], ident), [B_osb, B_cst], [PB[7][cc]])
                    S.op("dve", lambda e, cc=cc: e.tensor_scalar(out=tmp2[:, cc, :], in0=pq(7, cc), scalar1=prm[:, P_GNG + cc:P_GNG + cc + 1], scalar2=prm[:, P_GNB + cc:P_GNB + cc + 1], op0=ALU.mult, op1=ALU.add),
                         [PB[7][cc], B_prm], [B_tmp2])
                S.op("pool", lambda e: e.tensor_tensor(out=tmp2, in0=tmp2, in1=bon, op=ALU.add), [B_tmp2, B_bon], [B_tmp2])
                S.op("pool", lambda e: e.tensor_tensor(out=orw, in0=tmp2, in1=gT, op=ALU.mult), [B_tmp2, B_gT], [B_orw])
                if i == 1:
                    tap("orw", orw, B_orw, [128, 4, 128], BF16)
                S.dma(ch_orw, orw_s[i], orw, reads=[B_orw], writes=[B_orws])
                yield

            def run_all(g):
                for _ in g:
                    pass

            def adv(g):
                try:
                    next(g)
                    return True
                except StopIteration:
                    return False

            run_all(front(0))
            if NT > 1:
                run_all(front(1))
            run_all(neu(0))
            NNEU = 21
            NFRONT = 10
            for i in range(NT):
                if i < 16:
                    issue_cast(i)
                g_st = stt(i)
                g_neu = neu(i + 1) if i + 1 < NT else iter(())
                g_fr = front(i + 2) if i + 2 < NT else iter(())
                a_st = a_neu = a_fr = True
                rnd = 0
                fdone = 0
                while a_st or a_neu or a_fr:
                    if a_st:
                        a_st = adv(g_st)
                    if a_neu:
                        a_neu = adv(g_neu)
                    want = (rnd + 1) * NFRONT // NNEU + 1 if (a_neu or a_st) else 10 ** 9
                    while a_fr and fdone < want:
                        a_fr = adv(g_fr)
                        fdone += 1
                    rnd += 1
            for k_ in range(min(NT, 16), 16):
                issue_cast(k_)
            tap("STfin", ST, B_STp[0], [128, 4, 64])
        phase_R()
        S.barrier()
        B_h1toks = Buf("h1toks")
        B_h1ns = Buf("h1ns")

        def phase_C():
            top[0] = base_top
            wC, B_wC = alloc("wC", [8, 3072], BF16)
            wco, B_wco = alloc("wco", [4, 1024], BF16)
            wro, B_wro = alloc("wro", [4, 1024], BF16)
            wo, B_wo = alloc("wo", [8, 1024], BF16)
            diag, B_diag = alloc("diag", [124, 128], BF16)
            S.dma(S.chan("wC0"), wC[:, :, 0:1024], w_in_d[:, 0:1024].rearrange("(c p) n -> p c n", p=128), writes=[B_wC], eng="pool")
            S.dma(S.chan("wC1"), wC[:, :, 1024:3072], w_in_d[:, 2816:4864].rearrange("(c p) n -> p c n", p=128), writes=[B_wC], eng="pool")
            S.dma(S.chan("wco"), wco, wco_d.rearrange("(c p) n -> p c n", p=128), writes=[B_wco], eng="pool")
            S.dma(S.chan("wro"), wro, wro_d.rearrange("(c p) n -> p c n", p=128), writes=[B_wro], eng="pool")
            S.dma(S.chan("wo"), wo, wo_d.rearrange("(c p) n -> p c n", p=128), writes=[B_wo], eng="pool")
            for cc in range(4):
                for j in range(31):
                    S.op("dve", lambda e, cc=cc, j=j: e.tensor_scalar(out=diag[:, cc * 31 + j, :], in0=ident, scalar1=prm[:, P_CW + cc * 31 + j:P_CW + cc * 31 + j + 1], scalar2=None, op0=ALU.mult),
                         [B_cst, B_prm], [B_diag])
            xts = [alloc("cxt%d" % k, [8, 128]) for k in range(3)]
            chx = [S.chan("cx%d" % k) for k in range(3)]
            xb, B_xb = alloc("cxb", [8, 128], BF16)
            sq, B_sq = alloc("csq", [8, 128], BF16)
            rs, B_rs = alloc("crs", [128])
            zcs = [alloc("zc%d" % k, [24, 128]) for k in range(2)]
            ubuf, B_ubuf = alloc("ubuf", [4, 158], BF16)
            ysb, B_ysb = alloc("ysb", [4, 128])
            ysq, B_ysq = alloc("ysq", [4, 128])
            mv, B_mv = alloc("mv", [128])
            actc, B_actc = alloc("actc", [4, 128], BF16)
            orwt, B_orwt = alloc("orwt", [4, 128], BF16)
            t1, B_t1 = alloc("t1", [8, 128])
            t2, B_t2 = alloc("t2", [8, 128])
            gateds = [alloc("gated%d" % k, [8, 128], BF16) for k in range(2)]
            t1b, B_t1b = alloc("t1b", [8, 128])
            h1T, B_h1T = alloc("h1T", [8, 128])
            h1sq, B_h1sq = alloc("h1sq", [8, 128], BF16)
            rs2, B_rs2 = alloc("rs2", [128])
            h1n, B_h1n = alloc("h1n", [8, 128], BF16)
            h1tok, B_h1tok = alloc("h1tok", [1024])
            ch_orwl = S.chan("orwl")
            ch_h1tok = S.chan("h1tok")
            ch_h1n = S.chan("h1n")
            S.op("dve", lambda e: e.memset(ubuf, 0.0), [], [B_ubuf])
            def cfront(i):
                zc, B_zc = zcs[i % 2]
                xt, B_xt = xts[i % 3]
                load_x(i, xt, B_xt, chx[i % 3], xb, B_xb, sq, B_sq, rs, B_rs, (1, 0))
                nchunk = 8 if i == 0 else 24
                for gi in range(nchunk // 4):
                    bank = gi % 2
                    for jj in range(4):
                        j = gi * 4 + jj
                        col = j * 128
                        for dc in range(8):
                            S.op("pe", lambda e, bank=bank, jj=jj, dc=dc, col=col: e.matmul(pq(bank, jj), lhsT=wC[:, dc, col:col + 128], rhs=xb[:, dc, :], start=(dc == 0), stop=(dc == 7)),
                                 [B_wC, B_xb], [PB[bank][0]])
                    S.op("dve", lambda e, bank=bank, gi=gi: e.tensor_tensor(out=zc[:, gi * 4:gi * 4 + 4, :], in0=PS[bank][:, :].rearrange("p (a b) -> p a b", b=128),
                                                                         in1=rs.unsqueeze(1).to_broadcast([128, 4, 128]), op=ALU.mult),
                         [PB[bank][0], B_rs], [B_zc])
                yield

            def cb1(i):
                zc, B_zc = zcs[i % 2]
                gated, B_gated = gateds[i % 2]
                S.op("act", lambda e: e.activation(out=zc[:, 4:8, :], in_=zc[:, 4:8, :], func=AF.Sigmoid), [B_zc], [B_zc])
                S.op("dve", lambda e: e.tensor_tensor(out=ubuf[:, :, 30:158], in0=zc[:, 0:4, :], in1=zc[:, 4:8, :], op=ALU.mult), [B_zc], [B_ubuf])
                yield
                for cc in range(4):
                    for j in range(31):
                        S.op("pe", lambda e, cc=cc, j=j: e.matmul(pq(2, cc), lhsT=diag[:, cc * 31 + j, :], rhs=ubuf[:, cc, j:j + 128], start=(j == 0), stop=(j == 30)),
                             [B_diag, B_ubuf], [PB[2][0]])
                S.op("pool", lambda e: e.tensor_copy(out=ubuf[:, :, 0:30], in_=ubuf[:, :, 128:158]), [B_ubuf], [B_ubuf])
                if i == 0:
                    S.op("act", lambda e: e.copy(out=ysb.rearrange("p a b -> p (a b)"), in_=PS[2][:, :]), [PB[2][0]], [B_ysb])
                    return
                yield
                for cc in range(4):
                    S.op("act", lambda e, cc=cc: e.activation(out=ysb[:, cc, :], in_=pq(2, cc), func=AF.Identity, bias=prm[:, P_CB + cc:P_CB + cc + 1]), [PB[2][0], B_prm], [B_ysb])
                    S.op("act", lambda e, cc=cc: e.activation(out=ysq[:, cc, :], in_=pq(2, cc), func=AF.Square, bias=prm[:, P_CB + cc:P_CB + cc + 1]), [PB[2][0], B_prm], [B_ysq])
                for cc in range(4):
                    S.op("pe", lambda e, cc=cc: e.matmul(pq(3, 0), lhsT=cst[:, C_O512, :], rhs=ysb[:, cc, :], start=(cc == 0), stop=(cc == 3)), [B_cst, B_ysb], [PB[3][0]])
                for cc in range(4):
                    S.op("pe", lambda e, cc=cc: e.matmul(pq(3, 1), lhsT=cst[:, C_O512, :], rhs=ysq[:, cc, :], start=(cc == 0), stop=(cc == 3)), [B_cst, B_ysq], [PB[3][0]])
                S.op("act", lambda e: e.activation(out=mv, in_=pq(3, 0), func=AF.Square), [PB[3][0]], [B_mv])
                S.op("dve", lambda e: e.tensor_tensor(out=mv, in0=pq(3, 1), in1=mv, op=ALU.subtract), [PB[3][0], B_mv], [B_mv])
                S.op("act", lambda e: e.activation(out=mv, in_=mv, func=AF.Sqrt, bias=prm[:, P_EPS5:P_EPS5 + 1]), [B_mv, B_prm], [B_mv])
                S.op("dve", lambda e: e.reciprocal(out=mv, in_=mv), [B_mv], [B_mv])
                S.op("dve", lambda e: e.tensor_tensor(out=ysb, in0=ysb, in1=pq(3, 0).unsqueeze(1).to_broadcast([128, 4, 128]), op=ALU.subtract), [B_ysb, PB[3][0]], [B_ysb])
                S.op("dve", lambda e: e.tensor_tensor(out=ysb, in0=ysb, in1=mv.unsqueeze(1).to_broadcast([128, 4, 128]), op=ALU.mult), [B_ysb, B_mv], [B_ysb])
                for cc in range(4):
                    S.op("act", lambda e, cc=cc: e.activation(out=actc[:, cc, :], in_=ysb[:, cc, :], func=AF.Silu, scale=prm[:, P_LNG + cc:P_LNG + cc + 1], bias=prm[:, P_LNB + cc:P_LNB + cc + 1]),
                         [B_ysb, B_prm], [B_actc])
                yield
                S.dma(ch_orwl, orwt, orw_s[i], reads=[B_orws], writes=[B_orwt])
                for m_ in range(8):
                    for cc in range(4):
                        S.op("pe", lambda e, m_=m_, cc=cc: e.matmul(pq(4 + m_ // 4, m_ % 4), lhsT=wco[:, cc, m_ * 128:(m_ + 1) * 128], rhs=actc[:, cc, :], start=(cc == 0), stop=(cc == 3)),
                             [B_wco, B_actc], [PB[4 + m_ // 4][0]])
                S.op("act", lambda e: e.activation(out=zc[:, 8:24, :], in_=zc[:, 8:24, :], func=AF.Sigmoid), [B_zc], [B_zc])
                yield
                for hb_ in range(2):
                    S.op("dve", lambda e, hb_=hb_: e.tensor_tensor(out=t1[:, hb_ * 4:hb_ * 4 + 4, :], in0=PS[4 + hb_][:, :].rearrange("p (a b) -> p a b", b=128), in1=zc[:, 8 + hb_ * 4:12 + hb_ * 4, :], op=ALU.mult),
                         [PB[4 + hb_][0], B_zc], [B_t1])
                for m_ in range(8):
                    for cc in range(4):
                        S.op("pe", lambda e, m_=m_, cc=cc: e.matmul(pq(4 + m_ // 4, m_ % 4), lhsT=wro[:, cc, m_ * 128:(m_ + 1) * 128], rhs=orwt[:, cc, :], start=(cc == 0), stop=(cc == 3)),
                             [B_wro, B_orwt], [PB[4 + m_ // 4][0]])
                yield
                for hb_ in range(2):
                    S.op("dve", lambda e, hb_=hb_: e.tensor_tensor(out=t2[:, hb_ * 4:hb_ * 4 + 4, :], in0=PS[4 + hb_][:, :].rearrange("p (a b) -> p a b", b=128), in1=zc[:, 16 + hb_ * 4:20 + hb_ * 4, :], op=ALU.mult),
                         [PB[4 + hb_][0], B_zc], [B_t2])
                S.op("pool", lambda e: e.tensor_tensor(out=gated, in0=t1, in1=t2, op=ALU.add), [B_t1, B_t2], [B_gated])
                yield


            def cb2(i):
                if i == 0:
                    return
                    yield
                xt, B_xt = xts[i % 3]
                gated, B_gated = gateds[i % 2]
                for m_ in range(8):
                    for kc in range(8):
                        S.op("pe", lambda e, m_=m_, kc=kc: e.matmul(pq(6 + m_ // 4, m_ % 4), lhsT=wo[:, kc, m_ * 128:(m_ + 1) * 128], rhs=gated[:, kc, :], start=(kc == 0), stop=(kc == 7)),
                             [B_wo, B_gated], [PB[6 + m_ // 4][0]])
                for hb_ in range(2):
                    S.op("dve", lambda e, hb_=hb_, xt=xt: e.tensor_tensor(out=h1T[:, hb_ * 4:hb_ * 4 + 4, :], in0=PS[6 + hb_][:, :].rearrange("p (a b) -> p a b", b=128), in1=xt[:, hb_ * 4:hb_ * 4 + 4, :], op=ALU.add),
                         [PB[6 + hb_][0], B_xt], [B_h1T])
                yield
                for m_ in range(8):
                    S.op("pe", lambda e, m_=m_: e.transpose(pq(6 + m_ // 4, m_ % 4), h1T[:, m_, :], ident), [B_h1T, B_cst], [PB[6 + m_ // 4][0]])
                for hb_ in range(2):
                    S.op("act", lambda e, hb_=hb_: e.copy(out=h1tok[:, hb_ * 512:(hb_ + 1) * 512], in_=PS[6 + hb_][:, :]), [PB[6 + hb_][0]], [B_h1tok])
                S.dma(ch_h1tok, h1tok_s[i - 1], h1tok, reads=[B_h1tok], writes=[B_h1toks])
                yield
                S.op("act", lambda e: e.activation(out=h1sq, in_=h1T, func=AF.Square), [B_h1T], [B_h1sq])
                for dc in range(8):
                    S.op("pe", lambda e, dc=dc: e.matmul(pq(6, 0), lhsT=ones_b, rhs=h1sq[:, dc, :], start=(dc == 0), stop=(dc == 7)), [B_onesb, B_h1sq], [PB[6][0]])
                S.op("act", lambda e: e.activation(out=rs2, in_=pq(6, 0), func=AF.Sqrt, scale=1.0 / 1024, bias=prm[:, P_EPS6:P_EPS6 + 1]), [PB[6][0], B_prm], [B_rs2])
                S.op("dve", lambda e: e.reciprocal(out=rs2, in_=rs2), [B_rs2], [B_rs2])
                S.op("dve", lambda e: e.tensor_tensor(out=t1b, in0=h1T, in1=rs2.unsqueeze(1).to_broadcast([128, 8, 128]), op=ALU.mult), [B_h1T, B_rs2], [B_t1b])
                S.op("pool", lambda e: e.tensor_tensor(out=h1n, in0=t1b, in1=prm[:, P_GFFN:P_GFFN + 8].unsqueeze(2).to_broadcast([128, 8, 128]), op=ALU.mult), [B_t1b, B_prm], [B_h1n])
                S.dma(ch_h1n, h1nT_s[i - 1], h1n, reads=[B_h1n], writes=[B_h1ns])
                if i == 1:
                    tap("h1tok", h1tok, B_h1tok, [128, 1024])
                    tap("h1n", h1n, B_h1n, [128, 8, 128], BF16)
                yield

            def _adv(g):
                try:
                    next(g)
                    return True
                except StopIteration:
                    return False

            def _run(g):
                for _ in g:
                    pass

            _run(cfront(0))
            if NT > 1:
                _run(cfront(1))
            _run(cb1(0))
            for i in range(NT):
                g2 = cb2(i)
                g1 = cb1(i + 1) if i + 1 < NT else iter(())
                gf = cfront(i + 2) if i + 2 < NT else iter(())
                a1 = a2 = af = True
                while a1 or a2 or af:
                    if a2:
                        a2 = _adv(g2)
                    if a1:
                        a1 = _adv(g1)
                    if af:
                        af = _adv(gf)
        phase_C()
        S.barrier()

        B_sels = Buf("sels")

        def phase_Q():
            top[0] = base_top
            wq, B_wq = alloc("wq", [8, 2048], BF16)
            skT, B_skT = alloc("skT", [16, 128], BF16)
            S.dma(S.chan("wq"), wq, wq_d.rearrange("(c p) n -> p c n", p=128), writes=[B_wq], eng="pool")
            S.dma(S.chan("skT"), skT, skT_d, writes=[B_skT], eng="pool")
            hns = [alloc("hn%d" % k, [8, 128], BF16) for k in range(2)]
            ch_hns = [S.chan("hn%d" % k) for k in range(2)]
            qT, B_qT = alloc("qT", [16, 128], BF16)
            ssbs = [alloc("ssb%d" % k, [16, 128]) for k in range(2)]
            wk16, _ = alloc("wk16", [16, 128])
            B_wkg = [Buf("wk%d" % g_) for g_ in range(16)]
            B_ssb4s = [[Buf("ssb%d_%d" % (k, g_)) for g_ in range(4)] for k in range(2)]
            B_topsg = [Buf("tops%d" % g_) for g_ in range(16)]
            B_topig = [Buf("topi%d" % g_) for g_ in range(16)]
            B_candh = [Buf("cand%d" % g_) for g_ in range(8)]
            B_bestsh = [Buf("bests%d" % g_) for g_ in range(8)]
            B_bestch = [Buf("bestc%d" % g_) for g_ in range(8)]
            B_eqh = [Buf("eq%d" % g_) for g_ in range(16)]
            B_sel3h = [Buf("sel3_%d" % g_) for g_ in range(16)]
            B_sel3g = Buf("sel3g")
            B_ju2 = Buf("ju2")
            TS3 = [(alloc("tops%d" % k, [16, 16])[0], alloc("topi%d" % k, [16, 16], U32)[0], alloc("topif%d" % k, [16, 16])[0]) for k in range(2)]
            TB3 = [([Buf("tops%d_%d" % (k, g_)) for g_ in range(16)], [Buf("topi%d_%d" % (k, g_)) for g_ in range(16)], Buf("topif%d" % k)) for k in range(2)]
            wk2, _ = alloc("wk2", [8, 256])
            B_wk2h = [Buf("wk2_%d" % h) for h in range(8)]
            cand, B_cand = alloc("cand", [8, 256])
            bests, B_bests = alloc("bests", [8, 16])
            bestc, B_bestc = alloc("bestc", [8, 16], U32)
            ju, B_ju = alloc("ju", [2, 8, 16], U32)
            j1, B_j1 = alloc("j1", [8, 16])
            j2, B_j2 = alloc("j2", [8, 16])
            eq, B_eq = alloc("eq", [16, 16, 16])
            ee, B_ee = alloc("ee", [8, 16])
            zz, B_zz = alloc("zz", [8])
            sel3, B_sel3 = alloc("sel3", [3, 128])
            selT, B_selT = alloc("selT", [3, 128])
            ch_hn = S.chan("hn")
            ch_sel = S.chan("sel")
            iota16 = cst[:, C_IOTA, 0:16]
            def q_front(i):
                hn, B_hn = hns[i % 2]
                ssb = ssbs[i % 2][0]
                B_ssb4 = B_ssb4s[i % 2]
                S.dma(ch_hns[i % 2], hn, h1nT_s[i], reads=[B_h1ns], writes=[B_hn])
                for g_ in range(16):
                    for dc in range(8):
                        S.op("pe", lambda e, g_=g_, dc=dc: e.matmul(pq(g_ // 4, g_ % 4), lhsT=wq[:, dc, g_ * 128:(g_ + 1) * 128], rhs=hn[:, dc, :], start=(dc == 0), stop=(dc == 7)),
                             [B_wq, B_hn], [PB[g_ // 4][0]])
                    if g_ % 4 == 3:
                        S.op("act", lambda e, g_=g_: e.copy(out=qT[:, g_ - 3:g_ + 1, :], in_=PS[g_ // 4][:, :].rearrange("p (a b) -> p a b", b=128)), [PB[g_ // 4][0]], [B_qT])
                for g_ in range(16):
                    S.op("pe", lambda e, g_=g_: e.matmul(pq(4 + g_ // 4, g_ % 4), lhsT=qT[:, g_, :], rhs=skT[:, g_, :], start=True, stop=True), [B_qT, B_skT], [PB[4 + g_ // 4][0]])
                    if g_ % 4 == 3:
                        S.op("act", lambda e, g_=g_: e.copy(out=ssb[:, g_ - 3:g_ + 1, :], in_=PS[4 + g_ // 4][:, :].rearrange("p (a b) -> p a b", b=128)), [PB[4 + g_ // 4][0]], [B_ssb4[g_ // 4]])

            def q_b1(i):
                tops, topi, topif = TS3[i % 2]
                B_topsg, B_topig, B_topif = TB3[i % 2]
                ssb = ssbs[i % 2][0]
                B_ssb4 = B_ssb4s[i % 2]
                for g_ in range(0, 8):
                    S.op("dve", lambda e, g_=g_: e.max(out=tops[:, g_, 0:8], in_=ssb[:, g_, :]), [B_ssb4[g_ // 4]], [B_topsg[g_]])
                yield
                for g_ in range(8, 16):
                    S.op("dve", lambda e, g_=g_: e.max(out=tops[:, g_, 0:8], in_=ssb[:, g_, :]), [B_ssb4[g_ // 4]], [B_topsg[g_]])
                yield
                for g_ in range(0, 8):
                    S.op("dve", lambda e, g_=g_: e.max_index(out=topi[:, g_, 0:8], in_max=tops[:, g_, 0:8], in_values=ssb[:, g_, :]), [B_ssb4[g_ // 4], B_topsg[g_]], [B_topig[g_]])
                yield
                for g_ in range(8, 16):
                    S.op("dve", lambda e, g_=g_: e.max_index(out=topi[:, g_, 0:8], in_max=tops[:, g_, 0:8], in_values=ssb[:, g_, :]), [B_ssb4[g_ // 4], B_topsg[g_]], [B_topig[g_]])
                yield
                for g_ in range(0, 8):
                    S.op("dve", lambda e, g_=g_: e.match_replace(out=wk16[:, g_, :], in_to_replace=tops[:, g_, 0:8], in_values=ssb[:, g_, :], imm_value=-1e30), [B_ssb4[g_ // 4], B_topsg[g_]], [B_wkg[g_]])
                yield
                for g_ in range(8, 16):
                    S.op("dve", lambda e, g_=g_: e.match_replace(out=wk16[:, g_, :], in_to_replace=tops[:, g_, 0:8], in_values=ssb[:, g_, :], imm_value=-1e30), [B_ssb4[g_ // 4], B_topsg[g_]], [B_wkg[g_]])
                yield
                for g_ in range(0, 8):
                    S.op("dve", lambda e, g_=g_: e.max(out=tops[:, g_, 8:16], in_=wk16[:, g_, :]), [B_wkg[g_]], [B_topsg[g_]])
                yield
                for g_ in range(8, 16):
                    S.op("dve", lambda e, g_=g_: e.max(out=tops[:, g_, 8:16], in_=wk16[:, g_, :]), [B_wkg[g_]], [B_topsg[g_]])
                yield
                for g_ in range(0, 8):
                    S.op("dve", lambda e, g_=g_: e.max_index(out=topi[:, g_, 8:16], in_max=tops[:, g_, 8:16], in_values=wk16[:, g_, :]), [B_wkg[g_], B_topsg[g_]], [B_topig[g_]])
                yield
                for g_ in range(8, 16):
                    S.op("dve", lambda e, g_=g_: e.max_index(out=topi[:, g_, 8:16], in_max=tops[:, g_, 8:16], in_values=wk16[:, g_, :]), [B_wkg[g_], B_topsg[g_]], [B_topig[g_]])
                S.op("pool", lambda e: e.tensor_copy(out=topif, in_=topi), B_topig, [B_topif])
                yield

            def q_b2(i):
                tops, topi, topif = TS3[i % 2]
                B_topsg, B_topig, B_topif = TB3[i % 2]
                yield
                for h in range(8):
                    S.op("pool", lambda e, h=h: e.tensor_tensor(out=cand[:, h, :].rearrange("p (a b) -> p a b", b=16),
                                                              in0=tops[:, 2 * h, :].unsqueeze(2).to_broadcast([128, 16, 16]),
                                                              in1=tops[:, 2 * h + 1, :].unsqueeze(1).to_broadcast([128, 16, 16]), op=ALU.add), [B_topsg[2 * h], B_topsg[2 * h + 1]], [B_candh[h]])
                yield
                for h in range(8):
                    S.op("dve", lambda e, h=h: e.max(out=bests[:, h, 0:8], in_=cand[:, h, :]), [B_candh[h]], [B_bestsh[h]])
                yield
                for h in range(8):
                    S.op("dve", lambda e, h=h: e.max_index(out=bestc[:, h, 0:8], in_max=bests[:, h, 0:8], in_values=cand[:, h, :]), [B_candh[h], B_bestsh[h]], [B_bestch[h]])
                yield
                for h in range(8):
                    S.op("dve", lambda e, h=h: e.match_replace(out=wk2[:, h, :], in_to_replace=bests[:, h, 0:8], in_values=cand[:, h, :], imm_value=-1e30),
                         [B_candh[h], B_bestsh[h]], [B_wk2h[h]])
                yield
                for h in range(8):
                    S.op("dve", lambda e, h=h: e.max(out=bests[:, h, 8:16], in_=wk2[:, h, :]), [B_wk2h[h]], [B_bestsh[h]])
                yield
                for h in range(8):
                    S.op("dve", lambda e, h=h: e.max_index(out=bestc[:, h, 8:16], in_max=bests[:, h, 8:16], in_values=wk2[:, h, :]),
                         [B_wk2h[h], B_bestsh[h]], [B_bestch[h]])
                S.op("dve", lambda e: e.tensor_single_scalar(out=ju[:, 0, :, :], in_=bestc, scalar=4, op=ALU.logical_shift_right), B_bestch, [B_ju])
                S.op("dve", lambda e: e.tensor_single_scalar(out=ju[:, 1, :, :], in_=bestc, scalar=15, op=ALU.bitwise_and), B_bestch, [B_ju2])
                S.op("pool", lambda e: e.tensor_copy(out=j1, in_=ju[:, 0, :, :]), [B_ju], [B_j1])
                S.op("pool", lambda e: e.tensor_copy(out=j2, in_=ju[:, 1, :, :]), [B_ju2], [B_j2])
                yield
                for half, jj_ in ((0, j1), (1, j2)):
                    Bj = B_j1 if half == 0 else B_j2
                    for h in range(8):
                        S.op("dve", lambda e, h=h, jj_=jj_, half=half: e.tensor_tensor(out=eq[:, half * 8 + h, :, :], in0=jj_[:, h, :].unsqueeze(2).to_broadcast([128, 16, 16]),
                                                                          in1=iota16.unsqueeze(1).to_broadcast([128, 16, 16]), op=ALU.is_equal), [Bj, B_cst], [B_eqh[half * 8 + h]])
                    for h in range(8):
                        S.op("pool", lambda e, h=h, half=half: e.tensor_tensor(out=eq[:, half * 8 + h, :, :], in0=eq[:, half * 8 + h, :, :],
                                                                            in1=topif[:, 2 * h + half, :].unsqueeze(1).to_broadcast([128, 16, 16]), op=ALU.mult), [B_eqh[half * 8 + h], B_topif], [B_eqh[half * 8 + h]])
                yield
                for half in range(2):
                    for h in range(8):
                        S.op("dve", lambda e, h=h, half=half: e.tensor_reduce(out=sel3[:, half, h * 16:(h + 1) * 16], in_=eq[:, half * 8 + h, :, :], axis=AX.X, op=ALU.add), [B_eqh[half * 8 + h]], [B_sel3h[half * 8 + h]])
                B_bests_all = B_bestsh
                yield
                S.op("dve", lambda e: e.tensor_tensor(out=ee, in0=bests, in1=bests[:, :, 0:1].to_broadcast([128, 8, 16]), op=ALU.subtract), B_bestsh, [B_ee])
                S.op("act", lambda e: e.activation(out=ee, in_=ee, func=AF.Exp), [B_ee], [B_ee])
                S.op("dve", lambda e: e.tensor_reduce(out=zz, in_=ee, axis=AX.X, op=ALU.add), [B_ee], [B_zz])
                S.op("dve", lambda e: e.reciprocal(out=zz, in_=zz), [B_zz], [B_zz])
                S.op("dve", lambda e: e.tensor_tensor(out=sel3[:, 2, :].rearrange("p (h j) -> p h j", j=16), in0=ee, in1=zz.unsqueeze(2).to_broadcast([128, 8, 16]), op=ALU.mult), [B_ee, B_zz], [B_sel3g])
                yield
                for k in range(3):
                    S.op("pe", lambda e, k=k: e.transpose(pq(0, k), sel3[:, k, :], ident), B_sel3h + [B_sel3g, B_cst], [PB[0][0]])
                S.op("act", lambda e: e.copy(out=selT.rearrange("p a b -> p (a b)"), in_=PS[0][:, 0:384]), [PB[0][0]], [B_selT])
                S.dma(ch_sel, sel_s[i], selT, reads=[B_selT], writes=[B_sels])
                if i == 0:
                    tap("sel3", sel3, B_sel3g, [128, 3, 128])


                yield
            def _adv(g):
                try:
                    next(g)
                    return True
                except StopIteration:
                    return False

            q_front(0)
            if NRT > 1:
                q_front(1)
            for _ in q_b1(0):
                pass
            for i in range(NRT):
                if i + 2 < NRT:
                    q_front(i + 2)
                g2 = q_b2(i)
                g1 = q_b1(i + 1) if i + 1 < NRT else iter(())
                a1 = a2 = True
                while a1 or a2:
                    if a2:
                        a2 = _adv(g2)
                    if a1:
                        a1 = _adv(g1)
        phase_Q()
        S.barrier()

        def phase_E():
            top[0] = base_top
            NS = NRT // 2
            act3s = [alloc("act3_%d" % k, [256, 128], BF16) for k in range(2)]
            hn2s = [alloc("hn2_%d" % k, [8, 256], BF16) for k in range(2)]
            selAs = [alloc("selA_%d" % k, [2, 3, 128]) for k in range(2)]
            Ub = [alloc("Ub%d" % k, [8, 128], BF16) for k in range(8)]
            Vb = [alloc("Vb%d" % k, [1024], BF16) for k in range(8)]
            Aoh = [alloc("Aoh%d" % k, [4, 128], BF16) for k in range(4)]
            Boh = [alloc("Boh%d" % k, [4, 128], BF16) for k in range(4)]
            ysb, B_ysb = alloc("eysb", [1024])
            h1t, B_h1t = alloc("h1t", [1024])
            gfin, B_gfin = alloc("gfin", [1024])
            ob, B_ob = alloc("ob", [1024])
            stat, B_stat = alloc("stat", [4])
            S.dma(S.chan("gfin"), gfin, gfin_d.partition_broadcast(128)[:, 0, :], writes=[B_gfin])
            ch_hn2 = [S.chan("hn2_%d" % k) for k in range(2)]
            ch_selA = [S.chan("selA_%d" % k) for k in range(2)]
            ch_U = [S.chan("U%d" % k) for k in range(8)]
            ch_V = [S.chan("V%d" % k) for k in range(8)]
            ch_h1t = S.chan("h1t")
            ch_out = S.chan("out")
            iota_bc = cst[:, C_IOTA, :].unsqueeze(1).to_broadcast([128, 4, 128])
            uctr = [0]
            actr = [0]

            def loads(s_):
                hn2, B_hn2 = hn2s[s_ % 2]
                selA, B_selA = selAs[s_ % 2]
                for ts in range(2):
                    S.dma(ch_hn2[s_ % 2], hn2[:, :, ts * 128:(ts + 1) * 128], h1nT_s[s_ * 2 + ts], reads=[B_h1ns], writes=[B_hn2])
                    S.dma(ch_selA[s_ % 2], selA[:, ts, :, :], sel_s[s_ * 2 + ts], reads=[B_sels], writes=[B_selA])

            def a_iter(s_, i2):
                act3, B_act3 = act3s[s_ % 2]
                hn2, B_hn2 = hn2s[s_ % 2]
                uctr[0] += 1
                slot = uctr[0] % 8
                U_, B_U = Ub[slot]
                S.dma(ch_U[slot], U_.rearrange("p b c -> p (b c)"), uT_b[i2], reads=[B_uTb], writes=[B_U])
                bank = (actr[0] // 2) % 2
                half = actr[0] % 2
                actr[0] += 1
                for dc in range(8):
                    S.op("pe", lambda e, dc=dc: e.matmul(PS[bank][:, half * 256:(half + 1) * 256], lhsT=U_[:, dc, :], rhs=hn2[:, dc, :], start=(dc == 0), stop=(dc == 7)), [B_U, B_hn2], [PB[bank][0]])
                if half == 1:
                    S.op("act", lambda e: e.activation(out=act3[:, :, i2 - 1:i2 + 1], in_=PS[bank][:, :].rearrange("p (i t) -> p t i", i=2), func=AF.Gelu), [PB[bank][0]], [B_act3])

            def b_vars(s_, tg):
                act3, B_act3 = act3s[s_ % 2]
                selA, B_selA = selAs[s_ % 2]
                t0 = tg * 4
                A_, B_A = Aoh[tg % 4]
                Bm, B_B = Boh[tg % 4]
                return act3, B_act3, selA, B_selA, t0, t0 // 128, t0 % 128, A_, B_A, Bm, B_B, 2 + tg % 2

            def b_stage1(s_, tg):
                act3, B_act3, selA, B_selA, t0, ti, tt, A_, B_A, Bm, B_B, gb = b_vars(s_, tg)
                S.op("dve", lambda e: e.tensor_tensor(out=A_, in0=iota_bc, in1=selA[:, ti, 0, tt:tt + 4].unsqueeze(2).to_broadcast([128, 4, 128]), op=ALU.is_equal), [B_cst, B_selA], [B_A])
                S.op("pool", lambda e: e.tensor_tensor(out=A_, in0=A_, in1=selA[:, ti, 2, tt:tt + 4].unsqueeze(2).to_broadcast([128, 4, 128]), op=ALU.mult), [B_A, B_selA], [B_A])
                S.op("dve", lambda e: e.tensor_tensor(out=Bm, in0=iota_bc, in1=selA[:, ti, 1, tt:tt + 4].unsqueeze(2).to_broadcast([128, 4, 128]), op=ALU.is_equal), [B_cst, B_selA], [B_B])

            def b_stage2(s_, tg):
                act3, B_act3, selA, B_selA, t0, ti, tt, A_, B_A, Bm, B_B, gb = b_vars(s_, tg)
                for tk in range(4):
                    S.op("pe", lambda e, tk=tk: e.matmul(pq(gb, tk), lhsT=A_[:, tk, :], rhs=Bm[:, tk, :], start=True, stop=True), [B_A, B_B], [PB[gb][0]])

            def b_stage3(s_, tg):
                act3, B_act3, selA, B_selA, t0, ti, tt, A_, B_A, Bm, B_B, gb = b_vars(s_, tg)
                S.op("dve", lambda e: e.tensor_tensor(out=act3[:, t0:t0 + 4, :], in0=PS[gb][:, :].rearrange("p (a b) -> p a b", b=128), in1=act3[:, t0:t0 + 4, :], op=ALU.mult),
                     [PB[gb][0], B_act3], [B_act3])

            def c_phase(s_):
                act3, B_act3 = act3s[s_ % 2]
                vctr = 0
                for i2 in range(128):
                    vctr += 1
                    V_, B_V = Vb[vctr % 8]
                    S.dma(ch_V[vctr % 8], V_, vP_b[i2], reads=[B_vPb], writes=[B_V])
                    for ts in range(2):
                        for dh in range(2):
                            bk = 4 + ts * 2 + dh
                            S.op("pe", lambda e, V_=V_, i2=i2, ts=ts, dh=dh, bk=bk: e.matmul(PS[bk][:, :], lhsT=act3[:, ts * 128:(ts + 1) * 128, i2], rhs=V_[:, dh * 512:(dh + 1) * 512],
                                                                                       start=(i2 == 0), stop=(i2 == 127)), [B_act3, B_V], [PB[bk][0]])

            def d_phase(s_):
                for ts in range(2):
                    gi_ = s_ * 2 + ts
                    S.dma(ch_h1t, h1t, h1tok_s[gi_], reads=[B_h1toks], writes=[B_h1t])
                    for dh in range(2):
                        bk = 4 + ts * 2 + dh
                        S.op("dve", lambda e, bk=bk, dh=dh: e.tensor_tensor(out=ysb[:, dh * 512:(dh + 1) * 512], in0=PS[bk][:, :], in1=h1t[:, dh * 512:(dh + 1) * 512], op=ALU.add),
                             [PB[bk][0], B_h1t], [B_ysb])
                    S.op("pool", lambda e: e.tensor_tensor(out=ob, in0=ysb, in1=ysb, op=ALU.mult), [B_ysb], [B_ob])
                    S.op("dve", lambda e: e.tensor_reduce(out=stat[:, 0:1], in_=ob, axis=AX.X, op=ALU.add), [B_ob], [B_stat])
                    S.op("act", lambda e: e.activation(out=stat[:, 1:2], in_=stat[:, 0:1], func=AF.Sqrt, scale=1.0 / 1024, bias=prm[:, P_EPS6:P_EPS6 + 1]), [B_stat, B_prm], [B_stat])
                    S.op("dve", lambda e: e.reciprocal(out=stat[:, 2:3], in_=stat[:, 1:2]), [B_stat], [B_stat])
                    S.op("dve", lambda e: e.scalar_tensor_tensor(out=ob, in0=ysb, scalar=stat[:, 2:3], in1=gfin, op0=ALU.mult, op1=ALU.mult), [B_ysb, B_stat, B_gfin], [B_ob])
                    o_ = S.dma(ch_out, out_d[gi_ * 128:(gi_ + 1) * 128, :], ob, reads=[B_ob])
                    S.final_waits.append(o_)

            loads(0)
            for i2 in range(128):
                a_iter(0, i2)
            for s_ in range(NS):
                nxt = s_ + 1 < NS
                if nxt:
                    loads(s_ + 1)
                b_stage1(s_, 0)
                b_stage1(s_, 1)
                b_stage2(s_, 0)
                for tg in range(64):
                    if tg + 2 < 64:
                        b_stage1(s_, tg + 2)
                    if tg + 1 < 64:
                        b_stage2(s_, tg + 1)
                    if nxt:
                        a_iter(s_ + 1, 2 * tg)
                        a_iter(s_ + 1, 2 * tg + 1)
                    b_stage3(s_, tg)
                    if tg == 3 and s_ > 0:
                        d_phase(s_ - 1)
                c_phase(s_)
            d_phase(NS - 1)
        phase_E()
        S.barrier()
        S.emit(st)
    return nc, dbg


def host_prep(inp, b, NT):
    f = np.float32
    x = np.asarray(inp["x"])[b]
    nreal = (NT - 1) * 128
    seq = np.concatenate([np.zeros((NPAD, D), f), np.asarray(inp["meta_tokens"], f), x[:nreal]], axis=0)
    xT = np.ascontiguousarray(seq.reshape(NT, 128, 8, 128).transpose(0, 3, 2, 1))
    m = {"xT": xT}
    return m


def shared_prep(inp):
    f = np.float32
    g = lambda k: np.asarray(inp[k], f)
    m = {}
    m["w_in"] = np.ascontiguousarray(g("w_in")[0])
    m["w_conv_out"] = np.ascontiguousarray(g("w_conv_out")[0])
    m["w_rwkv_out"] = np.ascontiguousarray(g("w_rwkv_out")[0])
    m["w_o"] = np.ascontiguousarray(g("w_o")[0])
    m["w_q"] = np.ascontiguousarray(g("w_q")[0])
    m["skT"] = np.ascontiguousarray(g("sub_keys")[0].transpose(3, 0, 1, 2).reshape(128, 16, 128))
    u = g("expert_u")[0]
    m["uT"] = np.ascontiguousarray(u.reshape(128, 128, 8, 128).transpose(1, 3, 2, 0)).reshape(128, 128, 1024)
    v = g("expert_v")[0]
    m["vP"] = np.ascontiguousarray(v.reshape(128, 128, 1024).transpose(1, 0, 2))
    m["wa_up"] = np.ascontiguousarray(np.concatenate([g("w_up")[0], g("a_up")[0]], axis=0))
    m["g_up"] = np.ascontiguousarray(g("g_up")[0])
    m["w0row"] = np.ascontiguousarray(g("w0")[0].reshape(1, 512))
    m["gfin"] = np.ascontiguousarray(g("g_final").reshape(1, 1024))
    prm = np.zeros((128, NPRM), f)
    col = lambda a, n: np.asarray(a, f).reshape(n, 128).T
    prm[:, P_GMIX:P_GMIX + 8] = col(g("g_mix")[0], 8)
    prm[:, P_MU:P_MU + 14] = col(g("mu_shift")[0], 14)
    prm[:, P_A0:P_A0 + 4] = col(g("a0")[0], 4)
    prm[:, P_KK:P_KK + 4] = col(g("k_k")[0], 4)
    prm[:, P_KA:P_KA + 4] = col(g("k_a")[0], 4)
    prm[:, P_RK:P_RK + 4] = col(g("r_k")[0].reshape(512), 4)
    prm[:, P_GNG:P_GNG + 4] = col(g("gn_g")[0], 4)
    prm[:, P_GNB:P_GNB + 4] = col(g("gn_b")[0], 4)
    prm[:, P_CB:P_CB + 4] = col(g("conv_b")[0], 4)
    prm[:, P_LNG:P_LNG + 4] = col(g("conv_ln_g")[0], 4)
    prm[:, P_LNB:P_LNB + 4] = col(g("conv_ln_b")[0], 4)
    prm[:, P_GFFN:P_GFFN + 8] = col(g("g_ffn")[0], 8)
    prm[:, P_EPS6] = 1e-6
    prm[:, P_EPS5] = 1e-5
    prm[:, P_EPSG] = 64e-5
    cw = g("conv_w")[0]
    prm[:, P_CW:P_CW + 124] = cw.reshape(31, 4, 128).transpose(2, 1, 0).reshape(128, 124)
    m["params"] = prm
    cst = np.zeros((128, NCST, 128), f)
    ar = np.arange(128)
    cst[:, C_IDENT] = np.eye(128)
    cst[:, C_ONES] = 1.0
    cst[:, C_O512] = 1.0 / 512
    cst[:, C_BLK] = (ar[:, None] // 64 == ar[None, :] // 64)
    e05 = float(np.exp(np.float32(-0.5)))
    cst[:, C_TRII] = -e05 * (ar[:, None] <= ar[None, :])
    cst[:, C_TRIE] = -e05 * (ar[:, None] < ar[None, :])
    cst[:, C_MS] = (ar[:, None] < ar[None, :])
    cst[:, C_MSN] = -1.0 * (ar[:, None] < ar[None, :])
    cst[:, C_MLN] = -1.0 * (ar[None, :] < ar[:, None])
    cst[:, C_MI] = (ar[:, None] <= ar[None, :])
    cst[:, C_IOTA] = ar[None, :]
    cst[:, C_O1024] = 1.0 / 1024
    m["cst"] = cst
    return m


_CACHE = {}


def kernel(**inputs):
    NT = 33
    if "nc" not in _CACHE:
        _CACHE["nc"] = build_program(NT)[0]
    nc = _CACHE["nc"]
    sh = shared_prep(inputs)
    in_maps = []
    for b in range(8):
        m = dict(sh)
        m.update(host_prep(inputs, b, NT))
        in_maps.append(m)
    res = run_bass_kernel_spmd(nc, in_maps, core_ids=list(range(8)))
    return np.stack([r["out"] for r in res.results], axis=0)
```

```python
import numpy as np
import concourse.bass as bass
import concourse.mybir as mybir
from concourse.bass_utils import run_bass_kernel_spmd
from contextlib import ExitStack

F32 = mybir.dt.float32
BF16 = mybir.dt.bfloat16
U32 = mybir.dt.uint32
ALU = mybir.AluOpType
AF = mybir.ActivationFunctionType
AX = mybir.AxisListType


class Buf:
    __slots__ = ("name", "w", "r", "excl")

    def __init__(self, name, excl=False):
        self.name = name
        self.w = None
        self.r = []
        self.excl = excl


class Op:
    __slots__ = ("eng", "fn", "deps", "signal", "is_dma", "chan", "sem", "val")

    def __init__(self, eng, fn, is_dma=False, chan=None):
        self.eng = eng
        self.fn = fn
        self.deps = []
        self.signal = False
        self.is_dma = is_dma
        self.chan = chan
        self.sem = None
        self.val = 0


class Chan:
    __slots__ = ("name", "last", "count", "sem")

    def __init__(self, name):
        self.name = name
        self.last = None
        self.count = 0
        self.sem = None


EPOCH = 30000
CAST = True
ENGS = ("pe", "dve", "act", "pool", "sp")


class Sched:
    def __init__(self, nc):
        self.nc = nc
        self.ops = {e: [] for e in ENGS}
        self.chans = []
        self.final_waits = []

    def chan(self, name):
        c = Chan(name)
        self.chans.append(c)
        return c

    def _record(self, op, reads, writes):
        writes = writes + [b for b in reads if b.excl and not any(b is x for x in writes)]
        deps = []
        for b in reads:
            if b.w is not None:
                deps.append(b.w)
        for b in writes:
            if b.w is not None:
                deps.append(b.w)
            for r in b.r:
                if r.eng == op.eng and not r.is_dma and not op.is_dma:
                    continue
                deps.append(r)
        seen = set(id(d) for d in op.deps)
        for d in deps:
            if d is op or id(d) in seen:
                continue
            if d.eng == "pe" and op.eng == "pe" and not d.is_dma and not op.is_dma:
                continue
            seen.add(id(d))
            op.deps.append(d)
            d.signal = True
        for b in reads:
            b.r.append(op)
        for b in writes:
            b.w = op
            b.r = []
        self.ops[op.eng].append(op)
        return op

    def op(self, eng, fn, reads=(), writes=()):
        return self._record(Op(eng, fn), list(reads), list(writes))

    def dma(self, chan, out, in_, reads=(), writes=(), eng="sp", **kw):
        op = Op(eng, lambda e: e.dma_start(out=out, in_=in_, **kw), is_dma=True, chan=chan)
        if chan.last is not None:
            op.deps.append(chan.last)
            chan.last.signal = True
        chan.last = op
        op.signal = True
        return self._record(op, list(reads), list(writes))

    def barrier(self):
        lasts = []
        for e in ENGS:
            for o in reversed(self.ops[e]):
                if not o.is_dma and o.fn is not None:
                    lasts.append(o)
                    break
        for c in self.chans:
            if c.last is not None:
                lasts.append(c.last)
        for e in ENGS:
            op = Op(e, None)
            for d in lasts:
                if d.eng == e and not d.is_dma:
                    continue
                op.deps.append(d)
                d.signal = True
            self.ops[e].append(op)

    def emit(self, stack):
        nc = self.nc
        for c in self.chans:
            c.sem = stack.enter_context(nc.semaphore("c_" + c.name))
        for d in self.final_waits:
            d.signal = True
        for e, lst in self.ops.items():
            cur = None
            cnt = 0
            k = 0
            for op in lst:
                if op.is_dma:
                    op.chan.count += 16
                    op.sem = op.chan.sem
                    op.val = op.chan.count
                elif op.signal:
                    if cur is None or cnt >= EPOCH:
                        cur = stack.enter_context(nc.semaphore("e_%s_%d" % (e, k)))
                        k += 1
                        cnt = 0
                    cnt += 1
                    op.sem = cur
                    op.val = cnt
        block = stack.enter_context(nc.Block())

        def run(engine_obj, lst, tail=None):
            waited = {}

            def w(d):
                key = d.sem.name
                if waited.get(key, 0) >= d.val:
                    return
                engine_obj.wait_ge(d.sem, d.val)
                waited[key] = d.val

            for op in lst:
                for d in op.deps:
                    w(d)
                if op.fn is None:
                    continue
                ins = op.fn(engine_obj)
                if op.is_dma:
                    ins.then_inc(op.sem, 16)
                elif op.signal:
                    ins.then_inc(op.sem, 1)
            if tail:
                for d in tail:
                    w(d)

        ops = self.ops
        fw = self.final_waits

        @block.tensor
        def _(e):
            run(e, ops["pe"])

        @block.vector
        def _(e):
            run(e, ops["dve"])

        @block.scalar
        def _(e):
            run(e, ops["act"])

        @block.gpsimd
        def _(e):
            run(e, ops["pool"])

        @block.sync
        def _(e):
            run(e, ops["sp"], tail=fw)


D = 1024
NPAD = 112
C_IDENT, C_ONES, C_O512, C_BLK, C_TRII, C_TRIE, C_MS, C_MSN, C_MLN, C_MI, C_IOTA, C_O1024 = range(12)
NCST = 12
P_GMIX = 0
P_MU = 8
P_A0 = 22
P_KK = 26
P_KA = 30
P_RK = 34
P_GNG = 38
P_GNB = 42
P_CB = 46
P_LNG = 50
P_LNB = 54
P_GFFN = 58
P_EPS6 = 66
P_EPS5 = 67
P_EPSG = 68
P_CW = 69
NPRM = 69 + 124


def build_program(NT, debug=False):
    NRT = NT - 1
    assert NRT % 2 == 0
    nc = bass.Bass("TRN2", target_bir_lowering=False)
    din = lambda n, s, dt=F32: nc.dram_tensor(n, s, dt, kind="ExternalInput").ap()
    xT_d = din("xT", [NT, 128, 8, 128])
    w_in_d = din("w_in", [1024, 4864])
    wco_d = din("w_conv_out", [512, 1024])
    wro_d = din("w_rwkv_out", [512, 1024])
    wo_d = din("w_o", [1024, 1024])
    wq_d = din("w_q", [1024, 2048])
    skT_d = din("skT", [128, 16, 128])
    uT_d = din("uT", [128, 128, 8 * 128])
    vP_d = din("vP", [128, 128, 1024])
    waup_d = din("wa_up", [128, 512])
    gup_d = din("g_up", [128, 512])
    prm_d = din("params", [128, NPRM])
    w0_d = din("w0row", [1, 512])
    cst_d = din("cst", [128, NCST, 128])
    gfin_d = din("gfin", [1, 1024])
    out_d = nc.dram_tensor("out", [NRT * 128, 1024], F32, kind="ExternalOutput").ap()
    dscr = lambda n, s, dt: nc.dram_tensor(n, s, dt, kind="Internal").ap()
    orw_s = dscr("orw_s", [NT, 128, 4, 128], BF16)
    h1tok_s = dscr("h1tok_s", [NRT, 128, 1024], F32)
    h1nT_s = dscr("h1nT_s", [NRT, 128, 8, 128], BF16)
    sel_s = dscr("sel_s", [NRT, 128, 3, 128], F32)
    uT_b = dscr("uT_b", [128, 128, 8 * 128], BF16)
    vP_b = dscr("vP_b", [128, 128, 1024], BF16)
    dbg = {}

    st = ExitStack()
    with st:
        S = Sched(nc)
        ARENA = 53000
        arena = st.enter_context(nc.sbuf_tensor("arena", [128, ARENA], F32))
        PS = [st.enter_context(nc.psum_tensor("ps%d" % k, [128, 512], F32)) for k in range(8)]
        PB = []
        for k in range(8):
            _b = Buf("ps%d" % k, excl=True)
            PB.append([_b, _b, _b, _b])
        top = [0]

        def alloc(name, free, dt=F32):
            n = int(np.prod(free))
            words = n if dt in (F32, U32) else (n + 1) // 2
            words = (words + 7) // 8 * 8
            a = arena[:, top[0]:top[0] + words]
            top[0] += words
            assert top[0] <= ARENA, (name, top[0])
            if dt != F32:
                a = a.bitcast(dt)
            a = a[:, 0:n]
            if len(free) == 2:
                a = a.rearrange("p (a b) -> p a b", b=free[1])
            elif len(free) == 3:
                a = a.rearrange("p (a b c) -> p a b c", b=free[1], c=free[2])
            return a, Buf(name)

        def pq(k, q):
            return PS[k][:, q * 128:(q + 1) * 128]

        def tap(name, ap, buf, shape, dt=F32):
            if not debug:
                return
            d = nc.dram_tensor("dbg_" + name, list(shape), dt, kind="ExternalOutput").ap()
            c = S.chan("dbg_" + name)
            o = S.dma(c, d, ap, reads=[buf])
            S.final_waits.append(o)
            dbg[name] = (shape, dt)

        cst, B_cst = alloc("cst", [NCST, 128])
        prm, B_prm = alloc("prm", [NPRM])
        ones_b, B_onesb = alloc("ones_b", [128], BF16)
        ch_c = S.chan("cst")
        S.dma(ch_c, cst, cst_d, writes=[B_cst])
        ch_p = S.chan("prm")
        S.dma(ch_p, prm, prm_d, writes=[B_prm])
        S.op("dve", lambda e: e.tensor_copy(out=ones_b, in_=cst[:, C_ONES, :]), [B_cst], [B_onesb])
        ident = cst[:, C_IDENT, :]
        base_top = top[0]

        ch_cast = [S.chan("cast%d" % k) for k in range(4)]
        B_uTb = Buf("uTb")
        B_vPb = Buf("vPb")
        cast_ops = []
        def issue_cast(k):
            if not CAST:
                return
            sl = slice(k * 8, (k + 1) * 8)
            cast_ops.append(S.dma(ch_cast[k % 2], uT_b[sl], uT_d[sl], eng="pool"))
            cast_ops.append(S.dma(ch_cast[2 + k % 2], vP_b[sl], vP_d[sl], eng="pool"))

        def load_x(i, xt, B_xt, ch, xb, B_xb, sq, B_sq, rs, B_rs, psq):
            S.dma(ch, xt, xT_d[i], writes=[B_xt])
            S.op("pool", lambda e: e.tensor_tensor(out=xb, in0=xt, in1=prm[:, P_GMIX:P_GMIX + 8].unsqueeze(2).to_broadcast([128, 8, 128]), op=ALU.mult),
                 [B_xt, B_prm], [B_xb])
            S.op("act", lambda e: e.activation(out=sq, in_=xt, func=AF.Square), [B_xt], [B_sq])
            k, q = psq
            for dc in range(8):
                S.op("pe", lambda e, dc=dc: e.matmul(pq(k, q), lhsT=ones_b, rhs=sq[:, dc, :], start=(dc == 0), stop=(dc == 7)),
                     [B_onesb, B_sq], [PB[k][q]])
            S.op("act", lambda e: e.activation(out=rs, in_=pq(k, q), func=AF.Sqrt, scale=1.0 / 1024, bias=prm[:, P_EPS6:P_EPS6 + 1]),
                 [PB[k][q], B_prm], [B_rs])
            S.op("dve", lambda e: e.reciprocal(out=rs, in_=rs), [B_rs], [B_rs])

        B_orws = Buf("orws")
        ch_orw = S.chan("orw")

        def phase_R():
            wR, B_wR = alloc("wR", [8, 1792], BF16)
            waup, B_waup = alloc("waup", [512])
            gup, B_gup = alloc("gup", [512])
            w0r, B_w0r = alloc("w0r", [512])
            chw = S.chan("wR")
            S.dma(chw, wR, w_in_d[:, 1024:2816].rearrange("(c p) n -> p c n", p=128), writes=[B_wR], eng="pool")
            S.dma(S.chan("waup"), waup, waup_d, writes=[B_waup])
            S.dma(S.chan("gup"), gup, gup_d, writes=[B_gup])
            S.dma(S.chan("w0r"), w0r[0:1, :], w0_d, writes=[B_w0r])
            xts = [alloc("xt0", [8, 128])] * 2
            chx = [S.chan("x%d" % k) for k in range(2)]
            xb, B_xb = alloc("xb", [8, 128], BF16)
            sq, B_sq = alloc("sq", [8, 128], BF16)
            rs, B_rs = alloc("rs", [128])
            zr, B_zr = alloc("zr", [14, 129])
            zs, B_zs = alloc("zs", [14, 128])
            tl, B_tl = alloc("tl", [128])
            sgt, B_sgt = alloc("sgt", [512])
            cexc, B_cexc = alloc("cexc", [4, 128])
            cinv, B_cinv = alloc("cinv", [4, 128])
            aa, B_aa = alloc("aa", [4, 128])
            sgl, B_sgl = alloc("sgl", [128])
            kk, B_kk = alloc("kk", [4, 128])
            tmp, B_tmp = alloc("tmp", [4, 128])
            k2, B_k2 = alloc("k2", [4, 128])
            FS = [{n_: alloc("%s_%d" % (n_, k_), shp_) for n_, shp_ in (("rt", [4, 128]), ("kkt", [4, 128]), ("kt", [4, 128]), ("bt", [4, 128]), ("bon", [4, 128]), ("gT", [4, 128]), ("vtok", [512]), ("ktok", [512]), ("btok", [512]), ("cinc", [4, 128]))} for k_ in range(3)]
            tmp2, B_tmp2 = alloc("tmp2", [4, 128])
            osq, B_osq = tmp2.rearrange("p a b -> p (a b)"), B_tmp2
            ST, _ = alloc("ST", [4, 64])
            osb, B_osb = alloc("osb", [512])
            gst, B_gst = alloc("gst", [32])
            orw, B_orw = alloc("orw", [4, 128], BF16)
            KAS = {}
            for nm in ("Xa", "XTa", "Xb", "XTb", "Tb"):
                ap_, _ = alloc(nm + "8", [8, 128])
                KAS[nm] = (ap_, [Buf(nm + "_e"), Buf(nm + "_o")])
            KAD = []
            for k_ in range(2):
                d_ = {}
                for nm in ("Mk", "Ak", "Ab", "Ta"):
                    ap_, _ = alloc("%s8_%d" % (nm, k_), [8, 128])
                    d_[nm] = (ap_, [Buf("%s_e%d" % (nm, k_)), Buf("%s_o%d" % (nm, k_))])
                KAD.append(d_)
            XTs8, B_XTs8 = alloc("XTs8", [8, 64])
            NTs8, B_NTs8 = alloc("NTs8", [8, 64])
            tmpS, B_tmpS = alloc("tmpS", [4, 64])
            B_STp = [Buf("STe"), Buf("STo")]
            S.op("dve", lambda e: e.memset(zr, 0.0), [], [B_zr])
            S.op("dve", lambda e: e.memset(ST, 0.0), [], B_STp)
            slot_ctr = [0]

            def nslot():
                s_ = slot_ctr[0] % 3
                slot_ctr[0] += 1
                return 2 + s_

            def front(i):
                rt, B_rt = FS[i % 3]["rt"]
                kkt, B_kkt = FS[i % 3]["kkt"]
                kt, B_kt = FS[i % 3]["kt"]
                bt, B_bt = FS[i % 3]["bt"]
                bon, B_bon = FS[i % 3]["bon"]
                gT, B_gT = FS[i % 3]["gT"]
                vtok, B_vtok = FS[i % 3]["vtok"]
                ktok, B_ktok = FS[i % 3]["ktok"]
                btok, B_btok = FS[i % 3]["btok"]
                cinc, B_cinc = FS[i % 3]["cinc"]
                xt, B_xt = xts[i % 2]
                load_x(i, xt, B_xt, chx[i % 2], xb, B_xb, sq, B_sq, rs, B_rs, (1, 0))
                yield
                for gi, (j0, nj) in enumerate(((0, 4), (4, 4), (8, 4), (12, 2))):
                    for jj in range(nj):
                        j = j0 + jj
                        for dc in range(8):
                            S.op("pe", lambda e, j=j, jj=jj, dc=dc, gi=gi: e.matmul(pq(gi % 2, jj), lhsT=wR[:, dc, j * 128:(j + 1) * 128], rhs=xb[:, dc, :], start=(dc == 0), stop=(dc == 7)),
                                 [B_wR, B_xb], [PB[gi % 2][jj]])
                    S.op("dve", lambda e, gi=gi, j0=j0, nj=nj: e.tensor_tensor(out=zr[:, j0:j0 + nj, 1:129], in0=PS[gi % 2][:, 0:nj * 128].rearrange("p (a b) -> p a b", b=128),
                                                                           in1=rs.unsqueeze(1).to_broadcast([128, nj, 128]), op=ALU.mult),
                         PB[gi % 2][0:nj] + [B_rs], [B_zr])
                yield
                mu_bc = prm[:, P_MU:P_MU + 14].unsqueeze(2).to_broadcast([128, 14, 128])
                S.op("pool", lambda e: e.tensor_tensor(out=zs, in0=zr[:, :, 0:128], in1=zr[:, :, 1:129], op=ALU.subtract), [B_zr], [B_zs])
                S.op("pool", lambda e: e.tensor_tensor(out=zs, in0=zs, in1=mu_bc, op=ALU.mult), [B_zs, B_prm], [B_zs])
                S.op("pool", lambda e: e.tensor_tensor(out=zs, in0=zs, in1=zr[:, :, 1:129], op=ALU.add), [B_zs, B_zr], [B_zs])
                S.op("pool", lambda e: e.tensor_copy(out=zr[:, :, 0:1], in_=zr[:, :, 128:129]), [B_zr, B_zs], [B_zr])
                if i == 1:
                    tap("zs", zs, B_zs, [128, 14, 128])
                r_ = zs[:, 0:4, :]
                k_ = zs[:, 4:8, :]
                v_ = zs[:, 8:12, :]
                yield
                S.op("act", lambda e: e.activation(out=tl[0:64, :], in_=zs[0:64, 12, :], func=AF.Tanh), [B_zs], [B_tl])
                S.op("pe", lambda e: e.matmul(PS[0][:, :], lhsT=tl[0:64, :], rhs=waup[0:64, :], start=True, stop=False), [B_tl, B_waup], PB[0])
                S.op("pe", lambda e: e.matmul(PS[0][:, :], lhsT=cst[0:1, C_ONES, :], rhs=w0r[0:1, :], start=False, stop=True), [B_cst, B_w0r], PB[0])
                S.op("act", lambda e: e.activation(out=sgt, in_=PS[0][:, :], func=AF.Sigmoid), PB[0], [B_sgt])
                for cc in range(4):
                    S.op("pe", lambda e, cc=cc: e.matmul(pq(1, cc), lhsT=sgt[:, cc * 128:(cc + 1) * 128], rhs=cst[:, C_TRII, :], start=True, stop=True), [B_sgt, B_cst], [PB[1][cc]])
                    S.op("pe", lambda e, cc=cc: e.matmul(pq(0, cc), lhsT=sgt[:, cc * 128:(cc + 1) * 128], rhs=cst[:, C_TRIE, :], start=True, stop=True), [B_sgt, B_cst], [PB[0][cc]])
                S.op("act", lambda e: e.activation(out=cinc.rearrange("p a b -> p (a b)"), in_=PS[1][:, :], func=AF.Exp), PB[1], [B_cinc])
                S.op("act", lambda e: e.activation(out=cinv.rearrange("p a b -> p (a b)"), in_=PS[1][:, :], func=AF.Exp, scale=-1.0), PB[1], [B_cinv])
                S.op("act", lambda e: e.activation(out=cexc.rearrange("p a b -> p (a b)"), in_=PS[0][:, :], func=AF.Exp), PB[0], [B_cexc])
                yield
                for cc in range(4):
                    S.op("pe", lambda e, cc=cc: e.matmul(pq(1, cc), lhsT=waup[64:128, cc * 128:(cc + 1) * 128], rhs=zs[64:128, 12, :], start=True, stop=True), [B_waup, B_zs], [PB[1][cc]])
                    S.op("act", lambda e, cc=cc: e.activation(out=aa[:, cc, :], in_=pq(1, cc), func=AF.Sigmoid, bias=prm[:, P_A0 + cc:P_A0 + cc + 1]), [PB[1][cc], B_prm], [B_aa])
                S.op("act", lambda e: e.activation(out=sgl, in_=zs[:, 13, :], func=AF.Sigmoid), [B_zs], [B_sgl])
                for cc in range(4):
                    S.op("pe", lambda e, cc=cc: e.matmul(pq(0, cc), lhsT=gup[:, cc * 128:(cc + 1) * 128], rhs=sgl, start=True, stop=True), [B_gup, B_sgl], [PB[0][cc]])
                S.op("act", lambda e: e.copy(out=gT.rearrange("p a b -> p (a b)"), in_=PS[0][:, :]), PB[0], [B_gT])
                yield
                bc4 = lambda c0: prm[:, c0:c0 + 4].unsqueeze(2).to_broadcast([128, 4, 128])
                S.op("dve", lambda e: e.tensor_tensor(out=kk, in0=k_, in1=bc4(P_KK), op=ALU.mult), [B_zs, B_prm], [B_kk])
                S.op("pool", lambda e: e.tensor_tensor(out=tmp, in0=kk, in1=kk, op=ALU.mult), [B_kk], [B_tmp])
                for cc in range(4):
                    S.op("pe", lambda e, cc=cc: e.matmul(pq(1, cc), lhsT=cst[:, C_BLK, :], rhs=tmp[:, cc, :], start=True, stop=True), [B_cst, B_tmp], [PB[1][cc]])
                S.op("dve", lambda e: e.tensor_scalar(out=tmp.rearrange("p a b -> p (a b)"), in0=PS[1][:, :], scalar1=1e-24, scalar2=None, op0=ALU.max), PB[1], [B_tmp])
                S.op("act", lambda e: e.activation(out=tmp, in_=tmp, func=AF.Sqrt), [B_tmp], [B_tmp])
                S.op("dve", lambda e: e.reciprocal(out=tmp, in_=tmp), [B_tmp], [B_tmp])
                S.op("dve", lambda e: e.tensor_tensor(out=kk, in0=kk, in1=tmp, op=ALU.mult), [B_kk, B_tmp], [B_kk])
                S.op("pool", lambda e: e.tensor_scalar(out=k2, in0=aa, scalar1=-1.0, scalar2=None, op0=ALU.add), [B_aa], [B_k2])
                S.op("pool", lambda e: e.tensor_tensor(out=k2, in0=k2, in1=bc4(P_KA), op=ALU.mult), [B_k2, B_prm], [B_k2])
                S.op("dve", lambda e: e.scalar_tensor_tensor(out=k2, in0=k2, scalar=1.0, in1=k_, op0=ALU.add, op1=ALU.mult), [B_k2, B_zs], [B_k2])
                yield
                S.op("dve", lambda e: e.tensor_tensor(out=kkt, in0=kk, in1=cexc, op=ALU.mult), [B_kk, B_cexc], [B_kkt])
                S.op("dve", lambda e: e.tensor_tensor(out=bt, in0=kk, in1=aa, op=ALU.mult), [B_kk, B_aa], [B_bt])
                S.op("dve", lambda e: e.tensor_tensor(out=bt, in0=bt, in1=cinv, op=ALU.mult), [B_bt, B_cinv], [B_bt])
                S.op("pool", lambda e: e.tensor_tensor(out=kt, in0=k2, in1=cinv, op=ALU.mult), [B_k2, B_cinv], [B_kt])
                S.op("pool", lambda e: e.tensor_tensor(out=rt, in0=r_, in1=cinc, op=ALU.mult), [B_zs, B_cinc], [B_rt])
                S.op("pool", lambda e: e.tensor_tensor(out=tmp, in0=r_, in1=k2, op=ALU.mult), [B_zs, B_k2, B_kk], [B_tmp])
                S.op("pool", lambda e: e.tensor_tensor(out=tmp, in0=tmp, in1=bc4(P_RK), op=ALU.mult), [B_tmp, B_prm], [B_tmp])
                for cc in range(4):
                    S.op("pe", lambda e, cc=cc: e.matmul(pq(0, cc), lhsT=cst[:, C_BLK, :], rhs=tmp[:, cc, :], start=True, stop=True), [B_cst, B_tmp], [PB[0][cc]])
                S.op("dve", lambda e: e.tensor_tensor(out=bon, in0=PS[0][:, :].rearrange("p (a b) -> p a b", b=128), in1=v_, op=ALU.mult), PB[0] + [B_zs], [B_bon])
                yield
                for (src, Bsrc, dst, Bdst, bank) in ((v_, B_zs, vtok, B_vtok, 0), (kt, B_kt, ktok, B_ktok, 1), (bt, B_bt, btok, B_btok, 0)):
                    for cc in range(4):
                        S.op("pe", lambda e, src=src, cc=cc, bank=bank: e.transpose(pq(bank, cc), src[:, cc, :], ident), [Bsrc, B_cst], [PB[bank][cc]])
                    S.op("act", lambda e, dst=dst, bank=bank: e.copy(out=dst, in_=PS[bank][:, :]), PB[bank], [Bdst])
                if i == 1:
                    tap("kkt", kkt, B_kkt, [128, 4, 128])
                    tap("rt", rt, B_rt, [128, 4, 128])
                    tap("vtok", vtok, B_vtok, [128, 512])
                yield

            def hv(ap3, par, cc):
                return ap3[64 * par:64 * par + 64, cc, :]

            def neu(i):
                rt, B_rt = FS[i % 3]["rt"]
                kkt, B_kkt = FS[i % 3]["kkt"]
                kt, B_kt = FS[i % 3]["kt"]
                bt, B_bt = FS[i % 3]["bt"]
                bon, B_bon = FS[i % 3]["bon"]
                gT, B_gT = FS[i % 3]["gT"]
                vtok, B_vtok = FS[i % 3]["vtok"]
                ktok, B_ktok = FS[i % 3]["ktok"]
                btok, B_btok = FS[i % 3]["btok"]
                cinc, B_cinc = FS[i % 3]["cinc"]
                KA = dict(KAS)
                KA.update(KAD[i % 2])
                def hv(ap3, par, cc):
                    return ap3[64 * par:64 * par + 64, cc, :]

                def mm_mask(lsrc, Bl, rsrc, Br, mask_idx, kind, eng="dve"):
                    dst, Bd = KA[kind]
                    banks = [nslot(), nslot()]
                    for cc in range(4):
                        for par in range(2):
                            bank = banks[par]
                            S.op("pe", lambda e, par=par, cc=cc, bank=bank: e.matmul(pq(bank, cc), lhsT=hv(lsrc, par, cc), rhs=hv(rsrc, par, cc), start=True, stop=True), [Bl, Br], [PB[bank][0]])
                    for par in range(2):
                        bank = banks[par]
                        S.op(eng, lambda e, par=par, bank=bank: e.tensor_tensor(out=dst[:, par * 4:par * 4 + 4, :], in0=PS[bank][:, :].rearrange("p (a b) -> p a b", b=128),
                                                                           in1=cst[:, mask_idx, :].unsqueeze(1).to_broadcast([128, 4, 128]), op=ALU.mult), [PB[bank][0], B_cst], [Bd[par]])

                mm_mask(bt, B_bt, kkt, B_kkt, C_MSN, "Xa")
                yield
                mm_mask(kkt, B_kkt, bt, B_bt, C_MLN, "XTa")
                yield
                mm_mask(kt, B_kt, kkt, B_kkt, C_MS, "Mk")
                yield
                mm_mask(kt, B_kt, rt, B_rt, C_MI, "Ak")
                yield
                mm_mask(bt, B_bt, rt, B_rt, C_MI, "Ab")
                yield
                for par in range(2):
                    S.op("dve", lambda e, par=par: e.tensor_tensor(out=KA["Ta"][0][:, par * 4:par * 4 + 4, :], in0=KA["Xa"][0][:, par * 4:par * 4 + 4, :],
                                                                in1=ident.unsqueeze(1).to_broadcast([128, 4, 128]), op=ALU.add), [KA["Xa"][1][par], B_cst], [KA["Ta"][1][par]])
                X, XT, T = "Xa", "XTa", "Ta"
                Xn, XTn, Tn = "Xb", "XTb", "Tb"

                def lvl_mm(lk, rk, outk, eng, addk=None):
                    la, lB = KA[lk]
                    ra, rB = KA[rk]
                    oa, oB = KA[outk]
                    for par in range(2):
                        bank = nslot()
                        for cc in range(4):
                            hi = par * 4 + cc
                            S.op("pe", lambda e, hi=hi, cc=cc, bank=bank: e.matmul(pq(bank, cc), lhsT=la[:, hi, :], rhs=ra[:, hi, :], start=True, stop=True), [lB[par], rB[par]], [PB[bank][0]])
                        if addk is None:
                            S.op(eng, lambda e, par=par, bank=bank: e.copy(out=oa[:, par * 4:par * 4 + 4, :], in_=PS[bank][:, :].rearrange("p (a b) -> p a b", b=128)), [PB[bank][0]], [oB[par]])
                        else:
                            aa_, aB = KA[addk]
                            S.op(eng, lambda e, par=par, bank=bank: e.tensor_tensor(out=oa[:, par * 4:par * 4 + 4, :], in0=PS[bank][:, :].rearrange("p (a b) -> p a b", b=128),
                                                                               in1=aa_[:, par * 4:par * 4 + 4, :], op=ALU.add), [PB[bank][0], aB[par]], [oB[par]])

                for l in range(6):
                    if l < 5:
                        lvl_mm(XT, X, Xn, "act")
                    lvl_mm(X, XT, XTn, "act")
                    yield
                    lvl_mm(XTn, T, Tn, "dve", addk=T)
                    yield
                    X, Xn = Xn, X
                    XT, XTn = XTn, XT
                    T, Tn = Tn, T
                yield
                assert T == "Ta"
                yield

            def stt(i):
                rt, B_rt = FS[i % 3]["rt"]
                kkt, B_kkt = FS[i % 3]["kkt"]
                kt, B_kt = FS[i % 3]["kt"]
                bt, B_bt = FS[i % 3]["bt"]
                bon, B_bon = FS[i % 3]["bon"]
                gT, B_gT = FS[i % 3]["gT"]
                vtok, B_vtok = FS[i % 3]["vtok"]
                ktok, B_ktok = FS[i % 3]["ktok"]
                btok, B_btok = FS[i % 3]["btok"]
                cinc, B_cinc = FS[i % 3]["cinc"]
                KA = KAD[i % 2]
                Tinv, B_Tinv = KA["Ta"]
                Mk8, B_Mk8 = KA["Mk"]
                Ak8, B_Ak8 = KA["Ak"]
                Ab8, B_Ab8 = KA["Ab"]
                heads = [(par, cc) for par in range(2) for cc in range(4)]
                for (par, cc) in heads:
                    hi = par * 4 + cc
                    vt_h = vtok[:, cc * 128 + 64 * par: cc * 128 + 64 * par + 64]
                    S.op("pe", lambda e, par=par, cc=cc, hi=hi: e.matmul(PS[6][:, hi * 64:(hi + 1) * 64], lhsT=hv(kkt, par, cc), rhs=ST[64 * par:64 * par + 64, cc, :], start=True, stop=False),
                         [B_kkt, B_STp[par]], [PB[6][0]])
                    S.op("pe", lambda e, hi=hi, vt_h=vt_h: e.matmul(PS[6][:, hi * 64:(hi + 1) * 64], lhsT=Mk8[:, hi, :], rhs=vt_h, start=False, stop=True), [B_Mk8[par], B_vtok], [PB[6][0]])
                S.op("act", lambda e: e.mul(XTs8.rearrange("p a b -> p (a b)"), PS[6][:, :], -1.0), [PB[6][0]], [B_XTs8])
                yield
                for (par, cc) in heads:
                    hi = par * 4 + cc
                    S.op("pe", lambda e, hi=hi: e.matmul(PS[7][:, hi * 64:(hi + 1) * 64], lhsT=Tinv[:, hi, :], rhs=XTs8[:, hi, :], start=True, stop=True), [B_Tinv[par], B_XTs8], [PB[7][0]])
                S.op("act", lambda e: e.copy(out=NTs8.rearrange("p a b -> p (a b)"), in_=PS[7][:, :]), [PB[7][0]], [B_NTs8])
                yield
                for (par, cc) in heads:
                    hi = par * 4 + cc
                    h = 2 * cc + par
                    vt_h = vtok[:, cc * 128 + 64 * par: cc * 128 + 64 * par + 64]
                    osl = PS[5][:, h * 64:(h + 1) * 64]
                    S.op("pe", lambda e, par=par, cc=cc, osl=osl: e.matmul(osl, lhsT=hv(rt, par, cc), rhs=ST[64 * par:64 * par + 64, cc, :], start=True, stop=False), [B_rt, B_STp[par]], [PB[5][0]])
                    S.op("pe", lambda e, hi=hi, osl=osl: e.matmul(osl, lhsT=Ab8[:, hi, :], rhs=NTs8[:, hi, :], start=False, stop=False), [B_Ab8[par], B_NTs8], [PB[5][0]])
                    S.op("pe", lambda e, hi=hi, osl=osl, vt_h=vt_h: e.matmul(osl, lhsT=Ak8[:, hi, :], rhs=vt_h, start=False, stop=True), [B_Ak8[par], B_vtok], [PB[5][0]])
                yield
                for (par, cc) in heads:
                    hi = par * 4 + cc
                    vt_h = vtok[:, cc * 128 + 64 * par: cc * 128 + 64 * par + 64]
                    S.op("pe", lambda e, hi=hi, cc=cc: e.matmul(PS[6][:, hi * 64:(hi + 1) * 64], lhsT=btok[:, cc * 128:(cc + 1) * 128], rhs=NTs8[:, hi, :], start=True, stop=False), [B_btok, B_NTs8], [PB[6][0]])
                    S.op("pe", lambda e, hi=hi, cc=cc, vt_h=vt_h: e.matmul(PS[6][:, hi * 64:(hi + 1) * 64], lhsT=ktok[:, cc * 128:(cc + 1) * 128], rhs=vt_h, start=False, stop=True), [B_ktok, B_vtok], [PB[6][0]])
                for par in range(2):
                    sl = slice(64 * par, 64 * par + 64)
                    S.op("dve", lambda e, par=par, sl=sl: e.tensor_tensor(out=tmpS[sl, :, :], in0=PS[6][sl, par * 256:(par + 1) * 256].rearrange("p (c v) -> p c v", v=64), in1=ST[sl, :, :], op=ALU.add),
                         [PB[6][0], B_STp[par]], [B_tmpS])
                    S.op("dve", lambda e, par=par, sl=sl: e.tensor_tensor(out=ST[sl, :, :], in0=tmpS[sl, :, :], in1=cinc[sl, :, 127:128].to_broadcast([64, 4, 64]), op=ALU.mult),
                         [B_tmpS, B_cinc], [B_STp[par]])
                if i == 0:
                    return
                yield
                S.op("act", lambda e: e.copy(out=osb, in_=PS[5][:, :]), PB[5], [B_osb])
                S.op("act", lambda e: e.activation(out=osq, in_=PS[5][:, :], func=AF.Square), PB[5], [B_osq])
                if i == 1:
                    tap("osb", osb, B_osb, [128, 512])
                o3 = osb.rearrange("p (h v) -> p h v", v=64)
                S.op("dve", lambda e: e.tensor_reduce(out=gst[:, 0:8], in_=o3, axis=AX.X, op=ALU.add), [B_osb], [B_gst])
                S.op("dve", lambda e: e.tensor_reduce(out=gst[:, 8:16], in_=osq.rearrange("p (h v) -> p h v", v=64), axis=AX.X, op=ALU.add), [B_osq, B_gst], [B_gst])
                S.op("dve", lambda e: e.tensor_scalar(out=gst[:, 0:16], in0=gst[:, 0:16], scalar1=1.0 / 64, scalar2=None, op0=ALU.mult), [B_gst], [B_gst])
                S.op("dve", lambda e: e.tensor_tensor(out=gst[:, 16:24], in0=gst[:, 0:8], in1=gst[:, 0:8], op=ALU.mult), [B_gst], [B_gst])
                S.op("dve", lambda e: e.tensor_tensor(out=gst[:, 16:24], in0=gst[:, 8:16], in1=gst[:, 16:24], op=ALU.subtract), [B_gst], [B_gst])
                S.op("act", lambda e: e.activation(out=gst[:, 24:32], in_=gst[:, 16:24], func=AF.Sqrt, bias=prm[:, P_EPSG:P_EPSG + 1]), [B_gst, B_prm], [B_gst])
                S.op("dve", lambda e: e.reciprocal(out=gst[:, 24:32], in_=gst[:, 24:32]), [B_gst], [B_gst])
                S.op("dve", lambda e: e.tensor_tensor(out=o3, in0=o3, in1=gst[:, 0:8].unsqueeze(2).to_broadcast([128, 8, 64]), op=ALU.subtract), [B_osb, B_gst], [B_osb])
                S.op("dve", lambda e: e.tensor_tensor(out=o3, in0=o3, in1=gst[:, 24:32].unsqueeze(2).to_broadcast([128, 8, 64]), op=ALU.mult), [B_osb, B_gst], [B_osb])
                yield
                for cc in range(4):
                    S.op("pe", lambda e, cc=cc: e.transpose(pq(7, cc), osb[:, cc * 128:(cc + 1) * 128], ident), [B_osb, B_cst], [PB[7][cc]])
                    S.op("dve", lambda e, cc=cc: e.tensor_scalar(out=tmp2[:, cc, :], in0=pq(7, cc), scalar1=prm[:, P_GNG + cc:P_GNG + cc + 1], scalar2=prm[:, P_GNB + cc:P_GNB + cc + 1], op0=ALU.mult, op1=ALU.add),
                         [PB[7][cc], B_prm], [B_tmp2])
                S.op("pool", lambda e: e.tensor_tensor(out=tmp2, in0=tmp2, in1=bon, op=ALU.add), [B_tmp2, B_bon], [B_tmp2])
                S.op("pool", lambda e: e.tensor_tensor(out=orw, in0=tmp2, in1=gT, op=ALU.mult), [B_tmp2, B_gT], [B_orw])
                if i == 1:
                    tap("orw", orw, B_orw, [128, 4, 128], BF16)
                S.dma(ch_orw, orw_s[i], orw, reads=[B_orw], writes=[B_orws])
                yield

            def run_all(g):
                for _ in g:
                    pass

            def adv(g):
                try:
                    next(g)
                    return True
                except StopIteration:
                    return False

            run_all(front(0))
            if NT > 1:
                run_all(front(1))
            run_all(neu(0))
            NNEU = 21
            NFRONT = 10
            for i in range(NT):
                if i < 16:
                    issue_cast(i)
                g_st = stt(i)
                g_neu = neu(i + 1) if i + 1 < NT else iter(())
                g_fr = front(i + 2) if i + 2 < NT else iter(())
                a_st = a_neu = a_fr = True
                rnd = 0
                fdone = 0
                while a_st or a_neu or a_fr:
                    if a_st:
                        a_st = adv(g_st)
                    if a_neu:
                        a_neu = adv(g_neu)
                    want = (rnd + 1) * NFRONT // NNEU + 1 if (a_neu or a_st) else 10 ** 9
                    while a_fr and fdone < want:
                        a_fr = adv(g_fr)
                        fdone += 1
                    rnd += 1
            for k_ in range(min(NT, 16), 16):
                issue_cast(k_)
            tap("STfin", ST, B_STp[0], [128, 4, 64])
        phase_R()
        S.barrier()
        B_h1toks = Buf("h1toks")
        B_h1ns = Buf("h1ns")

        def phase_C():
            top[0] = base_top
            wC, B_wC = alloc("wC", [8, 3072], BF16)
            wco, B_wco = alloc("wco", [4, 1024], BF16)
            wro, B_wro = alloc("wro", [4, 1024], BF16)
            wo, B_wo = alloc("wo", [8, 1024], BF16)
            diag, B_diag = alloc("diag", [124, 128], BF16)
            S.dma(S.chan("wC0"), wC[:, :, 0:1024], w_in_d[:, 0:1024].rearrange("(c p) n -> p c n", p=128), writes=[B_wC], eng="pool")
            S.dma(S.chan("wC1"), wC[:, :, 1024:3072], w_in_d[:, 2816:4864].rearrange("(c p) n -> p c n", p=128), writes=[B_wC], eng="pool")
            S.dma(S.chan("wco"), wco, wco_d.rearrange("(c p) n -> p c n", p=128), writes=[B_wco], eng="pool")
            S.dma(S.chan("wro"), wro, wro_d.rearrange("(c p) n -> p c n", p=128), writes=[B_wro], eng="pool")
            S.dma(S.chan("wo"), wo, wo_d.rearrange("(c p) n -> p c n", p=128), writes=[B_wo], eng="pool")
            for cc in range(4):
                for j in range(31):
                    S.op("dve", lambda e, cc=cc, j=j: e.tensor_scalar(out=diag[:, cc * 31 + j, :], in0=ident, scalar1=prm[:, P_CW + cc * 31 + j:P_CW + cc * 31 + j + 1], scalar2=None, op0=ALU.mult),
                         [B_cst, B_prm], [B_diag])
            xts = [alloc("cxt%d" % k, [8, 128]) for k in range(3)]
            chx = [S.chan("cx%d" % k) for k in range(3)]
            xb, B_xb = alloc("cxb", [8, 128], BF16)
            sq, B_sq = alloc("csq", [8, 128], BF16)
            rs, B_rs = alloc("crs", [128])
            zcs = [alloc("zc%d" % k, [24, 128]) for k in range(2)]
            ubuf, B_ubuf = alloc("ubuf", [4, 158], BF16)
            ysb, B_ysb = alloc("ysb", [4, 128])
            ysq, B_ysq = alloc("ysq", [4, 128])
            mv, B_mv = alloc("mv", [128])
            actc, B_actc = alloc("actc", [4, 128], BF16)
            orwt, B_orwt = alloc("orwt", [4, 128], BF16)
            t1, B_t1 = alloc("t1", [8, 128])
            t2, B_t2 = alloc("t2", [8, 128])
            gateds = [alloc("gated%d" % k, [8, 128], BF16) for k in range(2)]
            t1b, B_t1b = alloc("t1b", [8, 128])
            h1T, B_h1T = alloc("h1T", [8, 128])
            h1sq, B_h1sq = alloc("h1sq", [8, 128], BF16)
            rs2, B_rs2 = alloc("rs2", [128])
            h1n, B_h1n = alloc("h1n", [8, 128], BF16)
            h1tok, B_h1tok = alloc("h1tok", [1024])
            ch_orwl = S.chan("orwl")
            ch_h1tok = S.chan("h1tok")
            ch_h1n = S.chan("h1n")
            S.op("dve", lambda e: e.memset(ubuf, 0.0), [], [B_ubuf])
            def cfront(i):
                zc, B_zc = zcs[i % 2]
                xt, B_xt = xts[i % 3]
                load_x(i, xt, B_xt, chx[i % 3], xb, B_xb, sq, B_sq, rs, B_rs, (1, 0))
                nchunk = 8 if i == 0 else 24
                for gi in range(nchunk // 4):
                    bank = gi % 2
                    for jj in range(4):
                        j = gi * 4 + jj
                        col = j * 128
                        for dc in range(8):
                            S.op("pe", lambda e, bank=bank, jj=jj, dc=dc, col=col: e.matmul(pq(bank, jj), lhsT=wC[:, dc, col:col + 128], rhs=xb[:, dc, :], start=(dc == 0), stop=(dc == 7)),
                                 [B_wC, B_xb], [PB[bank][0]])
                    S.op("dve", lambda e, bank=bank, gi=gi: e.tensor_tensor(out=zc[:, gi * 4:gi * 4 + 4, :], in0=PS[bank][:, :].rearrange("p (a b) -> p a b", b=128),
                                                                         in1=rs.unsqueeze(1).to_broadcast([128, 4, 128]), op=ALU.mult),
                         [PB[bank][0], B_rs], [B_zc])
                yield

            def cb1(i):
                zc, B_zc = zcs[i % 2]
                gated, B_gated = gateds[i % 2]
                S.op("act", lambda e: e.activation(out=zc[:, 4:8, :], in_=zc[:, 4:8, :], func=AF.Sigmoid), [B_zc], [B_zc])
                S.op("dve", lambda e: e.tensor_tensor(out=ubuf[:, :, 30:158], in0=zc[:, 0:4, :], in1=zc[:, 4:8, :], op=ALU.mult), [B_zc], [B_ubuf])
                yield
                for cc in range(4):
                    for j in range(31):
                        S.op("pe", lambda e, cc=cc, j=j: e.matmul(pq(2, cc), lhsT=diag[:, cc * 31 + j, :], rhs=ubuf[:, cc, j:j + 128], start=(j == 0), stop=(j == 30)),
                             [B_diag, B_ubuf], [PB[2][0]])
                S.op("pool", lambda e: e.tensor_copy(out=ubuf[:, :, 0:30], in_=ubuf[:, :, 128:158]), [B_ubuf], [B_ubuf])
                if i == 0:
                    S.op("act", lambda e: e.copy(out=ysb.rearrange("p a b -> p (a b)"), in_=PS[2][:, :]), [PB[2][0]], [B_ysb])
                    return
                yield
                for cc in range(4):
                    S.op("act", lambda e, cc=cc: e.activation(out=ysb[:, cc, :], in_=pq(2, cc), func=AF.Identity, bias=prm[:, P_CB + cc:P_CB + cc + 1]), [PB[2][0], B_prm], [B_ysb])
                    S.op("act", lambda e, cc=cc: e.activation(out=ysq[:, cc, :], in_=pq(2, cc), func=AF.Square, bias=prm[:, P_CB + cc:P_CB + cc + 1]), [PB[2][0], B_prm], [B_ysq])
                for cc in range(4):
                    S.op("pe", lambda e, cc=cc: e.matmul(pq(3, 0), lhsT=cst[:, C_O512, :], rhs=ysb[:, cc, :], start=(cc == 0), stop=(cc == 3)), [B_cst, B_ysb], [PB[3][0]])
                for cc in range(4):
                    S.op("pe", lambda e, cc=cc: e.matmul(pq(3, 1), lhsT=cst[:, C_O512, :], rhs=ysq[:, cc, :], start=(cc == 0), stop=(cc == 3)), [B_cst, B_ysq], [PB[3][0]])
                S.op("act", lambda e: e.activation(out=mv, in_=pq(3, 0), func=AF.Square), [PB[3][0]], [B_mv])
                S.op("dve", lambda e: e.tensor_tensor(out=mv, in0=pq(3, 1), in1=mv, op=ALU.subtract), [PB[3][0], B_mv], [B_mv])
                S.op("act", lambda e: e.activation(out=mv, in_=mv, func=AF.Sqrt, bias=prm[:, P_EPS5:P_EPS5 + 1]), [B_mv, B_prm], [B_mv])
                S.op("dve", lambda e: e.reciprocal(out=mv, in_=mv), [B_mv], [B_mv])
                S.op("dve", lambda e: e.tensor_tensor(out=ysb, in0=ysb, in1=pq(3, 0).unsqueeze(1).to_broadcast([128, 4, 128]), op=ALU.subtract), [B_ysb, PB[3][0]], [B_ysb])
                S.op("dve", lambda e: e.tensor_tensor(out=ysb, in0=ysb, in1=mv.unsqueeze(1).to_broadcast([128, 4, 128]), op=ALU.mult), [B_ysb, B_mv], [B_ysb])
                for cc in range(4):
                    S.op("act", lambda e, cc=cc: e.activation(out=actc[:, cc, :], in_=ysb[:, cc, :], func=AF.Silu, scale=prm[:, P_LNG + cc:P_LNG + cc + 1], bias=prm[:, P_LNB + cc:P_LNB + cc + 1]),
                         [B_ysb, B_prm], [B_actc])
                yield
                S.dma(ch_orwl, orwt, orw_s[i], reads=[B_orws], writes=[B_orwt])
                for m_ in range(8):
                    for cc in range(4):
                        S.op("pe", lambda e, m_=m_, cc=cc: e.matmul(pq(4 + m_ // 4, m_ % 4), lhsT=wco[:, cc, m_ * 128:(m_ + 1) * 128], rhs=actc[:, cc, :], start=(cc == 0), stop=(cc == 3)),
                             [B_wco, B_actc], [PB[4 + m_ // 4][0]])
                S.op("act", lambda e: e.activation(out=zc[:, 8:24, :], in_=zc[:, 8:24, :], func=AF.Sigmoid), [B_zc], [B_zc])
                yield
                for hb_ in range(2):
                    S.op("dve", lambda e, hb_=hb_: e.tensor_tensor(out=t1[:, hb_ * 4:hb_ * 4 + 4, :], in0=PS[4 + hb_][:, :].rearrange("p (a b) -> p a b", b=128), in1=zc[:, 8 + hb_ * 4:12 + hb_ * 4, :], op=ALU.mult),
                         [PB[4 + hb_][0], B_zc], [B_t1])
                for m_ in range(8):
                    for cc in range(4):
                        S.op("pe", lambda e, m_=m_, cc=cc: e.matmul(pq(4 + m_ // 4, m_ % 4), lhsT=wro[:, cc, m_ * 128:(m_ + 1) * 128], rhs=orwt[:, cc, :], start=(cc == 0), stop=(cc == 3)),
                             [B_wro, B_orwt], [PB[4 + m_ // 4][0]])
                yield
                for hb_ in range(2):
                    S.op("dve", lambda e, hb_=hb_: e.tensor_tensor(out=t2[:, hb_ * 4:hb_ * 4 + 4, :], in0=PS[4 + hb_][:, :].rearrange("p (a b) -> p a b", b=128), in1=zc[:, 16 + hb_ * 4:20 + hb_ * 4, :], op=ALU.mult),
                         [PB[4 + hb_][0], B_zc], [B_t2])
                S.op("pool", lambda e: e.tensor_tensor(out=gated, in0=t1, in1=t2, op=ALU.add), [B_t1, B_t2], [B_gated])
                yield


            def cb2(i):
                if i == 0:
                    return
                    yield
                xt, B_xt = xts[i % 3]
                gated, B_gated = gateds[i % 2]
                for m_ in range(8):
                    for kc in range(8):
                        S.op("pe", lambda e, m_=m_, kc=kc: e.matmul(pq(6 + m_ // 4, m_ % 4), lhsT=wo[:, kc, m_ * 128:(m_ + 1) * 128], rhs=gated[:, kc, :], start=(kc == 0), stop=(kc == 7)),
                             [B_wo, B_gated], [PB[6 + m_ // 4][0]])
                for hb_ in range(2):
                    S.op("dve", lambda e, hb_=hb_, xt=xt: e.tensor_tensor(out=h1T[:, hb_ * 4:hb_ * 4 + 4, :], in0=PS[6 + hb_][:, :].rearrange("p (a b) -> p a b", b=128), in1=xt[:, hb_ * 4:hb_ * 4 + 4, :], op=ALU.add),
                         [PB[6 + hb_][0], B_xt], [B_h1T])
                yield
                for m_ in range(8):
                    S.op("pe", lambda e, m_=m_: e.transpose(pq(6 + m_ // 4, m_ % 4), h1T[:, m_, :], ident), [B_h1T, B_cst], [PB[6 + m_ // 4][0]])
                for hb_ in range(2):
                    S.op("act", lambda e, hb_=hb_: e.copy(out=h1tok[:, hb_ * 512:(hb_ + 1) * 512], in_=PS[6 + hb_][:, :]), [PB[6 + hb_][0]], [B_h1tok])
                S.dma(ch_h1tok, h1tok_s[i - 1], h1tok, reads=[B_h1tok], writes=[B_h1toks])
                yield
                S.op("act", lambda e: e.activation(out=h1sq, in_=h1T, func=AF.Square), [B_h1T], [B_h1sq])
                for dc in range(8):
                    S.op("pe", lambda e, dc=dc: e.matmul(pq(6, 0), lhsT=ones_b, rhs=h1sq[:, dc, :], start=(dc == 0), stop=(dc == 7)), [B_onesb, B_h1sq], [PB[6][0]])
                S.op("act", lambda e: e.activation(out=rs2, in_=pq(6, 0), func=AF.Sqrt, scale=1.0 / 1024, bias=prm[:, P_EPS6:P_EPS6 + 1]), [PB[6][0], B_prm], [B_rs2])
                S.op("dve", lambda e: e.reciprocal(out=rs2, in_=rs2), [B_rs2], [B_rs2])
                S.op("dve", lambda e: e.tensor_tensor(out=t1b, in0=h1T, in1=rs2.unsqueeze(1).to_broadcast([128, 8, 128]), op=ALU.mult), [B_h1T, B_rs2], [B_t1b])
                S.op("pool", lambda e: e.tensor_tensor(out=h1n, in0=t1b, in1=prm[:, P_GFFN:P_GFFN + 8].unsqueeze(2).to_broadcast([128, 8, 128]), op=ALU.mult), [B_t1b, B_prm], [B_h1n])
                S.dma(ch_h1n, h1nT_s[i - 1], h1n, reads=[B_h1n], writes=[B_h1ns])
                if i == 1:
                    tap("h1tok", h1tok, B_h1tok, [128, 1024])
                    tap("h1n", h1n, B_h1n, [128, 8, 128], BF16)
                yield

            def _adv(g):
                try:
                    next(g)
                    return True
                except StopIteration:
                    return False

            def _run(g):
                for _ in g:
                    pass

            _run(cfront(0))
            if NT > 1:
                _run(cfront(1))
            _run(cb1(0))
            for i in range(NT):
                g2 = cb2(i)
                g1 = cb1(i + 1) if i + 1 < NT else iter(())
                gf = cfront(i + 2) if i + 2 < NT else iter(())
                a1 = a2 = af = True
                while a1 or a2 or af:
                    if a2:
                        a2 = _adv(g2)
                    if a1:
                        a1 = _adv(g1)
                    if af:
                        af = _adv(gf)
        phase_C()
        S.barrier()

        B_sels = Buf("sels")

        def phase_Q():
            top[0] = base_top
            wq, B_wq = alloc("wq", [8, 2048], BF16)
            skT, B_skT = alloc("skT", [16, 128], BF16)
            S.dma(S.chan("wq"), wq, wq_d.rearrange("(c p) n -> p c n", p=128), writes=[B_wq], eng="pool")
            S.dma(S.chan("skT"), skT, skT_d, writes=[B_skT], eng="pool")
            hns = [alloc("hn%d" % k, [8, 128], BF16) for k in range(2)]
            ch_hns = [S.chan("hn%d" % k) for k in range(2)]
            qT, B_qT = alloc("qT", [16, 128], BF16)
            ssbs = [alloc("ssb%d" % k, [16, 128]) for k in range(2)]
            wk16, _ = alloc("wk16", [16, 128])
            B_wkg = [Buf("wk%d" % g_) for g_ in range(16)]
            B_ssb4s = [[Buf("ssb%d_%d" % (k, g_)) for g_ in range(4)] for k in range(2)]
            B_topsg = [Buf("tops%d" % g_) for g_ in range(16)]
            B_topig = [Buf("topi%d" % g_) for g_ in range(16)]
            B_candh = [Buf("cand%d" % g_) for g_ in range(8)]
            B_bestsh = [Buf("bests%d" % g_) for g_ in range(8)]
            B_bestch = [Buf("bestc%d" % g_) for g_ in range(8)]
            B_eqh = [Buf("eq%d" % g_) for g_ in range(16)]
            B_sel3h = [Buf("sel3_%d" % g_) for g_ in range(16)]
            B_sel3g = Buf("sel3g")
            B_ju2 = Buf("ju2")
            TS3 = [(alloc("tops%d" % k, [16, 16])[0], alloc("topi%d" % k, [16, 16], U32)[0], alloc("topif%d" % k, [16, 16])[0]) for k in range(2)]
            TB3 = [([Buf("tops%d_%d" % (k, g_)) for g_ in range(16)], [Buf("topi%d_%d" % (k, g_)) for g_ in range(16)], Buf("topif%d" % k)) for k in range(2)]
            wk2, _ = alloc("wk2", [8, 256])
            B_wk2h = [Buf("wk2_%d" % h) for h in range(8)]
            cand, B_cand = alloc("cand", [8, 256])
            bests, B_bests = alloc("bests", [8, 16])
            bestc, B_bestc = alloc("bestc", [8, 16], U32)
            ju, B_ju = alloc("ju", [2, 8, 16], U32)
            j1, B_j1 = alloc("j1", [8, 16])
            j2, B_j2 = alloc("j2", [8, 16])
            eq, B_eq = alloc("eq", [16, 16, 16])
            ee, B_ee = alloc("ee", [8, 16])
            zz, B_zz = alloc("zz", [8])
            sel3, B_sel3 = alloc("sel3", [3, 128])
            selT, B_selT = alloc("selT", [3, 128])
            ch_hn = S.chan("hn")
            ch_sel = S.chan("sel")
            iota16 = cst[:, C_IOTA, 0:16]
            def q_front(i):
                hn, B_hn = hns[i % 2]
                ssb = ssbs[i % 2][0]
                B_ssb4 = B_ssb4s[i % 2]
                S.dma(ch_hns[i % 2], hn, h1nT_s[i], reads=[B_h1ns], writes=[B_hn])
                for g_ in range(16):
                    for dc in range(8):
                        S.op("pe", lambda e, g_=g_, dc=dc: e.matmul(pq(g_ // 4, g_ % 4), lhsT=wq[:, dc, g_ * 128:(g_ + 1) * 128], rhs=hn[:, dc, :], start=(dc == 0), stop=(dc == 7)),
                             [B_wq, B_hn], [PB[g_ // 4][0]])
                    if g_ % 4 == 3:
                        S.op("act", lambda e, g_=g_: e.copy(out=qT[:, g_ - 3:g_ + 1, :], in_=PS[g_ // 4][:, :].rearrange("p (a b) -> p a b", b=128)), [PB[g_ // 4][0]], [B_qT])
                for g_ in range(16):
                    S.op("pe", lambda e, g_=g_: e.matmul(pq(4 + g_ // 4, g_ % 4), lhsT=qT[:, g_, :], rhs=skT[:, g_, :], start=True, stop=True), [B_qT, B_skT], [PB[4 + g_ // 4][0]])
                    if g_ % 4 == 3:
                        S.op("act", lambda e, g_=g_: e.copy(out=ssb[:, g_ - 3:g_ + 1, :], in_=PS[4 + g_ // 4][:, :].rearrange("p (a b) -> p a b", b=128)), [PB[4 + g_ // 4][0]], [B_ssb4[g_ // 4]])

            def q_b1(i):
                tops, topi, topif = TS3[i % 2]
                B_topsg, B_topig, B_topif = TB3[i % 2]
                ssb = ssbs[i % 2][0]
                B_ssb4 = B_ssb4s[i % 2]
                for g_ in range(16):
                    S.op("dve", lambda e, g_=g_: e.max(out=tops[:, g_, 0:8], in_=ssb[:, g_, :]), [B_ssb4[g_ // 4]], [B_topsg[g_]])
                yield
                for g_ in range(16):
                    S.op("dve", lambda e, g_=g_: e.max_index(out=topi[:, g_, 0:8], in_max=tops[:, g_, 0:8], in_values=ssb[:, g_, :]), [B_ssb4[g_ // 4], B_topsg[g_]], [B_topig[g_]])
                yield
                for g_ in range(16):
                    S.op("dve", lambda e, g_=g_: e.match_replace(out=wk16[:, g_, :], in_to_replace=tops[:, g_, 0:8], in_values=ssb[:, g_, :], imm_value=-1e30), [B_ssb4[g_ // 4], B_topsg[g_]], [B_wkg[g_]])
                yield
                for g_ in range(16):
                    S.op("dve", lambda e, g_=g_: e.max(out=tops[:, g_, 8:16], in_=wk16[:, g_, :]), [B_wkg[g_]], [B_topsg[g_]])
                yield
                for g_ in range(16):
                    S.op("dve", lambda e, g_=g_: e.max_index(out=topi[:, g_, 8:16], in_max=tops[:, g_, 8:16], in_values=wk16[:, g_, :]), [B_wkg[g_], B_topsg[g_]], [B_topig[g_]])
                S.op("pool", lambda e: e.tensor_copy(out=topif, in_=topi), B_topig, [B_topif])
                yield

            def q_b2(i):
                tops, topi, topif = TS3[i % 2]
                B_topsg, B_topig, B_topif = TB3[i % 2]
                yield
                for h in range(8):
                    S.op("pool", lambda e, h=h: e.tensor_tensor(out=cand[:, h, :].rearrange("p (a b) -> p a b", b=16),
                                                              in0=tops[:, 2 * h, :].unsqueeze(2).to_broadcast([128, 16, 16]),
                                                              in1=tops[:, 2 * h + 1, :].unsqueeze(1).to_broadcast([128, 16, 16]), op=ALU.add), [B_topsg[2 * h], B_topsg[2 * h + 1]], [B_candh[h]])
                yield
                for h in range(8):
                    S.op("dve", lambda e, h=h: e.max(out=bests[:, h, 0:8], in_=cand[:, h, :]), [B_candh[h]], [B_bestsh[h]])
                yield
                for h in range(8):
                    S.op("dve", lambda e, h=h: e.max_index(out=bestc[:, h, 0:8], in_max=bests[:, h, 0:8], in_values=cand[:, h, :]), [B_candh[h], B_bestsh[h]], [B_bestch[h]])
                yield
                for h in range(8):
                    S.op("dve", lambda e, h=h: e.match_replace(out=wk2[:, h, :], in_to_replace=bests[:, h, 0:8], in_values=cand[:, h, :], imm_value=-1e30),
                         [B_candh[h], B_bestsh[h]], [B_wk2h[h]])
                yield
                for h in range(8):
                    S.op("dve", lambda e, h=h: e.max(out=bests[:, h, 8:16], in_=wk2[:, h, :]), [B_wk2h[h]], [B_bestsh[h]])
                yield
                for h in range(8):
                    S.op("dve", lambda e, h=h: e.max_index(out=bestc[:, h, 8:16], in_max=bests[:, h, 8:16], in_values=wk2[:, h, :]),
                         [B_wk2h[h], B_bestsh[h]], [B_bestch[h]])
                S.op("dve", lambda e: e.tensor_single_scalar(out=ju[:, 0, :, :], in_=bestc, scalar=4, op=ALU.logical_shift_right), B_bestch, [B_ju])
                S.op("dve", lambda e: e.tensor_single_scalar(out=ju[:, 1, :, :], in_=bestc, scalar=15, op=ALU.bitwise_and), B_bestch, [B_ju2])
                S.op("pool", lambda e: e.tensor_copy(out=j1, in_=ju[:, 0, :, :]), [B_ju], [B_j1])
                S.op("pool", lambda e: e.tensor_copy(out=j2, in_=ju[:, 1, :, :]), [B_ju2], [B_j2])
                yield
                for half, jj_ in ((0, j1), (1, j2)):
                    Bj = B_j1 if half == 0 else B_j2
                    for h in range(8):
                        S.op("dve", lambda e, h=h, jj_=jj_, half=half: e.tensor_tensor(out=eq[:, half * 8 + h, :, :], in0=jj_[:, h, :].unsqueeze(2).to_broadcast([128, 16, 16]),
                                                                          in1=iota16.unsqueeze(1).to_broadcast([128, 16, 16]), op=ALU.is_equal), [Bj, B_cst], [B_eqh[half * 8 + h]])
                    for h in range(8):
                        S.op("pool", lambda e, h=h, half=half: e.tensor_tensor(out=eq[:, half * 8 + h, :, :], in0=eq[:, half * 8 + h, :, :],
                                                                            in1=topif[:, 2 * h + half, :].unsqueeze(1).to_broadcast([128, 16, 16]), op=ALU.mult), [B_eqh[half * 8 + h], B_topif], [B_eqh[half * 8 + h]])
                yield
                for half in range(2):
                    for h in range(8):
                        S.op("dve", lambda e, h=h, half=half: e.tensor_reduce(out=sel3[:, half, h * 16:(h + 1) * 16], in_=eq[:, half * 8 + h, :, :], axis=AX.X, op=ALU.add), [B_eqh[half * 8 + h]], [B_sel3h[half * 8 + h]])
                B_bests_all = B_bestsh
                yield
                S.op("dve", lambda e: e.tensor_tensor(out=ee, in0=bests, in1=bests[:, :, 0:1].to_broadcast([128, 8, 16]), op=ALU.subtract), B_bestsh, [B_ee])
                S.op("act", lambda e: e.activation(out=ee, in_=ee, func=AF.Exp), [B_ee], [B_ee])
                S.op("dve", lambda e: e.tensor_reduce(out=zz, in_=ee, axis=AX.X, op=ALU.add), [B_ee], [B_zz])
                S.op("dve", lambda e: e.reciprocal(out=zz, in_=zz), [B_zz], [B_zz])
                S.op("dve", lambda e: e.tensor_tensor(out=sel3[:, 2, :].rearrange("p (h j) -> p h j", j=16), in0=ee, in1=zz.unsqueeze(2).to_broadcast([128, 8, 16]), op=ALU.mult), [B_ee, B_zz], [B_sel3g])
                yield
                for k in range(3):
                    S.op("pe", lambda e, k=k: e.transpose(pq(0, k), sel3[:, k, :], ident), B_sel3h + [B_sel3g, B_cst], [PB[0][0]])
                S.op("act", lambda e: e.copy(out=selT.rearrange("p a b -> p (a b)"), in_=PS[0][:, 0:384]), [PB[0][0]], [B_selT])
                S.dma(ch_sel, sel_s[i], selT, reads=[B_selT], writes=[B_sels])
                if i == 0:
                    tap("sel3", sel3, B_sel3g, [128, 3, 128])


                yield
            def _adv(g):
                try:
                    next(g)
                    return True
                except StopIteration:
                    return False

            q_front(0)
            if NRT > 1:
                q_front(1)
            for _ in q_b1(0):
                pass
            for i in range(NRT):
                if i + 2 < NRT:
                    q_front(i + 2)
                g2 = q_b2(i)
                g1 = q_b1(i + 1) if i + 1 < NRT else iter(())
                a1 = a2 = True
                while a1 or a2:
                    if a2:
                        a2 = _adv(g2)
                    if a2:
                        a2 = _adv(g2)
                    if a1:
                        a1 = _adv(g1)
        phase_Q()
        S.barrier()

        def phase_E():
            top[0] = base_top
            NS = NRT // 2
            act3s = [alloc("act3_%d" % k, [256, 128], BF16) for k in range(2)]
            hn2s = [alloc("hn2_%d" % k, [8, 256], BF16) for k in range(2)]
            selAs = [alloc("selA_%d" % k, [2, 3, 128]) for k in range(2)]
            Ub = [alloc("Ub%d" % k, [8, 128], BF16) for k in range(8)]
            Vb = [alloc("Vb%d" % k, [1024], BF16) for k in range(8)]
            Aoh = [alloc("Aoh%d" % k, [4, 128], BF16) for k in range(4)]
            Boh = [alloc("Boh%d" % k, [4, 128], BF16) for k in range(4)]
            ysb, B_ysb = alloc("eysb", [1024])
            h1t, B_h1t = alloc("h1t", [1024])
            gfin, B_gfin = alloc("gfin", [1024])
            ob, B_ob = alloc("ob", [1024])
            stat, B_stat = alloc("stat", [4])
            S.dma(S.chan("gfin"), gfin, gfin_d.partition_broadcast(128)[:, 0, :], writes=[B_gfin])
            ch_hn2 = [S.chan("hn2_%d" % k) for k in range(2)]
            ch_selA = [S.chan("selA_%d" % k) for k in range(2)]
            ch_U = [S.chan("U%d" % k) for k in range(8)]
            ch_V = [S.chan("V%d" % k) for k in range(8)]
            ch_h1t = S.chan("h1t")
            ch_out = S.chan("out")
            iota_bc = cst[:, C_IOTA, :].unsqueeze(1).to_broadcast([128, 4, 128])
            uctr = [0]
            actr = [0]

            def loads(s_):
                hn2, B_hn2 = hn2s[s_ % 2]
                selA, B_selA = selAs[s_ % 2]
                for ts in range(2):
                    S.dma(ch_hn2[s_ % 2], hn2[:, :, ts * 128:(ts + 1) * 128], h1nT_s[s_ * 2 + ts], reads=[B_h1ns], writes=[B_hn2])
                    S.dma(ch_selA[s_ % 2], selA[:, ts, :, :], sel_s[s_ * 2 + ts], reads=[B_sels], writes=[B_selA])

            def a_iter(s_, i2):
                act3, B_act3 = act3s[s_ % 2]
                hn2, B_hn2 = hn2s[s_ % 2]
                uctr[0] += 1
                slot = uctr[0] % 8
                U_, B_U = Ub[slot]
                S.dma(ch_U[slot], U_.rearrange("p b c -> p (b c)"), uT_b[i2], reads=[B_uTb], writes=[B_U])
                bank = (actr[0] // 2) % 2
                half = actr[0] % 2
                actr[0] += 1
                for dc in range(8):
                    S.op("pe", lambda e, dc=dc: e.matmul(PS[bank][:, half * 256:(half + 1) * 256], lhsT=U_[:, dc, :], rhs=hn2[:, dc, :], start=(dc == 0), stop=(dc == 7)), [B_U, B_hn2], [PB[bank][0]])
                if half == 1:
                    S.op("act", lambda e: e.activation(out=act3[:, :, i2 - 1:i2 + 1], in_=PS[bank][:, :].rearrange("p (i t) -> p t i", i=2), func=AF.Gelu), [PB[bank][0]], [B_act3])

            def b_vars(s_, tg):
                act3, B_act3 = act3s[s_ % 2]
                selA, B_selA = selAs[s_ % 2]
                t0 = tg * 4
                A_, B_A = Aoh[tg % 4]
                Bm, B_B = Boh[tg % 4]
                return act3, B_act3, selA, B_selA, t0, t0 // 128, t0 % 128, A_, B_A, Bm, B_B, 2 + tg % 2

            def b_stage1(s_, tg):
                act3, B_act3, selA, B_selA, t0, ti, tt, A_, B_A, Bm, B_B, gb = b_vars(s_, tg)
                S.op("dve", lambda e: e.tensor_tensor(out=A_, in0=iota_bc, in1=selA[:, ti, 0, tt:tt + 4].unsqueeze(2).to_broadcast([128, 4, 128]), op=ALU.is_equal), [B_cst, B_selA], [B_A])
                S.op("pool", lambda e: e.tensor_tensor(out=A_, in0=A_, in1=selA[:, ti, 2, tt:tt + 4].unsqueeze(2).to_broadcast([128, 4, 128]), op=ALU.mult), [B_A, B_selA], [B_A])
                S.op("dve", lambda e: e.tensor_tensor(out=Bm, in0=iota_bc, in1=selA[:, ti, 1, tt:tt + 4].unsqueeze(2).to_broadcast([128, 4, 128]), op=ALU.is_equal), [B_cst, B_selA], [B_B])

            def b_stage2(s_, tg):
                act3, B_act3, selA, B_selA, t0, ti, tt, A_, B_A, Bm, B_B, gb = b_vars(s_, tg)
                for tk in range(4):
                    S.op("pe", lambda e, tk=tk: e.matmul(pq(gb, tk), lhsT=A_[:, tk, :], rhs=Bm[:, tk, :], start=True, stop=True), [B_A, B_B], [PB[gb][0]])

            def b_stage3(s_, tg):
                act3, B_act3, selA, B_selA, t0, ti, tt, A_, B_A, Bm, B_B, gb = b_vars(s_, tg)
                S.op("dve", lambda e: e.tensor_tensor(out=act3[:, t0:t0 + 4, :], in0=PS[gb][:, :].rearrange("p (a b) -> p a b", b=128), in1=act3[:, t0:t0 + 4, :], op=ALU.mult),
                     [PB[gb][0], B_act3], [B_act3])

            def c_phase(s_):
                act3, B_act3 = act3s[s_ % 2]
                vctr = 0
                for i2 in range(128):
                    vctr += 1
                    V_, B_V = Vb[vctr % 8]
                    S.dma(ch_V[vctr % 8], V_, vP_b[i2], reads=[B_vPb], writes=[B_V])
                    for ts in range(2):
                        for dh in range(2):
                            bk = 4 + ts * 2 + dh
                            S.op("pe", lambda e, V_=V_, i2=i2, ts=ts, dh=dh, bk=bk: e.matmul(PS[bk][:, :], lhsT=act3[:, ts * 128:(ts + 1) * 128, i2], rhs=V_[:, dh * 512:(dh + 1) * 512],
                                                                                       start=(i2 == 0), stop=(i2 == 127)), [B_act3, B_V], [PB[bk][0]])

            def d_phase(s_):
                for ts in range(2):
                    gi_ = s_ * 2 + ts
                    S.dma(ch_h1t, h1t, h1tok_s[gi_], reads=[B_h1toks], writes=[B_h1t])
                    for dh in range(2):
                        bk = 4 + ts * 2 + dh
                        S.op("dve", lambda e, bk=bk, dh=dh: e.tensor_tensor(out=ysb[:, dh * 512:(dh + 1) * 512], in0=PS[bk][:, :], in1=h1t[:, dh * 512:(dh + 1) * 512], op=ALU.add),
                             [PB[bk][0], B_h1t], [B_ysb])
                    S.op("pool", lambda e: e.tensor_tensor(out=ob, in0=ysb, in1=ysb, op=ALU.mult), [B_ysb], [B_ob])
                    S.op("dve", lambda e: e.tensor_reduce(out=stat[:, 0:1], in_=ob, axis=AX.X, op=ALU.add), [B_ob], [B_stat])
                    S.op("act", lambda e: e.activation(out=stat[:, 1:2], in_=stat[:, 0:1], func=AF.Sqrt, scale=1.0 / 1024, bias=prm[:, P_EPS6:P_EPS6 + 1]), [B_stat, B_prm], [B_stat])
                    S.op("dve", lambda e: e.reciprocal(out=stat[:, 2:3], in_=stat[:, 1:2]), [B_stat], [B_stat])
                    S.op("dve", lambda e: e.scalar_tensor_tensor(out=ob, in0=ysb, scalar=stat[:, 2:3], in1=gfin, op0=ALU.mult, op1=ALU.mult), [B_ysb, B_stat, B_gfin], [B_ob])
                    o_ = S.dma(ch_out, out_d[gi_ * 128:(gi_ + 1) * 128, :], ob, reads=[B_ob])
                    S.final_waits.append(o_)

            loads(0)
            for i2 in range(128):
                a_iter(0, i2)
            for s_ in range(NS):
                nxt = s_ + 1 < NS
                if nxt:
                    loads(s_ + 1)
                b_stage1(s_, 0)
                b_stage1(s_, 1)
                b_stage2(s_, 0)
                for tg in range(64):
                    if tg + 2 < 64:
                        b_stage1(s_, tg + 2)
                    if tg + 1 < 64:
                        b_stage2(s_, tg + 1)
                    if nxt:
                        a_iter(s_ + 1, 2 * tg)
                        a_iter(s_ + 1, 2 * tg + 1)
                    b_stage3(s_, tg)
                    if tg == 3 and s_ > 0:
                        d_phase(s_ - 1)
                c_phase(s_)
            d_phase(NS - 1)
        phase_E()
        S.barrier()
        S.emit(st)
    return nc, dbg


def host_prep(inp, b, NT):
    f = np.float32
    x = np.asarray(inp["x"])[b]
    nreal = (NT - 1) * 128
    seq = np.concatenate([np.zeros((NPAD, D), f), np.asarray(inp["meta_tokens"], f), x[:nreal]], axis=0)
    xT = np.ascontiguousarray(seq.reshape(NT, 128, 8, 128).transpose(0, 3, 2, 1))
    m = {"xT": xT}
    return m


def shared_prep(inp):
    f = np.float32
    g = lambda k: np.asarray(inp[k], f)
    m = {}
    m["w_in"] = np.ascontiguousarray(g("w_in")[0])
    m["w_conv_out"] = np.ascontiguousarray(g("w_conv_out")[0])
    m["w_rwkv_out"] = np.ascontiguousarray(g("w_rwkv_out")[0])
    m["w_o"] = np.ascontiguousarray(g("w_o")[0])
    m["w_q"] = np.ascontiguousarray(g("w_q")[0])
    m["skT"] = np.ascontiguousarray(g("sub_keys")[0].transpose(3, 0, 1, 2).reshape(128, 16, 128))
    u = g("expert_u")[0]
    m["uT"] = np.ascontiguousarray(u.reshape(128, 128, 8, 128).transpose(1, 3, 2, 0)).reshape(128, 128, 1024)
    v = g("expert_v")[0]
    m["vP"] = np.ascontiguousarray(v.reshape(128, 128, 1024).transpose(1, 0, 2))
    m["wa_up"] = np.ascontiguousarray(np.concatenate([g("w_up")[0], g("a_up")[0]], axis=0))
    m["g_up"] = np.ascontiguousarray(g("g_up")[0])
    m["w0row"] = np.ascontiguousarray(g("w0")[0].reshape(1, 512))
    m["gfin"] = np.ascontiguousarray(g("g_final").reshape(1, 1024))
    prm = np.zeros((128, NPRM), f)
    col = lambda a, n: np.asarray(a, f).reshape(n, 128).T
    prm[:, P_GMIX:P_GMIX + 8] = col(g("g_mix")[0], 8)
    prm[:, P_MU:P_MU + 14] = col(g("mu_shift")[0], 14)
    prm[:, P_A0:P_A0 + 4] = col(g("a0")[0], 4)
    prm[:, P_KK:P_KK + 4] = col(g("k_k")[0], 4)
    prm[:, P_KA:P_KA + 4] = col(g("k_a")[0], 4)
    prm[:, P_RK:P_RK + 4] = col(g("r_k")[0].reshape(512), 4)
    prm[:, P_GNG:P_GNG + 4] = col(g("gn_g")[0], 4)
    prm[:, P_GNB:P_GNB + 4] = col(g("gn_b")[0], 4)
    prm[:, P_CB:P_CB + 4] = col(g("conv_b")[0], 4)
    prm[:, P_LNG:P_LNG + 4] = col(g("conv_ln_g")[0], 4)
    prm[:, P_LNB:P_LNB + 4] = col(g("conv_ln_b")[0], 4)
    prm[:, P_GFFN:P_GFFN + 8] = col(g("g_ffn")[0], 8)
    prm[:, P_EPS6] = 1e-6
    prm[:, P_EPS5] = 1e-5
    prm[:, P_EPSG] = 64e-5
    cw = g("conv_w")[0]
    prm[:, P_CW:P_CW + 124] = cw.reshape(31, 4, 128).transpose(2, 1, 0).reshape(128, 124)
    m["params"] = prm
    cst = np.zeros((128, NCST, 128), f)
    ar = np.arange(128)
    cst[:, C_IDENT] = np.eye(128)
    cst[:, C_ONES] = 1.0
    cst[:, C_O512] = 1.0 / 512
    cst[:, C_BLK] = (ar[:, None] // 64 == ar[None, :] // 64)
    e05 = float(np.exp(np.float32(-0.5)))
    cst[:, C_TRII] = -e05 * (ar[:, None] <= ar[None, :])
    cst[:, C_TRIE] = -e05 * (ar[:, None] < ar[None, :])
    cst[:, C_MS] = (ar[:, None] < ar[None, :])
    cst[:, C_MSN] = -1.0 * (ar[:, None] < ar[None, :])
    cst[:, C_MLN] = -1.0 * (ar[None, :] < ar[:, None])
    cst[:, C_MI] = (ar[:, None] <= ar[None, :])
    cst[:, C_IOTA] = ar[None, :]
    cst[:, C_O1024] = 1.0 / 1024
    m["cst"] = cst
    return m


_CACHE = {}


def kernel(**inputs):
    NT = 33
    if "nc" not in _CACHE:
        _CACHE["nc"] = build_program(NT)[0]
    nc = _CACHE["nc"]
    sh = shared_prep(inputs)
    in_maps = []
    for b in range(8):
        m = dict(sh)
        m.update(host_prep(inputs, b, NT))
        in_maps.append(m)
    res = run_bass_kernel_spmd(nc, in_maps, core_ids=list(range(8)))
    return np.stack([r["out"] for r in res.results], axis=0)
```

```python
import numpy as np
import concourse.bass as bass
import concourse.mybir as mybir
from concourse.bass_utils import run_bass_kernel_spmd
from contextlib import ExitStack

F32 = mybir.dt.float32
BF16 = mybir.dt.bfloat16
U32 = mybir.dt.uint32
ALU = mybir.AluOpType
AF = mybir.ActivationFunctionType
AX = mybir.AxisListType


class Buf:
    __slots__ = ("name", "w", "r", "excl")

    def __init__(self, name, excl=False):
        self.name = name
        self.w = None
        self.r = []
        self.excl = excl


class Op:
    __slots__ = ("eng", "fn", "deps", "signal", "is_dma", "chan", "sem", "val")

    def __init__(self, eng, fn, is_dma=False, chan=None):
        self.eng = eng
        self.fn = fn
        self.deps = []
        self.signal = False
        self.is_dma = is_dma
        self.chan = chan
        self.sem = None
        self.val = 0


class Chan:
    __slots__ = ("name", "last", "count", "sem")

    def __init__(self, name):
        self.name = name
        self.last = None
        self.count = 0
        self.sem = None


EPOCH = 30000
CAST = True
ENGS = ("pe", "dve", "act", "pool", "sp")


class Sched:
    def __init__(self, nc):
        self.nc = nc
        self.ops = {e: [] for e in ENGS}
        self.chans = []
        self.final_waits = []

    def chan(self, name):
        c = Chan(name)
        self.chans.append(c)
        return c

    def _record(self, op, reads, writes):
        writes = writes + [b for b in reads if b.excl and not any(b is x for x in writes)]
        deps = []
        for b in reads:
            if b.w is not None:
                deps.append(b.w)
        for b in writes:
            if b.w is not None:
                deps.append(b.w)
            for r in b.r:
                if r.eng == op.eng and not r.is_dma and not op.is_dma:
                    continue
                deps.append(r)
        seen = set(id(d) for d in op.deps)
        for d in deps:
            if d is op or id(d) in seen:
                continue
            if d.eng == "pe" and op.eng == "pe" and not d.is_dma and not op.is_dma:
                continue
            seen.add(id(d))
            op.deps.append(d)
            d.signal = True
        for b in reads:
            b.r.append(op)
        for b in writes:
            b.w = op
            b.r = []
        self.ops[op.eng].append(op)
        return op

    def op(self, eng, fn, reads=(), writes=()):
        return self._record(Op(eng, fn), list(reads), list(writes))

    def dma(self, chan, out, in_, reads=(), writes=(), eng="sp", **kw):
        op = Op(eng, lambda e: e.dma_start(out=out, in_=in_, **kw), is_dma=True, chan=chan)
        if chan.last is not None:
            op.deps.append(chan.last)
            chan.last.signal = True
        chan.last = op
        op.signal = True
        return self._record(op, list(reads), list(writes))

    def barrier(self):
        lasts = []
        for e in ENGS:
            for o in reversed(self.ops[e]):
                if not o.is_dma and o.fn is not None:
                    lasts.append(o)
                    break
        for c in self.chans:
            if c.last is not None:
                lasts.append(c.last)
        for e in ENGS:
            op = Op(e, None)
            for d in lasts:
                if d.eng == e and not d.is_dma:
                    continue
                op.deps.append(d)
                d.signal = True
            self.ops[e].append(op)

    def emit(self, stack):
        nc = self.nc
        for c in self.chans:
            c.sem = stack.enter_context(nc.semaphore("c_" + c.name))
        for d in self.final_waits:
            d.signal = True
        for e, lst in self.ops.items():
            cur = None
            cnt = 0
            k = 0
            for op in lst:
                if op.is_dma:
                    op.chan.count += 16
                    op.sem = op.chan.sem
                    op.val = op.chan.count
                elif op.signal:
                    if cur is None or cnt >= EPOCH:
                        cur = stack.enter_context(nc.semaphore("e_%s_%d" % (e, k)))
                        k += 1
                        cnt = 0
                    cnt += 1
                    op.sem = cur
                    op.val = cnt
        block = stack.enter_context(nc.Block())

        def run(engine_obj, lst, tail=None):
            waited = {}

            def w(d):
                key = d.sem.name
                if waited.get(key, 0) >= d.val:
                    return
                engine_obj.wait_ge(d.sem, d.val)
                waited[key] = d.val

            for op in lst:
                for d in op.deps:
                    w(d)
                if op.fn is None:
                    continue
                ins = op.fn(engine_obj)
                if op.is_dma:
                    ins.then_inc(op.sem, 16)
                elif op.signal:
                    ins.then_inc(op.sem, 1)
            if tail:
                for d in tail:
                    w(d)

        ops = self.ops
        fw = self.final_waits

        @block.tensor
        def _(e):
            run(e, ops["pe"])

        @block.vector
        def _(e):
            run(e, ops["dve"])

        @block.scalar
        def _(e):
            run(e, ops["act"])

        @block.gpsimd
        def _(e):
            run(e, ops["pool"])

        @block.sync
        def _(e):
            run(e, ops["sp"], tail=fw)


D = 1024
NPAD = 112
C_IDENT, C_ONES, C_O512, C_BLK, C_TRII, C_TRIE, C_MS, C_MSN, C_MLN, C_MI, C_IOTA, C_O1024 = range(12)
NCST = 12
P_GMIX = 0
P_MU = 8
P_A0 = 22
P_KK = 26
P_KA = 30
P_RK = 34
P_GNG = 38
P_GNB = 42
P_CB = 46
P_LNG = 50
P_LNB = 54
P_GFFN = 58
P_EPS6 = 66
P_EPS5 = 67
P_EPSG = 68
P_CW = 69
NPRM = 69 + 124


def build_program(NT, debug=False):
    NRT = NT - 1
    assert NRT % 2 == 0
    nc = bass.Bass("TRN2", target_bir_lowering=False)
    din = lambda n, s, dt=F32: nc.dram_tensor(n, s, dt, kind="ExternalInput").ap()
    xT_d = din("xT", [NT, 128, 8, 128])
    w_in_d = din("w_in", [1024, 4864])
    wco_d = din("w_conv_out", [512, 1024])
    wro_d = din("w_rwkv_out", [512, 1024])
    wo_d = din("w_o", [1024, 1024])
    wq_d = din("w_q", [1024, 2048])
    skT_d = din("skT", [128, 16, 128])
    uT_d = din("uT", [128, 128, 8 * 128])
    vP_d = din("vP", [128, 128, 1024])
    waup_d = din("wa_up", [128, 512])
    gup_d = din("g_up", [128, 512])
    prm_d = din("params", [128, NPRM])
    w0_d = din("w0row", [1, 512])
    cst_d = din("cst", [128, NCST, 128])
    gfin_d = din("gfin", [1, 1024])
    out_d = nc.dram_tensor("out", [NRT * 128, 1024], F32, kind="ExternalOutput").ap()
    dscr = lambda n, s, dt: nc.dram_tensor(n, s, dt, kind="Internal").ap()
    orw_s = dscr("orw_s", [NT, 128, 4, 128], BF16)
    h1tok_s = dscr("h1tok_s", [NRT, 128, 1024], F32)
    h1nT_s = dscr("h1nT_s", [NRT, 128, 8, 128], BF16)
    sel_s = dscr("sel_s", [NRT, 128, 3, 128], F32)
    uT_b = dscr("uT_b", [128, 128, 8 * 128], BF16)
    vP_b = dscr("vP_b", [128, 128, 1024], BF16)
    dbg = {}

    st = ExitStack()
    with st:
        S = Sched(nc)
        ARENA = 53000
        arena = st.enter_context(nc.sbuf_tensor("arena", [128, ARENA], F32))
        PS = [st.enter_context(nc.psum_tensor("ps%d" % k, [128, 512], F32)) for k in range(8)]
        PB = []
        for k in range(8):
            _b = Buf("ps%d" % k, excl=True)
            PB.append([_b, _b, _b, _b])
        top = [0]

        def alloc(name, free, dt=F32):
            n = int(np.prod(free))
            words = n if dt in (F32, U32) else (n + 1) // 2
            words = (words + 7) // 8 * 8
            a = arena[:, top[0]:top[0] + words]
            top[0] += words
            assert top[0] <= ARENA, (name, top[0])
            if dt != F32:
                a = a.bitcast(dt)
            a = a[:, 0:n]
            if len(free) == 2:
                a = a.rearrange("p (a b) -> p a b", b=free[1])
            elif len(free) == 3:
                a = a.rearrange("p (a b c) -> p a b c", b=free[1], c=free[2])
            return a, Buf(name)

        def pq(k, q):
            return PS[k][:, q * 128:(q + 1) * 128]

        def tap(name, ap, buf, shape, dt=F32):
            if not debug:
                return
            d = nc.dram_tensor("dbg_" + name, list(shape), dt, kind="ExternalOutput").ap()
            c = S.chan("dbg_" + name)
            o = S.dma(c, d, ap, reads=[buf])
            S.final_waits.append(o)
            dbg[name] = (shape, dt)

        cst, B_cst = alloc("cst", [NCST, 128])
        prm, B_prm = alloc("prm", [NPRM])
        ones_b, B_onesb = alloc("ones_b", [128], BF16)
        ch_c = S.chan("cst")
        S.dma(ch_c, cst, cst_d, writes=[B_cst])
        ch_p = S.chan("prm")
        S.dma(ch_p, prm, prm_d, writes=[B_prm])
        S.op("dve", lambda e: e.tensor_copy(out=ones_b, in_=cst[:, C_ONES, :]), [B_cst], [B_onesb])
        ident = cst[:, C_IDENT, :]
        base_top = top[0]

        ch_cast = [S.chan("cast%d" % k) for k in range(4)]
        B_uTb = Buf("uTb")
        B_vPb = Buf("vPb")
        cast_ops = []
        def issue_cast(k):
            if not CAST:
                return
            sl = slice(k * 8, (k + 1) * 8)
            cast_ops.append(S.dma(ch_cast[k % 2], uT_b[sl], uT_d[sl], eng="pool"))
            cast_ops.append(S.dma(ch_cast[2 + k % 2], vP_b[sl], vP_d[sl], eng="pool"))

        def load_x(i, xt, B_xt, ch, xb, B_xb, sq, B_sq, rs, B_rs, psq):
            S.dma(ch, xt, xT_d[i], writes=[B_xt])
            S.op("pool", lambda e: e.tensor_tensor(out=xb, in0=xt, in1=prm[:, P_GMIX:P_GMIX + 8].unsqueeze(2).to_broadcast([128, 8, 128]), op=ALU.mult),
                 [B_xt, B_prm], [B_xb])
            S.op("act", lambda e: e.activation(out=sq, in_=xt, func=AF.Square), [B_xt], [B_sq])
            k, q = psq
            for dc in range(8):
                S.op("pe", lambda e, dc=dc: e.matmul(pq(k, q), lhsT=ones_b, rhs=sq[:, dc, :], start=(dc == 0), stop=(dc == 7)),
                     [B_onesb, B_sq], [PB[k][q]])
            S.op("act", lambda e: e.activation(out=rs, in_=pq(k, q), func=AF.Sqrt, scale=1.0 / 1024, bias=prm[:, P_EPS6:P_EPS6 + 1]),
                 [PB[k][q], B_prm], [B_rs])
            S.op("dve", lambda e: e.reciprocal(out=rs, in_=rs), [B_rs], [B_rs])

        B_orws = Buf("orws")
        ch_orw = S.chan("orw")

        def phase_R():
            wR, B_wR = alloc("wR", [8, 1792], BF16)
            waup, B_waup = alloc("waup", [512])
            gup, B_gup = alloc("gup", [512])
            w0r, B_w0r = alloc("w0r", [512])
            chw = S.chan("wR")
            S.dma(chw, wR, w_in_d[:, 1024:2816].rearrange("(c p) n -> p c n", p=128), writes=[B_wR], eng="pool")
            S.dma(S.chan("waup"), waup, waup_d, writes=[B_waup])
            S.dma(S.chan("gup"), gup, gup_d, writes=[B_gup])
            S.dma(S.chan("w0r"), w0r[0:1, :], w0_d, writes=[B_w0r])
            xts = [alloc("xt0", [8, 128])] * 2
            chx = [S.chan("x%d" % k) for k in range(2)]
            xb, B_xb = alloc("xb", [8, 128], BF16)
            sq, B_sq = alloc("sq", [8, 128], BF16)
            rs, B_rs = alloc("rs", [128])
            zr, B_zr = alloc("zr", [14, 129])
            zs, B_zs = alloc("zs", [14, 128])
            tl, B_tl = alloc("tl", [128])
            sgt, B_sgt = alloc("sgt", [512])
            cexc, B_cexc = alloc("cexc", [4, 128])
            cinv, B_cinv = alloc("cinv", [4, 128])
            aa, B_aa = alloc("aa", [4, 128])
            sgl, B_sgl = alloc("sgl", [128])
            kk, B_kk = alloc("kk", [4, 128])
            tmp, B_tmp = alloc("tmp", [4, 128])
            k2, B_k2 = alloc("k2", [4, 128])
            FS = [{n_: alloc("%s_%d" % (n_, k_), shp_) for n_, shp_ in (("rt", [4, 128]), ("kkt", [4, 128]), ("kt", [4, 128]), ("bt", [4, 128]), ("bon", [4, 128]), ("gT", [4, 128]), ("vtok", [512]), ("ktok", [512]), ("btok", [512]), ("cinc", [4, 128]))} for k_ in range(3)]
            tmp2, B_tmp2 = alloc("tmp2", [4, 128])
            osq, B_osq = tmp2.rearrange("p a b -> p (a b)"), B_tmp2
            ST, _ = alloc("ST", [4, 64])
            osb, B_osb = alloc("osb", [512])
            gst, B_gst = alloc("gst", [32])
            orw, B_orw = alloc("orw", [4, 128], BF16)
            KAS = {}
            for nm in ("Xa", "XTa", "Xb", "XTb", "Tb"):
                ap_, _ = alloc(nm + "8", [8, 128])
                KAS[nm] = (ap_, [Buf(nm + "_e"), Buf(nm + "_o")])
            KAD = []
            for k_ in range(2):
                d_ = {}
                for nm in ("Mk", "Ak", "Ab", "Ta"):
                    ap_, _ = alloc("%s8_%d" % (nm, k_), [8, 128])
                    d_[nm] = (ap_, [Buf("%s_e%d" % (nm, k_)), Buf("%s_o%d" % (nm, k_))])
                KAD.append(d_)
            XTs8, B_XTs8 = alloc("XTs8", [8, 64])
            NTs8, B_NTs8 = alloc("NTs8", [8, 64])
            tmpS, B_tmpS = alloc("tmpS", [4, 64])
            B_STp = [Buf("STe"), Buf("STo")]
            S.op("dve", lambda e: e.memset(zr, 0.0), [], [B_zr])
            S.op("dve", lambda e: e.memset(ST, 0.0), [], B_STp)
            slot_ctr = [0]

            def nslot():
                s_ = slot_ctr[0] % 3
                slot_ctr[0] += 1
                return 2 + s_

            def front(i):
                rt, B_rt = FS[i % 3]["rt"]
                kkt, B_kkt = FS[i % 3]["kkt"]
                kt, B_kt = FS[i % 3]["kt"]
                bt, B_bt = FS[i % 3]["bt"]
                bon, B_bon = FS[i % 3]["bon"]
                gT, B_gT = FS[i % 3]["gT"]
                vtok, B_vtok = FS[i % 3]["vtok"]
                ktok, B_ktok = FS[i % 3]["ktok"]
                btok, B_btok = FS[i % 3]["btok"]
                cinc, B_cinc = FS[i % 3]["cinc"]
                xt, B_xt = xts[i % 2]
                load_x(i, xt, B_xt, chx[i % 2], xb, B_xb, sq, B_sq, rs, B_rs, (1, 0))
                yield
                for gi, (j0, nj) in enumerate(((0, 4), (4, 4), (8, 4), (12, 2))):
                    for jj in range(nj):
                        j = j0 + jj
                        for dc in range(8):
                            S.op("pe", lambda e, j=j, jj=jj, dc=dc, gi=gi: e.matmul(pq(gi % 2, jj), lhsT=wR[:, dc, j * 128:(j + 1) * 128], rhs=xb[:, dc, :], start=(dc == 0), stop=(dc == 7)),
                                 [B_wR, B_xb], [PB[gi % 2][jj]])
                    S.op("dve", lambda e, gi=gi, j0=j0, nj=nj: e.tensor_tensor(out=zr[:, j0:j0 + nj, 1:129], in0=PS[gi % 2][:, 0:nj * 128].rearrange("p (a b) -> p a b", b=128),
                                                                           in1=rs.unsqueeze(1).to_broadcast([128, nj, 128]), op=ALU.mult),
                         PB[gi % 2][0:nj] + [B_rs], [B_zr])
                yield
                mu_bc = prm[:, P_MU:P_MU + 14].unsqueeze(2).to_broadcast([128, 14, 128])
                S.op("pool", lambda e: e.tensor_tensor(out=zs, in0=zr[:, :, 0:128], in1=zr[:, :, 1:129], op=ALU.subtract), [B_zr], [B_zs])
                S.op("pool", lambda e: e.tensor_tensor(out=zs, in0=zs, in1=mu_bc, op=ALU.mult), [B_zs, B_prm], [B_zs])
                S.op("pool", lambda e: e.tensor_tensor(out=zs, in0=zs, in1=zr[:, :, 1:129], op=ALU.add), [B_zs, B_zr], [B_zs])
                S.op("pool", lambda e: e.tensor_copy(out=zr[:, :, 0:1], in_=zr[:, :, 128:129]), [B_zr, B_zs], [B_zr])
                if i == 1:
                    tap("zs", zs, B_zs, [128, 14, 128])
                r_ = zs[:, 0:4, :]
                k_ = zs[:, 4:8, :]
                v_ = zs[:, 8:12, :]
                yield
                S.op("act", lambda e: e.activation(out=tl[0:64, :], in_=zs[0:64, 12, :], func=AF.Tanh), [B_zs], [B_tl])
                S.op("pe", lambda e: e.matmul(PS[0][:, :], lhsT=tl[0:64, :], rhs=waup[0:64, :], start=True, stop=False), [B_tl, B_waup], PB[0])
                S.op("pe", lambda e: e.matmul(PS[0][:, :], lhsT=cst[0:1, C_ONES, :], rhs=w0r[0:1, :], start=False, stop=True), [B_cst, B_w0r], PB[0])
                S.op("act", lambda e: e.activation(out=sgt, in_=PS[0][:, :], func=AF.Sigmoid), PB[0], [B_sgt])
                for cc in range(4):
                    S.op("pe", lambda e, cc=cc: e.matmul(pq(1, cc), lhsT=sgt[:, cc * 128:(cc + 1) * 128], rhs=cst[:, C_TRII, :], start=True, stop=True), [B_sgt, B_cst], [PB[1][cc]])
                    S.op("pe", lambda e, cc=cc: e.matmul(pq(0, cc), lhsT=sgt[:, cc * 128:(cc + 1) * 128], rhs=cst[:, C_TRIE, :], start=True, stop=True), [B_sgt, B_cst], [PB[0][cc]])
                S.op("act", lambda e: e.activation(out=cinc.rearrange("p a b -> p (a b)"), in_=PS[1][:, :], func=AF.Exp), PB[1], [B_cinc])
                S.op("act", lambda e: e.activation(out=cinv.rearrange("p a b -> p (a b)"), in_=PS[1][:, :], func=AF.Exp, scale=-1.0), PB[1], [B_cinv])
                S.op("act", lambda e: e.activation(out=cexc.rearrange("p a b -> p (a b)"), in_=PS[0][:, :], func=AF.Exp), PB[0], [B_cexc])
                yield
                for cc in range(4):
                    S.op("pe", lambda e, cc=cc: e.matmul(pq(1, cc), lhsT=waup[64:128, cc * 128:(cc + 1) * 128], rhs=zs[64:128, 12, :], start=True, stop=True), [B_waup, B_zs], [PB[1][cc]])
                    S.op("act", lambda e, cc=cc: e.activation(out=aa[:, cc, :], in_=pq(1, cc), func=AF.Sigmoid, bias=prm[:, P_A0 + cc:P_A0 + cc + 1]), [PB[1][cc], B_prm], [B_aa])
                S.op("act", lambda e: e.activation(out=sgl, in_=zs[:, 13, :], func=AF.Sigmoid), [B_zs], [B_sgl])
                for cc in range(4):
                    S.op("pe", lambda e, cc=cc: e.matmul(pq(0, cc), lhsT=gup[:, cc * 128:(cc + 1) * 128], rhs=sgl, start=True, stop=True), [B_gup, B_sgl], [PB[0][cc]])
                S.op("act", lambda e: e.copy(out=gT.rearrange("p a b -> p (a b)"), in_=PS[0][:, :]), PB[0], [B_gT])
                yield
                bc4 = lambda c0: prm[:, c0:c0 + 4].unsqueeze(2).to_broadcast([128, 4, 128])
                S.op("dve", lambda e: e.tensor_tensor(out=kk, in0=k_, in1=bc4(P_KK), op=ALU.mult), [B_zs, B_prm], [B_kk])
                S.op("pool", lambda e: e.tensor_tensor(out=tmp, in0=kk, in1=kk, op=ALU.mult), [B_kk], [B_tmp])
                for cc in range(4):
                    S.op("pe", lambda e, cc=cc: e.matmul(pq(1, cc), lhsT=cst[:, C_BLK, :], rhs=tmp[:, cc, :], start=True, stop=True), [B_cst, B_tmp], [PB[1][cc]])
                S.op("dve", lambda e: e.tensor_scalar(out=tmp.rearrange("p a b -> p (a b)"), in0=PS[1][:, :], scalar1=1e-24, scalar2=None, op0=ALU.max), PB[1], [B_tmp])
                S.op("act", lambda e: e.activation(out=tmp, in_=tmp, func=AF.Sqrt), [B_tmp], [B_tmp])
                S.op("dve", lambda e: e.reciprocal(out=tmp, in_=tmp), [B_tmp], [B_tmp])
                S.op("dve", lambda e: e.tensor_tensor(out=kk, in0=kk, in1=tmp, op=ALU.mult), [B_kk, B_tmp], [B_kk])
                S.op("pool", lambda e: e.tensor_scalar(out=k2, in0=aa, scalar1=-1.0, scalar2=None, op0=ALU.add), [B_aa], [B_k2])
                S.op("pool", lambda e: e.tensor_tensor(out=k2, in0=k2, in1=bc4(P_KA), op=ALU.mult), [B_k2, B_prm], [B_k2])
                S.op("dve", lambda e: e.scalar_tensor_tensor(out=k2, in0=k2, scalar=1.0, in1=k_, op0=ALU.add, op1=ALU.mult), [B_k2, B_zs], [B_k2])
                yield
                S.op("dve", lambda e: e.tensor_tensor(out=kkt, in0=kk, in1=cexc, op=ALU.mult), [B_kk, B_cexc], [B_kkt])
                S.op("dve", lambda e: e.tensor_tensor(out=bt, in0=kk, in1=aa, op=ALU.mult), [B_kk, B_aa], [B_bt])
                S.op("dve", lambda e: e.tensor_tensor(out=bt, in0=bt, in1=cinv, op=ALU.mult), [B_bt, B_cinv], [B_bt])
                S.op("pool", lambda e: e.tensor_tensor(out=kt, in0=k2, in1=cinv, op=ALU.mult), [B_k2, B_cinv], [B_kt])
                S.op("pool", lambda e: e.tensor_tensor(out=rt, in0=r_, in1=cinc, op=ALU.mult), [B_zs, B_cinc], [B_rt])
                S.op("pool", lambda e: e.tensor_tensor(out=tmp, in0=r_, in1=k2, op=ALU.mult), [B_zs, B_k2, B_kk], [B_tmp])
                S.op("pool", lambda e: e.tensor_tensor(out=tmp, in0=tmp, in1=bc4(P_RK), op=ALU.mult), [B_tmp, B_prm], [B_tmp])
                for cc in range(4):
                    S.op("pe", lambda e, cc=cc: e.matmul(pq(0, cc), lhsT=cst[:, C_BLK, :], rhs=tmp[:, cc, :], start=True, stop=True), [B_cst, B_tmp], [PB[0][cc]])
                S.op("dve", lambda e: e.tensor_tensor(out=bon, in0=PS[0][:, :].rearrange("p (a b) -> p a b", b=128), in1=v_, op=ALU.mult), PB[0] + [B_zs], [B_bon])
                yield
                for (src, Bsrc, dst, Bdst, bank) in ((v_, B_zs, vtok, B_vtok, 0), (kt, B_kt, ktok, B_ktok, 1), (bt, B_bt, btok, B_btok, 0)):
                    for cc in range(4):
                        S.op("pe", lambda e, src=src, cc=cc, bank=bank: e.transpose(pq(bank, cc), src[:, cc, :], ident), [Bsrc, B_cst], [PB[bank][cc]])
                    S.op("act", lambda e, dst=dst, bank=bank: e.copy(out=dst, in_=PS[bank][:, :]), PB[bank], [Bdst])
                if i == 1:
                    tap("kkt", kkt, B_kkt, [128, 4, 128])
                    tap("rt", rt, B_rt, [128, 4, 128])
                    tap("vtok", vtok, B_vtok, [128, 512])
                yield

            def hv(ap3, par, cc):
                return ap3[64 * par:64 * par + 64, cc, :]

            def neu(i):
                rt, B_rt = FS[i % 3]["rt"]
                kkt, B_kkt = FS[i % 3]["kkt"]
                kt, B_kt = FS[i % 3]["kt"]
                bt, B_bt = FS[i % 3]["bt"]
                bon, B_bon = FS[i % 3]["bon"]
                gT, B_gT = FS[i % 3]["gT"]
                vtok, B_vtok = FS[i % 3]["vtok"]
                ktok, B_ktok = FS[i % 3]["ktok"]
                btok, B_btok = FS[i % 3]["btok"]
                cinc, B_cinc = FS[i % 3]["cinc"]
                KA = dict(KAS)
                KA.update(KAD[i % 2])
                def hv(ap3, par, cc):
                    return ap3[64 * par:64 * par + 64, cc, :]

                def mm_mask(lsrc, Bl, rsrc, Br, mask_idx, kind, eng="dve"):
                    dst, Bd = KA[kind]
                    banks = [nslot(), nslot()]
                    for cc in range(4):
                        for par in range(2):
                            bank = banks[par]
                            S.op("pe", lambda e, par=par, cc=cc, bank=bank: e.matmul(pq(bank, cc), lhsT=hv(lsrc, par, cc), rhs=hv(rsrc, par, cc), start=True, stop=True), [Bl, Br], [PB[bank][0]])
                    for par in range(2):
                        bank = banks[par]
                        S.op(eng, lambda e, par=par, bank=bank: e.tensor_tensor(out=dst[:, par * 4:par * 4 + 4, :], in0=PS[bank][:, :].rearrange("p (a b) -> p a b", b=128),
                                                                           in1=cst[:, mask_idx, :].unsqueeze(1).to_broadcast([128, 4, 128]), op=ALU.mult), [PB[bank][0], B_cst], [Bd[par]])

                mm_mask(bt, B_bt, kkt, B_kkt, C_MSN, "Xa")
                yield
                mm_mask(kkt, B_kkt, bt, B_bt, C_MLN, "XTa")
                yield
                mm_mask(kt, B_kt, kkt, B_kkt, C_MS, "Mk")
                yield
                mm_mask(kt, B_kt, rt, B_rt, C_MI, "Ak")
                yield
                mm_mask(bt, B_bt, rt, B_rt, C_MI, "Ab")
                yield
                for par in range(2):
                    S.op("dve", lambda e, par=par: e.tensor_tensor(out=KA["Ta"][0][:, par * 4:par * 4 + 4, :], in0=KA["Xa"][0][:, par * 4:par * 4 + 4, :],
                                                                in1=ident.unsqueeze(1).to_broadcast([128, 4, 128]), op=ALU.add), [KA["Xa"][1][par], B_cst], [KA["Ta"][1][par]])
                X, XT, T = "Xa", "XTa", "Ta"
                Xn, XTn, Tn = "Xb", "XTb", "Tb"

                def lvl_mm(lk, rk, outk, eng, addk=None):
                    la, lB = KA[lk]
                    ra, rB = KA[rk]
                    oa, oB = KA[outk]
                    for par in range(2):
                        bank = nslot()
                        for cc in range(4):
                            hi = par * 4 + cc
                            S.op("pe", lambda e, hi=hi, cc=cc, bank=bank: e.matmul(pq(bank, cc), lhsT=la[:, hi, :], rhs=ra[:, hi, :], start=True, stop=True), [lB[par], rB[par]], [PB[bank][0]])
                        if addk is None:
                            S.op(eng, lambda e, par=par, bank=bank: e.copy(out=oa[:, par * 4:par * 4 + 4, :], in_=PS[bank][:, :].rearrange("p (a b) -> p a b", b=128)), [PB[bank][0]], [oB[par]])
                        else:
                            aa_, aB = KA[addk]
                            S.op(eng, lambda e, par=par, bank=bank: e.tensor_tensor(out=oa[:, par * 4:par * 4 + 4, :], in0=PS[bank][:, :].rearrange("p (a b) -> p a b", b=128),
                                                                               in1=aa_[:, par * 4:par * 4 + 4, :], op=ALU.add), [PB[bank][0], aB[par]], [oB[par]])

                for l in range(6):
                    if l < 5:
                        lvl_mm(XT, X, Xn, "act")
                    lvl_mm(X, XT, XTn, "act")
                    yield
                    lvl_mm(XTn, T, Tn, "dve", addk=T)
                    yield
                    X, Xn = Xn, X
                    XT, XTn = XTn, XT
                    T, Tn = Tn, T
                yield
                assert T == "Ta"
                yield

            def stt(i):
                rt, B_rt = FS[i % 3]["rt"]
                kkt, B_kkt = FS[i % 3]["kkt"]
                kt, B_kt = FS[i % 3]["kt"]
                bt, B_bt = FS[i % 3]["bt"]
                bon, B_bon = FS[i % 3]["bon"]
                gT, B_gT = FS[i % 3]["gT"]
                vtok, B_vtok = FS[i % 3]["vtok"]
                ktok, B_ktok = FS[i % 3]["ktok"]
                btok, B_btok = FS[i % 3]["btok"]
                cinc, B_cinc = FS[i % 3]["cinc"]
                KA = KAD[i % 2]
                Tinv, B_Tinv = KA["Ta"]
                Mk8, B_Mk8 = KA["Mk"]
                Ak8, B_Ak8 = KA["Ak"]
                Ab8, B_Ab8 = KA["Ab"]
                heads = [(par, cc) for par in range(2) for cc in range(4)]
                for (par, cc) in heads:
                    hi = par * 4 + cc
                    vt_h = vtok[:, cc * 128 + 64 * par: cc * 128 + 64 * par + 64]
                    S.op("pe", lambda e, par=par, cc=cc, hi=hi: e.matmul(PS[6][:, hi * 64:(hi + 1) * 64], lhsT=hv(kkt, par, cc), rhs=ST[64 * par:64 * par + 64, cc, :], start=True, stop=False),
                         [B_kkt, B_STp[par]], [PB[6][0]])
                    S.op("pe", lambda e, hi=hi, vt_h=vt_h: e.matmul(PS[6][:, hi * 64:(hi + 1) * 64], lhsT=Mk8[:, hi, :], rhs=vt_h, start=False, stop=True), [B_Mk8[par], B_vtok], [PB[6][0]])
                S.op("act", lambda e: e.mul(XTs8.rearrange("p a b -> p (a b)"), PS[6][:, :], -1.0), [PB[6][0]], [B_XTs8])
                yield
                for (par, cc) in heads:
                    hi = par * 4 + cc
                    S.op("pe", lambda e, hi=hi: e.matmul(PS[7][:, hi * 64:(hi + 1) * 64], lhsT=Tinv[:, hi, :], rhs=XTs8[:, hi, :], start=True, stop=True), [B_Tinv[par], B_XTs8], [PB[7][0]])
                S.op("act", lambda e: e.copy(out=NTs8.rearrange("p a b -> p (a b)"), in_=PS[7][:, :]), [PB[7][0]], [B_NTs8])
                yield
                for (par, cc) in heads:
                    hi = par * 4 + cc
                    h = 2 * cc + par
                    vt_h = vtok[:, cc * 128 + 64 * par: cc * 128 + 64 * par + 64]
                    osl = PS[5][:, h * 64:(h + 1) * 64]
                    S.op("pe", lambda e, par=par, cc=cc, osl=osl: e.matmul(osl, lhsT=hv(rt, par, cc), rhs=ST[64 * par:64 * par + 64, cc, :], start=True, stop=False), [B_rt, B_STp[par]], [PB[5][0]])
                    S.op("pe", lambda e, hi=hi, osl=osl: e.matmul(osl, lhsT=Ab8[:, hi, :], rhs=NTs8[:, hi, :], start=False, stop=False), [B_Ab8[par], B_NTs8], [PB[5][0]])
                    S.op("pe", lambda e, hi=hi, osl=osl, vt_h=vt_h: e.matmul(osl, lhsT=Ak8[:, hi, :], rhs=vt_h, start=False, stop=True), [B_Ak8[par], B_vtok], [PB[5][0]])
                yield
                for (par, cc) in heads:
                    hi = par * 4 + cc
                    vt_h = vtok[:, cc * 128 + 64 * par: cc * 128 + 64 * par + 64]
                    S.op("pe", lambda e, hi=hi, cc=cc: e.matmul(PS[6][:, hi * 64:(hi + 1) * 64], lhsT=btok[:, cc * 128:(cc + 1) * 128], rhs=NTs8[:, hi, :], start=True, stop=False), [B_btok, B_NTs8], [PB[6][0]])
                    S.op("pe", lambda e, hi=hi, cc=cc, vt_h=vt_h: e.matmul(PS[6][:, hi * 64:(hi + 1) * 64], lhsT=ktok[:, cc * 128:(cc + 1) * 128], rhs=vt_h, start=False, stop=True), [B_ktok, B_vtok], [PB[6][0]])
                for par in range(2):
                    sl = slice(64 * par, 64 * par + 64)
                    S.op("dve", lambda e, par=par, sl=sl: e.tensor_tensor(out=tmpS[sl, :, :], in0=PS[6][sl, par * 256:(par + 1) * 256].rearrange("p (c v) -> p c v", v=64), in1=ST[sl, :, :], op=ALU.add),
                         [PB[6][0], B_STp[par]], [B_tmpS])
                    S.op("dve", lambda e, par=par, sl=sl: e.tensor_tensor(out=ST[sl, :, :], in0=tmpS[sl, :, :], in1=cinc[sl, :, 127:128].to_broadcast([64, 4, 64]), op=ALU.mult),
                         [B_tmpS, B_cinc], [B_STp[par]])
                if i == 0:
                    return
                yield
                S.op("act", lambda e: e.copy(out=osb, in_=PS[5][:, :]), PB[5], [B_osb])
                S.op("act", lambda e: e.activation(out=osq, in_=PS[5][:, :], func=AF.Square), PB[5], [B_osq])
                if i == 1:
                    tap("osb", osb, B_osb, [128, 512])
                o3 = osb.rearrange("p (h v) -> p h v", v=64)
                S.op("dve", lambda e: e.tensor_reduce(out=gst[:, 0:8], in_=o3, axis=AX.X, op=ALU.add), [B_osb], [B_gst])
                S.op("dve", lambda e: e.tensor_reduce(out=gst[:, 8:16], in_=osq.rearrange("p (h v) -> p h v", v=64), axis=AX.X, op=ALU.add), [B_osq, B_gst], [B_gst])
                S.op("dve", lambda e: e.tensor_scalar(out=gst[:, 0:16], in0=gst[:, 0:16], scalar1=1.0 / 64, scalar2=None, op0=ALU.mult), [B_gst], [B_gst])
                S.op("dve", lambda e: e.tensor_tensor(out=gst[:, 16:24], in0=gst[:, 0:8], in1=gst[:, 0:8], op=ALU.mult), [B_gst], [B_gst])
                S.op("dve", lambda e: e.tensor_tensor(out=gst[:, 16:24], in0=gst[:, 8:16], in1=gst[:, 16:24], op=ALU.subtract), [B_gst], [B_gst])
                S.op("act", lambda e: e.activation(out=gst[:, 24:32], in_=gst[:, 16:24], func=AF.Sqrt, bias=prm[:, P_EPSG:P_EPSG + 1]), [B_gst, B_prm], [B_gst])
                S.op("dve", lambda e: e.reciprocal(out=gst[:, 24:32], in_=gst[:, 24:32]), [B_gst], [B_gst])
                S.op("dve", lambda e: e.tensor_tensor(out=o3, in0=o3, in1=gst[:, 0:8].unsqueeze(2).to_broadcast([128, 8, 64]), op=ALU.subtract), [B_osb, B_gst], [B_osb])
                S.op("dve", lambda e: e.tensor_tensor(out=o3, in0=o3, in1=gst[:, 24:32].unsqueeze(2).to_broadcast([128, 8, 64]), op=ALU.mult), [B_osb, B_gst], [B_osb])
                yield
                for cc in range(4):
                    S.op("pe", lambda e, cc=cc: e.transpose(pq(7, cc), osb[:, cc * 128:(cc + 1) * 128], ident), [B_osb, B_cst], [PB[7][cc]])
                    S.op("dve", lambda e, cc=cc: e.tensor_scalar(out=tmp2[:, cc, :], in0=pq(7, cc), scalar1=prm[:, P_GNG + cc:P_GNG + cc + 1], scalar2=prm[:, P_GNB + cc:P_GNB + cc + 1], op0=ALU.mult, op1=ALU.add),
                         [PB[7][cc], B_prm], [B_tmp2])
                S.op("pool", lambda e: e.tensor_tensor(out=tmp2, in0=tmp2, in1=bon, op=ALU.add), [B_tmp2, B_bon], [B_tmp2])
                S.op("pool", lambda e: e.tensor_tensor(out=orw, in0=tmp2, in1=gT, op=ALU.mult), [B_tmp2, B_gT], [B_orw])
                if i == 1:
                    tap("orw", orw, B_orw, [128, 4, 128], BF16)
                S.dma(ch_orw, orw_s[i], orw, reads=[B_orw], writes=[B_orws])
                yield

            def run_all(g):
                for _ in g:
                    pass

            def adv(g):
                try:
                    next(g)
                    return True
                except StopIteration:
                    return False

            run_all(front(0))
            if NT > 1:
                run_all(front(1))
            run_all(neu(0))
            NNEU = 21
            NFRONT = 10
            for i in range(NT):
                if i < 16:
                    issue_cast(i)
                g_st = stt(i)
                g_neu = neu(i + 1) if i + 1 < NT else iter(())
                g_fr = front(i + 2) if i + 2 < NT else iter(())
                a_st = a_neu = a_fr = True
                rnd = 0
                fdone = 0
                while a_st or a_neu or a_fr:
                    if a_st:
                        a_st = adv(g_st)
                    if a_neu:
                        a_neu = adv(g_neu)
                    want = (rnd + 1) * NFRONT // NNEU + 1 if (a_neu or a_st) else 10 ** 9
                    while a_fr and fdone < want:
                        a_fr = adv(g_fr)
                        fdone += 1
                    rnd += 1
            for k_ in range(min(NT, 16), 16):
                issue_cast(k_)
            tap("STfin", ST, B_STp[0], [128, 4, 64])
        phase_R()
        S.barrier()
        B_h1toks = Buf("h1toks")
        B_h1ns = Buf("h1ns")

        def phase_C():
            top[0] = base_top
            wC, B_wC = alloc("wC", [8, 3072], BF16)
            wco, B_wco = alloc("wco", [4, 1024], BF16)
            wro, B_wro = alloc("wro", [4, 1024], BF16)
            wo, B_wo = alloc("wo", [8, 1024], BF16)
            diag, B_diag = alloc("diag", [124, 128], BF16)
            S.dma(S.chan("wC0"), wC[:, :, 0:1024], w_in_d[:, 0:1024].rearrange("(c p) n -> p c n", p=128), writes=[B_wC], eng="pool")
            S.dma(S.chan("wC1"), wC[:, :, 1024:3072], w_in_d[:, 2816:4864].rearrange("(c p) n -> p c n", p=128), writes=[B_wC], eng="pool")
            S.dma(S.chan("wco"), wco, wco_d.rearrange("(c p) n -> p c n", p=128), writes=[B_wco], eng="pool")
            S.dma(S.chan("wro"), wro, wro_d.rearrange("(c p) n -> p c n", p=128), writes=[B_wro], eng="pool")
            S.dma(S.chan("wo"), wo, wo_d.rearrange("(c p) n -> p c n", p=128), writes=[B_wo], eng="pool")
            for cc in range(4):
                for j in range(31):
                    S.op("dve", lambda e, cc=cc, j=j: e.tensor_scalar(out=diag[:, cc * 31 + j, :], in0=ident, scalar1=prm[:, P_CW + cc * 31 + j:P_CW + cc * 31 + j + 1], scalar2=None, op0=ALU.mult),
                         [B_cst, B_prm], [B_diag])
            xts = [alloc("cxt%d" % k, [8, 128]) for k in range(3)]
            chx = [S.chan("cx%d" % k) for k in range(3)]
            xb, B_xb = alloc("cxb", [8, 128], BF16)
            sq, B_sq = alloc("csq", [8, 128], BF16)
            rs, B_rs = alloc("crs", [128])
            zcs = [alloc("zc%d" % k, [24, 128]) for k in range(2)]
            ubuf, B_ubuf = alloc("ubuf", [4, 158], BF16)
            ysb, B_ysb = alloc("ysb", [4, 128])
            ysq, B_ysq = alloc("ysq", [4, 128])
            mv, B_mv = alloc("mv", [128])
            actc, B_actc = alloc("actc", [4, 128], BF16)
            orwt, B_orwt = alloc("orwt", [4, 128], BF16)
            t1, B_t1 = alloc("t1", [8, 128])
            t2, B_t2 = alloc("t2", [8, 128])
            gateds = [alloc("gated%d" % k, [8, 128], BF16) for k in range(2)]
            t1b, B_t1b = alloc("t1b", [8, 128])
            h1T, B_h1T = alloc("h1T", [8, 128])
            h1sq, B_h1sq = alloc("h1sq", [8, 128], BF16)
            rs2, B_rs2 = alloc("rs2", [128])
            h1n, B_h1n = alloc("h1n", [8, 128], BF16)
            h1tok, B_h1tok = alloc("h1tok", [1024])
            ch_orwl = S.chan("orwl")
            ch_h1tok = S.chan("h1tok")
            ch_h1n = S.chan("h1n")
            S.op("dve", lambda e: e.memset(ubuf, 0.0), [], [B_ubuf])
            def cfront(i):
                zc, B_zc = zcs[i % 2]
                xt, B_xt = xts[i % 3]
                load_x(i, xt, B_xt, chx[i % 3], xb, B_xb, sq, B_sq, rs, B_rs, (1, 0))
                nchunk = 8 if i == 0 else 24
                for gi in range(nchunk // 4):
                    bank = gi % 2
                    for jj in range(4):
                        j = gi * 4 + jj
                        col = j * 128
                        for dc in range(8):
                            S.op("pe", lambda e, bank=bank, jj=jj, dc=dc, col=col: e.matmul(pq(bank, jj), lhsT=wC[:, dc, col:col + 128], rhs=xb[:, dc, :], start=(dc == 0), stop=(dc == 7)),
                                 [B_wC, B_xb], [PB[bank][0]])
                    S.op("dve", lambda e, bank=bank, gi=gi: e.tensor_tensor(out=zc[:, gi * 4:gi * 4 + 4, :], in0=PS[bank][:, :].rearrange("p (a b) -> p a b", b=128),
                                                                         in1=rs.unsqueeze(1).to_broadcast([128, 4, 128]), op=ALU.mult),
                         [PB[bank][0], B_rs], [B_zc])
                yield

            def cb1(i):
                zc, B_zc = zcs[i % 2]
                gated, B_gated = gateds[i % 2]
                S.op("act", lambda e: e.activation(out=zc[:, 4:8, :], in_=zc[:, 4:8, :], func=AF.Sigmoid), [B_zc], [B_zc])
                S.op("dve", lambda e: e.tensor_tensor(out=ubuf[:, :, 30:158], in0=zc[:, 0:4, :], in1=zc[:, 4:8, :], op=ALU.mult), [B_zc], [B_ubuf])
                yield
                for cc in range(4):
                    for j in range(31):
                        S.op("pe", lambda e, cc=cc, j=j: e.matmul(pq(2, cc), lhsT=diag[:, cc * 31 + j, :], rhs=ubuf[:, cc, j:j + 128], start=(j == 0), stop=(j == 30)),
                             [B_diag, B_ubuf], [PB[2][0]])
                S.op("pool", lambda e: e.tensor_copy(out=ubuf[:, :, 0:30], in_=ubuf[:, :, 128:158]), [B_ubuf], [B_ubuf])
                if i == 0:
                    S.op("act", lambda e: e.copy(out=ysb.rearrange("p a b -> p (a b)"), in_=PS[2][:, :]), [PB[2][0]], [B_ysb])
                    return
                yield
                for cc in range(4):
                    S.op("act", lambda e, cc=cc: e.activation(out=ysb[:, cc, :], in_=pq(2, cc), func=AF.Identity, bias=prm[:, P_CB + cc:P_CB + cc + 1]), [PB[2][0], B_prm], [B_ysb])
                    S.op("act", lambda e, cc=cc: e.activation(out=ysq[:, cc, :], in_=pq(2, cc), func=AF.Square, bias=prm[:, P_CB + cc:P_CB + cc + 1]), [PB[2][0], B_prm], [B_ysq])
                for cc in range(4):
                    S.op("pe", lambda e, cc=cc: e.matmul(pq(3, 0), lhsT=cst[:, C_O512, :], rhs=ysb[:, cc, :], start=(cc == 0), stop=(cc == 3)), [B_cst, B_ysb], [PB[3][0]])
                for cc in range(4):
                    S.op("pe", lambda e, cc=cc: e.matmul(pq(3, 1), lhsT=cst[:, C_O512, :], rhs=ysq[:, cc, :], start=(cc == 0), stop=(cc == 3)), [B_cst, B_ysq], [PB[3][0]])
                S.op("act", lambda e: e.activation(out=mv, in_=pq(3, 0), func=AF.Square), [PB[3][0]], [B_mv])
                S.op("dve", lambda e: e.tensor_tensor(out=mv, in0=pq(3, 1), in1=mv, op=ALU.subtract), [PB[3][0], B_mv], [B_mv])
                S.op("act", lambda e: e.activation(out=mv, in_=mv, func=AF.Sqrt, bias=prm[:, P_EPS5:P_EPS5 + 1]), [B_mv, B_prm], [B_mv])
                S.op("dve", lambda e: e.reciprocal(out=mv, in_=mv), [B_mv], [B_mv])
                S.op("dve", lambda e: e.tensor_tensor(out=ysb, in0=ysb, in1=pq(3, 0).unsqueeze(1).to_broadcast([128, 4, 128]), op=ALU.subtract), [B_ysb, PB[3][0]], [B_ysb])
                S.op("dve", lambda e: e.tensor_tensor(out=ysb, in0=ysb, in1=mv.unsqueeze(1).to_broadcast([128, 4, 128]), op=ALU.mult), [B_ysb, B_mv], [B_ysb])
                for cc in range(4):
                    S.op("act", lambda e, cc=cc: e.activation(out=actc[:, cc, :], in_=ysb[:, cc, :], func=AF.Silu, scale=prm[:, P_LNG + cc:P_LNG + cc + 1], bias=prm[:, P_LNB + cc:P_LNB + cc + 1]),
                         [B_ysb, B_prm], [B_actc])
                yield
                S.dma(ch_orwl, orwt, orw_s[i], reads=[B_orws], writes=[B_orwt])
                for m_ in range(8):
                    for cc in range(4):
                        S.op("pe", lambda e, m_=m_, cc=cc: e.matmul(pq(4 + m_ // 4, m_ % 4), lhsT=wco[:, cc, m_ * 128:(m_ + 1) * 128], rhs=actc[:, cc, :], start=(cc == 0), stop=(cc == 3)),
                             [B_wco, B_actc], [PB[4 + m_ // 4][0]])
                S.op("act", lambda e: e.activation(out=zc[:, 8:24, :], in_=zc[:, 8:24, :], func=AF.Sigmoid), [B_zc], [B_zc])
                yield
                for hb_ in range(2):
                    S.op("dve", lambda e, hb_=hb_: e.tensor_tensor(out=t1[:, hb_ * 4:hb_ * 4 + 4, :], in0=PS[4 + hb_][:, :].rearrange("p (a b) -> p a b", b=128), in1=zc[:, 8 + hb_ * 4:12 + hb_ * 4, :], op=ALU.mult),
                         [PB[4 + hb_][0], B_zc], [B_t1])
                for m_ in range(8):
                    for cc in range(4):
                        S.op("pe", lambda e, m_=m_, cc=cc: e.matmul(pq(4 + m_ // 4, m_ % 4), lhsT=wro[:, cc, m_ * 128:(m_ + 1) * 128], rhs=orwt[:, cc, :], start=(cc == 0), stop=(cc == 3)),
                             [B_wro, B_orwt], [PB[4 + m_ // 4][0]])
                yield
                for hb_ in range(2):
                    S.op("dve", lambda e, hb_=hb_: e.tensor_tensor(out=t2[:, hb_ * 4:hb_ * 4 + 4, :], in0=PS[4 + hb_][:, :].rearrange("p (a b) -> p a b", b=128), in1=zc[:, 16 + hb_ * 4:20 + hb_ * 4, :], op=ALU.mult),
                         [PB[4 + hb_][0], B_zc], [B_t2])
                S.op("pool", lambda e: e.tensor_tensor(out=gated, in0=t1, in1=t2, op=ALU.add), [B_t1, B_t2], [B_gated])
                yield


            def cb2(i):
                if i == 0:
                    return
                    yield
                xt, B_xt = xts[i % 3]
                gated, B_gated = gateds[i % 2]
                for m_ in range(8):
                    for kc in range(8):
                        S.op("pe", lambda e, m_=m_, kc=kc: e.matmul(pq(6 + m_ // 4, m_ % 4), lhsT=wo[:, kc, m_ * 128:(m_ + 1) * 128], rhs=gated[:, kc, :], start=(kc == 0), stop=(kc == 7)),
                             [B_wo, B_gated], [PB[6 + m_ // 4][0]])
                for hb_ in range(2):
                    S.op("dve", lambda e, hb_=hb_, xt=xt: e.tensor_tensor(out=h1T[:, hb_ * 4:hb_ * 4 + 4, :], in0=PS[6 + hb_][:, :].rearrange("p (a b) -> p a b", b=128), in1=xt[:, hb_ * 4:hb_ * 4 + 4, :], op=ALU.add),
                         [PB[6 + hb_][0], B_xt], [B_h1T])
                yield
                for m_ in range(8):
                    S.op("pe", lambda e, m_=m_: e.transpose(pq(6 + m_ // 4, m_ % 4), h1T[:, m_, :], ident), [B_h1T, B_cst], [PB[6 + m_ // 4][0]])
                for hb_ in range(2):
                    S.op("act", lambda e, hb_=hb_: e.copy(out=h1tok[:, hb_ * 512:(hb_ + 1) * 512], in_=PS[6 + hb_][:, :]), [PB[6 + hb_][0]], [B_h1tok])
                S.dma(ch_h1tok, h1tok_s[i - 1], h1tok, reads=[B_h1tok], writes=[B_h1toks])
                yield
                S.op("act", lambda e: e.activation(out=h1sq, in_=h1T, func=AF.Square), [B_h1T], [B_h1sq])
                for dc in range(8):
                    S.op("pe", lambda e, dc=dc: e.matmul(pq(6, 0), lhsT=ones_b, rhs=h1sq[:, dc, :], start=(dc == 0), stop=(dc == 7)), [B_onesb, B_h1sq], [PB[6][0]])
                S.op("act", lambda e: e.activation(out=rs2, in_=pq(6, 0), func=AF.Sqrt, scale=1.0 / 1024, bias=prm[:, P_EPS6:P_EPS6 + 1]), [PB[6][0], B_prm], [B_rs2])
                S.op("dve", lambda e: e.reciprocal(out=rs2, in_=rs2), [B_rs2], [B_rs2])
                S.op("dve", lambda e: e.tensor_tensor(out=t1b, in0=h1T, in1=rs2.unsqueeze(1).to_broadcast([128, 8, 128]), op=ALU.mult), [B_h1T, B_rs2], [B_t1b])
                S.op("pool", lambda e: e.tensor_tensor(out=h1n, in0=t1b, in1=prm[:, P_GFFN:P_GFFN + 8].unsqueeze(2).to_broadcast([128, 8, 128]), op=ALU.mult), [B_t1b, B_prm], [B_h1n])
                S.dma(ch_h1n, h1nT_s[i - 1], h1n, reads=[B_h1n], writes=[B_h1ns])
                if i == 1:
                    tap("h1tok", h1tok, B_h1tok, [128, 1024])
                    tap("h1n", h1n, B_h1n, [128, 8, 128], BF16)
                yield

            def _adv(g):
                try:
                    next(g)
                    return True
                except StopIteration:
                    return False

            def _run(g):
                for _ in g:
                    pass

            _run(cfront(0))
            if NT > 1:
                _run(cfront(1))
            _run(cb1(0))
            for i in range(NT):
                g2 = cb2(i)
                g1 = cb1(i + 1) if i + 1 < NT else iter(())
                gf = cfront(i + 2) if i + 2 < NT else iter(())
                a1 = a2 = af = True
                while a1 or a2 or af:
                    if a2:
                        a2 = _adv(g2)
                    if a1:
                        a1 = _adv(g1)
                    if af:
                        af = _adv(gf)
        phase_C()
        S.barrier()

        B_sels = Buf("sels")

        def phase_Q():
            top[0] = base_top
            wq, B_wq = alloc("wq", [8, 2048], BF16)
            skT, B_skT = alloc("skT", [16, 128], BF16)
            S.dma(S.chan("wq"), wq, wq_d.rearrange("(c p) n -> p c n", p=128), writes=[B_wq], eng="pool")
            S.dma(S.chan("skT"), skT, skT_d, writes=[B_skT], eng="pool")
            hns = [alloc("hn%d" % k, [8, 128], BF16) for k in range(2)]
            ch_hns = [S.chan("hn%d" % k) for k in range(2)]
            qT, B_qT = alloc("qT", [16, 128], BF16)
            ssbs = [alloc("ssb%d" % k, [16, 128]) for k in range(2)]
            wk16, _ = alloc("wk16", [16, 128])
            B_wkg = [Buf("wk%d" % g_) for g_ in range(16)]
            B_ssb4s = [[Buf("ssb%d_%d" % (k, g_)) for g_ in range(4)] for k in range(2)]
            B_topsg = [Buf("tops%d" % g_) for g_ in range(16)]
            B_topig = [Buf("topi%d" % g_) for g_ in range(16)]
            B_candh = [Buf("cand%d" % g_) for g_ in range(8)]
            B_bestsh = [Buf("bests%d" % g_) for g_ in range(8)]
            B_bestch = [Buf("bestc%d" % g_) for g_ in range(8)]
            B_eqh = [Buf("eq%d" % g_) for g_ in range(16)]
            B_sel3h = [Buf("sel3_%d" % g_) for g_ in range(16)]
            B_sel3g = Buf("sel3g")
            B_ju2 = Buf("ju2")
            TS3 = [(alloc("tops%d" % k, [16, 16])[0], alloc("topi%d" % k, [16, 16], U32)[0], alloc("topif%d" % k, [16, 16])[0]) for k in range(2)]
            TB3 = [([Buf("tops%d_%d" % (k, g_)) for g_ in range(16)], [Buf("topi%d_%d" % (k, g_)) for g_ in range(16)], Buf("topif%d" % k)) for k in range(2)]
            wk2, _ = alloc("wk2", [8, 256])
            B_wk2h = [Buf("wk2_%d" % h) for h in range(8)]
            cand, B_cand = alloc("cand", [8, 256])
            bests, B_bests = alloc("bests", [8, 16])
            bestc, B_bestc = alloc("bestc", [8, 16], U32)
            ju, B_ju = alloc("ju", [2, 8, 16], U32)
            j1, B_j1 = alloc("j1", [8, 16])
            j2, B_j2 = alloc("j2", [8, 16])
            eq, B_eq = alloc("eq", [16, 16, 16])
            ee, B_ee = alloc("ee", [8, 16])
            zz, B_zz = alloc("zz", [8])
            sel3, B_sel3 = alloc("sel3", [3, 128])
            selT, B_selT = alloc("selT", [3, 128])
            ch_hn = S.chan("hn")
            ch_sel = S.chan("sel")
            iota16 = cst[:, C_IOTA, 0:16]
            def q_front(i):
                hn, B_hn = hns[i % 2]
                ssb = ssbs[i % 2][0]
                B_ssb4 = B_ssb4s[i % 2]
                S.dma(ch_hns[i % 2], hn, h1nT_s[i], reads=[B_h1ns], writes=[B_hn])
                for g_ in range(16):
                    for dc in range(8):
                        S.op("pe", lambda e, g_=g_, dc=dc: e.matmul(pq(g_ // 4, g_ % 4), lhsT=wq[:, dc, g_ * 128:(g_ + 1) * 128], rhs=hn[:, dc, :], start=(dc == 0), stop=(dc == 7)),
                             [B_wq, B_hn], [PB[g_ // 4][0]])
                    if g_ % 4 == 3:
                        S.op("act", lambda e, g_=g_: e.copy(out=qT[:, g_ - 3:g_ + 1, :], in_=PS[g_ // 4][:, :].rearrange("p (a b) -> p a b", b=128)), [PB[g_ // 4][0]], [B_qT])
                for g_ in range(16):
                    S.op("pe", lambda e, g_=g_: e.matmul(pq(4 + g_ // 4, g_ % 4), lhsT=qT[:, g_, :], rhs=skT[:, g_, :], start=True, stop=True), [B_qT, B_skT], [PB[4 + g_ // 4][0]])
                    if g_ % 4 == 3:
                        S.op("act", lambda e, g_=g_: e.copy(out=ssb[:, g_ - 3:g_ + 1, :], in_=PS[4 + g_ // 4][:, :].rearrange("p (a b) -> p a b", b=128)), [PB[4 + g_ // 4][0]], [B_ssb4[g_ // 4]])

            def q_b1(i):
                tops, topi, topif = TS3[i % 2]
                B_topsg, B_topig, B_topif = TB3[i % 2]
                ssb = ssbs[i % 2][0]
                B_ssb4 = B_ssb4s[i % 2]
                for g_ in range(16):
                    S.op("dve", lambda e, g_=g_: e.max(out=tops[:, g_, 0:8], in_=ssb[:, g_, :]), [B_ssb4[g_ // 4]], [B_topsg[g_]])
                yield
                for g_ in range(16):
                    S.op("dve", lambda e, g_=g_: e.max_index(out=topi[:, g_, 0:8], in_max=tops[:, g_, 0:8], in_values=ssb[:, g_, :]), [B_ssb4[g_ // 4], B_topsg[g_]], [B_topig[g_]])
                yield
                for g_ in range(16):
                    S.op("dve", lambda e, g_=g_: e.match_replace(out=wk16[:, g_, :], in_to_replace=tops[:, g_, 0:8], in_values=ssb[:, g_, :], imm_value=-1e30), [B_ssb4[g_ // 4], B_topsg[g_]], [B_wkg[g_]])
                yield
                for g_ in range(16):
                    S.op("dve", lambda e, g_=g_: e.max(out=tops[:, g_, 8:16], in_=wk16[:, g_, :]), [B_wkg[g_]], [B_topsg[g_]])
                yield
                for g_ in range(16):
                    S.op("dve", lambda e, g_=g_: e.max_index(out=topi[:, g_, 8:16], in_max=tops[:, g_, 8:16], in_values=wk16[:, g_, :]), [B_wkg[g_], B_topsg[g_]], [B_topig[g_]])
                S.op("pool", lambda e: e.tensor_copy(out=topif, in_=topi), B_topig, [B_topif])
                yield

            def q_b2(i):
                tops, topi, topif = TS3[i % 2]
                B_topsg, B_topig, B_topif = TB3[i % 2]
                yield
                tops4 = tops.rearrange("p (h c) j -> p h c j", c=2)
                S.op("pool", lambda e, tops4=tops4: e.tensor_tensor(out=cand.rearrange("p h (a b) -> p h a b", b=16),
                                                                in0=tops4[:, :, 0, :].unsqueeze(3).to_broadcast([128, 8, 16, 16]),
                                                                in1=tops4[:, :, 1, :].unsqueeze(2).to_broadcast([128, 8, 16, 16]), op=ALU.add), B_topsg, B_candh)
                yield
                for h in range(8):
                    S.op("dve", lambda e, h=h: e.max(out=bests[:, h, 0:8], in_=cand[:, h, :]), [B_candh[h]], [B_bestsh[h]])
                yield
                for h in range(8):
                    S.op("dve", lambda e, h=h: e.max_index(out=bestc[:, h, 0:8], in_max=bests[:, h, 0:8], in_values=cand[:, h, :]), [B_candh[h], B_bestsh[h]], [B_bestch[h]])
                yield
                for h in range(8):
                    S.op("dve", lambda e, h=h: e.match_replace(out=wk2[:, h, :], in_to_replace=bests[:, h, 0:8], in_values=cand[:, h, :], imm_value=-1e30),
                         [B_candh[h], B_bestsh[h]], [B_wk2h[h]])
                yield
                for h in range(8):
                    S.op("dve", lambda e, h=h: e.max(out=bests[:, h, 8:16], in_=wk2[:, h, :]), [B_wk2h[h]], [B_bestsh[h]])
                yield
                for h in range(8):
                    S.op("dve", lambda e, h=h: e.max_index(out=bestc[:, h, 8:16], in_max=bests[:, h, 8:16], in_values=wk2[:, h, :]),
                         [B_wk2h[h], B_bestsh[h]], [B_bestch[h]])
                S.op("dve", lambda e: e.tensor_single_scalar(out=ju[:, 0, :, :], in_=bestc, scalar=4, op=ALU.logical_shift_right), B_bestch, [B_ju])
                S.op("dve", lambda e: e.tensor_single_scalar(out=ju[:, 1, :, :], in_=bestc, scalar=15, op=ALU.bitwise_and), B_bestch, [B_ju2])
                S.op("pool", lambda e: e.tensor_copy(out=j1, in_=ju[:, 0, :, :]), [B_ju], [B_j1])
                S.op("pool", lambda e: e.tensor_copy(out=j2, in_=ju[:, 1, :, :]), [B_ju2], [B_j2])
                yield
                for half, jj_ in ((0, j1), (1, j2)):
                    Bj = B_j1 if half == 0 else B_j2
                    eqh = eq[:, half * 8:half * 8 + 8, :, :]
                    Beq = B_eqh[half * 8:half * 8 + 8]
                    tf4 = topif.rearrange("p (h c) j -> p h c j", c=2)[:, :, half, :]
                    S.op("dve", lambda e, jj_=jj_, eqh=eqh: e.tensor_tensor(out=eqh, in0=jj_.unsqueeze(3).to_broadcast([128, 8, 16, 16]),
                                                                   in1=iota16.unsqueeze(1).unsqueeze(1).to_broadcast([128, 8, 16, 16]), op=ALU.is_equal), [Bj, B_cst], Beq)
                    S.op("pool", lambda e, eqh=eqh, tf4=tf4: e.tensor_tensor(out=eqh, in0=eqh, in1=tf4.unsqueeze(2).to_broadcast([128, 8, 16, 16]), op=ALU.mult), Beq + [B_topif], Beq)
                yield
                for half in range(2):
                    eqh = eq[:, half * 8:half * 8 + 8, :, :]
                    Beq = B_eqh[half * 8:half * 8 + 8]
                    S.op("dve", lambda e, half=half, eqh=eqh: e.tensor_reduce(out=sel3[:, half, :].rearrange("p (h j) -> p h j", j=16), in_=eqh, axis=AX.X, op=ALU.add), Beq, B_sel3h[half * 8:half * 8 + 8])
                B_bests_all = B_bestsh
                yield
                S.op("dve", lambda e: e.tensor_tensor(out=ee, in0=bests, in1=bests[:, :, 0:1].to_broadcast([128, 8, 16]), op=ALU.subtract), B_bestsh, [B_ee])
                S.op("act", lambda e: e.activation(out=ee, in_=ee, func=AF.Exp), [B_ee], [B_ee])
                S.op("dve", lambda e: e.tensor_reduce(out=zz, in_=ee, axis=AX.X, op=ALU.add), [B_ee], [B_zz])
                S.op("dve", lambda e: e.reciprocal(out=zz, in_=zz), [B_zz], [B_zz])
                S.op("dve", lambda e: e.tensor_tensor(out=sel3[:, 2, :].rearrange("p (h j) -> p h j", j=16), in0=ee, in1=zz.unsqueeze(2).to_broadcast([128, 8, 16]), op=ALU.mult), [B_ee, B_zz], [B_sel3g])
                yield
                for k in range(3):
                    S.op("pe", lambda e, k=k: e.transpose(pq(0, k), sel3[:, k, :], ident), B_sel3h + [B_sel3g, B_cst], [PB[0][0]])
                S.op("act", lambda e: e.copy(out=selT.rearrange("p a b -> p (a b)"), in_=PS[0][:, 0:384]), [PB[0][0]], [B_selT])
                S.dma(ch_sel, sel_s[i], selT, reads=[B_selT], writes=[B_sels])
                if i == 0:
                    tap("sel3", sel3, B_sel3g, [128, 3, 128])


                yield
            def _adv(g):
                try:
                    next(g)
                    return True
                except StopIteration:
                    return False

            q_front(0)
            if NRT > 1:
                q_front(1)
            for _ in q_b1(0):
                pass
            for i in range(NRT):
                if i + 2 < NRT:
                    q_front(i + 2)
                g2 = q_b2(i)
                g1 = q_b1(i + 1) if i + 1 < NRT else iter(())
                a1 = a2 = True
                while a1 or a2:
                    if a2:
                        a2 = _adv(g2)
                    if a2:
                        a2 = _adv(g2)
                    if a1:
                        a1 = _adv(g1)
        phase_Q()
        S.barrier()

        def phase_E():
            top[0] = base_top
            NS = NRT // 2
            act3s = [alloc("act3_%d" % k, [256, 128], BF16) for k in range(2)]
            hn2s = [alloc("hn2_%d" % k, [8, 256], BF16) for k in range(2)]
            selAs = [alloc("selA_%d" % k, [2, 3, 128]) for k in range(2)]
            Ub = [alloc("Ub%d" % k, [8, 128], BF16) for k in range(8)]
            Vb = [alloc("Vb%d" % k, [1024], BF16) for k in range(8)]
            Aoh = [alloc("Aoh%d" % k, [4, 128], BF16) for k in range(4)]
            Boh = [alloc("Boh%d" % k, [4, 128], BF16) for k in range(4)]
            ysb, B_ysb = alloc("eysb", [1024])
            h1t, B_h1t = alloc("h1t", [1024])
            gfin, B_gfin = alloc("gfin", [1024])
            ob, B_ob = alloc("ob", [1024])
            stat, B_stat = alloc("stat", [4])
            S.dma(S.chan("gfin"), gfin, gfin_d.partition_broadcast(128)[:, 0, :], writes=[B_gfin])
            ch_hn2 = [S.chan("hn2_%d" % k) for k in range(2)]
            ch_selA = [S.chan("selA_%d" % k) for k in range(2)]
            ch_U = [S.chan("U%d" % k) for k in range(8)]
            ch_V = [S.chan("V%d" % k) for k in range(8)]
            ch_h1t = S.chan("h1t")
            ch_out = S.chan("out")
            iota_bc = cst[:, C_IOTA, :].unsqueeze(1).to_broadcast([128, 4, 128])
            uctr = [0]
            actr = [0]

            def loads(s_):
                hn2, B_hn2 = hn2s[s_ % 2]
                selA, B_selA = selAs[s_ % 2]
                for ts in range(2):
                    S.dma(ch_hn2[s_ % 2], hn2[:, :, ts * 128:(ts + 1) * 128], h1nT_s[s_ * 2 + ts], reads=[B_h1ns], writes=[B_hn2])
                    S.dma(ch_selA[s_ % 2], selA[:, ts, :, :], sel_s[s_ * 2 + ts], reads=[B_sels], writes=[B_selA])

            def a_iter(s_, i2):
                act3, B_act3 = act3s[s_ % 2]
                hn2, B_hn2 = hn2s[s_ % 2]
                uctr[0] += 1
                slot = uctr[0] % 8
                U_, B_U = Ub[slot]
                S.dma(ch_U[slot], U_.rearrange("p b c -> p (b c)"), uT_b[i2], reads=[B_uTb], writes=[B_U])
                bank = (actr[0] // 2) % 2
                half = actr[0] % 2
                actr[0] += 1
                for dc in range(8):
                    S.op("pe", lambda e, dc=dc: e.matmul(PS[bank][:, half * 256:(half + 1) * 256], lhsT=U_[:, dc, :], rhs=hn2[:, dc, :], start=(dc == 0), stop=(dc == 7)), [B_U, B_hn2], [PB[bank][0]])
                if half == 1:
                    S.op("act", lambda e: e.activation(out=act3[:, :, i2 - 1:i2 + 1], in_=PS[bank][:, :].rearrange("p (i t) -> p t i", i=2), func=AF.Gelu), [PB[bank][0]], [B_act3])

            def b_vars(s_, tg):
                act3, B_act3 = act3s[s_ % 2]
                selA, B_selA = selAs[s_ % 2]
                t0 = tg * 4
                A_, B_A = Aoh[tg % 4]
                Bm, B_B = Boh[tg % 4]
                return act3, B_act3, selA, B_selA, t0, t0 // 128, t0 % 128, A_, B_A, Bm, B_B, 2 + tg % 2

            def b_stage1(s_, tg):
                act3, B_act3, selA, B_selA, t0, ti, tt, A_, B_A, Bm, B_B, gb = b_vars(s_, tg)
                S.op("dve", lambda e: e.tensor_tensor(out=A_, in0=iota_bc, in1=selA[:, ti, 0, tt:tt + 4].unsqueeze(2).to_broadcast([128, 4, 128]), op=ALU.is_equal), [B_cst, B_selA], [B_A])
                S.op("pool", lambda e: e.tensor_tensor(out=A_, in0=A_, in1=selA[:, ti, 2, tt:tt + 4].unsqueeze(2).to_broadcast([128, 4, 128]), op=ALU.mult), [B_A, B_selA], [B_A])
                S.op("dve", lambda e: e.tensor_tensor(out=Bm, in0=iota_bc, in1=selA[:, ti, 1, tt:tt + 4].unsqueeze(2).to_broadcast([128, 4, 128]), op=ALU.is_equal), [B_cst, B_selA], [B_B])

            def b_stage2(s_, tg):
                act3, B_act3, selA, B_selA, t0, ti, tt, A_, B_A, Bm, B_B, gb = b_vars(s_, tg)
                for tk in range(4):
                    S.op("pe", lambda e, tk=tk: e.matmul(pq(gb, tk), lhsT=A_[:, tk, :], rhs=Bm[:, tk, :], start=True, stop=True), [B_A, B_B], [PB[gb][0]])

            def b_stage3(s_, tg):
                act3, B_act3, selA, B_selA, t0, ti, tt, A_, B_A, Bm, B_B, gb = b_vars(s_, tg)
                S.op("dve", lambda e: e.tensor_tensor(out=act3[:, t0:t0 + 4, :], in0=PS[gb][:, :].rearrange("p (a b) -> p a b", b=128), in1=act3[:, t0:t0 + 4, :], op=ALU.mult),
                     [PB[gb][0], B_act3], [B_act3])

            def c_phase(s_):
                act3, B_act3 = act3s[s_ % 2]
                vctr = 0
                for i2 in range(128):
                    vctr += 1
                    V_, B_V = Vb[vctr % 8]
                    S.dma(ch_V[vctr % 8], V_, vP_b[i2], reads=[B_vPb], writes=[B_V])
                    for ts in range(2):
                        for dh in range(2):
                            bk = 4 + ts * 2 + dh
                            S.op("pe", lambda e, V_=V_, i2=i2, ts=ts, dh=dh, bk=bk: e.matmul(PS[bk][:, :], lhsT=act3[:, ts * 128:(ts + 1) * 128, i2], rhs=V_[:, dh * 512:(dh + 1) * 512],
                                                                                       start=(i2 == 0), stop=(i2 == 127)), [B_act3, B_V], [PB[bk][0]])

            def d_phase(s_):
                for ts in range(2):
                    gi_ = s_ * 2 + ts
                    S.dma(ch_h1t, h1t, h1tok_s[gi_], reads=[B_h1toks], writes=[B_h1t])
                    for dh in range(2):
                        bk = 4 + ts * 2 + dh
                        S.op("dve", lambda e, bk=bk, dh=dh: e.tensor_tensor(out=ysb[:, dh * 512:(dh + 1) * 512], in0=PS[bk][:, :], in1=h1t[:, dh * 512:(dh + 1) * 512], op=ALU.add),
                             [PB[bk][0], B_h1t], [B_ysb])
                    S.op("pool", lambda e: e.tensor_tensor(out=ob, in0=ysb, in1=ysb, op=ALU.mult), [B_ysb], [B_ob])
                    S.op("dve", lambda e: e.tensor_reduce(out=stat[:, 0:1], in_=ob, axis=AX.X, op=ALU.add), [B_ob], [B_stat])
                    S.op("act", lambda e: e.activation(out=stat[:, 1:2], in_=stat[:, 0:1], func=AF.Sqrt, scale=1.0 / 1024, bias=prm[:, P_EPS6:P_EPS6 + 1]), [B_stat, B_prm], [B_stat])
                    S.op("dve", lambda e: e.reciprocal(out=stat[:, 2:3], in_=stat[:, 1:2]), [B_stat], [B_stat])
                    S.op("dve", lambda e: e.scalar_tensor_tensor(out=ob, in0=ysb, scalar=stat[:, 2:3], in1=gfin, op0=ALU.mult, op1=ALU.mult), [B_ysb, B_stat, B_gfin], [B_ob])
                    o_ = S.dma(ch_out, out_d[gi_ * 128:(gi_ + 1) * 128, :], ob, reads=[B_ob])
                    S.final_waits.append(o_)

            loads(0)
            for i2 in range(128):
                a_iter(0, i2)
            for s_ in range(NS):
                nxt = s_ + 1 < NS
                if nxt:
                    loads(s_ + 1)
                b_stage1(s_, 0)
                b_stage1(s_, 1)
                b_stage2(s_, 0)
                for tg in range(64):
                    if tg + 2 < 64:
                        b_stage1(s_, tg + 2)
                    if tg + 1 < 64:
                        b_stage2(s_, tg + 1)
                    if nxt:
                        a_iter(s_ + 1, 2 * tg)
                        a_iter(s_ + 1, 2 * tg + 1)
                    b_stage3(s_, tg)
                    if tg == 3 and s_ > 0:
                        d_phase(s_ - 1)
                c_phase(s_)
            d_phase(NS - 1)
        phase_E()
        S.barrier()
        S.emit(st)
    return nc, dbg


def host_prep(inp, b, NT):
    f = np.float32
    x = np.asarray(inp["x"])[b]
    nreal = (NT - 1) * 128
    seq = np.concatenate([np.zeros((NPAD, D), f), np.asarray(inp["meta_tokens"], f), x[:nreal]], axis=0)
    xT = np.ascontiguousarray(seq.reshape(NT, 128, 8, 128).transpose(0, 3, 2, 1))
    m = {"xT": xT}
    return m


def shared_prep(inp):
    f = np.float32
    g = lambda k: np.asarray(inp[k], f)
    m = {}
    m["w_in"] = np.ascontiguousarray(g("w_in")[0])
    m["w_conv_out"] = np.ascontiguousarray(g("w_conv_out")[0])
    m["w_rwkv_out"] = np.ascontiguousarray(g("w_rwkv_out")[0])
    m["w_o"] = np.ascontiguousarray(g("w_o")[0])
    m["w_q"] = np.ascontiguousarray(g("w_q")[0])
    m["skT"] = np.ascontiguousarray(g("sub_keys")[0].transpose(3, 0, 1, 2).reshape(128, 16, 128))
    u = g("expert_u")[0]
    m["uT"] = np.ascontiguousarray(u.reshape(128, 128, 8, 128).transpose(1, 3, 2, 0)).reshape(128, 128, 1024)
    v = g("expert_v")[0]
    m["vP"] = np.ascontiguousarray(v.reshape(128, 128, 1024).transpose(1, 0, 2))
    m["wa_up"] = np.ascontiguousarray(np.concatenate([g("w_up")[0], g("a_up")[0]], axis=0))
    m["g_up"] = np.ascontiguousarray(g("g_up")[0])
    m["w0row"] = np.ascontiguousarray(g("w0")[0].reshape(1, 512))
    m["gfin"] = np.ascontiguousarray(g("g_final").reshape(1, 1024))
    prm = np.zeros((128, NPRM), f)
    col = lambda a, n: np.asarray(a, f).reshape(n, 128).T
    prm[:, P_GMIX:P_GMIX + 8] = col(g("g_mix")[0], 8)
    prm[:, P_MU:P_MU + 14] = col(g("mu_shift")[0], 14)
    prm[:, P_A0:P_A0 + 4] = col(g("a0")[0], 4)
    prm[:, P_KK:P_KK + 4] = col(g("k_k")[0], 4)
    prm[:, P_KA:P_KA + 4] = col(g("k_a")[0], 4)
    prm[:, P_RK:P_RK + 4] = col(g("r_k")[0].reshape(512), 4)
    prm[:, P_GNG:P_GNG + 4] = col(g("gn_g")[0], 4)
    prm[:, P_GNB:P_GNB + 4] = col(g("gn_b")[0], 4)
    prm[:, P_CB:P_CB + 4] = col(g("conv_b")[0], 4)
    prm[:, P_LNG:P_LNG + 4] = col(g("conv_ln_g")[0], 4)
    prm[:, P_LNB:P_LNB + 4] = col(g("conv_ln_b")[0], 4)
    prm[:, P_GFFN:P_GFFN + 8] = col(g("g_ffn")[0], 8)
    prm[:, P_EPS6] = 1e-6
    prm[:, P_EPS5] = 1e-5
    prm[:, P_EPSG] = 64e-5
    cw = g("conv_w")[0]
    prm[:, P_CW:P_CW + 124] = cw.reshape(31, 4, 128).transpose(2, 1, 0).reshape(128, 124)
    m["params"] = prm
    cst = np.zeros((128, NCST, 128), f)
    ar = np.arange(128)
    cst[:, C_IDENT] = np.eye(128)
    cst[:, C_ONES] = 1.0
    cst[:, C_O512] = 1.0 / 512
    cst[:, C_BLK] = (ar[:, None] // 64 == ar[None, :] // 64)
    e05 = float(np.exp(np.float32(-0.5)))
    cst[:, C_TRII] = -e05 * (ar[:, None] <= ar[None, :])
    cst[:, C_TRIE] = -e05 * (ar[:, None] < ar[None, :])
    cst[:, C_MS] = (ar[:, None] < ar[None, :])
    cst[:, C_MSN] = -1.0 * (ar[:, None] < ar[None, :])
    cst[:, C_MLN] = -1.0 * (ar[None, :] < ar[:, None])
    cst[:, C_MI] = (ar[:, None] <= ar[None, :])
    cst[:, C_IOTA] = ar[None, :]
    cst[:, C_O1024] = 1.0 / 1024
    m["cst"] = cst
    return m


_CACHE = {}


def kernel(**inputs):
    NT = 33
    if "nc" not in _CACHE:
        _CACHE["nc"] = build_program(NT)[0]
    nc = _CACHE["nc"]
    sh = shared_prep(inputs)
    in_maps = []
    for b in range(8):
        m = dict(sh)
        m.update(host_prep(inputs, b, NT))
        in_maps.append(m)
    res = run_bass_kernel_spmd(nc, in_maps, core_ids=list(range(8)))
    return np.stack([r["out"] for r in res.results], axis=0)
```

```python
import numpy as np
import concourse.bass as bass
import concourse.mybir as mybir
from concourse.bass_utils import run_bass_kernel_spmd
from contextlib import ExitStack

F32 = mybir.dt.float32
BF16 = mybir.dt.bfloat16
U32 = mybir.dt.uint32
ALU = mybir.AluOpType
AF = mybir.ActivationFunctionType
AX = mybir.AxisListType


class Buf:
    __slots__ = ("name", "w", "r", "excl")

    def __init__(self, name, excl=False):
        self.name = name
        self.w = None
        self.r = []
        self.excl = excl


class Op:
    __slots__ = ("eng", "fn", "deps", "signal", "is_dma", "chan", "sem", "val")

    def __init__(self, eng, fn, is_dma=False, chan=None):
        self.eng = eng
        self.fn = fn
        self.deps = []
        self.signal = False
        self.is_dma = is_dma
        self.chan = chan
        self.sem = None
        self.val = 0


class Chan:
    __slots__ = ("name", "last", "count", "sem")

    def __init__(self, name):
        self.name = name
        self.last = None
        self.count = 0
        self.sem = None


EPOCH = 30000
CAST = True
A0_IN_Q = True
ENGS = ("pe", "dve", "act", "pool", "sp")


class Sched:
    def __init__(self, nc):
        self.nc = nc
        self.ops = {e: [] for e in ENGS}
        self.chans = []
        self.final_waits = []

    def chan(self, name):
        c = Chan(name)
        self.chans.append(c)
        return c

    def _record(self, op, reads, writes):
        writes = writes + [b for b in reads if b.excl and not any(b is x for x in writes)]
        deps = []
        for b in reads:
            if b.w is not None:
                deps.append(b.w)
        for b in writes:
            if b.w is not None:
                deps.append(b.w)
            for r in b.r:
                if r.eng == op.eng and not r.is_dma and not op.is_dma:
                    continue
                deps.append(r)
        seen = set(id(d) for d in op.deps)
        for d in deps:
            if d is op or id(d) in seen:
                continue
            if d.eng == "pe" and op.eng == "pe" and not d.is_dma and not op.is_dma:
                continue
            seen.add(id(d))
            op.deps.append(d)
            d.signal = True
        for b in reads:
            b.r.append(op)
        for b in writes:
            b.w = op
            b.r = []
        self.ops[op.eng].append(op)
        return op

    def op(self, eng, fn, reads=(), writes=()):
        return self._record(Op(eng, fn), list(reads), list(writes))

    def dma(self, chan, out, in_, reads=(), writes=(), eng="sp", **kw):
        op = Op(eng, lambda e: e.dma_start(out=out, in_=in_, **kw), is_dma=True, chan=chan)
        if chan.last is not None:
            op.deps.append(chan.last)
            chan.last.signal = True
        chan.last = op
        op.signal = True
        return self._record(op, list(reads), list(writes))

    def barrier(self):
        lasts = []
        for e in ENGS:
            for o in reversed(self.ops[e]):
                if not o.is_dma and o.fn is not None:
                    lasts.append(o)
                    break
        for c in self.chans:
            if c.last is not None:
                lasts.append(c.last)
        for e in ENGS:
            op = Op(e, None)
            for d in lasts:
                if d.eng == e and not d.is_dma:
                    continue
                op.deps.append(d)
                d.signal = True
            self.ops[e].append(op)

    def emit(self, stack):
        nc = self.nc
        for c in self.chans:
            c.sem = stack.enter_context(nc.semaphore("c_" + c.name))
        for d in self.final_waits:
            d.signal = True
        for e, lst in self.ops.items():
            cur = None
            cnt = 0
            k = 0
            for op in lst:
                if op.is_dma:
                    op.chan.count += 16
                    op.sem = op.chan.sem
                    op.val = op.chan.count
                elif op.signal:
                    if cur is None or cnt >= EPOCH:
                        cur = stack.enter_context(nc.semaphore("e_%s_%d" % (e, k)))
                        k += 1
                        cnt = 0
                    cnt += 1
                    op.sem = cur
                    op.val = cnt
        block = stack.enter_context(nc.Block())

        def run(engine_obj, lst, tail=None):
            waited = {}

            def w(d):
                key = d.sem.name
                if waited.get(key, 0) >= d.val:
                    return
                engine_obj.wait_ge(d.sem, d.val)
                waited[key] = d.val

            for op in lst:
                for d in op.deps:
                    w(d)
                if op.fn is None:
                    continue
                ins = op.fn(engine_obj)
                if op.is_dma:
                    ins.then_inc(op.sem, 16)
                elif op.signal:
                    ins.then_inc(op.sem, 1)
            if tail:
                for d in tail:
                    w(d)

        ops = self.ops
        fw = self.final_waits

        @block.tensor
        def _(e):
            run(e, ops["pe"])

        @block.vector
        def _(e):
            run(e, ops["dve"])

        @block.scalar
        def _(e):
            run(e, ops["act"])

        @block.gpsimd
        def _(e):
            run(e, ops["pool"])

        @block.sync
        def _(e):
            run(e, ops["sp"], tail=fw)


D = 1024
NPAD = 112
C_IDENT, C_ONES, C_O512, C_BLK, C_TRII, C_TRIE, C_MS, C_MSN, C_MLN, C_MI, C_IOTA, C_O1024 = range(12)
NCST = 12
P_GMIX = 0
P_MU = 8
P_A0 = 22
P_KK = 26
P_KA = 30
P_RK = 34
P_GNG = 38
P_GNB = 42
P_CB = 46
P_LNG = 50
P_LNB = 54
P_GFFN = 58
P_EPS6 = 66
P_EPS5 = 67
P_EPSG = 68
P_CW = 69
NPRM = 69 + 124


def build_program(NT, debug=False):
    NRT = NT - 1
    assert NRT % 2 == 0
    nc = bass.Bass("TRN2", target_bir_lowering=False)
    din = lambda n, s, dt=F32: nc.dram_tensor(n, s, dt, kind="ExternalInput").ap()
    xT_d = din("xT", [NT, 128, 8, 128])
    w_in_d = din("w_in", [1024, 4864])
    wco_d = din("w_conv_out", [512, 1024])
    wro_d = din("w_rwkv_out", [512, 1024])
    wo_d = din("w_o", [1024, 1024])
    wq_d = din("w_q", [1024, 2048])
    skT_d = din("skT", [128, 16, 128])
    uT_d = din("uT", [128, 128, 8 * 128])
    vP_d = din("vP", [128, 128, 1024])
    waup_d = din("wa_up", [128, 512])
    gup_d = din("g_up", [128, 512])
    prm_d = din("params", [128, NPRM])
    w0_d = din("w0row", [1, 512])
    cst_d = din("cst", [128, NCST, 128])
    gfin_d = din("gfin", [1, 1024])
    out_d = nc.dram_tensor("out", [NRT * 128, 1024], F32, kind="ExternalOutput").ap()
    dscr = lambda n, s, dt: nc.dram_tensor(n, s, dt, kind="Internal").ap()
    orw_s = dscr("orw_s", [NT, 128, 4, 128], BF16)
    h1tok_s = dscr("h1tok_s", [NRT, 128, 1024], F32)
    h1nT_s = dscr("h1nT_s", [NRT, 128, 8, 128], BF16)
    sel_s = dscr("sel_s", [NRT, 128, 3, 128], F32)
    uT_b = dscr("uT_b", [128, 128, 8 * 128], BF16)
    vP_b = dscr("vP_b", [128, 128, 1024], BF16)
    dbg = {}

    st = ExitStack()
    with st:
        S = Sched(nc)
        ARENA = 53000
        arena = st.enter_context(nc.sbuf_tensor("arena", [128, ARENA], F32))
        PS = [st.enter_context(nc.psum_tensor("ps%d" % k, [128, 512], F32)) for k in range(8)]
        PB = []
        for k in range(8):
            _b = Buf("ps%d" % k, excl=True)
            PB.append([_b, _b, _b, _b])
        top = [0]

        def alloc(name, free, dt=F32):
            n = int(np.prod(free))
            words = n if dt in (F32, U32) else (n + 1) // 2
            words = (words + 7) // 8 * 8
            a = arena[:, top[0]:top[0] + words]
            top[0] += words
            assert top[0] <= ARENA, (name, top[0])
            if dt != F32:
                a = a.bitcast(dt)
            a = a[:, 0:n]
            if len(free) == 2:
                a = a.rearrange("p (a b) -> p a b", b=free[1])
            elif len(free) == 3:
                a = a.rearrange("p (a b c) -> p a b c", b=free[1], c=free[2])
            return a, Buf(name)

        def pq(k, q):
            return PS[k][:, q * 128:(q + 1) * 128]

        def tap(name, ap, buf, shape, dt=F32):
            if not debug:
                return
            d = nc.dram_tensor("dbg_" + name, list(shape), dt, kind="ExternalOutput").ap()
            c = S.chan("dbg_" + name)
            o = S.dma(c, d, ap, reads=[buf])
            S.final_waits.append(o)
            dbg[name] = (shape, dt)

        cst, B_cst = alloc("cst", [NCST, 128])
        prm, B_prm = alloc("prm", [NPRM])
        ones_b, B_onesb = alloc("ones_b", [128], BF16)
        ch_c = S.chan("cst")
        S.dma(ch_c, cst, cst_d, writes=[B_cst])
        ch_p = S.chan("prm")
        S.dma(ch_p, prm, prm_d, writes=[B_prm])
        S.op("dve", lambda e: e.tensor_copy(out=ones_b, in_=cst[:, C_ONES, :]), [B_cst], [B_onesb])
        ident = cst[:, C_IDENT, :]
        base_top = top[0]

        ch_cast = [S.chan("cast%d" % k) for k in range(4)]
        B_uTb = Buf("uTb")
        B_vPb = Buf("vPb")
        cast_ops = []
        def issue_cast(k):
            if not CAST:
                return
            sl = slice(k * 8, (k + 1) * 8)
            cast_ops.append(S.dma(ch_cast[k % 2], uT_b[sl], uT_d[sl], eng="pool"))
            cast_ops.append(S.dma(ch_cast[2 + k % 2], vP_b[sl], vP_d[sl], eng="pool"))

        def load_x(i, xt, B_xt, ch, xb, B_xb, sq, B_sq, rs, B_rs, psq):
            S.dma(ch, xt, xT_d[i], writes=[B_xt])
            S.op("pool", lambda e: e.tensor_tensor(out=xb, in0=xt, in1=prm[:, P_GMIX:P_GMIX + 8].unsqueeze(2).to_broadcast([128, 8, 128]), op=ALU.mult),
                 [B_xt, B_prm], [B_xb])
            S.op("act", lambda e: e.activation(out=sq, in_=xt, func=AF.Square), [B_xt], [B_sq])
            k, q = psq
            for dc in range(8):
                S.op("pe", lambda e, dc=dc: e.matmul(pq(k, q), lhsT=ones_b, rhs=sq[:, dc, :], start=(dc == 0), stop=(dc == 7)),
                     [B_onesb, B_sq], [PB[k][q]])
            S.op("act", lambda e: e.activation(out=rs, in_=pq(k, q), func=AF.Sqrt, scale=1.0 / 1024, bias=prm[:, P_EPS6:P_EPS6 + 1]),
                 [PB[k][q], B_prm], [B_rs])
            S.op("dve", lambda e: e.reciprocal(out=rs, in_=rs), [B_rs], [B_rs])

        B_orws = Buf("orws")
        ch_orw = S.chan("orw")

        def phase_R():
            wR, B_wR = alloc("wR", [8, 1792], BF16)
            waup, B_waup = alloc("waup", [512])
            gup, B_gup = alloc("gup", [512])
            w0r, B_w0r = alloc("w0r", [512])
            chw = S.chan("wR")
            S.dma(chw, wR, w_in_d[:, 1024:2816].rearrange("(c p) n -> p c n", p=128), writes=[B_wR], eng="pool")
            S.dma(S.chan("waup"), waup, waup_d, writes=[B_waup])
            S.dma(S.chan("gup"), gup, gup_d, writes=[B_gup])
            S.dma(S.chan("w0r"), w0r[0:1, :], w0_d, writes=[B_w0r])
            xts = [alloc("xt0", [8, 128])] * 2
            chx = [S.chan("x%d" % k) for k in range(2)]
            xb, B_xb = alloc("xb", [8, 128], BF16)
            sq, B_sq = alloc("sq", [8, 128], BF16)
            rs, B_rs = alloc("rs", [128])
            zr, B_zr = alloc("zr", [14, 129])
            zs, B_zs = alloc("zs", [14, 128])
            tl, B_tl = alloc("tl", [128])
            sgt, B_sgt = alloc("sgt", [512])
            cexc, B_cexc = alloc("cexc", [4, 128])
            cinv, B_cinv = alloc("cinv", [4, 128])
            aa, B_aa = alloc("aa", [4, 128])
            sgl, B_sgl = alloc("sgl", [128])
            kk, B_kk = alloc("kk", [4, 128])
            tmp, B_tmp = alloc("tmp", [4, 128])
            k2, B_k2 = alloc("k2", [4, 128])
            FS = [{n_: alloc("%s_%d" % (n_, k_), shp_) for n_, shp_ in (("rt", [4, 128]), ("kkt", [4, 128]), ("kt", [4, 128]), ("bt", [4, 128]), ("bon", [4, 128]), ("gT", [4, 128]), ("vtok", [512]), ("ktok", [512]), ("btok", [512]), ("cinc", [4, 128]))} for k_ in range(3)]
            tmp2, B_tmp2 = alloc("tmp2", [4, 128])
            osq, B_osq = tmp2.rearrange("p a b -> p (a b)"), B_tmp2
            ST, _ = alloc("ST", [4, 64])
            osb, B_osb = alloc("osb", [512])
            gst, B_gst = alloc("gst", [32])
            orw, B_orw = alloc("orw", [4, 128], BF16)
            KAS = {}
            for nm in ("Xa", "XTa", "Xb", "XTb", "Tb"):
                ap_, _ = alloc(nm + "8", [8, 128])
                KAS[nm] = (ap_, [Buf(nm + "_e"), Buf(nm + "_o")])
            KAD = []
            for k_ in range(2):
                d_ = {}
                for nm in ("Mk", "Ak", "Ab", "Ta"):
                    ap_, _ = alloc("%s8_%d" % (nm, k_), [8, 128])
                    d_[nm] = (ap_, [Buf("%s_e%d" % (nm, k_)), Buf("%s_o%d" % (nm, k_))])
                KAD.append(d_)
            XTs8, B_XTs8 = alloc("XTs8", [8, 64])
            NTs8, B_NTs8 = alloc("NTs8", [8, 64])
            tmpS, B_tmpS = alloc("tmpS", [4, 64])
            B_STp = [Buf("STe"), Buf("STo")]
            S.op("dve", lambda e: e.memset(zr, 0.0), [], [B_zr])
            S.op("dve", lambda e: e.memset(ST, 0.0), [], B_STp)
            slot_ctr = [0]

            def nslot():
                s_ = slot_ctr[0] % 3
                slot_ctr[0] += 1
                return 2 + s_

            def front(i):
                rt, B_rt = FS[i % 3]["rt"]
                kkt, B_kkt = FS[i % 3]["kkt"]
                kt, B_kt = FS[i % 3]["kt"]
                bt, B_bt = FS[i % 3]["bt"]
                bon, B_bon = FS[i % 3]["bon"]
                gT, B_gT = FS[i % 3]["gT"]
                vtok, B_vtok = FS[i % 3]["vtok"]
                ktok, B_ktok = FS[i % 3]["ktok"]
                btok, B_btok = FS[i % 3]["btok"]
                cinc, B_cinc = FS[i % 3]["cinc"]
                xt, B_xt = xts[i % 2]
                load_x(i, xt, B_xt, chx[i % 2], xb, B_xb, sq, B_sq, rs, B_rs, (1, 0))
                yield
                for gi, (j0, nj) in enumerate(((0, 4), (4, 4), (8, 4), (12, 2))):
                    for jj in range(nj):
                        j = j0 + jj
                        for dc in range(8):
                            S.op("pe", lambda e, j=j, jj=jj, dc=dc, gi=gi: e.matmul(pq(gi % 2, jj), lhsT=wR[:, dc, j * 128:(j + 1) * 128], rhs=xb[:, dc, :], start=(dc == 0), stop=(dc == 7)),
                                 [B_wR, B_xb], [PB[gi % 2][jj]])
                    S.op("dve", lambda e, gi=gi, j0=j0, nj=nj: e.tensor_tensor(out=zr[:, j0:j0 + nj, 1:129], in0=PS[gi % 2][:, 0:nj * 128].rearrange("p (a b) -> p a b", b=128),
                                                                           in1=rs.unsqueeze(1).to_broadcast([128, nj, 128]), op=ALU.mult),
                         PB[gi % 2][0:nj] + [B_rs], [B_zr])
                yield
                mu_bc = prm[:, P_MU:P_MU + 14].unsqueeze(2).to_broadcast([128, 14, 128])
                S.op("pool", lambda e: e.tensor_tensor(out=zs, in0=zr[:, :, 0:128], in1=zr[:, :, 1:129], op=ALU.subtract), [B_zr], [B_zs])
                S.op("pool", lambda e: e.tensor_tensor(out=zs, in0=zs, in1=mu_bc, op=ALU.mult), [B_zs, B_prm], [B_zs])
                S.op("pool", lambda e: e.tensor_tensor(out=zs, in0=zs, in1=zr[:, :, 1:129], op=ALU.add), [B_zs, B_zr], [B_zs])
                S.op("pool", lambda e: e.tensor_copy(out=zr[:, :, 0:1], in_=zr[:, :, 128:129]), [B_zr, B_zs], [B_zr])
                if i == 1:
                    tap("zs", zs, B_zs, [128, 14, 128])
                r_ = zs[:, 0:4, :]
                k_ = zs[:, 4:8, :]
                v_ = zs[:, 8:12, :]
                yield
                S.op("act", lambda e: e.activation(out=tl[0:64, :], in_=zs[0:64, 12, :], func=AF.Tanh), [B_zs], [B_tl])
                S.op("pe", lambda e: e.matmul(PS[0][:, :], lhsT=tl[0:64, :], rhs=waup[0:64, :], start=True, stop=False), [B_tl, B_waup], PB[0])
                S.op("pe", lambda e: e.matmul(PS[0][:, :], lhsT=cst[0:1, C_ONES, :], rhs=w0r[0:1, :], start=False, stop=True), [B_cst, B_w0r], PB[0])
                S.op("act", lambda e: e.activation(out=sgt, in_=PS[0][:, :], func=AF.Sigmoid), PB[0], [B_sgt])
                for cc in range(4):
                    S.op("pe", lambda e, cc=cc: e.matmul(pq(1, cc), lhsT=sgt[:, cc * 128:(cc + 1) * 128], rhs=cst[:, C_TRII, :], start=True, stop=True), [B_sgt, B_cst], [PB[1][cc]])
                    S.op("pe", lambda e, cc=cc: e.matmul(pq(0, cc), lhsT=sgt[:, cc * 128:(cc + 1) * 128], rhs=cst[:, C_TRIE, :], start=True, stop=True), [B_sgt, B_cst], [PB[0][cc]])
                S.op("act", lambda e: e.activation(out=cinc.rearrange("p a b -> p (a b)"), in_=PS[1][:, :], func=AF.Exp), PB[1], [B_cinc])
                S.op("act", lambda e: e.activation(out=cinv.rearrange("p a b -> p (a b)"), in_=PS[1][:, :], func=AF.Exp, scale=-1.0), PB[1], [B_cinv])
                S.op("act", lambda e: e.activation(out=cexc.rearrange("p a b -> p (a b)"), in_=PS[0][:, :], func=AF.Exp), PB[0], [B_cexc])
                yield
                for cc in range(4):
                    S.op("pe", lambda e, cc=cc: e.matmul(pq(1, cc), lhsT=waup[64:128, cc * 128:(cc + 1) * 128], rhs=zs[64:128, 12, :], start=True, stop=True), [B_waup, B_zs], [PB[1][cc]])
                    S.op("act", lambda e, cc=cc: e.activation(out=aa[:, cc, :], in_=pq(1, cc), func=AF.Sigmoid, bias=prm[:, P_A0 + cc:P_A0 + cc + 1]), [PB[1][cc], B_prm], [B_aa])
                S.op("act", lambda e: e.activation(out=sgl, in_=zs[:, 13, :], func=AF.Sigmoid), [B_zs], [B_sgl])
                for cc in range(4):
                    S.op("pe", lambda e, cc=cc: e.matmul(pq(0, cc), lhsT=gup[:, cc * 128:(cc + 1) * 128], rhs=sgl, start=True, stop=True), [B_gup, B_sgl], [PB[0][cc]])
                S.op("act", lambda e: e.copy(out=gT.rearrange("p a b -> p (a b)"), in_=PS[0][:, :]), PB[0], [B_gT])
                yield
                bc4 = lambda c0: prm[:, c0:c0 + 4].unsqueeze(2).to_broadcast([128, 4, 128])
                S.op("dve", lambda e: e.tensor_tensor(out=kk, in0=k_, in1=bc4(P_KK), op=ALU.mult), [B_zs, B_prm], [B_kk])
                S.op("pool", lambda e: e.tensor_tensor(out=tmp, in0=kk, in1=kk, op=ALU.mult), [B_kk], [B_tmp])
                for cc in range(4):
                    S.op("pe", lambda e, cc=cc: e.matmul(pq(1, cc), lhsT=cst[:, C_BLK, :], rhs=tmp[:, cc, :], start=True, stop=True), [B_cst, B_tmp], [PB[1][cc]])
                S.op("dve", lambda e: e.tensor_scalar(out=tmp.rearrange("p a b -> p (a b)"), in0=PS[1][:, :], scalar1=1e-24, scalar2=None, op0=ALU.max), PB[1], [B_tmp])
                S.op("act", lambda e: e.activation(out=tmp, in_=tmp, func=AF.Sqrt), [B_tmp], [B_tmp])
                S.op("dve", lambda e: e.reciprocal(out=tmp, in_=tmp), [B_tmp], [B_tmp])
                S.op("dve", lambda e: e.tensor_tensor(out=kk, in0=kk, in1=tmp, op=ALU.mult), [B_kk, B_tmp], [B_kk])
                S.op("pool", lambda e: e.tensor_scalar(out=k2, in0=aa, scalar1=-1.0, scalar2=None, op0=ALU.add), [B_aa], [B_k2])
                S.op("pool", lambda e: e.tensor_tensor(out=k2, in0=k2, in1=bc4(P_KA), op=ALU.mult), [B_k2, B_prm], [B_k2])
                S.op("dve", lambda e: e.scalar_tensor_tensor(out=k2, in0=k2, scalar=1.0, in1=k_, op0=ALU.add, op1=ALU.mult), [B_k2, B_zs], [B_k2])
                yield
                S.op("dve", lambda e: e.tensor_tensor(out=kkt, in0=kk, in1=cexc, op=ALU.mult), [B_kk, B_cexc], [B_kkt])
                S.op("dve", lambda e: e.tensor_tensor(out=bt, in0=kk, in1=aa, op=ALU.mult), [B_kk, B_aa], [B_bt])
                S.op("dve", lambda e: e.tensor_tensor(out=bt, in0=bt, in1=cinv, op=ALU.mult), [B_bt, B_cinv], [B_bt])
                S.op("pool", lambda e: e.tensor_tensor(out=kt, in0=k2, in1=cinv, op=ALU.mult), [B_k2, B_cinv], [B_kt])
                S.op("pool", lambda e: e.tensor_tensor(out=rt, in0=r_, in1=cinc, op=ALU.mult), [B_zs, B_cinc], [B_rt])
                S.op("pool", lambda e: e.tensor_tensor(out=tmp, in0=r_, in1=k2, op=ALU.mult), [B_zs, B_k2, B_kk], [B_tmp])
                S.op("pool", lambda e: e.tensor_tensor(out=tmp, in0=tmp, in1=bc4(P_RK), op=ALU.mult), [B_tmp, B_prm], [B_tmp])
                for cc in range(4):
                    S.op("pe", lambda e, cc=cc: e.matmul(pq(0, cc), lhsT=cst[:, C_BLK, :], rhs=tmp[:, cc, :], start=True, stop=True), [B_cst, B_tmp], [PB[0][cc]])
                S.op("dve", lambda e: e.tensor_tensor(out=bon, in0=PS[0][:, :].rearrange("p (a b) -> p a b", b=128), in1=v_, op=ALU.mult), PB[0] + [B_zs], [B_bon])
                yield
                for (src, Bsrc, dst, Bdst, bank) in ((v_, B_zs, vtok, B_vtok, 0), (kt, B_kt, ktok, B_ktok, 1), (bt, B_bt, btok, B_btok, 0)):
                    for cc in range(4):
                        S.op("pe", lambda e, src=src, cc=cc, bank=bank: e.transpose(pq(bank, cc), src[:, cc, :], ident), [Bsrc, B_cst], [PB[bank][cc]])
                    S.op("act", lambda e, dst=dst, bank=bank: e.copy(out=dst, in_=PS[bank][:, :]), PB[bank], [Bdst])
                if i == 1:
                    tap("kkt", kkt, B_kkt, [128, 4, 128])
                    tap("rt", rt, B_rt, [128, 4, 128])
                    tap("vtok", vtok, B_vtok, [128, 512])
                yield

            def hv(ap3, par, cc):
                return ap3[64 * par:64 * par + 64, cc, :]

            def neu(i):
                rt, B_rt = FS[i % 3]["rt"]
                kkt, B_kkt = FS[i % 3]["kkt"]
                kt, B_kt = FS[i % 3]["kt"]
                bt, B_bt = FS[i % 3]["bt"]
                bon, B_bon = FS[i % 3]["bon"]
                gT, B_gT = FS[i % 3]["gT"]
                vtok, B_vtok = FS[i % 3]["vtok"]
                ktok, B_ktok = FS[i % 3]["ktok"]
                btok, B_btok = FS[i % 3]["btok"]
                cinc, B_cinc = FS[i % 3]["cinc"]
                KA = dict(KAS)
                KA.update(KAD[i % 2])
                def hv(ap3, par, cc):
                    return ap3[64 * par:64 * par + 64, cc, :]

                def mm_mask(lsrc, Bl, rsrc, Br, mask_idx, kind, eng="dve"):
                    dst, Bd = KA[kind]
                    banks = [nslot(), nslot()]
                    for cc in range(4):
                        for par in range(2):
                            bank = banks[par]
                            S.op("pe", lambda e, par=par, cc=cc, bank=bank: e.matmul(pq(bank, cc), lhsT=hv(lsrc, par, cc), rhs=hv(rsrc, par, cc), start=True, stop=True), [Bl, Br], [PB[bank][0]])
                    for par in range(2):
                        bank = banks[par]
                        S.op(eng, lambda e, par=par, bank=bank: e.tensor_tensor(out=dst[:, par * 4:par * 4 + 4, :], in0=PS[bank][:, :].rearrange("p (a b) -> p a b", b=128),
                                                                           in1=cst[:, mask_idx, :].unsqueeze(1).to_broadcast([128, 4, 128]), op=ALU.mult), [PB[bank][0], B_cst], [Bd[par]])

                mm_mask(bt, B_bt, kkt, B_kkt, C_MSN, "Xa")
                yield
                mm_mask(kkt, B_kkt, bt, B_bt, C_MLN, "XTa")
                yield
                mm_mask(kt, B_kt, kkt, B_kkt, C_MS, "Mk")
                yield
                mm_mask(kt, B_kt, rt, B_rt, C_MI, "Ak")
                yield
                mm_mask(bt, B_bt, rt, B_rt, C_MI, "Ab")
                yield
                for par in range(2):
                    S.op("dve", lambda e, par=par: e.tensor_tensor(out=KA["Ta"][0][:, par * 4:par * 4 + 4, :], in0=KA["Xa"][0][:, par * 4:par * 4 + 4, :],
                                                                in1=ident.unsqueeze(1).to_broadcast([128, 4, 128]), op=ALU.add), [KA["Xa"][1][par], B_cst], [KA["Ta"][1][par]])
                X, XT, T = "Xa", "XTa", "Ta"
                Xn, XTn, Tn = "Xb", "XTb", "Tb"

                def lvl_mm(lk, rk, outk, eng, addk=None):
                    la, lB = KA[lk]
                    ra, rB = KA[rk]
                    oa, oB = KA[outk]
                    for par in range(2):
                        bank = nslot()
                        for cc in range(4):
                            hi = par * 4 + cc
                            S.op("pe", lambda e, hi=hi, cc=cc, bank=bank: e.matmul(pq(bank, cc), lhsT=la[:, hi, :], rhs=ra[:, hi, :], start=True, stop=True), [lB[par], rB[par]], [PB[bank][0]])
                        if addk is None:
                            S.op(eng, lambda e, par=par, bank=bank: e.copy(out=oa[:, par * 4:par * 4 + 4, :], in_=PS[bank][:, :].rearrange("p (a b) -> p a b", b=128)), [PB[bank][0]], [oB[par]])
                        else:
                            aa_, aB = KA[addk]
                            S.op(eng, lambda e, par=par, bank=bank: e.tensor_tensor(out=oa[:, par * 4:par * 4 + 4, :], in0=PS[bank][:, :].rearrange("p (a b) -> p a b", b=128),
                                                                               in1=aa_[:, par * 4:par * 4 + 4, :], op=ALU.add), [PB[bank][0], aB[par]], [oB[par]])

                for l in range(6):
                    if l < 5:
                        lvl_mm(XT, X, Xn, "act")
                    lvl_mm(X, XT, XTn, "act")
                    yield
                    lvl_mm(XTn, T, Tn, "dve", addk=T)
                    yield
                    X, Xn = Xn, X
                    XT, XTn = XTn, XT
                    T, Tn = Tn, T
                yield
                assert T == "Ta"
                yield

            def stt(i):
                rt, B_rt = FS[i % 3]["rt"]
                kkt, B_kkt = FS[i % 3]["kkt"]
                kt, B_kt = FS[i % 3]["kt"]
                bt, B_bt = FS[i % 3]["bt"]
                bon, B_bon = FS[i % 3]["bon"]
                gT, B_gT = FS[i % 3]["gT"]
                vtok, B_vtok = FS[i % 3]["vtok"]
                ktok, B_ktok = FS[i % 3]["ktok"]
                btok, B_btok = FS[i % 3]["btok"]
                cinc, B_cinc = FS[i % 3]["cinc"]
                KA = KAD[i % 2]
                Tinv, B_Tinv = KA["Ta"]
                Mk8, B_Mk8 = KA["Mk"]
                Ak8, B_Ak8 = KA["Ak"]
                Ab8, B_Ab8 = KA["Ab"]
                heads = [(par, cc) for par in range(2) for cc in range(4)]
                for (par, cc) in heads:
                    hi = par * 4 + cc
                    vt_h = vtok[:, cc * 128 + 64 * par: cc * 128 + 64 * par + 64]
                    S.op("pe", lambda e, par=par, cc=cc, hi=hi: e.matmul(PS[6][:, hi * 64:(hi + 1) * 64], lhsT=hv(kkt, par, cc), rhs=ST[64 * par:64 * par + 64, cc, :], start=True, stop=False),
                         [B_kkt, B_STp[par]], [PB[6][0]])
                    S.op("pe", lambda e, hi=hi, vt_h=vt_h: e.matmul(PS[6][:, hi * 64:(hi + 1) * 64], lhsT=Mk8[:, hi, :], rhs=vt_h, start=False, stop=True), [B_Mk8[par], B_vtok], [PB[6][0]])
                S.op("act", lambda e: e.mul(XTs8.rearrange("p a b -> p (a b)"), PS[6][:, :], -1.0), [PB[6][0]], [B_XTs8])
                yield
                for (par, cc) in heads:
                    hi = par * 4 + cc
                    S.op("pe", lambda e, hi=hi: e.matmul(PS[7][:, hi * 64:(hi + 1) * 64], lhsT=Tinv[:, hi, :], rhs=XTs8[:, hi, :], start=True, stop=True), [B_Tinv[par], B_XTs8], [PB[7][0]])
                S.op("act", lambda e: e.copy(out=NTs8.rearrange("p a b -> p (a b)"), in_=PS[7][:, :]), [PB[7][0]], [B_NTs8])
                yield
                for (par, cc) in heads:
                    hi = par * 4 + cc
                    h = 2 * cc + par
                    vt_h = vtok[:, cc * 128 + 64 * par: cc * 128 + 64 * par + 64]
                    osl = PS[5][:, h * 64:(h + 1) * 64]
                    S.op("pe", lambda e, par=par, cc=cc, osl=osl: e.matmul(osl, lhsT=hv(rt, par, cc), rhs=ST[64 * par:64 * par + 64, cc, :], start=True, stop=False), [B_rt, B_STp[par]], [PB[5][0]])
                    S.op("pe", lambda e, hi=hi, osl=osl: e.matmul(osl, lhsT=Ab8[:, hi, :], rhs=NTs8[:, hi, :], start=False, stop=False), [B_Ab8[par], B_NTs8], [PB[5][0]])
                    S.op("pe", lambda e, hi=hi, osl=osl, vt_h=vt_h: e.matmul(osl, lhsT=Ak8[:, hi, :], rhs=vt_h, start=False, stop=True), [B_Ak8[par], B_vtok], [PB[5][0]])
                yield
                for (par, cc) in heads:
                    hi = par * 4 + cc
                    vt_h = vtok[:, cc * 128 + 64 * par: cc * 128 + 64 * par + 64]
                    S.op("pe", lambda e, hi=hi, cc=cc: e.matmul(PS[6][:, hi * 64:(hi + 1) * 64], lhsT=btok[:, cc * 128:(cc + 1) * 128], rhs=NTs8[:, hi, :], start=True, stop=False), [B_btok, B_NTs8], [PB[6][0]])
                    S.op("pe", lambda e, hi=hi, cc=cc, vt_h=vt_h: e.matmul(PS[6][:, hi * 64:(hi + 1) * 64], lhsT=ktok[:, cc * 128:(cc + 1) * 128], rhs=vt_h, start=False, stop=True), [B_ktok, B_vtok], [PB[6][0]])
                for par in range(2):
                    sl = slice(64 * par, 64 * par + 64)
                    S.op("dve", lambda e, par=par, sl=sl: e.tensor_tensor(out=tmpS[sl, :, :], in0=PS[6][sl, par * 256:(par + 1) * 256].rearrange("p (c v) -> p c v", v=64), in1=ST[sl, :, :], op=ALU.add),
                         [PB[6][0], B_STp[par]], [B_tmpS])
                    S.op("dve", lambda e, par=par, sl=sl: e.tensor_tensor(out=ST[sl, :, :], in0=tmpS[sl, :, :], in1=cinc[sl, :, 127:128].to_broadcast([64, 4, 64]), op=ALU.mult),
                         [B_tmpS, B_cinc], [B_STp[par]])
                if i == 0:
                    return
                yield
                S.op("act", lambda e: e.copy(out=osb, in_=PS[5][:, :]), PB[5], [B_osb])
                S.op("act", lambda e: e.activation(out=osq, in_=PS[5][:, :], func=AF.Square), PB[5], [B_osq])
                if i == 1:
                    tap("osb", osb, B_osb, [128, 512])
                o3 = osb.rearrange("p (h v) -> p h v", v=64)
                S.op("dve", lambda e: e.tensor_reduce(out=gst[:, 0:8], in_=o3, axis=AX.X, op=ALU.add), [B_osb], [B_gst])
                S.op("dve", lambda e: e.tensor_reduce(out=gst[:, 8:16], in_=osq.rearrange("p (h v) -> p h v", v=64), axis=AX.X, op=ALU.add), [B_osq, B_gst], [B_gst])
                S.op("dve", lambda e: e.tensor_scalar(out=gst[:, 0:16], in0=gst[:, 0:16], scalar1=1.0 / 64, scalar2=None, op0=ALU.mult), [B_gst], [B_gst])
                S.op("dve", lambda e: e.tensor_tensor(out=gst[:, 16:24], in0=gst[:, 0:8], in1=gst[:, 0:8], op=ALU.mult), [B_gst], [B_gst])
                S.op("dve", lambda e: e.tensor_tensor(out=gst[:, 16:24], in0=gst[:, 8:16], in1=gst[:, 16:24], op=ALU.subtract), [B_gst], [B_gst])
                S.op("act", lambda e: e.activation(out=gst[:, 24:32], in_=gst[:, 16:24], func=AF.Sqrt, bias=prm[:, P_EPSG:P_EPSG + 1]), [B_gst, B_prm], [B_gst])
                S.op("dve", lambda e: e.reciprocal(out=gst[:, 24:32], in_=gst[:, 24:32]), [B_gst], [B_gst])
                S.op("dve", lambda e: e.tensor_tensor(out=o3, in0=o3, in1=gst[:, 0:8].unsqueeze(2).to_broadcast([128, 8, 64]), op=ALU.subtract), [B_osb, B_gst], [B_osb])
                S.op("dve", lambda e: e.tensor_tensor(out=o3, in0=o3, in1=gst[:, 24:32].unsqueeze(2).to_broadcast([128, 8, 64]), op=ALU.mult), [B_osb, B_gst], [B_osb])
                yield
                for cc in range(4):
                    S.op("pe", lambda e, cc=cc: e.transpose(pq(7, cc), osb[:, cc * 128:(cc + 1) * 128], ident), [B_osb, B_cst], [PB[7][cc]])
                    S.op("dve", lambda e, cc=cc: e.tensor_scalar(out=tmp2[:, cc, :], in0=pq(7, cc), scalar1=prm[:, P_GNG + cc:P_GNG + cc + 1], scalar2=prm[:, P_GNB + cc:P_GNB + cc + 1], op0=ALU.mult, op1=ALU.add),
                         [PB[7][cc], B_prm], [B_tmp2])
                S.op("pool", lambda e: e.tensor_tensor(out=tmp2, in0=tmp2, in1=bon, op=ALU.add), [B_tmp2, B_bon], [B_tmp2])
                S.op("pool", lambda e: e.tensor_tensor(out=orw, in0=tmp2, in1=gT, op=ALU.mult), [B_tmp2, B_gT], [B_orw])
                if i == 1:
                    tap("orw", orw, B_orw, [128, 4, 128], BF16)
                S.dma(ch_orw, orw_s[i], orw, reads=[B_orw], writes=[B_orws])
                yield

            def run_all(g):
                for _ in g:
                    pass

            def adv(g):
                try:
                    next(g)
                    return True
                except StopIteration:
                    return False

            run_all(front(0))
            if NT > 1:
                run_all(front(1))
            run_all(neu(0))
            NNEU = 21
            NFRONT = 10
            for i in range(NT):
                if i < 16:
                    issue_cast(i)
                g_st = stt(i)
                g_neu = neu(i + 1) if i + 1 < NT else iter(())
                g_fr = front(i + 2) if i + 2 < NT else iter(())
                a_st = a_neu = a_fr = True
                rnd = 0
                fdone = 0
                while a_st or a_neu or a_fr:
                    if a_st:
                        a_st = adv(g_st)
                    if a_neu:
                        a_neu = adv(g_neu)
                    want = (rnd + 1) * NFRONT // NNEU + 1 if (a_neu or a_st) else 10 ** 9
                    while a_fr and fdone < want:
                        a_fr = adv(g_fr)
                        fdone += 1
                    rnd += 1
            for k_ in range(min(NT, 16), 16):
                issue_cast(k_)
            tap("STfin", ST, B_STp[0], [128, 4, 64])
        phase_R()
        S.barrier()
        B_h1toks = Buf("h1toks")
        B_h1ns = Buf("h1ns")

        def phase_C():
            top[0] = base_top
            wC, B_wC = alloc("wC", [8, 3072], BF16)
            wco, B_wco = alloc("wco", [4, 1024], BF16)
            wro, B_wro = alloc("wro", [4, 1024], BF16)
            wo, B_wo = alloc("wo", [8, 1024], BF16)
            diag, B_diag = alloc("diag", [124, 128], BF16)
            S.dma(S.chan("wC0"), wC[:, :, 0:1024], w_in_d[:, 0:1024].rearrange("(c p) n -> p c n", p=128), writes=[B_wC], eng="pool")
            S.dma(S.chan("wC1"), wC[:, :, 1024:3072], w_in_d[:, 2816:4864].rearrange("(c p) n -> p c n", p=128), writes=[B_wC], eng="pool")
            S.dma(S.chan("wco"), wco, wco_d.rearrange("(c p) n -> p c n", p=128), writes=[B_wco], eng="pool")
            S.dma(S.chan("wro"), wro, wro_d.rearrange("(c p) n -> p c n", p=128), writes=[B_wro], eng="pool")
            S.dma(S.chan("wo"), wo, wo_d.rearrange("(c p) n -> p c n", p=128), writes=[B_wo], eng="pool")
            for cc in range(4):
                for j in range(31):
                    S.op("dve", lambda e, cc=cc, j=j: e.tensor_scalar(out=diag[:, cc * 31 + j, :], in0=ident, scalar1=prm[:, P_CW + cc * 31 + j:P_CW + cc * 31 + j + 1], scalar2=None, op0=ALU.mult),
                         [B_cst, B_prm], [B_diag])
            xts = [alloc("cxt%d" % k, [8, 128]) for k in range(3)]
            chx = [S.chan("cx%d" % k) for k in range(3)]
            xb, B_xb = alloc("cxb", [8, 128], BF16)
            sq, B_sq = alloc("csq", [8, 128], BF16)
            rs, B_rs = alloc("crs", [128])
            zcs = [alloc("zc%d" % k, [24, 128]) for k in range(2)]
            ubuf, B_ubuf = alloc("ubuf", [4, 158], BF16)
            ysb, B_ysb = alloc("ysb", [4, 128])
            ysq, B_ysq = alloc("ysq", [4, 128])
            mv, B_mv = alloc("mv", [128])
            actc, B_actc = alloc("actc", [4, 128], BF16)
            orwt, B_orwt = alloc("orwt", [4, 128], BF16)
            t1, B_t1 = alloc("t1", [8, 128])
            t2, B_t2 = alloc("t2", [8, 128])
            gateds = [alloc("gated%d" % k, [8, 128], BF16) for k in range(2)]
            t1b, B_t1b = alloc("t1b", [8, 128])
            h1T, B_h1T = alloc("h1T", [8, 128])
            h1sq, B_h1sq = alloc("h1sq", [8, 128], BF16)
            rs2, B_rs2 = alloc("rs2", [128])
            h1n, B_h1n = alloc("h1n", [8, 128], BF16)
            h1tok, B_h1tok = alloc("h1tok", [1024])
            ch_orwl = S.chan("orwl")
            ch_h1tok = S.chan("h1tok")
            ch_h1n = S.chan("h1n")
            S.op("dve", lambda e: e.memset(ubuf, 0.0), [], [B_ubuf])
            def cfront(i):
                zc, B_zc = zcs[i % 2]
                xt, B_xt = xts[i % 3]
                load_x(i, xt, B_xt, chx[i % 3], xb, B_xb, sq, B_sq, rs, B_rs, (1, 0))
                nchunk = 8 if i == 0 else 24
                for gi in range(nchunk // 4):
                    bank = gi % 2
                    for jj in range(4):
                        j = gi * 4 + jj
                        col = j * 128
                        for dc in range(8):
                            S.op("pe", lambda e, bank=bank, jj=jj, dc=dc, col=col: e.matmul(pq(bank, jj), lhsT=wC[:, dc, col:col + 128], rhs=xb[:, dc, :], start=(dc == 0), stop=(dc == 7)),
                                 [B_wC, B_xb], [PB[bank][0]])
                    S.op("dve", lambda e, bank=bank, gi=gi: e.tensor_tensor(out=zc[:, gi * 4:gi * 4 + 4, :], in0=PS[bank][:, :].rearrange("p (a b) -> p a b", b=128),
                                                                         in1=rs.unsqueeze(1).to_broadcast([128, 4, 128]), op=ALU.mult),
                         [PB[bank][0], B_rs], [B_zc])
                yield

            def cb1(i):
                zc, B_zc = zcs[i % 2]
                gated, B_gated = gateds[i % 2]
                S.op("act", lambda e: e.activation(out=zc[:, 4:8, :], in_=zc[:, 4:8, :], func=AF.Sigmoid), [B_zc], [B_zc])
                S.op("dve", lambda e: e.tensor_tensor(out=ubuf[:, :, 30:158], in0=zc[:, 0:4, :], in1=zc[:, 4:8, :], op=ALU.mult), [B_zc], [B_ubuf])
                yield
                for cc in range(4):
                    for j in range(31):
                        S.op("pe", lambda e, cc=cc, j=j: e.matmul(pq(2, cc), lhsT=diag[:, cc * 31 + j, :], rhs=ubuf[:, cc, j:j + 128], start=(j == 0), stop=(j == 30)),
                             [B_diag, B_ubuf], [PB[2][0]])
                S.op("pool", lambda e: e.tensor_copy(out=ubuf[:, :, 0:30], in_=ubuf[:, :, 128:158]), [B_ubuf], [B_ubuf])
                if i == 0:
                    S.op("act", lambda e: e.copy(out=ysb.rearrange("p a b -> p (a b)"), in_=PS[2][:, :]), [PB[2][0]], [B_ysb])
                    return
                yield
                for cc in range(4):
                    S.op("act", lambda e, cc=cc: e.activation(out=ysb[:, cc, :], in_=pq(2, cc), func=AF.Identity, bias=prm[:, P_CB + cc:P_CB + cc + 1]), [PB[2][0], B_prm], [B_ysb])
                    S.op("act", lambda e, cc=cc: e.activation(out=ysq[:, cc, :], in_=pq(2, cc), func=AF.Square, bias=prm[:, P_CB + cc:P_CB + cc + 1]), [PB[2][0], B_prm], [B_ysq])
                for cc in range(4):
                    S.op("pe", lambda e, cc=cc: e.matmul(pq(3, 0), lhsT=cst[:, C_O512, :], rhs=ysb[:, cc, :], start=(cc == 0), stop=(cc == 3)), [B_cst, B_ysb], [PB[3][0]])
                for cc in range(4):
                    S.op("pe", lambda e, cc=cc: e.matmul(pq(3, 1), lhsT=cst[:, C_O512, :], rhs=ysq[:, cc, :], start=(cc == 0), stop=(cc == 3)), [B_cst, B_ysq], [PB[3][0]])
                S.op("act", lambda e: e.activation(out=mv, in_=pq(3, 0), func=AF.Square), [PB[3][0]], [B_mv])
                S.op("dve", lambda e: e.tensor_tensor(out=mv, in0=pq(3, 1), in1=mv, op=ALU.subtract), [PB[3][0], B_mv], [B_mv])
                S.op("act", lambda e: e.activation(out=mv, in_=mv, func=AF.Sqrt, bias=prm[:, P_EPS5:P_EPS5 + 1]), [B_mv, B_prm], [B_mv])
                S.op("dve", lambda e: e.reciprocal(out=mv, in_=mv), [B_mv], [B_mv])
                S.op("dve", lambda e: e.tensor_tensor(out=ysb, in0=ysb, in1=pq(3, 0).unsqueeze(1).to_broadcast([128, 4, 128]), op=ALU.subtract), [B_ysb, PB[3][0]], [B_ysb])
                S.op("dve", lambda e: e.tensor_tensor(out=ysb, in0=ysb, in1=mv.unsqueeze(1).to_broadcast([128, 4, 128]), op=ALU.mult), [B_ysb, B_mv], [B_ysb])
                for cc in range(4):
                    S.op("act", lambda e, cc=cc: e.activation(out=actc[:, cc, :], in_=ysb[:, cc, :], func=AF.Silu, scale=prm[:, P_LNG + cc:P_LNG + cc + 1], bias=prm[:, P_LNB + cc:P_LNB + cc + 1]),
                         [B_ysb, B_prm], [B_actc])
                yield
                S.dma(ch_orwl, orwt, orw_s[i], reads=[B_orws], writes=[B_orwt])
                for m_ in range(8):
                    for cc in range(4):
                        S.op("pe", lambda e, m_=m_, cc=cc: e.matmul(pq(4 + m_ // 4, m_ % 4), lhsT=wco[:, cc, m_ * 128:(m_ + 1) * 128], rhs=actc[:, cc, :], start=(cc == 0), stop=(cc == 3)),
                             [B_wco, B_actc], [PB[4 + m_ // 4][0]])
                S.op("act", lambda e: e.activation(out=zc[:, 8:24, :], in_=zc[:, 8:24, :], func=AF.Sigmoid), [B_zc], [B_zc])
                yield
                for hb_ in range(2):
                    S.op("dve", lambda e, hb_=hb_: e.tensor_tensor(out=t1[:, hb_ * 4:hb_ * 4 + 4, :], in0=PS[4 + hb_][:, :].rearrange("p (a b) -> p a b", b=128), in1=zc[:, 8 + hb_ * 4:12 + hb_ * 4, :], op=ALU.mult),
                         [PB[4 + hb_][0], B_zc], [B_t1])
                for m_ in range(8):
                    for cc in range(4):
                        S.op("pe", lambda e, m_=m_, cc=cc: e.matmul(pq(4 + m_ // 4, m_ % 4), lhsT=wro[:, cc, m_ * 128:(m_ + 1) * 128], rhs=orwt[:, cc, :], start=(cc == 0), stop=(cc == 3)),
                             [B_wro, B_orwt], [PB[4 + m_ // 4][0]])
                yield
                for hb_ in range(2):
                    S.op("dve", lambda e, hb_=hb_: e.tensor_tensor(out=t2[:, hb_ * 4:hb_ * 4 + 4, :], in0=PS[4 + hb_][:, :].rearrange("p (a b) -> p a b", b=128), in1=zc[:, 16 + hb_ * 4:20 + hb_ * 4, :], op=ALU.mult),
                         [PB[4 + hb_][0], B_zc], [B_t2])
                S.op("pool", lambda e: e.tensor_tensor(out=gated, in0=t1, in1=t2, op=ALU.add), [B_t1, B_t2], [B_gated])
                yield


            def cb2(i):
                if i == 0:
                    return
                    yield
                xt, B_xt = xts[i % 3]
                gated, B_gated = gateds[i % 2]
                for m_ in range(8):
                    for kc in range(8):
                        S.op("pe", lambda e, m_=m_, kc=kc: e.matmul(pq(6 + m_ // 4, m_ % 4), lhsT=wo[:, kc, m_ * 128:(m_ + 1) * 128], rhs=gated[:, kc, :], start=(kc == 0), stop=(kc == 7)),
                             [B_wo, B_gated], [PB[6 + m_ // 4][0]])
                for hb_ in range(2):
                    S.op("dve", lambda e, hb_=hb_, xt=xt: e.tensor_tensor(out=h1T[:, hb_ * 4:hb_ * 4 + 4, :], in0=PS[6 + hb_][:, :].rearrange("p (a b) -> p a b", b=128), in1=xt[:, hb_ * 4:hb_ * 4 + 4, :], op=ALU.add),
                         [PB[6 + hb_][0], B_xt], [B_h1T])
                yield
                for m_ in range(8):
                    S.op("pe", lambda e, m_=m_: e.transpose(pq(6 + m_ // 4, m_ % 4), h1T[:, m_, :], ident), [B_h1T, B_cst], [PB[6 + m_ // 4][0]])
                for hb_ in range(2):
                    S.op("act", lambda e, hb_=hb_: e.copy(out=h1tok[:, hb_ * 512:(hb_ + 1) * 512], in_=PS[6 + hb_][:, :]), [PB[6 + hb_][0]], [B_h1tok])
                S.dma(ch_h1tok, h1tok_s[i - 1], h1tok, reads=[B_h1tok], writes=[B_h1toks])
                yield
                S.op("act", lambda e: e.activation(out=h1sq, in_=h1T, func=AF.Square), [B_h1T], [B_h1sq])
                for dc in range(8):
                    S.op("pe", lambda e, dc=dc: e.matmul(pq(6, 0), lhsT=ones_b, rhs=h1sq[:, dc, :], start=(dc == 0), stop=(dc == 7)), [B_onesb, B_h1sq], [PB[6][0]])
                S.op("act", lambda e: e.activation(out=rs2, in_=pq(6, 0), func=AF.Sqrt, scale=1.0 / 1024, bias=prm[:, P_EPS6:P_EPS6 + 1]), [PB[6][0], B_prm], [B_rs2])
                S.op("dve", lambda e: e.reciprocal(out=rs2, in_=rs2), [B_rs2], [B_rs2])
                S.op("dve", lambda e: e.tensor_tensor(out=t1b, in0=h1T, in1=rs2.unsqueeze(1).to_broadcast([128, 8, 128]), op=ALU.mult), [B_h1T, B_rs2], [B_t1b])
                S.op("pool", lambda e: e.tensor_tensor(out=h1n, in0=t1b, in1=prm[:, P_GFFN:P_GFFN + 8].unsqueeze(2).to_broadcast([128, 8, 128]), op=ALU.mult), [B_t1b, B_prm], [B_h1n])
                S.dma(ch_h1n, h1nT_s[i - 1], h1n, reads=[B_h1n], writes=[B_h1ns])
                if i == 1:
                    tap("h1tok", h1tok, B_h1tok, [128, 1024])
                    tap("h1n", h1n, B_h1n, [128, 8, 128], BF16)
                yield

            def _adv(g):
                try:
                    next(g)
                    return True
                except StopIteration:
                    return False

            def _run(g):
                for _ in g:
                    pass

            _run(cfront(0))
            if NT > 1:
                _run(cfront(1))
            _run(cb1(0))
            for i in range(NT):
                g2 = cb2(i)
                g1 = cb1(i + 1) if i + 1 < NT else iter(())
                gf = cfront(i + 2) if i + 2 < NT else iter(())
                a1 = a2 = af = True
                while a1 or a2 or af:
                    if a2:
                        a2 = _adv(g2)
                    if a1:
                        a1 = _adv(g1)
                    if af:
                        af = _adv(gf)
        phase_C()
        S.barrier()

        B_sels = Buf("sels")

        def phase_Q():
            top[0] = base_top
            act3_0, B_act3_0 = alloc("act3_0", [256, 128], BF16)
            hn2_0, B_hn2_0 = alloc("hn2_0", [8, 256], BF16)
            Ub0 = [alloc("Ub%d" % k, [8, 128], BF16) for k in range(8)]
            ch_U0 = [S.chan("U0_%d" % k) for k in range(8)]
            ch_hn20 = S.chan("hn2_0q")

            def a0_iter(i2):
                slot = i2 % 8
                U_, B_U = Ub0[slot]
                S.dma(ch_U0[slot], U_.rearrange("p b c -> p (b c)"), uT_b[i2], reads=[B_uTb], writes=[B_U])
                bank = (i2 // 2) % 2
                half = i2 % 2
                for dc in range(8):
                    S.op("pe", lambda e, dc=dc: e.matmul(PS[bank][:, half * 256:(half + 1) * 256], lhsT=U_[:, dc, :], rhs=hn2_0[:, dc, :], start=(dc == 0), stop=(dc == 7)), [B_U, B_hn2_0], [PB[bank][0]])
                if half == 1:
                    S.op("act", lambda e: e.activation(out=act3_0[:, :, i2 - 1:i2 + 1], in_=PS[bank][:, :].rearrange("p (i t) -> p t i", i=2), func=AF.Gelu), [PB[bank][0]], [B_act3_0])
            wq, B_wq = alloc("wq", [8, 2048], BF16)
            skT, B_skT = alloc("skT", [16, 128], BF16)
            S.dma(S.chan("wq"), wq, wq_d.rearrange("(c p) n -> p c n", p=128), writes=[B_wq], eng="pool")
            S.dma(S.chan("skT"), skT, skT_d, writes=[B_skT], eng="pool")
            hns = [alloc("hn%d" % k, [8, 128], BF16) for k in range(2)]
            ch_hns = [S.chan("hn%d" % k) for k in range(2)]
            qT, B_qT = alloc("qT", [16, 128], BF16)
            ssbs = [alloc("ssb%d" % k, [16, 128]) for k in range(2)]
            wk16, _ = alloc("wk16", [16, 128])
            B_wkg = [Buf("wk%d" % g_) for g_ in range(16)]
            B_ssb4s = [[Buf("ssb%d_%d" % (k, g_)) for g_ in range(4)] for k in range(2)]
            B_topsg = [Buf("tops%d" % g_) for g_ in range(16)]
            B_topig = [Buf("topi%d" % g_) for g_ in range(16)]
            B_candh = [Buf("cand%d" % g_) for g_ in range(8)]
            B_bestsh = [Buf("bests%d" % g_) for g_ in range(8)]
            B_bestch = [Buf("bestc%d" % g_) for g_ in range(8)]
            B_eqh = [Buf("eq%d" % g_) for g_ in range(16)]
            B_sel3h = [Buf("sel3_%d" % g_) for g_ in range(16)]
            B_sel3g = Buf("sel3g")
            B_ju2 = Buf("ju2")
            TS3 = [(alloc("tops%d" % k, [16, 16])[0], alloc("topi%d" % k, [16, 16], U32)[0], alloc("topif%d" % k, [16, 16])[0]) for k in range(2)]
            TB3 = [([Buf("tops%d_%d" % (k, g_)) for g_ in range(16)], [Buf("topi%d_%d" % (k, g_)) for g_ in range(16)], Buf("topif%d" % k)) for k in range(2)]
            wk2, _ = alloc("wk2", [8, 256])
            B_wk2h = [Buf("wk2_%d" % h) for h in range(8)]
            cand, B_cand = alloc("cand", [8, 256])
            bests, B_bests = alloc("bests", [8, 16])
            bestc, B_bestc = alloc("bestc", [8, 16], U32)
            ju, B_ju = alloc("ju", [2, 8, 16], U32)
            j1, B_j1 = alloc("j1", [8, 16])
            j2, B_j2 = alloc("j2", [8, 16])
            eq, B_eq = alloc("eq", [16, 16, 16])
            ee, B_ee = alloc("ee", [8, 16])
            zz, B_zz = alloc("zz", [8])
            sel3, B_sel3 = alloc("sel3", [3, 128])
            selT, B_selT = alloc("selT", [3, 128])
            ch_hn = S.chan("hn")
            ch_sel = S.chan("sel")
            iota16 = cst[:, C_IOTA, 0:16]
            def q_front(i):
                hn, B_hn = hns[i % 2]
                ssb = ssbs[i % 2][0]
                B_ssb4 = B_ssb4s[i % 2]
                S.dma(ch_hns[i % 2], hn, h1nT_s[i], reads=[B_h1ns], writes=[B_hn])
                for g_ in range(16):
                    for dc in range(8):
                        S.op("pe", lambda e, g_=g_, dc=dc: e.matmul(pq(g_ // 4, g_ % 4), lhsT=wq[:, dc, g_ * 128:(g_ + 1) * 128], rhs=hn[:, dc, :], start=(dc == 0), stop=(dc == 7)),
                             [B_wq, B_hn], [PB[g_ // 4][0]])
                    if g_ % 4 == 3:
                        S.op("act", lambda e, g_=g_: e.copy(out=qT[:, g_ - 3:g_ + 1, :], in_=PS[g_ // 4][:, :].rearrange("p (a b) -> p a b", b=128)), [PB[g_ // 4][0]], [B_qT])
                for g_ in range(16):
                    S.op("pe", lambda e, g_=g_: e.matmul(pq(4 + g_ // 4, g_ % 4), lhsT=qT[:, g_, :], rhs=skT[:, g_, :], start=True, stop=True), [B_qT, B_skT], [PB[4 + g_ // 4][0]])
                    if g_ % 4 == 3:
                        S.op("act", lambda e, g_=g_: e.copy(out=ssb[:, g_ - 3:g_ + 1, :], in_=PS[4 + g_ // 4][:, :].rearrange("p (a b) -> p a b", b=128)), [PB[4 + g_ // 4][0]], [B_ssb4[g_ // 4]])

            def q_b1(i):
                tops, topi, topif = TS3[i % 2]
                B_topsg, B_topig, B_topif = TB3[i % 2]
                ssb = ssbs[i % 2][0]
                B_ssb4 = B_ssb4s[i % 2]
                for g_ in range(16):
                    S.op("dve", lambda e, g_=g_: e.max(out=tops[:, g_, 0:8], in_=ssb[:, g_, :]), [B_ssb4[g_ // 4]], [B_topsg[g_]])
                yield
                for g_ in range(16):
                    S.op("dve", lambda e, g_=g_: e.max_index(out=topi[:, g_, 0:8], in_max=tops[:, g_, 0:8], in_values=ssb[:, g_, :]), [B_ssb4[g_ // 4], B_topsg[g_]], [B_topig[g_]])
                yield
                for g_ in range(16):
                    S.op("dve", lambda e, g_=g_: e.match_replace(out=wk16[:, g_, :], in_to_replace=tops[:, g_, 0:8], in_values=ssb[:, g_, :], imm_value=-1e30), [B_ssb4[g_ // 4], B_topsg[g_]], [B_wkg[g_]])
                yield
                for g_ in range(16):
                    S.op("dve", lambda e, g_=g_: e.max(out=tops[:, g_, 8:16], in_=wk16[:, g_, :]), [B_wkg[g_]], [B_topsg[g_]])
                yield
                for g_ in range(16):
                    S.op("dve", lambda e, g_=g_: e.max_index(out=topi[:, g_, 8:16], in_max=tops[:, g_, 8:16], in_values=wk16[:, g_, :]), [B_wkg[g_], B_topsg[g_]], [B_topig[g_]])
                S.op("pool", lambda e: e.tensor_copy(out=topif, in_=topi), B_topig, [B_topif])
                yield

            def q_b2(i):
                tops, topi, topif = TS3[i % 2]
                B_topsg, B_topig, B_topif = TB3[i % 2]
                yield
                tops4 = tops.rearrange("p (h c) j -> p h c j", c=2)
                S.op("pool", lambda e, tops4=tops4: e.tensor_tensor(out=cand.rearrange("p h (a b) -> p h a b", b=16),
                                                                in0=tops4[:, :, 0, :].unsqueeze(3).to_broadcast([128, 8, 16, 16]),
                                                                in1=tops4[:, :, 1, :].unsqueeze(2).to_broadcast([128, 8, 16, 16]), op=ALU.add), B_topsg, B_candh)
                yield
                for h in range(8):
                    S.op("dve", lambda e, h=h: e.max(out=bests[:, h, 0:8], in_=cand[:, h, :]), [B_candh[h]], [B_bestsh[h]])
                yield
                for h in range(8):
                    S.op("dve", lambda e, h=h: e.max_index(out=bestc[:, h, 0:8], in_max=bests[:, h, 0:8], in_values=cand[:, h, :]), [B_candh[h], B_bestsh[h]], [B_bestch[h]])
                yield
                for h in range(8):
                    S.op("dve", lambda e, h=h: e.match_replace(out=wk2[:, h, :], in_to_replace=bests[:, h, 0:8], in_values=cand[:, h, :], imm_value=-1e30),
                         [B_candh[h], B_bestsh[h]], [B_wk2h[h]])
                yield
                for h in range(8):
                    S.op("dve", lambda e, h=h: e.max(out=bests[:, h, 8:16], in_=wk2[:, h, :]), [B_wk2h[h]], [B_bestsh[h]])
                yield
                for h in range(8):
                    S.op("dve", lambda e, h=h: e.max_index(out=bestc[:, h, 8:16], in_max=bests[:, h, 8:16], in_values=wk2[:, h, :]),
                         [B_wk2h[h], B_bestsh[h]], [B_bestch[h]])
                S.op("dve", lambda e: e.tensor_single_scalar(out=ju[:, 0, :, :], in_=bestc, scalar=4, op=ALU.logical_shift_right), B_bestch, [B_ju])
                S.op("dve", lambda e: e.tensor_single_scalar(out=ju[:, 1, :, :], in_=bestc, scalar=15, op=ALU.bitwise_and), B_bestch, [B_ju2])
                S.op("pool", lambda e: e.tensor_copy(out=j1, in_=ju[:, 0, :, :]), [B_ju], [B_j1])
                S.op("pool", lambda e: e.tensor_copy(out=j2, in_=ju[:, 1, :, :]), [B_ju2], [B_j2])
                yield
                for half, jj_ in ((0, j1), (1, j2)):
                    Bj = B_j1 if half == 0 else B_j2
                    eqh = eq[:, half * 8:half * 8 + 8, :, :]
                    Beq = B_eqh[half * 8:half * 8 + 8]
                    tf4 = topif.rearrange("p (h c) j -> p h c j", c=2)[:, :, half, :]
                    S.op("dve", lambda e, jj_=jj_, eqh=eqh: e.tensor_tensor(out=eqh, in0=jj_.unsqueeze(3).to_broadcast([128, 8, 16, 16]),
                                                                   in1=iota16.unsqueeze(1).unsqueeze(1).to_broadcast([128, 8, 16, 16]), op=ALU.is_equal), [Bj, B_cst], Beq)
                    S.op("pool", lambda e, eqh=eqh, tf4=tf4: e.tensor_tensor(out=eqh, in0=eqh, in1=tf4.unsqueeze(2).to_broadcast([128, 8, 16, 16]), op=ALU.mult), Beq + [B_topif], Beq)
                yield
                for half in range(2):
                    eqh = eq[:, half * 8:half * 8 + 8, :, :]
                    Beq = B_eqh[half * 8:half * 8 + 8]
                    S.op("dve", lambda e, half=half, eqh=eqh: e.tensor_reduce(out=sel3[:, half, :].rearrange("p (h j) -> p h j", j=16), in_=eqh, axis=AX.X, op=ALU.add), Beq, B_sel3h[half * 8:half * 8 + 8])
                B_bests_all = B_bestsh
                yield
                S.op("dve", lambda e: e.tensor_tensor(out=ee, in0=bests, in1=bests[:, :, 0:1].to_broadcast([128, 8, 16]), op=ALU.subtract), B_bestsh, [B_ee])
                S.op("act", lambda e: e.activation(out=ee, in_=ee, func=AF.Exp), [B_ee], [B_ee])
                S.op("dve", lambda e: e.tensor_reduce(out=zz, in_=ee, axis=AX.X, op=ALU.add), [B_ee], [B_zz])
                S.op("dve", lambda e: e.reciprocal(out=zz, in_=zz), [B_zz], [B_zz])
                S.op("dve", lambda e: e.tensor_tensor(out=sel3[:, 2, :].rearrange("p (h j) -> p h j", j=16), in0=ee, in1=zz.unsqueeze(2).to_broadcast([128, 8, 16]), op=ALU.mult), [B_ee, B_zz], [B_sel3g])
                yield
                for k in range(3):
                    S.op("pe", lambda e, k=k: e.transpose(pq(0, k), sel3[:, k, :], ident), B_sel3h + [B_sel3g, B_cst], [PB[0][0]])
                S.op("act", lambda e: e.copy(out=selT.rearrange("p a b -> p (a b)"), in_=PS[0][:, 0:384]), [PB[0][0]], [B_selT])
                S.dma(ch_sel, sel_s[i], selT, reads=[B_selT], writes=[B_sels])
                if i == 0:
                    tap("sel3", sel3, B_sel3g, [128, 3, 128])


                yield
            def _adv(g):
                try:
                    next(g)
                    return True
                except StopIteration:
                    return False

            q_front(0)
            if NRT > 1:
                q_front(1)
            for _ in q_b1(0):
                pass
            if A0_IN_Q:
                for ts in range(2):
                    S.dma(ch_hn20, hn2_0[:, :, ts * 128:(ts + 1) * 128], h1nT_s[ts], reads=[B_h1ns], writes=[B_hn2_0])
            a0_per = -(-128 // NRT)
            for i in range(NRT):
                if i + 2 < NRT:
                    q_front(i + 2)
                if A0_IN_Q:
                    for i2 in range(i * a0_per, min(128, (i + 1) * a0_per)):
                        a0_iter(i2)
                g2 = q_b2(i)
                g1 = q_b1(i + 1) if i + 1 < NRT else iter(())
                a1 = a2 = True
                while a1 or a2:
                    if a2:
                        a2 = _adv(g2)
                    if a2:
                        a2 = _adv(g2)
                    if a1:
                        a1 = _adv(g1)
        phase_Q()
        S.barrier()

        def phase_E():
            top[0] = base_top
            NS = NRT // 2
            _a0 = alloc("act3_0", [256, 128], BF16)
            _h0 = alloc("hn2_0", [8, 256], BF16)
            Ub = [alloc("Ub%d" % k, [8, 128], BF16) for k in range(8)]
            act3s = [_a0, alloc("act3_1", [256, 128], BF16)]
            hn2s = [_h0, alloc("hn2_1", [8, 256], BF16)]
            selAs = [alloc("selA_%d" % k, [2, 3, 128]) for k in range(2)]
            Vb = [alloc("Vb%d" % k, [1024], BF16) for k in range(8)]
            Aoh = [alloc("Aoh%d" % k, [4, 128], BF16) for k in range(4)]
            Boh = [alloc("Boh%d" % k, [4, 128], BF16) for k in range(4)]
            ysb, B_ysb = alloc("eysb", [1024])
            h1t, B_h1t = alloc("h1t", [1024])
            gfin, B_gfin = alloc("gfin", [1024])
            ob, B_ob = alloc("ob", [1024])
            stat, B_stat = alloc("stat", [4])
            S.dma(S.chan("gfin"), gfin, gfin_d.partition_broadcast(128)[:, 0, :], writes=[B_gfin])
            ch_hn2 = [S.chan("hn2_%d" % k) for k in range(2)]
            ch_selA = [S.chan("selA_%d" % k) for k in range(2)]
            ch_U = [S.chan("U%d" % k) for k in range(8)]
            ch_V = [S.chan("V%d" % k) for k in range(8)]
            ch_h1t = S.chan("h1t")
            ch_out = S.chan("out")
            iota_bc = cst[:, C_IOTA, :].unsqueeze(1).to_broadcast([128, 4, 128])
            uctr = [0]
            actr = [0]

            def loads(s_):
                hn2, B_hn2 = hn2s[s_ % 2]
                selA, B_selA = selAs[s_ % 2]
                for ts in range(2):
                    S.dma(ch_hn2[s_ % 2], hn2[:, :, ts * 128:(ts + 1) * 128], h1nT_s[s_ * 2 + ts], reads=[B_h1ns], writes=[B_hn2])
                    S.dma(ch_selA[s_ % 2], selA[:, ts, :, :], sel_s[s_ * 2 + ts], reads=[B_sels], writes=[B_selA])

            def a_iter(s_, i2):
                act3, B_act3 = act3s[s_ % 2]
                hn2, B_hn2 = hn2s[s_ % 2]
                uctr[0] += 1
                slot = uctr[0] % 8
                U_, B_U = Ub[slot]
                S.dma(ch_U[slot], U_.rearrange("p b c -> p (b c)"), uT_b[i2], reads=[B_uTb], writes=[B_U])
                bank = (actr[0] // 2) % 2
                half = actr[0] % 2
                actr[0] += 1
                for dc in range(8):
                    S.op("pe", lambda e, dc=dc: e.matmul(PS[bank][:, half * 256:(half + 1) * 256], lhsT=U_[:, dc, :], rhs=hn2[:, dc, :], start=(dc == 0), stop=(dc == 7)), [B_U, B_hn2], [PB[bank][0]])
                if half == 1:
                    S.op("act", lambda e: e.activation(out=act3[:, :, i2 - 1:i2 + 1], in_=PS[bank][:, :].rearrange("p (i t) -> p t i", i=2), func=AF.Gelu), [PB[bank][0]], [B_act3])

            def b_vars(s_, tg):
                act3, B_act3 = act3s[s_ % 2]
                selA, B_selA = selAs[s_ % 2]
                t0 = tg * 4
                A_, B_A = Aoh[tg % 4]
                Bm, B_B = Boh[tg % 4]
                return act3, B_act3, selA, B_selA, t0, t0 // 128, t0 % 128, A_, B_A, Bm, B_B, 2 + tg % 2

            def b_stage1(s_, tg):
                act3, B_act3, selA, B_selA, t0, ti, tt, A_, B_A, Bm, B_B, gb = b_vars(s_, tg)
                S.op("dve", lambda e: e.tensor_tensor(out=A_, in0=iota_bc, in1=selA[:, ti, 0, tt:tt + 4].unsqueeze(2).to_broadcast([128, 4, 128]), op=ALU.is_equal), [B_cst, B_selA], [B_A])
                S.op("pool", lambda e: e.tensor_tensor(out=A_, in0=A_, in1=selA[:, ti, 2, tt:tt + 4].unsqueeze(2).to_broadcast([128, 4, 128]), op=ALU.mult), [B_A, B_selA], [B_A])
                S.op("dve", lambda e: e.tensor_tensor(out=Bm, in0=iota_bc, in1=selA[:, ti, 1, tt:tt + 4].unsqueeze(2).to_broadcast([128, 4, 128]), op=ALU.is_equal), [B_cst, B_selA], [B_B])

            def b_stage2(s_, tg):
                act3, B_act3, selA, B_selA, t0, ti, tt, A_, B_A, Bm, B_B, gb = b_vars(s_, tg)
                for tk in range(4):
                    S.op("pe", lambda e, tk=tk: e.matmul(pq(gb, tk), lhsT=A_[:, tk, :], rhs=Bm[:, tk, :], start=True, stop=True), [B_A, B_B], [PB[gb][0]])

            def b_stage3(s_, tg):
                act3, B_act3, selA, B_selA, t0, ti, tt, A_, B_A, Bm, B_B, gb = b_vars(s_, tg)
                S.op("dve", lambda e: e.tensor_tensor(out=act3[:, t0:t0 + 4, :], in0=PS[gb][:, :].rearrange("p (a b) -> p a b", b=128), in1=act3[:, t0:t0 + 4, :], op=ALU.mult),
                     [PB[gb][0], B_act3], [B_act3])

            def c_phase(s_):
                act3, B_act3 = act3s[s_ % 2]
                vctr = 0
                for i2 in range(128):
                    vctr += 1
                    V_, B_V = Vb[vctr % 8]
                    S.dma(ch_V[vctr % 8], V_, vP_b[i2], reads=[B_vPb], writes=[B_V])
                    for ts in range(2):
                        for dh in range(2):
                            bk = 4 + ts * 2 + dh
                            S.op("pe", lambda e, V_=V_, i2=i2, ts=ts, dh=dh, bk=bk: e.matmul(PS[bk][:, :], lhsT=act3[:, ts * 128:(ts + 1) * 128, i2], rhs=V_[:, dh * 512:(dh + 1) * 512],
                                                                                       start=(i2 == 0), stop=(i2 == 127)), [B_act3, B_V], [PB[bk][0]])

            def d_phase(s_):
                for ts in range(2):
                    gi_ = s_ * 2 + ts
                    S.dma(ch_h1t, h1t, h1tok_s[gi_], reads=[B_h1toks], writes=[B_h1t])
                    for dh in range(2):
                        bk = 4 + ts * 2 + dh
                        S.op("dve", lambda e, bk=bk, dh=dh: e.tensor_tensor(out=ysb[:, dh * 512:(dh + 1) * 512], in0=PS[bk][:, :], in1=h1t[:, dh * 512:(dh + 1) * 512], op=ALU.add),
                             [PB[bk][0], B_h1t], [B_ysb])
                    S.op("pool", lambda e: e.tensor_tensor(out=ob, in0=ysb, in1=ysb, op=ALU.mult), [B_ysb], [B_ob])
                    S.op("dve", lambda e: e.tensor_reduce(out=stat[:, 0:1], in_=ob, axis=AX.X, op=ALU.add), [B_ob], [B_stat])
                    S.op("act", lambda e: e.activation(out=stat[:, 1:2], in_=stat[:, 0:1], func=AF.Sqrt, scale=1.0 / 1024, bias=prm[:, P_EPS6:P_EPS6 + 1]), [B_stat, B_prm], [B_stat])
                    S.op("dve", lambda e: e.reciprocal(out=stat[:, 2:3], in_=stat[:, 1:2]), [B_stat], [B_stat])
                    S.op("dve", lambda e: e.scalar_tensor_tensor(out=ob, in0=ysb, scalar=stat[:, 2:3], in1=gfin, op0=ALU.mult, op1=ALU.mult), [B_ysb, B_stat, B_gfin], [B_ob])
                    o_ = S.dma(ch_out, out_d[gi_ * 128:(gi_ + 1) * 128, :], ob, reads=[B_ob])
                    S.final_waits.append(o_)

            if A0_IN_Q:
                selA0, B_selA0 = selAs[0]
                for ts in range(2):
                    S.dma(ch_selA[0], selA0[:, ts, :, :], sel_s[ts], reads=[B_sels], writes=[B_selA0])
            else:
                loads(0)
                for i2 in range(128):
                    a_iter(0, i2)
            for s_ in range(NS):
                nxt = s_ + 1 < NS
                if nxt:
                    loads(s_ + 1)
                b_stage1(s_, 0)
                b_stage1(s_, 1)
                b_stage2(s_, 0)
                for tg in range(64):
                    if tg + 2 < 64:
                        b_stage1(s_, tg + 2)
                    if tg + 1 < 64:
                        b_stage2(s_, tg + 1)
                    if nxt:
                        a_iter(s_ + 1, 2 * tg)
                        a_iter(s_ + 1, 2 * tg + 1)
                    b_stage3(s_, tg)
                    if tg == 3 and s_ > 0:
                        d_phase(s_ - 1)
                c_phase(s_)
            d_phase(NS - 1)
        phase_E()
        S.barrier()
        S.emit(st)
    return nc, dbg


def host_prep(inp, b, NT):
    f = np.float32
    x = np.asarray(inp["x"])[b]
    nreal = (NT - 1) * 128
    seq = np.concatenate([np.zeros((NPAD, D), f), np.asarray(inp["meta_tokens"], f), x[:nreal]], axis=0)
    xT = np.ascontiguousarray(seq.reshape(NT, 128, 8, 128).transpose(0, 3, 2, 1))
    m = {"xT": xT}
    return m


def shared_prep(inp):
    f = np.float32
    g = lambda k: np.asarray(inp[k], f)
    m = {}
    m["w_in"] = np.ascontiguousarray(g("w_in")[0])
    m["w_conv_out"] = np.ascontiguousarray(g("w_conv_out")[0])
    m["w_rwkv_out"] = np.ascontiguousarray(g("w_rwkv_out")[0])
    m["w_o"] = np.ascontiguousarray(g("w_o")[0])
    m["w_q"] = np.ascontiguousarray(g("w_q")[0])
    m["skT"] = np.ascontiguousarray(g("sub_keys")[0].transpose(3, 0, 1, 2).reshape(128, 16, 128))
    u = g("expert_u")[0]
    m["uT"] = np.ascontiguousarray(u.reshape(128, 128, 8, 128).transpose(1, 3, 2, 0)).reshape(128, 128, 1024)
    v = g("expert_v")[0]
    m["vP"] = np.ascontiguousarray(v.reshape(128, 128, 1024).transpose(1, 0, 2))
    m["wa_up"] = np.ascontiguousarray(np.concatenate([g("w_up")[0], g("a_up")[0]], axis=0))
    m["g_up"] = np.ascontiguousarray(g("g_up")[0])
    m["w0row"] = np.ascontiguousarray(g("w0")[0].reshape(1, 512))
    m["gfin"] = np.ascontiguousarray(g("g_final").reshape(1, 1024))
    prm = np.zeros((128, NPRM), f)
    col = lambda a, n: np.asarray(a, f).reshape(n, 128).T
    prm[:, P_GMIX:P_GMIX + 8] = col(g("g_mix")[0], 8)
    prm[:, P_MU:P_MU + 14] = col(g("mu_shift")[0], 14)
    prm[:, P_A0:P_A0 + 4] = col(g("a0")[0], 4)
    prm[:, P_KK:P_KK + 4] = col(g("k_k")[0], 4)
    prm[:, P_KA:P_KA + 4] = col(g("k_a")[0], 4)
    prm[:, P_RK:P_RK + 4] = col(g("r_k")[0].reshape(512), 4)
    prm[:, P_GNG:P_GNG + 4] = col(g("gn_g")[0], 4)
    prm[:, P_GNB:P_GNB + 4] = col(g("gn_b")[0], 4)
    prm[:, P_CB:P_CB + 4] = col(g("conv_b")[0], 4)
    prm[:, P_LNG:P_LNG + 4] = col(g("conv_ln_g")[0], 4)
    prm[:, P_LNB:P_LNB + 4] = col(g("conv_ln_b")[0], 4)
    prm[:, P_GFFN:P_GFFN + 8] = col(g("g_ffn")[0], 8)
    prm[:, P_EPS6] = 1e-6
    prm[:, P_EPS5] = 1e-5
    prm[:, P_EPSG] = 64e-5
    cw = g("conv_w")[0]
    prm[:, P_CW:P_CW + 124] = cw.reshape(31, 4, 128).transpose(2, 1, 0).reshape(128, 124)
    m["params"] = prm
    cst = np.zeros((128, NCST, 128), f)
    ar = np.arange(128)
    cst[:, C_IDENT] = np.eye(128)
    cst[:, C_ONES] = 1.0
    cst[:, C_O512] = 1.0 / 512
    cst[:, C_BLK] = (ar[:, None] // 64 == ar[None, :] // 64)
    e05 = float(np.exp(np.float32(-0.5)))
    cst[:, C_TRII] = -e05 * (ar[:, None] <= ar[None, :])
    cst[:, C_TRIE] = -e05 * (ar[:, None] < ar[None, :])
    cst[:, C_MS] = (ar[:, None] < ar[None, :])
    cst[:, C_MSN] = -1.0 * (ar[:, None] < ar[None, :])
    cst[:, C_MLN] = -1.0 * (ar[None, :] < ar[:, None])
    cst[:, C_MI] = (ar[:, None] <= ar[None, :])
    cst[:, C_IOTA] = ar[None, :]
    cst[:, C_O1024] = 1.0 / 1024
    m["cst"] = cst
    return m


_CACHE = {}


def kernel(**inputs):
    NT = 33
    if "nc" not in _CACHE:
        _CACHE["nc"] = build_program(NT)[0]
    nc = _CACHE["nc"]
    sh = shared_prep(inputs)
    in_maps = []
    for b in range(8):
        m = dict(sh)
        m.update(host_prep(inputs, b, NT))
        in_maps.append(m)
    res = run_bass_kernel_spmd(nc, in_maps, core_ids=list(range(8)))
    return np.stack([r["out"] for r in res.results], axis=0)
```
